# Optimizing a Trainium2 kernel written in Bass

```python
import math
import jax, jax.numpy as jnp
from jax import lax
import numpy as np

D_MODEL = 1024
BATCH = 16
SEQ = 2048
DEPTH = 2

GRID_W = 64
CTX_LEN = 256
N_EVEN = (DEPTH + 1) // 2
N_ODD = DEPTH // 2
EPS = 1e-6
CHUNK = 64
CONV_W = 4
MIX_WIDTH = D_MODEL

A_WIDTH = D_MODEL // 2
A_DK = 128
A_DV = 128
A_HEADS = A_WIDTH // A_DV
A_QK = A_HEADS * A_DK
A_QKV = 2 * A_QK + A_WIDTH
A_COLS = A_QKV + A_WIDTH + 4 * A_HEADS
B_WIDTH = D_MODEL // 2
B_BLOCKS = 8
B_BDIM = B_WIDTH // B_BLOCKS
LRU_C = 8.0
B_COLS = 2 * B_WIDTH
C_WIDTH = D_MODEL // 2
C_DV = 128
C_HEADS = C_WIDTH // C_DV
C_DQK = C_DV // 2
C_QK = C_HEADS * C_DQK
C_STATE_COLS = 2 * C_QK + C_WIDTH + 4 * C_HEADS
D_WIDTH = D_MODEL - C_WIDTH
POOL_SIZES = (2, 4, 8, 16)
D_GROUPS = len(POOL_SIZES)
D_GDIM = D_WIDTH // D_GROUPS
EVEN_IN = A_COLS + B_COLS
ODD_IN = C_STATE_COLS + C_WIDTH + D_WIDTH
PEER_HEADS = 8
N_KEYS = 128
N_EXPERTS = N_KEYS * N_KEYS
PEER_TOPK = 16
PEER_QDIM = 256
PEER_BLOCK = 128

kernel_name = 'hybrid_gdn_rglru_mlstm_pool_peer_dit'


def _split(t, sizes):
    cuts = [int(s) for s in np.cumsum(sizes)[:-1]]
    return jnp.split(t, cuts, axis=-1)


def _dirflip(t, d, axis):
    return t if d == 0 else jnp.flip(t, axis=axis)


def _rmsnorm(x, g):
    xf = x.astype(jnp.float32)
    y = xf * lax.rsqrt(jnp.mean(xf * xf, axis=-1, keepdims=True) + EPS)
    return (y * g.astype(jnp.float32)).astype(x.dtype)


def _modulate(h, shift, scale):
    return h * (1 + scale) + shift


def _l2norm(t):
    return t * lax.rsqrt(jnp.sum(t * t, axis=-1, keepdims=True) + EPS)


def _dwconv(x, w):
    return lax.conv_general_dilated(x, w[:, None, :].astype(x.dtype), (1,), ((CONV_W // 2 - 1, CONV_W // 2),),
                                    dimension_numbers=('NWC', 'WIO', 'NWC'), feature_group_count=x.shape[-1])


def _to_col_major(h, rows):
    b, l, d = h.shape
    return h.reshape(b, rows, GRID_W, d).transpose(0, 2, 1, 3).reshape(b, l, d)


def _from_col_major(h, rows):
    b, l, d = h.shape
    return h.reshape(b, GRID_W, rows, d).transpose(0, 2, 1, 3).reshape(b, l, d)


def _gdn_chunk(q, k, v, beta, g, s0):
    bsz, nh, L, dk = q.shape
    dv = v.shape[-1]
    nc = L // CHUNK
    q, k, v = (t.reshape(bsz, nh, nc, CHUNK, t.shape[-1]) for t in (q, k, v))
    beta = beta.reshape(bsz, nh, nc, CHUNK)
    gc = jnp.cumsum(g.reshape(bsz, nh, nc, CHUNK), axis=-1)
    incl = jnp.tril(jnp.ones((CHUNK, CHUNK), dtype=bool))
    strict = jnp.tril(jnp.ones((CHUNK, CHUNK), dtype=bool), -1)
    gamma = jnp.exp(jnp.where(incl, gc[..., :, None] - gc[..., None, :], -jnp.inf))
    kk = jnp.einsum('bhncd,bhnsd->bhncs', k, k)
    m_low = jnp.where(strict, beta[..., None] * kk * gamma, 0.0)
    rhs = jnp.concatenate([beta[..., None] * v, beta[..., None] * k * jnp.exp(gc)[..., None]], axis=-1)
    sol = lax.linalg.triangular_solve(m_low, rhs, left_side=True, lower=True, unit_diagonal=True)
    u, w = sol[..., :dv], sol[..., dv:]
    qk = jnp.einsum('bhncd,bhnsd->bhncs', q, k) * gamma
    q_dec = q * jnp.exp(gc)[..., None]
    k_dec = k * jnp.exp(gc[..., -1:] - gc)[..., None]
    g_last = jnp.exp(gc[..., -1])

    def step(s, xs):
        u_c, w_c, qk_c, qd_c, kd_c, gl_c = xs
        v_new = u_c - jnp.einsum('bhcd,bhde->bhce', w_c, s)
        o_c = jnp.einsum('bhcd,bhde->bhce', qd_c, s) + jnp.einsum('bhcs,bhse->bhce', qk_c, v_new)
        s = s * gl_c[..., None, None] + jnp.einsum('bhcd,bhce->bhde', kd_c, v_new)
        return s, o_c

    xs = tuple(jnp.moveaxis(t, 2, 0) for t in (u, w, qk, q_dec, k_dec, g_last))
    s_fin, o = lax.scan(step, s0, xs)
    return s_fin, jnp.moveaxis(o, 0, 2).reshape(bsz, nh, L, dv)


def _gdn_prep(p, conv_w, alog, dtb):
    bsz, L, _ = p.shape
    qkv, z, alpha, beta = _split(p, (A_QKV, A_WIDTH, 2 * A_HEADS, 2 * A_HEADS))
    qkv = jax.nn.silu(_dwconv(qkv, conv_w)).astype(jnp.float32)
    q, k, v = _split(qkv, (A_QK, A_QK, A_WIDTH))
    q = _l2norm(q.reshape(bsz, L, A_HEADS, A_DK).transpose(0, 2, 1, 3)) * A_DK ** -0.5
    k = _l2norm(k.reshape(bsz, L, A_HEADS, A_DK).transpose(0, 2, 1, 3))
    v = v.reshape(bsz, L, A_HEADS, A_DV).transpose(0, 2, 1, 3)
    g = -jnp.exp(alog) * jax.nn.softplus(alpha.astype(jnp.float32).reshape(bsz, L, 2, A_HEADS) + dtb)
    bt = jax.nn.sigmoid(beta.astype(jnp.float32).reshape(bsz, L, 2, A_HEADS))
    return q, k, v, g.transpose(2, 0, 3, 1), bt.transpose(2, 0, 3, 1), z


def _gdn_out(o, z, norm_w):
    bsz, nh, L, dv = o.shape
    o = o.transpose(0, 2, 1, 3)
    o = o * lax.rsqrt(jnp.mean(o * o, axis=-1, keepdims=True) + EPS) * norm_w.astype(jnp.float32)
    zg = jax.nn.silu(z.astype(jnp.float32)).reshape(bsz, L, nh, dv)
    return (o * zg).reshape(bsz, L, nh * dv).astype(z.dtype)


def _gdn_mixer(pc, pl, conv_w, alog, dtb, norm_w):
    qc, kc, vc, gc, bc, zc = _gdn_prep(pc, conv_w, alog, dtb)
    ql, kl, vl, gl, bl, zl = _gdn_prep(pl, conv_w, alog, dtb)
    zero = jnp.zeros((pl.shape[0], A_HEADS, A_DK, A_DV), jnp.float32)
    oc, ol = [], []
    for d in range(2):
        s_c, o_c = _gdn_chunk(*(_dirflip(t, d, 2) for t in (qc, kc, vc, bc[d], gc[d])), zero)
        _, o_l = _gdn_chunk(*(_dirflip(t, d, 2) for t in (ql, kl, vl, bl[d], gl[d])), s_c)
        oc.append(_dirflip(o_c, d, 2))
        ol.append(_dirflip(o_l, d, 2))
    return _gdn_out(oc[0] + oc[1], zc, norm_w), _gdn_out(ol[0] + ol[1], zl, norm_w)


def _lru_coeffs(x, wa, ba, wx, bx, lam):
    bsz, L, _ = x.shape
    xb = x.reshape(bsz, L, B_BLOCKS, B_BDIM)
    r = jax.nn.sigmoid(jnp.einsum('blnd,nde->blne', xb, wa) + ba)
    i = jax.nn.sigmoid(jnp.einsum('blnd,nde->blne', xb, wx) + bx)
    log_a = -LRU_C * jax.nn.softplus(-lam) * r
    a = jnp.exp(log_a)
    b = jnp.sqrt(-jnp.expm1(2.0 * log_a)) * (i * xb)
    return a.reshape(bsz, L, B_WIDTH), b.reshape(bsz, L, B_WIDTH)


def _linear_scan(a, b, h0):
    def comb(left, right):
        return left[0] * right[0], right[0] * left[1] + right[1]
    a_cum, h = lax.associative_scan(comb, (a, b), axis=1)
    return h + a_cum * h0[:, None, :]


def _lru_mixer(pc, pl, conv_w, conv_b, wa, ba, wx, bx, lam):
    def prep(p):
        xb, gate = _split(p, (B_WIDTH, B_WIDTH))
        return (_dwconv(xb, conv_w) + conv_b).astype(jnp.float32), gate
    xc, gtc = prep(pc)
    xl, gtl = prep(pl)
    hs_c, hs_l = [], []
    for d in range(2):
        ac, bcf = _lru_coeffs(_dirflip(xc, d, 1), wa[d], ba[d], wx[d], bx[d], lam[d])
        al, blf = _lru_coeffs(_dirflip(xl, d, 1), wa[d], ba[d], wx[d], bx[d], lam[d])
        h_c = _linear_scan(ac, bcf, jnp.zeros_like(xc[:, 0]))
        h_l = _linear_scan(al, blf, h_c[:, -1])
        hs_c.append(_dirflip(h_c, d, 1))
        hs_l.append(_dirflip(h_l, d, 1))
    out = lambda h, gate: (h * jax.nn.gelu(gate.astype(jnp.float32))).astype(gate.dtype)
    return out(hs_c[0] + hs_c[1], gtc), out(hs_l[0] + hs_l[1], gtl)


def _even_mixer(hc, hl, w_in, w_out, a_conv, a_alog, a_dtb, a_norm, b_conv_w, b_conv_b, b_wa, b_ba, b_wx, b_bx, b_lam):
    pa_c, pb_c = _split(hc @ w_in, (A_COLS, B_COLS))
    pa_l, pb_l = _split(hl @ w_in, (A_COLS, B_COLS))
    ya_c, ya_l = _gdn_mixer(pa_c, pa_l, a_conv, a_alog, a_dtb, a_norm)
    yb_c, yb_l = _lru_mixer(pb_c, pb_l, b_conv_w, b_conv_b, b_wa, b_ba, b_wx, b_bx, b_lam)
    yc = jnp.concatenate([ya_c, yb_c], axis=-1) @ w_out
    yl = jnp.concatenate([ya_l, yb_l], axis=-1) @ w_out
    return yc, yl


def _mlstm_chunk(q, k, v, logi, logf, state, with_out):
    bsz, nh, L, dk = q.shape
    dv = v.shape[-1]
    nc = L // CHUNK
    q, k, v = (t.reshape(bsz, nh, nc, CHUNK, t.shape[-1]) for t in (q, k, v))
    logi = logi.reshape(bsz, nh, nc, CHUNK)
    bcum = jnp.cumsum(logf.reshape(bsz, nh, nc, CHUNK), axis=-1)
    b_last = bcum[..., -1]
    w_end = b_last[..., None] - bcum + logi
    m_chunk = jnp.max(w_end, axis=-1)
    e_end = jnp.exp(w_end - m_chunk[..., None])
    c_chunk = jnp.einsum('bhncd,bhnce->bhnde', k * e_end[..., None], v)
    n_chunk = jnp.einsum('bhncd,bhnc->bhnd', k, e_end)

    def step(carry, xs):
        c_s, n_s, m_s = carry
        cc, ncc, mc, bl = xs
        m_new = jnp.maximum(bl + m_s, mc)
        a_old = jnp.exp(bl + m_s - m_new)
        a_new = jnp.exp(mc - m_new)
        c_new = a_old[..., None, None] * c_s + a_new[..., None, None] * cc
        n_new = a_old[..., None] * n_s + a_new[..., None] * ncc
        return (c_new, n_new, m_new), ((c_s, n_s, m_s) if with_out else None)

    xs = tuple(jnp.moveaxis(t, 2, 0) for t in (c_chunk, n_chunk, m_chunk, b_last))
    final, prev = lax.scan(step, state, xs)
    if not with_out:
        return final, None
    c0, n0, m0 = (jnp.moveaxis(t, 0, 2) for t in prev)
    incl = jnp.tril(jnp.ones((CHUNK, CHUNK), dtype=bool))
    log_d = jnp.where(incl, bcum[..., :, None] - bcum[..., None, :] + logi[..., None, :], -jnp.inf)
    m_intra = jnp.max(log_d, axis=-1)
    s_qk = jnp.einsum('bhncd,bhnsd->bhncs', q, k) * jnp.exp(log_d - m_intra[..., None])
    m_inter = bcum + m0[..., None]
    m_tot = jnp.maximum(m_inter, m_intra)
    w_inter = jnp.exp(m_inter - m_tot)
    w_intra = jnp.exp(m_intra - m_tot)
    num = (w_inter[..., None] * jnp.einsum('bhncd,bhnde->bhnce', q, c0)
           + w_intra[..., None] * jnp.einsum('bhncs,bhnse->bhnce', s_qk, v))
    den = w_inter * jnp.einsum('bhncd,bhnd->bhnc', q, n0) + w_intra * jnp.sum(s_qk, axis=-1)
    h = num / jnp.maximum(jnp.abs(den), jnp.exp(-m_tot))[..., None]
    return final, h.reshape(bsz, nh, L, dv)


def _mlstm_prep(s, ibias, fbias):
    bsz, L, _ = s.shape
    s = s.astype(jnp.float32)
    q, k, v, ig, fg = _split(s, (C_QK, C_QK, C_WIDTH, 2 * C_HEADS, 2 * C_HEADS))
    q = q.reshape(bsz, L, C_HEADS, C_DQK).transpose(0, 2, 1, 3)
    k = k.reshape(bsz, L, C_HEADS, C_DQK).transpose(0, 2, 1, 3) * C_DQK ** -0.5
    v = v.reshape(bsz, L, C_HEADS, C_DV).transpose(0, 2, 1, 3)
    logi = (ig.reshape(bsz, L, 2, C_HEADS) + ibias).transpose(2, 0, 3, 1)
    logf = jax.nn.log_sigmoid(fg.reshape(bsz, L, 2, C_HEADS) + fbias).transpose(2, 0, 3, 1)
    return q, k, v, logi, logf


def _mlstm_out(h, o, norm_w):
    bsz, nh, L, dv = h.shape
    h = h.transpose(0, 2, 1, 3)
    h = h * lax.rsqrt(jnp.mean(h * h, axis=-1, keepdims=True) + EPS) * norm_w.astype(jnp.float32).reshape(nh, dv)
    return (h.reshape(bsz, L, nh * dv) * jax.nn.sigmoid(o.astype(jnp.float32))).astype(o.dtype)


def _pool_mixer(x, seg, w_grp, scale):
    bsz, L, _ = x.shape
    xs = x.astype(jnp.float32).reshape(bsz, L // seg, seg, D_GROUPS, D_GDIM)
    csum = jnp.concatenate([jnp.zeros_like(xs[:, :, :1]), jnp.cumsum(xs, axis=2)], axis=2)
    t = jnp.arange(seg)
    outs = []
    for gi, w in enumerate(POOL_SIZES):
        lo = jnp.maximum(t - w // 2, 0)
        hi = jnp.minimum(t - w // 2 + w, seg)
        cg = csum[..., gi, :]
        mean = (jnp.take(cg, hi, axis=2) - jnp.take(cg, lo, axis=2)) / (hi - lo).astype(jnp.float32)[:, None]
        outs.append(mean - xs[..., gi, :])
    pooled = jnp.stack(outs, axis=3)
    y = jnp.einsum('bnsgd,gde->bnsge', pooled, w_grp).reshape(bsz, L, D_WIDTH) * scale
    return y.astype(x.dtype)


def _odd_mixer(hc, hl, rows, w_in, w_out, ibias, fbias, norm_w, d_w, d_scale, with_ctx):
    sl, ol, dl = _split(hl @ w_in, (C_STATE_COLS, C_WIDTH, D_WIDTH))
    if with_ctx:
        sc, oc, dc = _split(hc @ w_in, (C_STATE_COLS, C_WIDTH, D_WIDTH))
    else:
        sc = hc @ w_in[:, :C_STATE_COLS]
    ql, kl, vl, il, fl = _mlstm_prep(sl, ibias, fbias)
    qc, kc, vc, ic, fc = _mlstm_prep(sc, ibias, fbias)
    bsz = hl.shape[0]
    zero = (jnp.zeros((bsz, C_HEADS, C_DQK, C_DV), jnp.float32), jnp.zeros((bsz, C_HEADS, C_DQK), jnp.float32),
            jnp.zeros((bsz, C_HEADS), jnp.float32))
    hs_c, hs_l = [], []
    for d in range(2):
        st_c, h_c = _mlstm_chunk(*(_dirflip(t, d, 2) for t in (qc, kc, vc, ic[d], fc[d])), zero, with_ctx)
        _, h_l = _mlstm_chunk(*(_dirflip(t, d, 2) for t in (ql, kl, vl, il[d], fl[d])), st_c, True)
        hs_l.append(_dirflip(h_l, d, 2))
        if with_ctx:
            hs_c.append(_dirflip(h_c, d, 2))
    yl = jnp.concatenate([_mlstm_out(hs_l[0] + hs_l[1], ol, norm_w), _pool_mixer(dl, rows, d_w, d_scale)], axis=-1) @ w_out
    if not with_ctx:
        return None, yl
    yc = jnp.concatenate([_mlstm_out(hs_c[0] + hs_c[1], oc, norm_w), _pool_mixer(dc, dc.shape[1], d_w, d_scale)], axis=-1) @ w_out
    return yc, yl


def _peer(h, wq, keys, u_tab, v_tab):
    n_tok, dm = h.shape
    q = (h @ wq).reshape(n_tok, PEER_HEADS, 2, PEER_QDIM // 2)
    s = jnp.einsum('thpd,hpkd->thpk', q, keys)
    s1, i1 = lax.top_k(s[:, :, 0], PEER_TOPK)
    s2, i2 = lax.top_k(s[:, :, 1], PEER_TOPK)
    cand = (s1[..., :, None] + s2[..., None, :]).reshape(n_tok, PEER_HEADS, PEER_TOPK * PEER_TOPK)
    sc, ci = lax.top_k(cand, PEER_TOPK)
    idx = (jnp.take_along_axis(i1, ci // PEER_TOPK, axis=-1) * N_KEYS
           + jnp.take_along_axis(i2, ci % PEER_TOPK, axis=-1))
    gate = jax.nn.softmax(sc.astype(jnp.float32), axis=-1).astype(h.dtype)
    nb = n_tok // PEER_BLOCK

    def block(args):
        hb, ib, gb = args
        act = jax.nn.gelu(jnp.einsum('thkd,td->thk', jnp.take(u_tab, ib, axis=0), hb))
        return jnp.einsum('thk,thkd->td', gb * act, jnp.take(v_tab, ib, axis=0))

    out = lax.map(block, (h.reshape(nb, PEER_BLOCK, dm), idx.reshape(nb, PEER_BLOCK, PEER_HEADS, PEER_TOPK),
                          gate.reshape(nb, PEER_BLOCK, PEER_HEADS, PEER_TOPK)))
    return out.reshape(n_tok, dm)


def setup_inputs(seed: int = 0) -> dict:
    key = jax.random.key(seed)
    ks = iter(jax.random.split(key, 40))
    nrm = lambda shape, scale: jax.random.normal(next(ks), shape, jnp.float32) * scale
    gain = lambda shape: 1.0 + nrm(shape, 0.05)
    D = D_MODEL
    x = nrm((BATCH, SEQ, D), 1.0)
    c = nrm((BATCH, D), 1.0)
    ctx = nrm((BATCH, CTX_LEN, D), 1.0)
    c_ctx = nrm((D,), 1.0)
    ada_w = nrm((DEPTH, D, 6 * D), 0.3 * D ** -0.5)
    ada_b = nrm((DEPTH, 6 * D), 0.02)
    norm_mix = gain((DEPTH, D))
    norm_ffn = gain((DEPTH, D))
    final_norm = gain((D,))
    peer_wq = nrm((DEPTH, D, PEER_HEADS * PEER_QDIM), D ** -0.5)
    peer_keys = nrm((DEPTH, PEER_HEADS, 2, N_KEYS, PEER_QDIM // 2), (PEER_QDIM // 2) ** -0.5)
    peer_u = nrm((DEPTH, N_EXPERTS, D), D ** -0.5)
    peer_v = nrm((DEPTH, N_EXPERTS, D), 1.0)
    ev_w_in = nrm((N_EVEN, D, EVEN_IN), D ** -0.5)
    ev_w_out = nrm((N_EVEN, MIX_WIDTH, D), MIX_WIDTH ** -0.5)
    a_conv = nrm((N_EVEN, CONV_W, A_QKV), 0.5)
    a_alog = jnp.log(jax.random.uniform(next(ks), (N_EVEN, 2, A_HEADS), jnp.float32, 1.0, 16.0))
    dt = jnp.exp(jax.random.uniform(next(ks), (N_EVEN, 2, A_HEADS), jnp.float32, math.log(1e-3), math.log(1e-1)))
    a_dtb = dt + jnp.log(-jnp.expm1(-dt))
    a_norm = gain((N_EVEN, A_DV))
    b_conv_w = nrm((N_EVEN, CONV_W, B_WIDTH), 0.5)
    b_conv_b = nrm((N_EVEN, B_WIDTH), 0.02)
    b_wa = nrm((N_EVEN, 2, B_BLOCKS, B_BDIM, B_BDIM), B_BDIM ** -0.5)
    b_ba = nrm((N_EVEN, 2, B_BLOCKS, B_BDIM), 0.02)
    b_wx = nrm((N_EVEN, 2, B_BLOCKS, B_BDIM, B_BDIM), B_BDIM ** -0.5)
    b_bx = nrm((N_EVEN, 2, B_BLOCKS, B_BDIM), 0.02)
    a8 = jax.random.uniform(next(ks), (N_EVEN, 2, B_BLOCKS, B_BDIM), jnp.float32, 0.9, 0.999)
    p_lam = a8 ** (1.0 / LRU_C)
    b_lam = jnp.log(p_lam) - jnp.log1p(-p_lam)
    od_w_in = nrm((N_ODD, D, ODD_IN), D ** -0.5)
    od_w_out = nrm((N_ODD, MIX_WIDTH, D), MIX_WIDTH ** -0.5)
    c_ibias = nrm((N_ODD, 2, C_HEADS), 0.1)
    c_fbias = jnp.linspace(3.0, 6.0, C_HEADS, dtype=jnp.float32) + nrm((N_ODD, 2, C_HEADS), 0.1)
    c_norm = gain((N_ODD, C_WIDTH))
    d_w = nrm((N_ODD, D_GROUPS, D_GDIM, D_GDIM), D_GDIM ** -0.5)
    d_scale = 1.0 + nrm((N_ODD, D_WIDTH), 0.1)
    return {'x': x, 'c': c, 'ctx': ctx, 'c_ctx': c_ctx, 'ada_w': ada_w, 'ada_b': ada_b,
            'norm_mix': norm_mix, 'norm_ffn': norm_ffn, 'final_norm': final_norm,
            'peer_wq': peer_wq, 'peer_keys': peer_keys, 'peer_u': peer_u, 'peer_v': peer_v,
            'ev_w_in': ev_w_in, 'ev_w_out': ev_w_out, 'a_conv': a_conv, 'a_alog': a_alog, 'a_dtb': a_dtb,
            'a_norm': a_norm, 'b_conv_w': b_conv_w, 'b_conv_b': b_conv_b, 'b_wa': b_wa, 'b_ba': b_ba,
            'b_wx': b_wx, 'b_bx': b_bx, 'b_lam': b_lam, 'od_w_in': od_w_in, 'od_w_out': od_w_out,
            'c_ibias': c_ibias, 'c_fbias': c_fbias, 'c_norm': c_norm, 'd_w': d_w, 'd_scale': d_scale}


def reference(x, c, ctx, c_ctx, ada_w, ada_b, norm_mix, norm_ffn, final_norm, peer_wq, peer_keys, peer_u, peer_v,
              ev_w_in, ev_w_out, a_conv, a_alog, a_dtb, a_norm, b_conv_w, b_conv_b, b_wa, b_ba, b_wx, b_bx, b_lam,
              od_w_in, od_w_out, c_ibias, c_fbias, c_norm, d_w, d_scale):
    bsz, seq, dm = x.shape
    rows = seq // GRID_W
    n_ctx = ctx.shape[1]
    xl, xc = x, ctx
    for l in range(DEPTH):
        with_ctx = l < DEPTH - 1
        sh1, sc1, g1, sh2, sc2, g2 = (m[:, None, :] for m in jnp.split(jax.nn.silu(c) @ ada_w[l] + ada_b[l], 6, axis=-1))
        n_mod = 6 if with_ctx else 2
        cmods = jnp.split(jax.nn.silu(c_ctx) @ ada_w[l][:, :n_mod * dm] + ada_b[l][:n_mod * dm], n_mod)
        hl = _modulate(_rmsnorm(xl, norm_mix[l]), sh1, sc1)
        hc = _modulate(_rmsnorm(xc, norm_mix[l]), cmods[0], cmods[1])
        j = l // 2
        if l % 2 == 0:
            yc, yl = _even_mixer(hc, hl, ev_w_in[j], ev_w_out[j], a_conv[j], a_alog[j], a_dtb[j], a_norm[j],
                                 b_conv_w[j], b_conv_b[j], b_wa[j], b_ba[j], b_wx[j], b_bx[j], b_lam[j])
        else:
            yc, yl = _odd_mixer(hc, _to_col_major(hl, rows), rows, od_w_in[j], od_w_out[j], c_ibias[j], c_fbias[j],
                                c_norm[j], d_w[j], d_scale[j], with_ctx)
            yl = _from_col_major(yl, rows)
        xl = xl + g1 * yl
        hl = _modulate(_rmsnorm(xl, norm_ffn[l]), sh2, sc2)
        if with_ctx:
            xc = xc + cmods[2] * yc
            hc = _modulate(_rmsnorm(xc, norm_ffn[l]), cmods[3], cmods[4])
            f = _peer(jnp.concatenate([hc.reshape(-1, dm), hl.reshape(-1, dm)], axis=0),
                      peer_wq[l], peer_keys[l], peer_u[l], peer_v[l])
            n_ct = bsz * n_ctx
            xc = xc + cmods[5] * f[:n_ct].reshape(xc.shape)
            xl = xl + g2 * f[n_ct:].reshape(xl.shape)
        else:
            xl = xl + g2 * _peer(hl.reshape(-1, dm), peer_wq[l], peer_keys[l], peer_u[l], peer_v[l]).reshape(xl.shape)
    return _rmsnorm(xl, final_norm)
```

```python
import numpy as np
from contextlib import ExitStack
import concourse.bass as bass
import concourse.mybir as mybir
from concourse.bass_utils import run_bass_kernel_spmd

F32 = mybir.dt.float32
U32 = mybir.dt.uint32
I32 = mybir.dt.int32
AF = mybir.ActivationFunctionType
ALU = mybir.AluOpType
AX = mybir.AxisListType

NCORES = 8
NB = 2
D = 1024
KC = 8
LC = 256
LL = 2048
LT = LC + LL
NT = LT // 128
EPS = 1e-6
GRID_W = 64
ROWS = LL // GRID_W


class KB:
    def __init__(self, nc, es, ndma=16):
        self.nc = nc
        self.eng = {'pe': nc.tensor, 'act': nc.scalar, 'dve': nc.vector, 'pool': nc.gpsimd, 'sp': nc.sync}
        self.sems = {}
        self.val = {}
        for e in ['pe', 'act', 'dve', 'pool']:
            self.sems[e] = es.enter_context(nc.semaphore('s_' + e))
            self.val[e] = 0
        self.dq = {}
        for q in ['sp', 'pool', 'act']:
            ids = []
            for i in range(ndma):
                sid = ('d', q, i)
                self.sems[sid] = es.enter_context(nc.semaphore(f'd_{q}_{i}'))
                self.val[sid] = 0
                ids.append(sid)
            self.dq[q] = [ids, 0]
        self.seen = {e: {} for e in self.eng}
        self.lastw = {}
        self.readers = {}
        self.n_inst = 0

    def _waits(self, eng, reads, writes, extra=()):
        need = {}

        def add(ev):
            if ev is None:
                return
            sid, v = ev
            if need.get(sid, 0) < v:
                need[sid] = v
        for r in reads:
            add(self.lastw.get(r))
        for w in writes:
            add(self.lastw.get(w))
            for ev in self.readers.get(w, {}).items():
                add(ev)
        for ev in extra:
            add(ev)
        e = self.eng[eng]
        for sid, v in need.items():
            if sid == 'pe' and eng == 'pe':
                continue
            if self.seen[eng].get(sid, 0) >= v:
                continue
            self.seen[eng][sid] = v
            e.wait_ge(self.sems[sid], v)
            self.n_inst += 1

    def _record(self, ev, reads, writes):
        sid, v = ev
        for w in writes:
            self.lastw[w] = ev
            self.readers[w] = {}
        for r in reads:
            d = self.readers.setdefault(r, {})
            if d.get(sid, 0) < v:
                d[sid] = v

    def I(self, eng, reads, writes, fn):
        self._waits(eng, reads, writes)
        ins = fn(self.eng[eng])
        self.val[eng] += 1
        ins.then_inc(self.sems[eng], 1)
        self._record((eng, self.val[eng]), reads, writes)
        self.n_inst += 1
        return ins

    def DMA(self, q, reads, writes, fn):
        ids, rr = self.dq[q]
        sid = ids[rr % len(ids)]
        self.dq[q][1] = rr + 1
        self._waits(q, reads, writes, extra=[(sid, self.val[sid])] if self.val[sid] else ())
        ins = fn(self.eng[q])
        self.val[sid] += 16
        ins.then_inc(self.sems[sid], 16)
        self._record((sid, self.val[sid]), reads, writes)
        self.n_inst += 1
        return ins

    def barrier(self):
        for eng in self.eng:
            e = self.eng[eng]
            for sid, v in self.val.items():
                if v == 0 or self.seen[eng].get(sid, 0) >= v:
                    continue
                if sid == eng:
                    continue
                self.seen[eng][sid] = v
                e.wait_ge(self.sems[sid], v)
                self.n_inst += 1

    def finish(self):
        e = self.eng['sp']
        for sid, v in self.val.items():
            if v and self.seen['sp'].get(sid, 0) < v:
                e.wait_ge(self.sems[sid], v)


class Ring:
    def __init__(self, items):
        self.items = items
        self.i = 0

    def next(self):
        it = self.items[self.i % len(self.items)]
        self.i += 1
        return it


def bcast_rows(ap_1d_row, nparts):
    return ap_1d_row.to_broadcast([nparts, ap_1d_row.shape[-1]])


def build_program(stop_after=None, dbg=None):
    nc = bass.Bass("TRN2", target_bir_lowering=False)
    es = ExitStack()
    kb = KB(nc, es)
    dbg = dbg or {}

    def din(name, shape, dt=F32):
        return nc.dram_tensor(name, list(shape), dt, kind="ExternalInput").ap()

    def dscr(name, shape, dt=F32):
        return nc.dram_tensor(name, list(shape), dt, kind="Internal").ap()

    def dout(name, shape, dt=F32):
        return nc.dram_tensor(name, list(shape), dt, kind="ExternalOutput").ap()

    uid = [0]

    def sb(name, shape, dt=F32, stack=es):
        uid[0] += 1
        return stack.enter_context(nc.sbuf_tensor(f"{name}_u{uid[0]}", list(shape), dt))

    xin = din("xin", [NB, LT, D])
    c3T = din("c3T", [128, KC, 3])
    ada_w = din("ada_w", [2, KC, 128, 6 * D])
    ada_b = din("ada_b", [2, 1, 6 * D])
    norms = din("norms", [5, 1, D])
    ident_d = din("ident", [128, 128])
    modsD = dscr("modsD", [2, 3, 6 * D])
    out_d = dout("out", [NB, LL, D])
    dbg_out = {}
    for k, shp in dbg.items():
        dbg_out[k] = dout("dbg_" + k, shp)

    ident = sb("ident_sb", [128, 128])
    ps = [es.enter_context(nc.psum_tensor(f"ps{i}", [128, 512], F32)) for i in range(8)]
    psk = [f"ps{i}" for i in range(8)]
    kb.DMA('sp', [], ['ident'], lambda e: e.dma_start(out=ident[:], in_=ident_d[:, :]))
    epsc = sb("epsc", [128, 1])
    kb.I('dve', [], ['epsc'], lambda e: e.memset(epsc[:], EPS))

    with ExitStack() as ph:
        sT = sb("sT", [128, KC, 3], stack=ph)
        kb.DMA('sp', [], ['sT'], lambda e: e.dma_start(out=sT[:], in_=c3T[:, :, :]))
        kb.I('act', ['sT'], ['sT'], lambda e: e.activation(out=sT[:], in_=sT[:], func=AF.Silu))
        wbuf = [sb(f"adaw{i}", [128, 3072], stack=ph) for i in range(3)]
        wring = Ring(list(range(3)))
        bias_t = sb("adab", [3, 6 * D], stack=ph)
        mods_t = sb("mods_t", [3, 6 * D], stack=ph)
        for l in range(2):
            kb.DMA('sp', ['mods_st'], ['adab'], lambda e: e.dma_start(out=bias_t[:], in_=ada_b[l].to_broadcast([3, 6 * D])))
            for half in range(2):
                for k in range(KC):
                    wi = wring.next()
                    kb.DMA('sp', [], [f'adaw{wi}'], lambda e: e.dma_start(out=wbuf[wi][:], in_=ada_w[l, k, :, half * 3072:(half + 1) * 3072]))
                    for j in range(6):
                        kb.I('pe', ['sT', f'adaw{wi}'], [psk[j]], lambda e: e.matmul(ps[j][0:3, :], lhsT=sT[:, k, :], rhs=wbuf[wi][:, j * 512:(j + 1) * 512], start=(k == 0), stop=(k == KC - 1)))
                for j in range(6):
                    c0 = half * 3072 + j * 512
                    kb.I('dve', [psk[j], 'adab'], ['mods_t'], lambda e: e.tensor_tensor(out=mods_t[:, c0:c0 + 512], in0=ps[j][0:3, :], in1=bias_t[:, c0:c0 + 512], op=ALU.add))
            kb.DMA('sp', ['mods_t'], [('modsD', l), 'mods_st'], lambda e: e.dma_start(out=modsD[l], in_=mods_t[:]))
    kb.barrier()
    if 'mods' in dbg_out:
        kb.DMA('sp', [('modsD', 0), ('modsD', 1)], ['dbg_mods'], lambda e: e.dma_start(out=dbg_out['mods'], in_=modsD))
    if stop_after == 'mods':
        kb.finish(); es.close(); return nc

    ev_w_in = din("ev_w_in", [128, KC, 3088])
    ev_w_out = din("ev_w_out", [128, KC, D])
    a_convT = din("a_convT", [1536, 4])
    a_gc = din("a_gc", [1, 16])
    a_norm = din("a_norm", [1, 128])
    lru_pc = din("lru_pc", [512, 11])
    lru_w = din("lru_w", [2, 2, 4, 128, 128])
    masks_d = din("masks", [6, 128, 128])
    mixT = dscr("mixT", [NB, KC, 128, LT])
    l1only = (stop_after or '').startswith('l1only')
    xs = din("xs_in", [NB, LT, D]) if l1only else dscr("xs", [NB, LT, D])

    ones = sb("ones_sb", [128, 128])
    kb.I('dve', [], ['ones'], lambda e: e.memset(ones[:], 1.0))
    onec = sb("onec", [128, 1])
    kb.I('dve', [], ['onec'], lambda e: e.memset(onec[:], 1.0))
    msk = sb("msk", [128, 6, 128])
    for i in range(6):
        kb.DMA('sp', [], ['msk'], lambda e: e.dma_start(out=msk[:, i, :], in_=masks_d[i]))
    psr = Ring(list(range(8)))

    def mm(out, lhsT, rhs, reads, writes, start=True, stop=True):
        kb.I('pe', reads, writes, lambda e: e.matmul(out, lhsT=lhsT, rhs=rhs, start=start, stop=stop))

    def tr(out, in_, reads, writes):
        kb.I('pe', reads + ['ident'], writes, lambda e: e.transpose(out=out, in_=in_, identity=ident[:]))

    TOKBLKS = [(0, 256)] + [(256 + i * 512, 512) for i in range(4)]

    def norm_mod_T(ph, l, b, src_ap_fn, norm_idx, slot_sh, slot_sc, hT, keep_h=None, tiles=range(NT)):
        with ExitStack() as st:
            nw = sb("nm_nw", [128, D], stack=st)
            kb.DMA('sp', [], ['nm_nw'], lambda e: e.dma_start(out=nw[:], in_=norms[norm_idx].to_broadcast([128, D])))
            md = {}
            for kind, row in (('c', 2), ('l', b)):
                A = sb("nm_A" + kind, [128, D], stack=st)
                S = sb("nm_S" + kind, [128, D], stack=st)
                kb.DMA('sp', [('modsD', l)], ['nm_A' + kind], lambda e: e.dma_start(out=A[:], in_=modsD[l, row:row + 1, slot_sc * D:(slot_sc + 1) * D].to_broadcast([128, D])))
                kb.DMA('sp', [('modsD', l)], ['nm_S' + kind], lambda e: e.dma_start(out=S[:], in_=modsD[l, row:row + 1, slot_sh * D:(slot_sh + 1) * D].to_broadcast([128, D])))
                kb.I('dve', ['nm_A' + kind, 'nm_nw'], ['nm_A' + kind], lambda e: e.scalar_tensor_tensor(out=A[:], in0=A[:], scalar=1.0, in1=nw[:], op0=ALU.add, op1=ALU.mult))
                md[kind] = (A, S)
            xt = [sb(f"nm_x{i}", [128, D], stack=st) for i in range(2)]
            ht = [sb(f"nm_h{i}", [128, D], stack=st) for i in range(2)]
            sq = sb("nm_sq", [128, D], stack=st)
            ss = [sb(f"nm_ss{i}", [128, 1], stack=st) for i in range(2)]
            for t in tiles:
                i = t % 2
                kind = 'c' if t < 2 else 'l'
                A, S = md[kind]
                src, srck = src_ap_fn(b, t)
                kb.DMA('sp', [srck], [f'nm_x{i}'], lambda e: e.dma_start(out=xt[i][:], in_=src))
                kb.I('act', [f'nm_x{i}'], ['nm_sq', f'nm_ss{i}'], lambda e: e.activation(out=sq[:], in_=xt[i][:], func=AF.Square, accum_out=ss[i][:]))
                kb.I('act', [f'nm_ss{i}', 'epsc'], [f'nm_ss{i}'], lambda e: e.activation(out=ss[i][:], in_=ss[i][:], func=AF.Sqrt, scale=1.0 / D, bias=epsc[:]))
                kb.I('dve', [f'nm_ss{i}'], [f'nm_ss{i}'], lambda e: e.reciprocal(out=ss[i][:], in_=ss[i][:]))
                kb.I('dve', [f'nm_x{i}', f'nm_ss{i}', 'nm_A' + kind], [f'nm_h{i}'], lambda e: e.scalar_tensor_tensor(out=ht[i][:], in0=xt[i][:], scalar=ss[i][:], in1=A[:], op0=ALU.mult, op1=ALU.mult))
                kb.I('dve', [f'nm_h{i}', 'nm_S' + kind], [f'nm_h{i}'], lambda e: e.tensor_tensor(out=ht[i][:], in0=ht[i][:], in1=S[:], op=ALU.add))
                if keep_h is not None:
                    keep_h(t, ht[i], f'nm_h{i}')
                for half in range(2):
                    pb = psr.next()
                    for kk in range(4):
                        k = half * 4 + kk
                        tr(ps[pb][:, kk * 128:(kk + 1) * 128], ht[i][:, k * 128:(k + 1) * 128], [f'nm_h{i}'], [psk[pb]])
                    kb.I('act', [psk[pb]], [('hT', t)], lambda e: e.activation(out=hT[:, half * 4:half * 4 + 4, t * 128:(t + 1) * 128], in_=ps[pb][:, :].rearrange("p (k t) -> p k t", k=4), func=AF.Copy))
            kb.barrier()

    def proj_fm(hT, w_sb, wkey, dst_fn, evac):
        for (t0, n) in TOKBLKS:
            pb = psr.next()
            for k in range(KC):
                mm(ps[pb][:, 0:n], w_sb[:, k, :], hT[:, k, t0:t0 + n], [wkey] + [('hT', tt) for tt in range(t0 // 128, (t0 + n) // 128)], [psk[pb]], start=(k == 0), stop=(k == KC - 1))
            evac(pb, t0, n)

    def layer0_mixer(b):
        with ExitStack() as ph:
            hT = sb("hT", [128, KC, LT], stack=ph)
            norm_mod_T(ph, 0, b, lambda b_, t: (xin[b_, t * 128:(t + 1) * 128, :], 'xin'), 0, 0, 1, hT)
            if 'hT0' in dbg_out and b == 0:
                kb.DMA('sp', [('hT', t) for t in range(NT)], ['dbg_hT0'], lambda e: e.dma_start(out=dbg_out['hT0'], in_=hT[:]))
            if stop_after == 'hT0':
                return
            with ExitStack() as st:
                wx = sb("l_wx", [128, KC, 128], stack=st)
                wg = sb("l_wg", [128, KC, 128], stack=st)
                pc = sb("l_pc", [128, 11], stack=st)
                cl = sb("l_cl", [128, 4], stack=st)
                gw = sb("l_gw", [128, 4, 128], stack=st)
                raw = sb("l_raw", [128, LT + 6], stack=st)
                xb = sb("l_xb", [128, LT], stack=st)
                gg = sb("l_gg", [128, LT], stack=st)
                t1 = sb("l_t1", [128, LT], stack=st)
                t2 = sb("l_t2", [128, LT], stack=st)
                av = sb("l_a", [128, LT], stack=st)
                bv = sb("l_b", [128, LT], stack=st)
                hf = sb("l_hf", [128, LT], stack=st)
                hb = sb("l_hb", [128, LT], stack=st)
                kb.I('dve', [], ['l_raw'], lambda e: e.memset(raw[:], 0.0))
                RC, RL = 0, LC + 3

                def rawpos(t0):
                    return (RC + 1 + t0) if t0 < LC else (RL + 1 + (t0 - LC))
                for ct in range(4):
                    c0 = 2064 + ct * 128
                    kb.DMA('sp', [], ['l_wx'], lambda e: e.dma_start(out=wx[:], in_=ev_w_in[:, :, c0:c0 + 128]))
                    kb.DMA('sp', [], ['l_wg'], lambda e: e.dma_start(out=wg[:], in_=ev_w_in[:, :, c0 + 512:c0 + 640]))
                    kb.DMA('sp', [], ['l_pc'], lambda e: e.dma_start(out=pc[:], in_=lru_pc[ct * 128:(ct + 1) * 128, :]))
                    for ax in range(2):
                        for d in range(2):
                            kb.DMA('sp', [], ['l_gw'], lambda e: e.dma_start(out=gw[:, ax * 2 + d, :], in_=lru_w[ax, d, ct]))
                    for d in range(2):
                        kb.I('act', ['l_pc'], ['l_cl'], lambda e: e.activation(out=cl[:, d:d + 1], in_=pc[:, 7 + 3 * d:8 + 3 * d], func=AF.Exp, scale=-1.0))
                    kb.I('act', ['l_cl', 'onec'], ['l_cl'], lambda e: e.activation(out=cl[:, 0:2], in_=cl[:, 0:2], func=AF.Ln, bias=onec[:]))
                    kb.I('dve', ['l_cl'], ['l_cl'], lambda e: e.tensor_scalar(out=cl[:, 2:4], in0=cl[:, 0:2], scalar1=-16.0, scalar2=None, op0=ALU.mult))
                    kb.I('dve', ['l_cl'], ['l_cl'], lambda e: e.tensor_scalar(out=cl[:, 0:2], in0=cl[:, 0:2], scalar1=-8.0, scalar2=None, op0=ALU.mult))
                    proj_fm(hT, wx, 'l_wx', None, lambda pb, t0, n: kb.I('act', [psk[pb]], ['l_raw'], lambda e: e.activation(out=raw[:, rawpos(t0):rawpos(t0) + n], in_=ps[pb][:, 0:n], func=AF.Copy)))
                    for (o0, r0, L) in ((0, RC, LC), (LC, RL, LL)):
                        kb.I('dve', ['l_raw', 'l_pc'], ['l_xb'], lambda e: e.tensor_scalar(out=xb[:, o0:o0 + L], in0=raw[:, r0:r0 + L], scalar1=pc[:, 0:1], scalar2=pc[:, 4:5], op0=ALU.mult, op1=ALU.add))
                        for j in range(1, 4):
                            kb.I('dve', ['l_raw', 'l_pc', 'l_xb'], ['l_xb'], lambda e: e.scalar_tensor_tensor(out=xb[:, o0:o0 + L], in0=raw[:, r0 + j:r0 + j + L], scalar=pc[:, j:j + 1], in1=xb[:, o0:o0 + L], op0=ALU.mult, op1=ALU.add))
                    proj_fm(hT, wg, 'l_wg', None, lambda pb, t0, n: kb.I('act', [psk[pb]], ['l_t1'], lambda e: e.activation(out=t1[:, t0:t0 + n], in_=ps[pb][:, 0:n], func=AF.Copy)))
                    kb.I('dve', ['l_t1'], ['l_t2'], lambda e: e.tensor_tensor(out=t2[:], in0=t1[:], in1=t1[:], op=ALU.mult))
                    kb.I('dve', ['l_t2'], ['l_t2'], lambda e: e.tensor_scalar(out=t2[:], in0=t2[:], scalar1=0.044715, scalar2=1.0, op0=ALU.mult, op1=ALU.add))
                    kb.I('dve', ['l_t2', 'l_t1'], ['l_t2'], lambda e: e.tensor_tensor(out=t2[:], in0=t2[:], in1=t1[:], op=ALU.mult))
                    kb.I('act', ['l_t2'], ['l_t2'], lambda e: e.activation(out=t2[:], in_=t2[:], func=AF.Sigmoid, scale=1.5957691216057308))
                    kb.I('dve', ['l_t2', 'l_t1'], ['l_gg'], lambda e: e.tensor_tensor(out=gg[:], in0=t2[:], in1=t1[:], op=ALU.mult))
                    for d in range(2):
                        for (t0, n) in TOKBLKS:
                            for ax, dst, dk_ in ((0, t1, 'l_t1'), (1, t2, 'l_t2')):
                                pb = psr.next()
                                mm(ps[pb][:, 0:n], gw[:, ax * 2 + d, :], xb[:, t0:t0 + n], ['l_gw', 'l_xb'], [psk[pb]])
                                bcol = 5 + 3 * d + ax
                                kb.I('act', [psk[pb], 'l_pc'], [dk_], lambda e: e.activation(out=dst[:, t0:t0 + n], in_=ps[pb][:, 0:n], func=AF.Sigmoid, bias=pc[:, bcol:bcol + 1]))
                        kb.I('act', ['l_t1', 'l_cl'], ['l_a'], lambda e: e.activation(out=av[:], in_=t1[:], func=AF.Exp, scale=cl[:, d:d + 1]))
                        kb.I('act', ['l_t1', 'l_cl'], ['l_b'], lambda e: e.activation(out=bv[:], in_=t1[:], func=AF.Exp, scale=cl[:, 2 + d:3 + d]))
                        kb.I('dve', ['l_b'], ['l_b'], lambda e: e.tensor_scalar(out=bv[:], in0=bv[:], scalar1=-1.0, scalar2=1.0, op0=ALU.mult, op1=ALU.add))
                        kb.I('dve', ['l_b'], ['l_b'], lambda e: e.tensor_scalar(out=bv[:], in0=bv[:], scalar1=0.0, scalar2=None, op0=ALU.max))
                        kb.I('act', ['l_b'], ['l_b'], lambda e: e.activation(out=bv[:], in_=bv[:], func=AF.Sqrt))
                        kb.I('dve', ['l_b', 'l_t2'], ['l_b'], lambda e: e.tensor_tensor(out=bv[:], in0=bv[:], in1=t2[:], op=ALU.mult))
                        kb.I('dve', ['l_b', 'l_xb'], ['l_b'], lambda e: e.tensor_tensor(out=bv[:], in0=bv[:], in1=xb[:], op=ALU.mult))
                        if d == 0:
                            kb.I('dve', ['l_a', 'l_b'], ['l_hf'], lambda e: e.tensor_tensor_scan(out=hf[:, :], data0=av[:, :], data1=bv[:, :], initial=0.0, op0=ALU.mult, op1=ALU.add))
                        else:
                            kb.I('dve', ['l_a', 'l_b'], ['l_hb'], lambda e: e.tensor_tensor_scan(out=hb[:, LC - 1::-1], data0=av[:, LC - 1::-1], data1=bv[:, LC - 1::-1], initial=0.0, op0=ALU.mult, op1=ALU.add))
                            kb.I('dve', ['l_a', 'l_b', 'l_hb'], ['l_hb'], lambda e: e.tensor_tensor_scan(out=hb[:, LT - 1:LC - 1:-1], data0=av[:, LT - 1:LC - 1:-1], data1=bv[:, LT - 1:LC - 1:-1], initial=hb[:, 0:1], op0=ALU.mult, op1=ALU.add))
                    kb.I('dve', ['l_hf', 'l_hb'], ['l_hf'], lambda e: e.tensor_tensor(out=hf[:], in0=hf[:], in1=hb[:], op=ALU.add))
                    kb.I('dve', ['l_hf', 'l_gg'], ['l_hf'], lambda e: e.tensor_tensor(out=hf[:], in0=hf[:], in1=gg[:], op=ALU.mult))
                    kb.DMA('sp', ['l_hf'], [('mixT', b)], lambda e: e.dma_start(out=mixT[b, 4 + ct], in_=hf[:]))
                kb.barrier()
            if stop_after == 'lru':
                return
            gdn(ph, b, hT)
            kb.barrier()

    def gdn(ph, b, hT):
        with ExitStack() as st:
            wh = sb("g_wh", [128, 4, KC, 128], stack=st)
            wg16 = sb("g_wg16", [128, KC, 16], stack=st)
            cw = sb("g_cw", [128, 3, 4], stack=st)
            gcst = sb("g_gcst", [128, 16], stack=st)
            anw = sb("g_anw", [128, 128], stack=st)
            raw = sb("g_raw", [128, LT + 6], stack=st)
            T3 = [sb(f"g_T{i}", [128, LT], stack=st) for i in range(3)]
            tmp = sb("g_tmp", [128, LT], stack=st)
            Ktm = sb("g_Ktm", [128, NT, 128], stack=st)
            Vtm = sb("g_Vtm", [128, NT, 128], stack=st)
            zs = sb("g_zs", [128, NT, 128], stack=st)
            O = sb("g_O", [128, NT, 128], stack=st)
            graw = sb("g_graw", [128, NT, 16], stack=st)
            gG = sb("g_g", [128, NT, 8], stack=st)
            gB = sb("g_bt", [128, NT, 8], stack=st)
            RC, RL = 0, LC + 3

            def rawpos(t0):
                return (RC + 1 + t0) if t0 < LC else (RL + 1 + (t0 - LC))
            kb.I('dve', [], ['g_raw'], lambda e: e.memset(raw[:], 0.0))
            kb.DMA('sp', [], ['g_gcst'], lambda e: e.dma_start(out=gcst[:], in_=a_gc[0:1, :].to_broadcast([128, 16])))
            kb.DMA('sp', [], ['g_anw'], lambda e: e.dma_start(out=anw[:], in_=a_norm[0:1, :].to_broadcast([128, 128])))
            kb.I('act', ['g_gcst'], ['g_gcst'], lambda e: e.activation(out=gcst[:, 0:8], in_=gcst[:, 0:8], func=AF.Exp))
            kb.I('dve', ['g_gcst'], ['g_gcst'], lambda e: e.tensor_scalar(out=gcst[:, 0:8], in0=gcst[:, 0:8], scalar1=-1.0, scalar2=None, op0=ALU.mult))
            kb.DMA('sp', [], ['g_wg16'], lambda e: e.dma_start(out=wg16[:], in_=ev_w_in[:, :, 2048:2064]))
            pb = psr.next()
            for n in range(NT):
                for k in range(KC):
                    mm(ps[pb][:, n * 16:(n + 1) * 16], hT[:, k, n * 128:(n + 1) * 128], wg16[:, k, :], ['g_wg16', ('hT', n)], [psk[pb]], start=(k == 0), stop=(k == KC - 1))
            kb.I('act', [psk[pb]], ['g_graw'], lambda e: e.activation(out=graw[:], in_=ps[pb][:, 0:NT * 16].rearrange("p (n c) -> p n c", c=16), func=AF.Copy))
            kb.I('dve', ['g_graw', 'g_gcst'], ['g_g'], lambda e: e.tensor_tensor(out=gG[:], in0=graw[:, :, 0:8], in1=gcst[:, 8:16].unsqueeze(1).to_broadcast([128, NT, 8]), op=ALU.add))
            kb.I('act', ['g_g'], ['g_g'], lambda e: e.activation(out=gG[:], in_=gG[:], func=AF.Exp))
            kb.I('act', ['g_g', 'onec'], ['g_g'], lambda e: e.activation(out=gG[:], in_=gG[:], func=AF.Ln, bias=onec[:]))
            kb.I('dve', ['g_g', 'g_gcst'], ['g_g'], lambda e: e.tensor_tensor(out=gG[:], in0=gG[:], in1=gcst[:, 0:8].unsqueeze(1).to_broadcast([128, NT, 8]), op=ALU.mult))
            kb.I('act', ['g_graw'], ['g_bt'], lambda e: e.activation(out=gB[:], in_=graw[:, :, 8:16], func=AF.Sigmoid))
            if 'gates' in dbg_out and b == 0:
                kb.DMA('sp', ['g_g'], ['dbg_gates'], lambda e: e.dma_start(out=dbg_out['gates'][:, :, 0:8], in_=gG[:]))
                kb.DMA('sp', ['g_bt'], ['dbg_gates'], lambda e: e.dma_start(out=dbg_out['gates'][:, :, 8:16], in_=gB[:]))

            for hd in range(4):
                for part in range(4):
                    c0 = part * 512 + hd * 128
                    kb.DMA('sp', [], ['g_wh'], lambda e: e.dma_start(out=wh[:, part], in_=ev_w_in[:, :, c0:c0 + 128]))
                for part in range(3):
                    c0 = part * 512 + hd * 128
                    kb.DMA('sp', [], ['g_cw'], lambda e: e.dma_start(out=cw[:, part, :], in_=a_convT[c0:c0 + 128, :]))
                for part in range(3):
                    Tp, tk = T3[part], f'g_T{part}'
                    proj_fm(hT, wh[:, part], 'g_wh', None, lambda pb, t0, n: kb.I('act', [psk[pb]], ['g_raw'], lambda e: e.activation(out=raw[:, rawpos(t0):rawpos(t0) + n], in_=ps[pb][:, 0:n], func=AF.Copy)))
                    for (o0, r0, L) in ((0, RC, LC), (LC, RL, LL)):
                        kb.I('dve', ['g_raw', 'g_cw'], ['g_tmp'], lambda e: e.tensor_scalar(out=tmp[:, o0:o0 + L], in0=raw[:, r0:r0 + L], scalar1=cw[:, part, 0:1], scalar2=None, op0=ALU.mult))
                        for j in range(1, 4):
                            kb.I('dve', ['g_raw', 'g_cw', 'g_tmp'], ['g_tmp'], lambda e: e.scalar_tensor_tensor(out=tmp[:, o0:o0 + L], in0=raw[:, r0 + j:r0 + j + L], scalar=cw[:, part, j:j + 1], in1=tmp[:, o0:o0 + L], op0=ALU.mult, op1=ALU.add))
                    kb.I('act', ['g_tmp'], [tk], lambda e: e.activation(out=Tp[:], in_=tmp[:], func=AF.Silu))
                    if part < 2:
                        kb.I('act', [tk], ['g_tmp'], lambda e: e.activation(out=tmp[:], in_=Tp[:], func=AF.Square))
                        for (t0, n) in TOKBLKS:
                            pb = psr.next()
                            mm(ps[pb][:, 0:n], ones[:], tmp[:, t0:t0 + n], ['ones', 'g_tmp'], [psk[pb]])
                            kb.I('act', [psk[pb], 'epsc', 'g_tmp'], ['g_tmp'], lambda e: e.activation(out=tmp[:, t0:t0 + n], in_=ps[pb][:, 0:n], func=AF.Sqrt, bias=epsc[:]))
                        kb.I('dve', ['g_tmp'], ['g_tmp'], lambda e: e.reciprocal(out=tmp[:], in_=tmp[:]))
                        sc_ = (128 ** -0.5) if part == 0 else 1.0
                        kb.I('dve', [tk, 'g_tmp'], [tk], lambda e: e.scalar_tensor_tensor(out=Tp[:], in0=Tp[:], scalar=sc_, in1=tmp[:], op0=ALU.mult, op1=ALU.mult))
                for src, dst, dk_ in ((T3[1], Ktm, 'g_Ktm'), (T3[2], Vtm, 'g_Vtm')):
                    sk = 'g_T1' if dst is Ktm else 'g_T2'
                    for n0 in range(0, NT, 4):
                        pb = psr.next()
                        nn = min(4, NT - n0)
                        for i in range(nn):
                            tr(ps[pb][:, i * 128:(i + 1) * 128], src[:, (n0 + i) * 128:(n0 + i + 1) * 128], [sk], [psk[pb]])
                        kb.I('act', [psk[pb]], [dk_], lambda e: e.activation(out=dst[:, n0:n0 + nn, :], in_=ps[pb][:, 0:nn * 128].rearrange("p (n c) -> p n c", c=128), func=AF.Copy))
                for n0 in range(0, NT, 4):
                    pb = psr.next()
                    nn = min(4, NT - n0)
                    for i in range(nn):
                        n = n0 + i
                        for k in range(KC):
                            mm(ps[pb][:, i * 128:(i + 1) * 128], hT[:, k, n * 128:(n + 1) * 128], wh[:, 3, k, :], ['g_wh', ('hT', n)], [psk[pb]], start=(k == 0), stop=(k == KC - 1))
                    kb.I('act', [psk[pb]], ['g_zs'], lambda e: e.activation(out=zs[:, n0:n0 + nn, :], in_=ps[pb][:, 0:nn * 128].rearrange("p (n c) -> p n c", c=128), func=AF.Silu))
                gdn_chains(st, b, hd, T3, Ktm, Vtm, gG, gB, O)
                ssq = sb(f"g_ssq{hd}", [128, NT], stack=st)
                O2 = tmp[:, :].rearrange("p (n c) -> p n c", c=128)
                kb.I('dve', ['g_O'], ['g_tmp'], lambda e: e.tensor_tensor(out=O2, in0=O[:], in1=O[:], op=ALU.mult))
                kb.I('dve', ['g_tmp'], ['g_ssq'], lambda e: e.tensor_reduce(out=ssq[:], in_=O2, axis=AX.X, op=ALU.add))
                kb.I('act', ['g_ssq', 'epsc'], ['g_ssq'], lambda e: e.activation(out=ssq[:], in_=ssq[:], func=AF.Sqrt, scale=1.0 / 128, bias=epsc[:]))
                kb.I('dve', ['g_ssq'], ['g_ssq'], lambda e: e.reciprocal(out=ssq[:], in_=ssq[:]))
                kb.I('dve', ['g_O', 'g_ssq'], ['g_O'], lambda e: e.tensor_tensor(out=O[:], in0=O[:], in1=ssq[:].unsqueeze(2).to_broadcast([128, NT, 128]), op=ALU.mult))
                kb.I('dve', ['g_O', 'g_anw'], ['g_O'], lambda e: e.tensor_tensor(out=O[:], in0=O[:], in1=anw[:].unsqueeze(1).to_broadcast([128, NT, 128]), op=ALU.mult))
                kb.I('dve', ['g_O', 'g_zs'], ['g_O'], lambda e: e.tensor_tensor(out=O[:], in0=O[:], in1=zs[:], op=ALU.mult))
                for n0 in range(0, NT, 4):
                    pb = psr.next()
                    nn = min(4, NT - n0)
                    for i in range(nn):
                        tr(ps[pb][:, i * 128:(i + 1) * 128], O[:, n0 + i, :], ['g_O'], [psk[pb]])
                    kb.I('act', [psk[pb]], ['g_tmp'], lambda e: e.activation(out=tmp[:, n0 * 128:(n0 + nn) * 128], in_=ps[pb][:, 0:nn * 128], func=AF.Copy))
                kb.DMA('sp', ['g_tmp'], [('mixT', b)], lambda e: e.dma_start(out=mixT[b, hd], in_=tmp[:]))
            kb.barrier()

    def gdn_chains(st0, b, hd, T3, Ktm, Vtm, gG, gB, O):
        QT, KT = T3[0], T3[1]
        with ExitStack() as st:
            NBUF = 2
            def mk(name, shape):
                return [sb(f"c_{name}{i}", shape, stack=st) for i in range(NBUF)]
            cs = mk("cs", [128, 8])
            Tg = mk("Tg", [128, 128]); dB = mk("dB", [128, 128])
            Ei = mk("Ei", [128, 128]); EmI = mk("EmI", [128, 128]); tt = mk("tt", [128, 128])
            Y = [mk("Ya", [128, 128]), mk("Yb", [128, 128])]
            YT = [mk("YTa", [128, 128]), mk("YTb", [128, 128])]
            R = [mk("Ra", [128, 128]), mk("Rb", [128, 128])]
            qkm = mk("qkm", [128, 128]); eg = mk("eg", [128, 128]); qd = mk("qd", [128, 128])
            rv = mk("rv", [128, 128]); rk = mk("rk", [128, 128]); U = mk("U", [128, 128]); WT = mk("WT", [128, 128])
            Kd = mk("Kd", [128, 128]); vn = mk("vn", [128, 128])
            S = [sb(f"c_S{i}", [128, 128], stack=st) for i in range(2)]
            it = 0
            for d in range(2):
                col = d * 4 + hd
                order = ([0, 1] + list(range(2, NT))) if d == 0 else ([1, 0] + list(range(NT - 1, 1, -1)))
                si = 0
                kb.I('dve', [], ['c_S0'], lambda e: e.memset(S[0][:], 0.0))
                for n in order:
                    i = it % NBUF
                    it += 1
                    K_ = lambda nm: f"c_{nm}{i}"
                    gcol = gG[:, n, col:col + 1]
                    bcol = gB[:, n, col:col + 1]
                    tok = slice(n * 128, (n + 1) * 128)
                    kb.I('dve', ['g_g', 'msk'], [K_('Tg')], lambda e: e.tensor_scalar(out=Tg[i][:], in0=msk[:, d, :], scalar1=gcol, scalar2=None, op0=ALU.mult))
                    kb.I('dve', ['g_bt', 'ident'], [K_('dB')], lambda e: e.tensor_scalar(out=dB[i][:], in0=ident[:], scalar1=bcol, scalar2=None, op0=ALU.mult))
                    pa = psr.next()
                    mm(ps[pa][:, 0:128], ones[:], Tg[i][:], ['ones', K_('Tg')], [psk[pa]])
                    mm(ps[pa][:, 128:256], msk[:, 4 + d, :], dB[i][:], ['msk', K_('dB')], [psk[pa]])
                    mm(ps[pa][:, 256:257], msk[:, d, :], gcol, ['msk', 'g_g'], [psk[pa]])
                    mm(ps[pa][:, 257:258], ones[:], gcol, ['ones', 'g_g'], [psk[pa]])
                    kb.I('act', [psk[pa]], [K_('cs')], lambda e: e.activation(out=cs[i][:, 0:2], in_=ps[pa][:, 256:258], func=AF.Copy))
                    kb.I('act', [K_('cs')], [K_('cs')], lambda e: e.activation(out=cs[i][:, 2:3], in_=cs[i][:, 0:1], func=AF.Exp))
                    kb.I('dve', [K_('cs'), 'g_bt'], [K_('cs')], lambda e: e.tensor_tensor(out=cs[i][:, 3:4], in0=cs[i][:, 2:3], in1=bcol, op=ALU.mult))
                    kb.I('act', [K_('cs')], [K_('cs')], lambda e: e.activation(out=cs[i][:, 4:5], in_=cs[i][:, 0:1], func=AF.Exp, scale=-1.0, bias=cs[i][:, 1:2]))
                    kb.I('act', [K_('cs')], [K_('cs')], lambda e: e.activation(out=cs[i][:, 5:6], in_=cs[i][:, 1:2], func=AF.Exp))
                    kb.I('dve', [psk[pa], K_('cs')], [K_('Ei')], lambda e: e.tensor_scalar(out=Ei[i][:], in0=ps[pa][:, 0:128], scalar1=cs[i][:, 0:1], scalar2=0.0, op0=ALU.subtract, op1=ALU.min))
                    kb.I('act', [K_('Ei')], [K_('Ei')], lambda e: e.activation(out=Ei[i][:], in_=Ei[i][:], func=AF.Exp))
                    kb.I('act', [psk[pa]], [K_('eg')], lambda e: e.activation(out=eg[i][:], in_=ps[pa][:, 0:128], func=AF.Exp))
                    kb.I('dve', [K_('Ei'), 'msk'], [K_('EmI')], lambda e: e.tensor_tensor(out=EmI[i][:], in0=Ei[i][:], in1=msk[:, 2 + d, :], op=ALU.mult))
                    kb.I('dve', [K_('Ei'), psk[pa]], [K_('tt')], lambda e: e.tensor_tensor(out=tt[i][:], in0=Ei[i][:], in1=ps[pa][:, 128:256], op=ALU.mult))
                    pq = psr.next()
                    mm(ps[pq][:, 0:128], KT[:, tok], KT[:, tok], ['g_T1'], [psk[pq]])
                    mm(ps[pq][:, 128:256], KT[:, tok], QT[:, tok], ['g_T1', 'g_T0'], [psk[pq]])
                    y0, yt0, r0 = Y[0][i], YT[0][i], R[0][i]
                    kb.I('dve', [psk[pq], K_('tt')], [K_('Ya')], lambda e: e.scalar_tensor_tensor(out=y0[:], in0=ps[pq][:, 0:128], scalar=-1.0, in1=tt[i][:], op0=ALU.mult, op1=ALU.mult))
                    kb.I('dve', [psk[pq], K_('EmI')], [K_('qkm')], lambda e: e.tensor_tensor(out=qkm[i][:], in0=ps[pq][:, 128:256], in1=EmI[i][:], op=ALU.mult))
                    kb.I('dve', [K_('eg'), 'g_T0'], [K_('qd')], lambda e: e.tensor_tensor(out=qd[i][:], in0=QT[:, tok], in1=eg[i][:], op=ALU.mult))
                    pz = psr.next()
                    tr(ps[pz][:, 0:128], y0[:], [K_('Ya')], [psk[pz]])
                    kb.I('act', [psk[pz]], [K_('YTa')], lambda e: e.activation(out=yt0[:], in_=ps[pz][:, 0:128], func=AF.Copy))
                    kb.I('dve', [K_('Ya'), 'ident'], [K_('Ra')], lambda e: e.tensor_tensor(out=r0[:], in0=y0[:], in1=ident[:], op=ALU.add))
                    cur = 0
                    names = ['a', 'b']
                    for lev in range(6):
                        nx = 1 - cur
                        yc, ytc, rc = Y[cur][i], YT[cur][i], R[cur][i]
                        yn, ytn, rn = Y[nx][i], YT[nx][i], R[nx][i]
                        kc_, kn_ = names[cur], names[nx]
                        pl = psr.next()
                        mm(ps[pl][:, 128:256], yc[:], ytc[:], [K_('Y' + kc_), K_('YT' + kc_)], [psk[pl]])
                        kb.I('act', [psk[pl]], [K_('YT' + kn_)], lambda e: e.activation(out=ytn[:], in_=ps[pl][:, 128:256], func=AF.Copy))
                        if lev < 5:
                            mm(ps[pl][:, 0:128], ytc[:], yc[:], [K_('Y' + kc_), K_('YT' + kc_)], [psk[pl]])
                            kb.I('act', [psk[pl]], [K_('Y' + kn_)], lambda e: e.activation(out=yn[:], in_=ps[pl][:, 0:128], func=AF.Copy))
                        mm(ps[pl][:, 256:384], ytn[:], rc[:], [K_('YT' + kn_), K_('R' + kc_)], [psk[pl]])
                        kb.I('dve', [psk[pl], K_('R' + kc_)], [K_('R' + kn_)], lambda e: e.tensor_tensor(out=rn[:], in0=rc[:], in1=ps[pl][:, 256:384], op=ALU.add))
                        cur = nx
                    Rf, Rk_ = R[cur][i], K_('R' + names[cur])
                    kb.I('dve', ['g_Vtm', 'g_bt'], [K_('rv')], lambda e: e.tensor_scalar(out=rv[i][:], in0=Vtm[:, n, :], scalar1=bcol, scalar2=None, op0=ALU.mult))
                    kb.I('dve', ['g_Ktm', K_('cs')], [K_('rk')], lambda e: e.tensor_scalar(out=rk[i][:], in0=Ktm[:, n, :], scalar1=cs[i][:, 3:4], scalar2=None, op0=ALU.mult))
                    kb.I('dve', ['g_Ktm', K_('cs')], [K_('Kd')], lambda e: e.tensor_scalar(out=Kd[i][:], in0=Ktm[:, n, :], scalar1=cs[i][:, 4:5], scalar2=None, op0=ALU.mult))
                    pu = psr.next()
                    mm(ps[pu][:, 0:128], Rf[:], rv[i][:], [Rk_, K_('rv')], [psk[pu]])
                    mm(ps[pu][:, 128:256], rk[i][:], Rf[:], [Rk_, K_('rk')], [psk[pu]])
                    kb.I('act', [psk[pu]], [K_('U')], lambda e: e.activation(out=U[i][:], in_=ps[pu][:, 0:128], func=AF.Copy))
                    kb.I('act', [psk[pu]], [K_('WT')], lambda e: e.activation(out=WT[i][:], in_=ps[pu][:, 128:256], func=AF.Copy))
                    Sc, Sn = S[si], S[1 - si]
                    skc, skn = f'c_S{si}', f'c_S{1 - si}'
                    p1 = psr.next()
                    mm(ps[p1][:, 0:128], WT[i][:], Sc[:], [K_('WT'), skc], [psk[p1]])
                    kb.I('dve', [psk[p1], K_('U')], [K_('vn')], lambda e: e.tensor_tensor(out=vn[i][:], in0=U[i][:], in1=ps[p1][:, 0:128], op=ALU.subtract))
                    mm(ps[p1][:, 128:256], qd[i][:], Sc[:], [K_('qd'), skc], [psk[p1]], start=True, stop=False)
                    mm(ps[p1][:, 128:256], qkm[i][:], vn[i][:], [K_('qkm'), K_('vn')], [psk[p1]], start=False, stop=True)
                    if d == 0:
                        kb.I('act', [psk[p1]], ['g_O'], lambda e: e.activation(out=O[:, n, :], in_=ps[p1][:, 128:256], func=AF.Copy))
                    else:
                        kb.I('dve', [psk[p1], 'g_O'], ['g_O'], lambda e: e.tensor_tensor(out=O[:, n, :], in0=O[:, n, :], in1=ps[p1][:, 128:256], op=ALU.add))
                    mm(ps[p1][:, 256:384], Kd[i][:], vn[i][:], [K_('Kd'), K_('vn')], [psk[p1]])
                    kb.I('dve', [psk[p1], skc, K_('cs')], [skn], lambda e: e.scalar_tensor_tensor(out=Sn[:], in0=Sc[:], scalar=cs[i][:, 5:6], in1=ps[p1][:, 256:384], op0=ALU.mult, op1=ALU.add))
                    si = 1 - si
            kb.barrier()

    def w_out_phase(b):
        with ExitStack() as st:
            wo = sb("o_w", [128, KC, D], stack=st)
            kb.DMA('sp', [], ['o_w'], lambda e: e.dma_start(out=wo[:], in_=ev_w_out[:, :, :]))
            gt = {}
            for kind, row in (('c', 2), ('l', b)):
                g = sb("o_g" + kind, [128, D], stack=st)
                kb.DMA('sp', [('modsD', 0)], ['o_g' + kind], lambda e: e.dma_start(out=g[:], in_=modsD[0, row:row + 1, 2 * D:3 * D].to_broadcast([128, D])))
                gt[kind] = g
            mt = [sb(f"o_m{i}", [128, KC, 128], stack=st) for i in range(2)]
            xt = [sb(f"o_x{i}", [128, D], stack=st) for i in range(2)]
            for t in range(NT):
                i = t % 2
                kind = 'c' if t < 2 else 'l'
                kb.DMA('sp', [('mixT', b)], [f'o_m{i}'], lambda e: e.dma_start(out=mt[i][:], in_=mixT[b, :, :, t * 128:(t + 1) * 128].rearrange("k p t -> p k t")))
                kb.DMA('sp', ['xin'], [f'o_x{i}'], lambda e: e.dma_start(out=xt[i][:], in_=xin[b, t * 128:(t + 1) * 128, :]))
                for hlf in range(2):
                    pb = psr.next()
                    for k in range(KC):
                        mm(ps[pb][:, :], mt[i][:, k, :], wo[:, k, hlf * 512:(hlf + 1) * 512], [f'o_m{i}', 'o_w'], [psk[pb]], start=(k == 0), stop=(k == KC - 1))
                    if 'yl0' in dbg_out and b == 0:
                        yb = sb(f"o_y{t}_{hlf}", [128, 512], stack=st)
                        kb.I('act', [psk[pb]], [f'o_y{t}_{hlf}'], lambda e: e.activation(out=yb[:], in_=ps[pb][:, :], func=AF.Copy))
                        kb.DMA('sp', [f'o_y{t}_{hlf}'], ['dbg_yl0'], lambda e: e.dma_start(out=dbg_out['yl0'][t * 128:(t + 1) * 128, hlf * 512:(hlf + 1) * 512], in_=yb[:]))
                    kb.I('dve', [psk[pb], 'o_g' + kind], [psk[pb]], lambda e: e.tensor_tensor(out=ps[pb][:, :], in0=ps[pb][:, :], in1=gt[kind][:, hlf * 512:(hlf + 1) * 512], op=ALU.mult))
                    kb.I('dve', [psk[pb], f'o_x{i}'], [f'o_x{i}'], lambda e: e.tensor_tensor(out=xt[i][:, hlf * 512:(hlf + 1) * 512], in0=xt[i][:, hlf * 512:(hlf + 1) * 512], in1=ps[pb][:, :], op=ALU.add))
                kb.DMA('sp', [f'o_x{i}'], [('xs', b)], lambda e: e.dma_start(out=xs[b, t * 128:(t + 1) * 128, :], in_=xt[i][:]))
            kb.barrier()

    peer_wq = din("peer_wq", [2, 128, KC, 2048])
    peer_kT = din("peer_kT", [2, 128, 16, 128])
    peer_u = [din(f"peer_u{i}", [16384, D]) for i in range(2)]
    peer_v = [din(f"peer_v{i}", [16384, D]) for i in range(2)]
    pconst = din("pconst", [1, 32])
    xs1 = dscr("xs1", [NB, LL, D])
    NEG = -1.0e30

    def peer_phase(l, b, tiles, src_fn, dst_fn, final=False):
        with ExitStack() as st:
            wqb = [sb(f"p_wq{i}", [128, KC, 512], stack=st) for i in range(2)]
            kT = sb("p_kT", [128, 16, 128], stack=st)
            kb.DMA('sp', [], ['p_kT'], lambda e: e.dma_start(out=kT[:], in_=peer_kT[l]))
            pc = sb("p_pc", [128, 32], stack=st)
            kb.DMA('sp', [], ['p_pc'], lambda e: e.dma_start(out=pc[:], in_=pconst[0:1, :].to_broadcast([128, 32])))
            nw = sb("p_nw", [128, D], stack=st)
            kb.DMA('sp', [], ['p_nw'], lambda e: e.dma_start(out=nw[:], in_=norms[2 + l].to_broadcast([128, D])))
            A = sb("p_A", [128, D], stack=st); S = sb("p_S", [128, D], stack=st); G = sb("p_G", [128, D], stack=st)
            fnw = None
            if final:
                fnw = sb("p_fnw", [128, D], stack=st)
                kb.DMA('sp', [], ['p_fnw'], lambda e: e.dma_start(out=fnw[:], in_=norms[4].to_broadcast([128, D])))
            xt = [sb(f"p_x{i}", [128, D], stack=st) for i in range(2)]
            h2 = sb("p_h2", [128, D], stack=st)
            sq = sb("p_sq", [128, D], stack=st)
            ss = sb("p_ss", [128, 1], stack=st)
            h2T = sb("p_h2T", [128, KC, 128], stack=st)
            qT = sb("p_qT", [128, 16, 128], stack=st)
            s1 = sb("p_s1", [128, 16, 128], stack=st)
            s2 = sb("p_s2", [128, 16, 128], stack=st)
            m8 = sb("p_m8", [128, 16, 16], stack=st)
            i8 = sb("p_i8", [128, 16, 16], U32, stack=st)
            i8f = sb("p_i8f", [128, 16, 16], stack=st)
            cand = sb("p_cand", [128, 8, 256], stack=st)
            cand2 = sb("p_cand2", [128, 8, 256], stack=st)
            sc = sb("p_sc", [128, 8, 16], stack=st)
            ci = sb("p_ci", [128, 8, 16], U32, stack=st)
            cf = sb("p_cf", [128, 8, 16], stack=st)
            fi = sb("p_fi", [128, 8, 16], stack=st)
            fj = sb("p_fj", [128, 8, 16], stack=st)
            oh = sb("p_oh", [128, 8, 16, 16], stack=st)
            g1 = sb("p_g1", [128, 8, 16], stack=st)
            g2_ = sb("p_g2", [128, 8, 16], stack=st)
            eid = sb("p_eid", [128, 128], I32, stack=st)
            gate = sb("p_gate", [128, 8, 16], stack=st)
            gsum = sb("p_gsum", [128, 8], stack=st)
            act = sb("p_act", [128, 128], stack=st)
            t1 = sb("p_t1", [128, 128], stack=st)
            wgt = sb("p_wgt", [128, 128], stack=st)
            NG = 4
            gb = [sb(f"p_gb{i}", [128, D], stack=st) for i in range(NG)]
            gring = Ring(list(range(NG)))
            acc = sb("p_acc", [128, D], stack=st)
            cur_kind = None
            for ti, t in enumerate(tiles):
                i = ti % 2
                kind = 'c' if t < 2 else 'l'
                row = 2 if kind == 'c' else b
                if kind != cur_kind:
                    cur_kind = kind
                    kb.DMA('sp', [('modsD', l)], ['p_A'], lambda e: e.dma_start(out=A[:], in_=modsD[l, row:row + 1, 4 * D:5 * D].to_broadcast([128, D])))
                    kb.DMA('sp', [('modsD', l)], ['p_S'], lambda e: e.dma_start(out=S[:], in_=modsD[l, row:row + 1, 3 * D:4 * D].to_broadcast([128, D])))
                    kb.DMA('sp', [('modsD', l)], ['p_G'], lambda e: e.dma_start(out=G[:], in_=modsD[l, row:row + 1, 5 * D:6 * D].to_broadcast([128, D])))
                    kb.I('dve', ['p_A', 'p_nw'], ['p_A'], lambda e: e.scalar_tensor_tensor(out=A[:], in0=A[:], scalar=1.0, in1=nw[:], op0=ALU.add, op1=ALU.mult))
                sap, skey = src_fn(t)
                kb.DMA('sp', [skey], [f'p_x{i}'], lambda e: e.dma_start(out=xt[i][:], in_=sap))
                kb.I('act', [f'p_x{i}'], ['p_sq', 'p_ss'], lambda e: e.activation(out=sq[:], in_=xt[i][:], func=AF.Square, accum_out=ss[:]))
                kb.I('act', ['p_ss', 'epsc'], ['p_ss'], lambda e: e.activation(out=ss[:], in_=ss[:], func=AF.Sqrt, scale=1.0 / D, bias=epsc[:]))
                kb.I('dve', ['p_ss'], ['p_ss'], lambda e: e.reciprocal(out=ss[:], in_=ss[:]))
                kb.I('dve', [f'p_x{i}', 'p_ss', 'p_A'], ['p_h2'], lambda e: e.scalar_tensor_tensor(out=h2[:], in0=xt[i][:], scalar=ss[:], in1=A[:], op0=ALU.mult, op1=ALU.mult))
                kb.I('dve', ['p_h2', 'p_S'], ['p_h2'], lambda e: e.tensor_tensor(out=h2[:], in0=h2[:], in1=S[:], op=ALU.add))
                for half in range(2):
                    pb = psr.next()
                    for kk in range(4):
                        k = half * 4 + kk
                        tr(ps[pb][:, kk * 128:(kk + 1) * 128], h2[:, k * 128:(k + 1) * 128], ['p_h2'], [psk[pb]])
                    kb.I('act', [psk[pb]], ['p_h2T'], lambda e: e.activation(out=h2T[:, half * 4:half * 4 + 4, :], in_=ps[pb][:, :].rearrange("p (k t) -> p k t", k=4), func=AF.Copy))
                for q0 in range(0, 16, 4):
                    pb = psr.next()
                    wi_ = (q0 // 4) % 2
                    wq = wqb[wi_]
                    kb.DMA('sp', [], [f'p_wq{wi_}'], lambda e: e.dma_start(out=wq[:], in_=peer_wq[l, :, :, q0 * 128:(q0 + 4) * 128]))
                    for qq in range(4):
                        for k in range(KC):
                            mm(ps[pb][:, qq * 128:(qq + 1) * 128], wq[:, k, qq * 128:(qq + 1) * 128], h2T[:, k, :], [f'p_wq{wi_}', 'p_h2T'], [psk[pb]], start=(k == 0), stop=(k == KC - 1))
                    kb.I('act', [psk[pb]], ['p_qT'], lambda e: e.activation(out=qT[:, q0:q0 + 4, :], in_=ps[pb][:, :].rearrange("p (k t) -> p k t", k=4), func=AF.Copy))
                for q0 in range(0, 16, 4):
                    pb = psr.next()
                    for qq in range(4):
                        hp = q0 + qq
                        mm(ps[pb][:, qq * 128:(qq + 1) * 128], qT[:, hp, :], kT[:, hp, :], ['p_qT', 'p_kT'], [psk[pb]])
                    kb.I('act', [psk[pb]], ['p_s1'], lambda e: e.activation(out=s1[:, q0:q0 + 4, :], in_=ps[pb][:, :].rearrange("p (k t) -> p k t", k=4), func=AF.Copy))
                for hp in range(16):
                    kb.I('dve', ['p_s1'], ['p_m8'], lambda e: e.max(out=m8[:, hp, 0:8], in_=s1[:, hp, :]))
                    kb.I('dve', ['p_s1', 'p_m8'], ['p_i8'], lambda e: e.max_index(out=i8[:, hp, 0:8], in_max=m8[:, hp, 0:8], in_values=s1[:, hp, :]))
                    kb.I('dve', ['p_s1', 'p_m8'], ['p_s2'], lambda e: e.match_replace(out=s2[:, hp, :], in_to_replace=m8[:, hp, 0:8], in_values=s1[:, hp, :], imm_value=NEG))
                    kb.I('dve', ['p_s2'], ['p_m8'], lambda e: e.max(out=m8[:, hp, 8:16], in_=s2[:, hp, :]))
                    kb.I('dve', ['p_s2', 'p_m8'], ['p_i8'], lambda e: e.max_index(out=i8[:, hp, 8:16], in_max=m8[:, hp, 8:16], in_values=s2[:, hp, :]))
                kb.I('dve', ['p_i8'], ['p_i8f'], lambda e: e.tensor_copy(out=i8f[:], in_=i8[:]))
                m8v = m8[:, :, :].rearrange("p (h two) k -> p h two k", two=2)
                i8v = i8f[:, :, :].rearrange("p (h two) k -> p h two k", two=2)
                candv = cand[:, :, :].rearrange("p h (i j) -> p h i j", j=16)
                kb.I('dve', ['p_m8'], ['p_cand'], lambda e: e.tensor_tensor(out=candv, in0=m8v[:, :, 0, :].unsqueeze(3).to_broadcast([128, 8, 16, 16]), in1=m8v[:, :, 1, :].unsqueeze(2).to_broadcast([128, 8, 16, 16]), op=ALU.add))
                for h in range(8):
                    kb.I('dve', ['p_cand'], ['p_sc'], lambda e: e.max(out=sc[:, h, 0:8], in_=cand[:, h, :]))
                    kb.I('dve', ['p_cand', 'p_sc'], ['p_ci'], lambda e: e.max_index(out=ci[:, h, 0:8], in_max=sc[:, h, 0:8], in_values=cand[:, h, :]))
                    kb.I('dve', ['p_cand', 'p_sc'], ['p_cand2'], lambda e: e.match_replace(out=cand2[:, h, :], in_to_replace=sc[:, h, 0:8], in_values=cand[:, h, :], imm_value=NEG))
                    kb.I('dve', ['p_cand2'], ['p_sc'], lambda e: e.max(out=sc[:, h, 8:16], in_=cand2[:, h, :]))
                    kb.I('dve', ['p_cand2', 'p_sc'], ['p_ci'], lambda e: e.max_index(out=ci[:, h, 8:16], in_max=sc[:, h, 8:16], in_values=cand2[:, h, :]))
                kb.I('dve', ['p_ci'], ['p_cf'], lambda e: e.tensor_copy(out=cf[:], in_=ci[:]))
                thr_b = pc[:, 0:16].unsqueeze(1).unsqueeze(1).to_broadcast([128, 8, 16, 16])
                iot_b = pc[:, 16:32].unsqueeze(1).unsqueeze(1).to_broadcast([128, 8, 16, 16])
                kb.I('dve', ['p_cf', 'p_pc'], ['p_oh'], lambda e: e.tensor_tensor(out=oh[:], in0=cf[:].unsqueeze(3).to_broadcast([128, 8, 16, 16]), in1=thr_b, op=ALU.is_ge))
                kb.I('dve', ['p_oh'], ['p_fi'], lambda e: e.tensor_reduce(out=fi[:], in_=oh[:], axis=AX.X, op=ALU.add))
                kb.I('dve', ['p_fi', 'p_cf'], ['p_fj'], lambda e: e.scalar_tensor_tensor(out=fj[:], in0=fi[:], scalar=-16.0, in1=cf[:], op0=ALU.mult, op1=ALU.add))
                for (fx, half_, gdst, gk) in ((fi, 0, g1, 'p_g1'), (fj, 1, g2_, 'p_g2')):
                    fk = 'p_fi' if half_ == 0 else 'p_fj'
                    kb.I('dve', [fk, 'p_pc'], ['p_oh'], lambda e: e.tensor_tensor(out=oh[:], in0=fx[:].unsqueeze(3).to_broadcast([128, 8, 16, 16]), in1=iot_b, op=ALU.is_equal))
                    kb.I('dve', ['p_oh', 'p_i8f'], ['p_oh'], lambda e: e.tensor_tensor(out=oh[:], in0=oh[:], in1=i8v[:, :, half_, :].unsqueeze(2).to_broadcast([128, 8, 16, 16]), op=ALU.mult))
                    kb.I('dve', ['p_oh'], [gk], lambda e: e.tensor_reduce(out=gdst[:], in_=oh[:], axis=AX.X, op=ALU.add))
                kb.I('dve', ['p_g1', 'p_g2'], ['p_g1'], lambda e: e.scalar_tensor_tensor(out=g1[:], in0=g1[:], scalar=128.0, in1=g2_[:], op0=ALU.mult, op1=ALU.add))
                kb.I('dve', ['p_g1'], ['p_eid'], lambda e: e.tensor_copy(out=eid[:, :].rearrange("p (h k) -> p h k", k=16), in_=g1[:]))
                kb.I('dve', ['p_sc'], ['p_gate'], lambda e: e.tensor_tensor(out=gate[:], in0=sc[:], in1=sc[:, :, 0:1].to_broadcast([128, 8, 16]), op=ALU.subtract))
                kb.I('act', ['p_gate'], ['p_gate'], lambda e: e.activation(out=gate[:], in_=gate[:], func=AF.Exp))
                kb.I('dve', ['p_gate'], ['p_gsum'], lambda e: e.tensor_reduce(out=gsum[:], in_=gate[:], axis=AX.X, op=ALU.add))
                kb.I('dve', ['p_gsum'], ['p_gsum'], lambda e: e.reciprocal(out=gsum[:], in_=gsum[:]))
                kb.I('dve', ['p_gate', 'p_gsum'], ['p_gate'], lambda e: e.tensor_tensor(out=gate[:], in0=gate[:], in1=gsum[:].unsqueeze(2).to_broadcast([128, 8, 16]), op=ALU.mult))
                for slot in range(128):
                    gi = gring.next()
                    kb.DMA('pool', ['p_eid'], [f'p_gb{gi}'], lambda e: e.indirect_dma_start(out=gb[gi][:, :], out_offset=None, in_=peer_u[l][:, :], in_offset=bass.IndirectOffsetOnAxis(ap=eid[:, slot:slot + 1], axis=0)))
                    kb.I('dve', [f'p_gb{gi}', 'p_h2'], ['p_sq', 'p_act'], lambda e: e.scalar_tensor_tensor(out=sq[:], in0=gb[gi][:], scalar=1.0, in1=h2[:], op0=ALU.mult, op1=ALU.mult, accum_out=act[:, slot:slot + 1]))
                kb.I('dve', ['p_act'], ['p_t1'], lambda e: e.tensor_tensor(out=t1[:], in0=act[:], in1=act[:], op=ALU.mult))
                kb.I('dve', ['p_t1'], ['p_t1'], lambda e: e.tensor_scalar(out=t1[:], in0=t1[:], scalar1=0.044715, scalar2=1.0, op0=ALU.mult, op1=ALU.add))
                kb.I('dve', ['p_t1', 'p_act'], ['p_t1'], lambda e: e.tensor_tensor(out=t1[:], in0=t1[:], in1=act[:], op=ALU.mult))
                kb.I('act', ['p_t1'], ['p_t1'], lambda e: e.activation(out=t1[:], in_=t1[:], func=AF.Sigmoid, scale=1.5957691216057308))
                kb.I('dve', ['p_t1', 'p_act'], ['p_t1'], lambda e: e.tensor_tensor(out=t1[:], in0=t1[:], in1=act[:], op=ALU.mult))
                kb.I('dve', ['p_t1', 'p_gate'], ['p_wgt'], lambda e: e.tensor_tensor(out=wgt[:], in0=t1[:], in1=gate[:, :, :].rearrange("p h k -> p (h k)"), op=ALU.mult))
                for slot in range(128):
                    gi = gring.next()
                    kb.DMA('pool', ['p_eid'], [f'p_gb{gi}'], lambda e: e.indirect_dma_start(out=gb[gi][:, :], out_offset=None, in_=peer_v[l][:, :], in_offset=bass.IndirectOffsetOnAxis(ap=eid[:, slot:slot + 1], axis=0)))
                    if slot == 0:
                        kb.I('dve', [f'p_gb{gi}', 'p_wgt'], ['p_acc'], lambda e: e.tensor_scalar(out=acc[:], in0=gb[gi][:], scalar1=wgt[:, 0:1], scalar2=None, op0=ALU.mult))
                    else:
                        kb.I('dve', [f'p_gb{gi}', 'p_wgt', 'p_acc'], ['p_acc'], lambda e: e.scalar_tensor_tensor(out=acc[:], in0=gb[gi][:], scalar=wgt[:, slot:slot + 1], in1=acc[:], op0=ALU.mult, op1=ALU.add))
                kb.I('dve', ['p_acc', 'p_G'], ['p_acc'], lambda e: e.tensor_tensor(out=acc[:], in0=acc[:], in1=G[:], op=ALU.mult))
                kb.I('dve', ['p_acc', f'p_x{i}'], [f'p_x{i}'], lambda e: e.tensor_tensor(out=xt[i][:], in0=xt[i][:], in1=acc[:], op=ALU.add))
                if final:
                    kb.I('act', [f'p_x{i}'], ['p_sq', 'p_ss'], lambda e: e.activation(out=sq[:], in_=xt[i][:], func=AF.Square, accum_out=ss[:]))
                    kb.I('act', ['p_ss', 'epsc'], ['p_ss'], lambda e: e.activation(out=ss[:], in_=ss[:], func=AF.Sqrt, scale=1.0 / D, bias=epsc[:]))
                    kb.I('dve', ['p_ss'], ['p_ss'], lambda e: e.reciprocal(out=ss[:], in_=ss[:]))
                    kb.I('dve', [f'p_x{i}', 'p_ss', 'p_fnw'], [f'p_x{i}'], lambda e: e.scalar_tensor_tensor(out=xt[i][:], in0=xt[i][:], scalar=ss[:], in1=fnw[:], op0=ALU.mult, op1=ALU.mult))
                dst_fn(t, xt[i], f'p_x{i}')
            kb.barrier()

    od_w_in = din("od_w_in", [128, KC, 2064])
    od_w_out = din("od_w_out", [128, KC, D])
    c_gc = din("c_gc", [1, 16])
    c_norm = din("c_norm", [1, 512])
    d_w = din("d_w", [4, 128, 128])
    d_scale = din("d_scale", [512, 1])
    poolm = din("poolm", [4, 128, 128])
    mixT1 = dscr("mixT1", [NB, KC, 128, LT])

    def xs_cm_src(b, t):
        if t < 2:
            return [(slice(0, 128), xs[b, t * 128:(t + 1) * 128, :])]
        v = xs[b, LC:LT, :].rearrange("(r w) d -> r w d", w=GRID_W)
        return [(slice(wi * 32, (wi + 1) * 32), v[:, (t - 2) * 4 + wi, :]) for wi in range(4)]

    def layer1_mixer(b):
        with ExitStack() as ph:
            hT = sb("hT1", [128, KC, LT], stack=ph)
            with ExitStack() as st:
                nw = sb("n1_nw", [128, D], stack=st)
                kb.DMA('sp', [], ['n1_nw'], lambda e: e.dma_start(out=nw[:], in_=norms[1].to_broadcast([128, D])))
                md = {}
                for kind, row in (('c', 2), ('l', b)):
                    A = sb("n1_A" + kind, [128, D], stack=st)
                    S = sb("n1_S" + kind, [128, D], stack=st)
                    kb.DMA('sp', [('modsD', 1)], ['n1_A' + kind], lambda e: e.dma_start(out=A[:], in_=modsD[1, row:row + 1, D:2 * D].to_broadcast([128, D])))
                    kb.DMA('sp', [('modsD', 1)], ['n1_S' + kind], lambda e: e.dma_start(out=S[:], in_=modsD[1, row:row + 1, 0:D].to_broadcast([128, D])))
                    kb.I('dve', ['n1_A' + kind, 'n1_nw'], ['n1_A' + kind], lambda e: e.scalar_tensor_tensor(out=A[:], in0=A[:], scalar=1.0, in1=nw[:], op0=ALU.add, op1=ALU.mult))
                    md[kind] = (A, S)
                xt = [sb(f"n1_x{i}", [128, D], stack=st) for i in range(2)]
                ht = [sb(f"n1_h{i}", [128, D], stack=st) for i in range(2)]
                sq = sb("n1_sq", [128, D], stack=st)
                ss = [sb(f"n1_ss{i}", [128, 1], stack=st) for i in range(2)]
                for t in range(NT):
                    i = t % 2
                    kind = 'c' if t < 2 else 'l'
                    A, S = md[kind]
                    for (psl, sap) in xs_cm_src(b, t):
                        kb.DMA('sp', [('xs', b)], [f'n1_x{i}'], lambda e: e.dma_start(out=xt[i][psl, :], in_=sap))
                    kb.I('act', [f'n1_x{i}'], ['n1_sq', f'n1_ss{i}'], lambda e: e.activation(out=sq[:], in_=xt[i][:], func=AF.Square, accum_out=ss[i][:]))
                    kb.I('act', [f'n1_ss{i}', 'epsc'], [f'n1_ss{i}'], lambda e: e.activation(out=ss[i][:], in_=ss[i][:], func=AF.Sqrt, scale=1.0 / D, bias=epsc[:]))
                    kb.I('dve', [f'n1_ss{i}'], [f'n1_ss{i}'], lambda e: e.reciprocal(out=ss[i][:], in_=ss[i][:]))
                    kb.I('dve', [f'n1_x{i}', f'n1_ss{i}', 'n1_A' + kind], [f'n1_h{i}'], lambda e: e.scalar_tensor_tensor(out=ht[i][:], in0=xt[i][:], scalar=ss[i][:], in1=A[:], op0=ALU.mult, op1=ALU.mult))
                    kb.I('dve', [f'n1_h{i}', 'n1_S' + kind], [f'n1_h{i}'], lambda e: e.tensor_tensor(out=ht[i][:], in0=ht[i][:], in1=S[:], op=ALU.add))
                    for half in range(2):
                        pb = psr.next()
                        for kk in range(4):
                            k = half * 4 + kk
                            tr(ps[pb][:, kk * 128:(kk + 1) * 128], ht[i][:, k * 128:(k + 1) * 128], [f'n1_h{i}'], [psk[pb]])
                        kb.I('act', [psk[pb]], [('hT', t)], lambda e: e.activation(out=hT[:, half * 4:half * 4 + 4, t * 128:(t + 1) * 128], in_=ps[pb][:, :].rearrange("p (k t) -> p k t", k=4), func=AF.Copy))
                kb.barrier()
            with ExitStack() as st:
                wd = sb("q_wd", [128, KC, 512], stack=st)
                kb.DMA('sp', [], ['q_wd'], lambda e: e.dma_start(out=wd[:], in_=od_w_in[:, :, 1552:2064]))
                pm = sb("q_pm", [128, 4, 128], stack=st)
                dw = sb("q_dw", [128, 4, 128], stack=st)
                dsc = sb("q_dsc", [128, 4], stack=st)
                for g in range(4):
                    kb.DMA('sp', [], ['q_pm'], lambda e: e.dma_start(out=pm[:, g, :], in_=poolm[g]))
                    kb.DMA('sp', [], ['q_dw'], lambda e: e.dma_start(out=dw[:, g, :], in_=d_w[g]))
                    kb.DMA('sp', [], ['q_dsc'], lambda e: e.dma_start(out=dsc[:, g:g + 1], in_=d_scale[g * 128:(g + 1) * 128, :]))
                dtm = sb("q_dtm", [128, 512], stack=st)
                pT = sb("q_pT", [128, 128], stack=st)
                yT = sb("q_yT", [128, 4, LT], stack=st)
                for t in range(2, NT):
                    pb = psr.next()
                    for k in range(KC):
                        mm(ps[pb][:, :], hT[:, k, t * 128:(t + 1) * 128], wd[:, k, :], ['q_wd', ('hT', t)], [psk[pb]], start=(k == 0), stop=(k == KC - 1))
                    kb.I('act', [psk[pb]], ['q_dtm'], lambda e: e.activation(out=dtm[:], in_=ps[pb][:, :], func=AF.Copy))
                    for g in range(4):
                        p2 = psr.next()
                        mm(ps[p2][:, 0:128], dtm[:, g * 128:(g + 1) * 128], pm[:, g, :], ['q_dtm', 'q_pm'], [psk[p2]])
                        kb.I('act', [psk[p2]], ['q_pT'], lambda e: e.activation(out=pT[:], in_=ps[p2][:, 0:128], func=AF.Copy))
                        mm(ps[p2][:, 128:256], dw[:, g, :], pT[:], ['q_dw', 'q_pT'], [psk[p2]])
                        kb.I('dve', [psk[p2], 'q_dsc'], ['q_yT'], lambda e: e.tensor_scalar(out=yT[:, g, t * 128:(t + 1) * 128], in0=ps[p2][:, 128:256], scalar1=dsc[:, g:g + 1], scalar2=None, op0=ALU.mult))
                for g in range(4):
                    kb.DMA('sp', ['q_yT'], [('mixT1', b)], lambda e: e.dma_start(out=mixT1[b, 4 + g, :, LC:LT], in_=yT[:, g, LC:LT]))
                kb.barrier()
            mlstm(b, hT)
            kb.barrier()

    def mlstm(b, hT):
        with ExitStack() as st:
            wh = sb("m_wh", [128, KC, 64 + 64 + 128 + 128], stack=st)
            wg16 = sb("m_wg16", [128, KC, 16], stack=st)
            gcst = sb("m_gcst", [128, 16], stack=st)
            cnw = sb("m_cnw", [128, 512], stack=st)
            QT = sb("m_QT", [64, LT], stack=st)
            KT = sb("m_KT", [64, LT], stack=st)
            Ktm = sb("m_Ktm", [128, NT, 64], stack=st)
            V1 = sb("m_V1", [128, NT, 132], stack=st)
            og = sb("m_og", [128, NT, 128], stack=st)
            O = sb("m_O", [128, NT, 128], stack=st)
            tmp = sb("m_tmp", [128, LT], stack=st)
            graw = sb("m_graw", [128, NT, 16], stack=st)
            LI = sb("m_li", [128, NT, 8], stack=st)
            LF = sb("m_lf", [128, NT, 8], stack=st)
            kb.DMA('sp', [], ['m_gcst'], lambda e: e.dma_start(out=gcst[:], in_=c_gc[0:1, :].to_broadcast([128, 16])))
            kb.DMA('sp', [], ['m_cnw'], lambda e: e.dma_start(out=cnw[:], in_=c_norm[0:1, :].to_broadcast([128, 512])))
            kb.DMA('sp', [], ['m_wg16'], lambda e: e.dma_start(out=wg16[:], in_=od_w_in[:, :, 1024:1040]))
            pb = psr.next()
            for n in range(NT):
                for k in range(KC):
                    mm(ps[pb][:, n * 16:(n + 1) * 16], hT[:, k, n * 128:(n + 1) * 128], wg16[:, k, :], ['m_wg16', ('hT', n)], [psk[pb]], start=(k == 0), stop=(k == KC - 1))
            kb.I('act', [psk[pb]], ['m_graw'], lambda e: e.activation(out=graw[:], in_=ps[pb][:, 0:NT * 16].rearrange("p (n c) -> p n c", c=16), func=AF.Copy))
            kb.I('dve', ['m_graw', 'm_gcst'], ['m_li'], lambda e: e.tensor_tensor(out=LI[:], in0=graw[:, :, 0:8], in1=gcst[:, 0:8].unsqueeze(1).to_broadcast([128, NT, 8]), op=ALU.add))
            kb.I('dve', ['m_graw', 'm_gcst'], ['m_lf'], lambda e: e.tensor_tensor(out=LF[:], in0=graw[:, :, 8:16], in1=gcst[:, 8:16].unsqueeze(1).to_broadcast([128, NT, 8]), op=ALU.add))
            kb.I('act', ['m_lf'], ['m_lf'], lambda e: e.activation(out=LF[:], in_=LF[:], func=AF.Exp, scale=-1.0))
            kb.I('act', ['m_lf', 'onec'], ['m_lf'], lambda e: e.activation(out=LF[:], in_=LF[:], func=AF.Ln, bias=onec[:]))
            kb.I('dve', ['m_lf'], ['m_lf'], lambda e: e.tensor_scalar(out=LF[:], in0=LF[:], scalar1=-1.0, scalar2=None, op0=ALU.mult))
            kb.I('dve', [], ['m_V1'], lambda e: e.memset(V1[:], 1.0))
            for hd in range(4):
                for (o0, c0, w_) in ((0, hd * 64, 64), (64, 256 + hd * 64, 64), (128, 512 + hd * 128, 128), (256, 1040 + hd * 128, 128)):
                    kb.DMA('sp', [], ['m_wh'], lambda e: e.dma_start(out=wh[:, :, o0:o0 + w_], in_=od_w_in[:, :, c0:c0 + w_]))
                for (dst, dk_, o0, scl) in ((QT, 'm_QT', 0, 1.0), (KT, 'm_KT', 64, 0.125)):
                    for (t0, n) in TOKBLKS:
                        pb = psr.next()
                        for k in range(KC):
                            mm(ps[pb][0:64, 0:n], wh[:, k, o0:o0 + 64], hT[:, k, t0:t0 + n], ['m_wh'] + [('hT', tt) for tt in range(t0 // 128, (t0 + n) // 128)], [psk[pb]], start=(k == 0), stop=(k == KC - 1))
                        kb.I('act', [psk[pb]], [dk_], lambda e: e.activation(out=dst[:, t0:t0 + n], in_=ps[pb][0:64, 0:n], func=AF.Copy, scale=scl))
                for n0 in range(0, NT, 8):
                    pb = psr.next()
                    nn = min(8, NT - n0)
                    for i in range(nn):
                        kb.I('pe', ['m_KT', 'ident'], [psk[pb]], lambda e: e.transpose(out=ps[pb][:, i * 64:(i + 1) * 64], in_=KT[:, (n0 + i) * 128:(n0 + i + 1) * 128], identity=ident[0:64, 0:64]))
                    kb.I('act', [psk[pb]], ['m_Ktm'], lambda e: e.activation(out=Ktm[:, n0:n0 + nn, :], in_=ps[pb][:, 0:nn * 64].rearrange("p (n c) -> p n c", c=64), func=AF.Copy))
                for (o0, dst, dk_, fn_, wdt) in ((128, V1, 'm_V1', AF.Copy, 128), (256, og, 'm_og', AF.Sigmoid, 128)):
                    for n0 in range(0, NT, 4):
                        pb = psr.next()
                        nn = min(4, NT - n0)
                        for i in range(nn):
                            n = n0 + i
                            for k in range(KC):
                                mm(ps[pb][:, i * 128:(i + 1) * 128], hT[:, k, n * 128:(n + 1) * 128], wh[:, k, o0:o0 + 128], ['m_wh', ('hT', n)], [psk[pb]], start=(k == 0), stop=(k == KC - 1))
                        kb.I('act', [psk[pb]], [dk_], lambda e: e.activation(out=dst[:, n0:n0 + nn, 0:128], in_=ps[pb][:, 0:nn * 128].rearrange("p (n c) -> p n c", c=128), func=fn_))
                mlstm_chains(b, hd, QT, KT, Ktm, V1, LI, LF, O)
                ssq = sb(f"m_ssq{hd}", [128, NT], stack=st)
                O2 = tmp[:, :].rearrange("p (n c) -> p n c", c=128)
                kb.I('dve', ['m_O'], ['m_tmp'], lambda e: e.tensor_tensor(out=O2, in0=O[:], in1=O[:], op=ALU.mult))
                kb.I('dve', ['m_tmp'], ['m_ssq'], lambda e: e.tensor_reduce(out=ssq[:], in_=O2, axis=AX.X, op=ALU.add))
                kb.I('act', ['m_ssq', 'epsc'], ['m_ssq'], lambda e: e.activation(out=ssq[:], in_=ssq[:], func=AF.Sqrt, scale=1.0 / 128, bias=epsc[:]))
                kb.I('dve', ['m_ssq'], ['m_ssq'], lambda e: e.reciprocal(out=ssq[:], in_=ssq[:]))
                kb.I('dve', ['m_O', 'm_ssq'], ['m_O'], lambda e: e.tensor_tensor(out=O[:], in0=O[:], in1=ssq[:].unsqueeze(2).to_broadcast([128, NT, 128]), op=ALU.mult))
                kb.I('dve', ['m_O', 'm_cnw'], ['m_O'], lambda e: e.tensor_tensor(out=O[:], in0=O[:], in1=cnw[:, hd * 128:(hd + 1) * 128].unsqueeze(1).to_broadcast([128, NT, 128]), op=ALU.mult))
                kb.I('dve', ['m_O', 'm_og'], ['m_O'], lambda e: e.tensor_tensor(out=O[:], in0=O[:], in1=og[:], op=ALU.mult))
                for n0 in range(0, NT, 4):
                    pb = psr.next()
                    nn = min(4, NT - n0)
                    for i in range(nn):
                        tr(ps[pb][:, i * 128:(i + 1) * 128], O[:, n0 + i, :], ['m_O'], [psk[pb]])
                    kb.I('act', [psk[pb]], ['m_tmp'], lambda e: e.activation(out=tmp[:, n0 * 128:(n0 + nn) * 128], in_=ps[pb][:, 0:nn * 128], func=AF.Copy))
                kb.DMA('sp', ['m_tmp'], [('mixT1', b)], lambda e: e.dma_start(out=mixT1[b, hd, :, LC:LT], in_=tmp[:, LC:LT]))
            kb.barrier()

    def mlstm_chains(b, hd, QT, KT, Ktm, V1, LI, LF, O):
        with ExitStack() as st:
            NBUF = 2

            def mk(name, shape):
                return [sb(f"k_{name}{i}", shape, stack=st) for i in range(NBUF)]
            cs = mk("cs", [128, 8])
            Tg = mk("Tg", [128, 128]); Ei = mk("Ei", [128, 128]); eg = mk("eg", [64, 128]); qd = mk("qd", [64, 128])
            ST = mk("ST", [128, 128]); Kd = mk("Kd", [128, 64])
            C = [sb(f"k_C{i}", [64, 132], stack=st) for i in range(2)]
            kb.I('dve', [], ['m_O'], lambda e: e.memset(O[:, 0:2, :], 0.0))
            it = 0
            for d in range(2):
                col = d * 4 + hd
                order = ([0, 1] + list(range(2, NT))) if d == 0 else ([1, 0] + list(range(NT - 1, 1, -1)))
                si = 0
                kb.I('dve', [], ['k_C0'], lambda e: e.memset(C[0][:], 0.0))
                for n in order:
                    i = it % NBUF
                    it += 1
                    K_ = lambda nm: f"k_{nm}{i}"
                    fcol = LF[:, n, col:col + 1]
                    icol = LI[:, n, col:col + 1]
                    tok = slice(n * 128, (n + 1) * 128)
                    Cc, Cn = C[si], C[1 - si]
                    ckc, ckn = f'k_C{si}', f'k_C{1 - si}'
                    kb.I('dve', ['m_lf', 'msk'], [K_('Tg')], lambda e: e.tensor_scalar(out=Tg[i][:], in0=msk[:, d, :], scalar1=fcol, scalar2=None, op0=ALU.mult))
                    pa = psr.next()
                    mm(ps[pa][:, 0:128], ones[:], Tg[i][:], ['ones', K_('Tg')], [psk[pa]])
                    mm(ps[pa][:, 256:257], msk[:, d, :], fcol, ['msk', 'm_lf'], [psk[pa]])
                    mm(ps[pa][:, 257:258], ones[:], fcol, ['ones', 'm_lf'], [psk[pa]])
                    kb.I('act', [psk[pa]], [K_('cs')], lambda e: e.activation(out=cs[i][:, 0:2], in_=ps[pa][:, 256:258], func=AF.Copy))
                    kb.I('dve', [K_('cs')], [K_('cs')], lambda e: e.tensor_tensor(out=cs[i][:, 2:3], in0=cs[i][:, 1:2], in1=cs[i][:, 0:1], op=ALU.subtract))
                    kb.I('dve', [K_('cs'), 'm_li'], [K_('cs')], lambda e: e.tensor_tensor(out=cs[i][:, 2:3], in0=cs[i][:, 2:3], in1=icol, op=ALU.add))
                    kb.I('act', [K_('cs')], [K_('cs')], lambda e: e.activation(out=cs[i][:, 3:5], in_=cs[i][:, 1:3], func=AF.Exp))
                    kb.I('dve', ['m_Ktm', K_('cs')], [K_('Kd')], lambda e: e.tensor_scalar(out=Kd[i][:], in0=Ktm[:, n, :], scalar1=cs[i][:, 4:5], scalar2=None, op0=ALU.mult))
                    if n >= 2:
                        kb.I('dve', [psk[pa], K_('cs')], [K_('Ei')], lambda e: e.tensor_scalar(out=Ei[i][:], in0=ps[pa][:, 0:128], scalar1=cs[i][:, 0:1], scalar2=0.0, op0=ALU.subtract, op1=ALU.min))
                        kb.I('act', [K_('Ei'), 'm_li'], [K_('Ei')], lambda e: e.activation(out=Ei[i][:], in_=Ei[i][:], func=AF.Exp, bias=icol))
                        kb.I('dve', [K_('Ei'), 'msk'], [K_('Ei')], lambda e: e.tensor_tensor(out=Ei[i][:], in0=Ei[i][:], in1=msk[:, 2 + d, :], op=ALU.mult))
                        kb.I('act', [psk[pa]], [K_('eg')], lambda e: e.activation(out=eg[i][:], in_=ps[pa][0:64, 0:128], func=AF.Exp))
                        kb.I('dve', [K_('eg'), 'm_QT'], [K_('qd')], lambda e: e.tensor_tensor(out=qd[i][:], in0=QT[:, tok], in1=eg[i][:], op=ALU.mult))
                        pq = psr.next()
                        mm(ps[pq][:, 0:128], KT[:, tok], QT[:, tok], ['m_KT', 'm_QT'], [psk[pq]])
                        kb.I('dve', [psk[pq], K_('Ei')], [K_('ST')], lambda e: e.tensor_tensor(out=ST[i][:], in0=ps[pq][:, 0:128], in1=Ei[i][:], op=ALU.mult))
                        mm(ps[pq][:, 128:257], qd[i][:], Cc[:, 0:129], [K_('qd'), ckc], [psk[pq]], start=True, stop=False)
                        mm(ps[pq][:, 128:257], ST[i][:], V1[:, n, 0:129], [K_('ST'), 'm_V1'], [psk[pq]], start=False, stop=True)
                        kb.I('act', [psk[pq]], [K_('cs')], lambda e: e.activation(out=cs[i][:, 5:6], in_=ps[pq][:, 256:257], func=AF.Abs))
                        kb.I('dve', [K_('cs')], [K_('cs')], lambda e: e.tensor_scalar(out=cs[i][:, 5:6], in0=cs[i][:, 5:6], scalar1=1.0, scalar2=None, op0=ALU.max))
                        kb.I('dve', [K_('cs')], [K_('cs')], lambda e: e.reciprocal(out=cs[i][:, 5:6], in_=cs[i][:, 5:6]))
                        if d == 0:
                            kb.I('dve', [psk[pq], K_('cs')], ['m_O'], lambda e: e.tensor_scalar(out=O[:, n, :], in0=ps[pq][:, 128:256], scalar1=cs[i][:, 5:6], scalar2=None, op0=ALU.mult))
                        else:
                            kb.I('dve', [psk[pq], K_('cs'), 'm_O'], ['m_O'], lambda e: e.scalar_tensor_tensor(out=O[:, n, :], in0=ps[pq][:, 128:256], scalar=cs[i][:, 5:6], in1=O[:, n, :], op0=ALU.mult, op1=ALU.add))
                    p3 = psr.next()
                    mm(ps[p3][0:64, 0:129], Kd[i][:], V1[:, n, 0:129], [K_('Kd'), 'm_V1'], [psk[p3]])
                    kb.I('dve', [psk[p3], ckc, K_('cs')], [ckn], lambda e: e.scalar_tensor_tensor(out=Cn[:, 0:129], in0=Cc[:, 0:129], scalar=cs[i][0:64, 3:4], in1=ps[p3][0:64, 0:129], op0=ALU.mult, op1=ALU.add))
                    si = 1 - si
            kb.barrier()

    def w_out1_phase(b):
        with ExitStack() as st:
            wo = sb("o1_w", [128, KC, D], stack=st)
            kb.DMA('sp', [], ['o1_w'], lambda e: e.dma_start(out=wo[:], in_=od_w_out[:, :, :]))
            g = sb("o1_g", [128, D], stack=st)
            kb.DMA('sp', [('modsD', 1)], ['o1_g'], lambda e: e.dma_start(out=g[:], in_=modsD[1, b:b + 1, 2 * D:3 * D].to_broadcast([128, D])))
            mt = [sb(f"o1_m{i}", [128, KC, 128], stack=st) for i in range(2)]
            xt = [sb(f"o1_x{i}", [128, D], stack=st) for i in range(2)]
            for t in range(2, NT):
                i = t % 2
                kb.DMA('sp', [('mixT1', b)], [f'o1_m{i}'], lambda e: e.dma_start(out=mt[i][:], in_=mixT1[b, :, :, t * 128:(t + 1) * 128].rearrange("k p t -> p k t")))
                for (psl, sap) in xs_cm_src(b, t):
                    kb.DMA('sp', [('xs', b)], [f'o1_x{i}'], lambda e: e.dma_start(out=xt[i][psl, :], in_=sap))
                for hlf in range(2):
                    pb = psr.next()
                    for k in range(KC):
                        mm(ps[pb][:, :], mt[i][:, k, :], wo[:, k, hlf * 512:(hlf + 1) * 512], [f'o1_m{i}', 'o1_w'], [psk[pb]], start=(k == 0), stop=(k == KC - 1))
                    kb.I('dve', [psk[pb], 'o1_g'], [psk[pb]], lambda e: e.tensor_tensor(out=ps[pb][:, :], in0=ps[pb][:, :], in1=g[:, hlf * 512:(hlf + 1) * 512], op=ALU.mult))
                    kb.I('dve', [psk[pb], f'o1_x{i}'], [f'o1_x{i}'], lambda e: e.tensor_tensor(out=xt[i][:, hlf * 512:(hlf + 1) * 512], in0=xt[i][:, hlf * 512:(hlf + 1) * 512], in1=ps[pb][:, :], op=ALU.add))
                kb.DMA('sp', [f'o1_x{i}'], [('xs1', b)], lambda e: e.dma_start(out=xs1[b, (t - 2) * 128:(t - 1) * 128, :], in_=xt[i][:]))
            kb.barrier()

    def layer1(b, tiles=None):
        layer1_mixer(b)
        w_out1_phase(b)
        ov = out_d[b].rearrange("(r w) d -> r w d", w=GRID_W)

        def dst1(t, tile, key):
            for wi in range(4):
                kb.DMA('sp', [key], [('out', b)], lambda e: e.dma_start(out=ov[:, (t - 2) * 4 + wi, :], in_=tile[wi * 32:(wi + 1) * 32, :]))
        peer_phase(1, b, tiles or list(range(2, NT)), lambda t: (xs1[b, (t - 2) * 128:(t - 1) * 128, :], ('xs1', b)), dst1, final=True)

    nb_run = 1 if (stop_after or '').endswith('_b0') else NB
    for b in range(0 if l1only else nb_run):
        layer0_mixer(b)
        if stop_after in ('hT0', 'lru'):
            break
        w_out_phase(b)
        if stop_after == 'mix0_b0':
            continue

        def dst0(t, tile, key, b=b):
            kb.DMA('sp', [key], [('xs', b)], lambda e: e.dma_start(out=xs[b, t * 128:(t + 1) * 128, :], in_=tile[:]))
        peer_tiles = list(range(NT)) if stop_after != 'peer0_b0' else [0, 2]
        peer_phase(0, b, peer_tiles, lambda t, b=b: (xs[b, t * 128:(t + 1) * 128, :], ('xs', b)), dst0)
    if stop_after is None or stop_after.startswith('l1'):
        for b in range(nb_run):
            if stop_after == 'l1mix_b0':
                layer1_mixer(b)
                w_out1_phase(b)
            elif l1only:
                layer1(b, tiles=[2, 17])
            else:
                layer1(b)
    if 'xs' in dbg_out:
        kb.barrier()
        kb.DMA('sp', [('xs', 0), ('xs', 1)], ['dbg_xs'], lambda e: e.dma_start(out=dbg_out['xs'], in_=xs))
    if 'xs1' in dbg_out:
        kb.barrier()
        kb.DMA('sp', [('xs1', 0), ('xs1', 1)], ['dbg_xs1'], lambda e: e.dma_start(out=dbg_out['xs1'], in_=xs1))

    kb.finish()
    es.close()
    return nc


def make_in_maps(inputs):
    x = np.asarray(inputs['x'], np.float32)
    ctx = np.asarray(inputs['ctx'], np.float32)
    c = np.asarray(inputs['c'], np.float32)
    c_ctx = np.asarray(inputs['c_ctx'], np.float32)
    ada_w = np.ascontiguousarray(np.asarray(inputs['ada_w'], np.float32).reshape(2, KC, 128, 6 * D))
    ada_b = np.ascontiguousarray(np.asarray(inputs['ada_b'], np.float32).reshape(2, 1, 6 * D))
    norms = np.stack([inputs['norm_mix'][0], inputs['norm_mix'][1], inputs['norm_ffn'][0], inputs['norm_ffn'][1],
                      inputs['final_norm']]).astype(np.float32).reshape(5, 1, D)
    ident = np.eye(128, dtype=np.float32)
    ev_w_in = np.ascontiguousarray(np.asarray(inputs['ev_w_in'][0], np.float32).reshape(KC, 128, 3088).transpose(1, 0, 2))
    ev_w_out = np.ascontiguousarray(np.asarray(inputs['ev_w_out'][0], np.float32).reshape(KC, 128, D).transpose(1, 0, 2))
    a_convT = np.ascontiguousarray(np.asarray(inputs['a_conv'][0], np.float32).T)
    a_gc = np.concatenate([np.asarray(inputs['a_alog'][0]).reshape(8), np.asarray(inputs['a_dtb'][0]).reshape(8)]).astype(np.float32).reshape(1, 16)
    a_norm = np.asarray(inputs['a_norm'][0], np.float32).reshape(1, 128)
    lru_pc = np.zeros((512, 11), np.float32)
    lru_pc[:, 0:4] = np.asarray(inputs['b_conv_w'][0]).T
    lru_pc[:, 4] = np.asarray(inputs['b_conv_b'][0])
    for d in range(2):
        lru_pc[:, 5 + 3 * d] = np.asarray(inputs['b_ba'][0, d]).reshape(512)
        lru_pc[:, 6 + 3 * d] = np.asarray(inputs['b_bx'][0, d]).reshape(512)
        lru_pc[:, 7 + 3 * d] = np.asarray(inputs['b_lam'][0, d]).reshape(512)
    lru_w = np.zeros((2, 2, 4, 128, 128), np.float32)
    for ax, nm in enumerate(('b_wa', 'b_wx')):
        w = np.asarray(inputs[nm][0], np.float32)
        for d in range(2):
            for ct in range(4):
                lru_w[ax, d, ct, 0:64, 0:64] = w[d, 2 * ct]
                lru_w[ax, d, ct, 64:128, 64:128] = w[d, 2 * ct + 1]
    ii = np.arange(128)
    kk_, aa_ = ii[:, None], ii[None, :]
    masks = np.stack([(kk_ <= aa_), (kk_ >= aa_), (aa_ >= kk_), (aa_ <= kk_), (kk_ > aa_), (kk_ < aa_)]).astype(np.float32)
    peer_wq = np.ascontiguousarray(np.asarray(inputs['peer_wq'], np.float32).reshape(2, KC, 128, 2048).transpose(0, 2, 1, 3))
    peer_kT = np.ascontiguousarray(np.asarray(inputs['peer_keys'], np.float32).reshape(2, 16, 128, 128).transpose(0, 3, 1, 2))
    peer_u = np.asarray(inputs['peer_u'], np.float32)
    peer_v = np.asarray(inputs['peer_v'], np.float32)
    peer_u0, peer_u1, peer_v0, peer_v1 = peer_u[0], peer_u[1], peer_v[0], peer_v[1]
    pconst = np.concatenate([16.0 * (np.arange(16) + 1), np.arange(16)]).astype(np.float32).reshape(1, 32)
    od_w_in = np.ascontiguousarray(np.asarray(inputs['od_w_in'][0], np.float32).reshape(KC, 128, 2064).transpose(1, 0, 2))
    od_w_out = np.ascontiguousarray(np.asarray(inputs['od_w_out'][0], np.float32).reshape(KC, 128, D).transpose(1, 0, 2))
    c_gc = np.concatenate([np.asarray(inputs['c_ibias'][0]).reshape(8), np.asarray(inputs['c_fbias'][0]).reshape(8)]).astype(np.float32).reshape(1, 16)
    c_norm = np.asarray(inputs['c_norm'][0], np.float32).reshape(1, 512)
    d_w = np.asarray(inputs['d_w'][0], np.float32)
    d_scale = np.asarray(inputs['d_scale'][0], np.float32).reshape(512, 1)
    poolm = np.zeros((4, 128, 128), np.float32)
    seg = ROWS
    for gi, w in enumerate((2, 4, 8, 16)):
        P = np.zeros((seg, seg), np.float32)
        for t_ in range(seg):
            lo = max(t_ - w // 2, 0)
            hi = min(t_ - w // 2 + w, seg)
            P[t_, lo:hi] = 1.0 / (hi - lo)
        P = P - np.eye(seg, dtype=np.float32)
        for sgi in range(128 // seg):
            poolm[gi, sgi * seg:(sgi + 1) * seg, sgi * seg:(sgi + 1) * seg] = P.T
    shared = dict(od_w_in=od_w_in, od_w_out=od_w_out, c_gc=c_gc, c_norm=c_norm, d_w=d_w, d_scale=d_scale, poolm=poolm, peer_wq=peer_wq, peer_kT=peer_kT, peer_u0=peer_u0, peer_u1=peer_u1, peer_v0=peer_v0, peer_v1=peer_v1, pconst=pconst, ada_w=ada_w, ada_b=ada_b, norms=norms, ident=ident, ev_w_in=ev_w_in, ev_w_out=ev_w_out, a_convT=a_convT,
                  a_gc=a_gc, a_norm=a_norm, lru_pc=lru_pc, lru_w=lru_w, masks=masks)
    maps = []
    for ci in range(NCORES):
        b0 = ci * NB
        xin = np.concatenate([ctx[b0:b0 + NB], x[b0:b0 + NB]], axis=1)
        c3 = np.stack([c[b0], c[b0 + 1], c_ctx], axis=1)
        c3T = np.ascontiguousarray(c3.reshape(KC, 128, 3).transpose(1, 0, 2))
        maps.append(dict(xin=np.ascontiguousarray(xin), c3T=c3T, **shared))
    return maps


def kernel(**inputs):
    nc = build_program()
    maps = make_in_maps(inputs)
    res = run_bass_kernel_spmd(nc, maps, core_ids=list(range(NCORES)))
    outs = [r["out"] for r in res.results]
    return np.concatenate(outs, axis=0).astype(np.float32)
```

```python
import numpy as np
from contextlib import ExitStack
import concourse.bass as bass
import concourse.mybir as mybir
from concourse.bass_utils import run_bass_kernel_spmd

F32 = mybir.dt.float32
U32 = mybir.dt.uint32
I32 = mybir.dt.int32
AF = mybir.ActivationFunctionType
ALU = mybir.AluOpType
AX = mybir.AxisListType

NCORES = 8
NB = 2
D = 1024
KC = 8
LC = 256
LL = 2048
LT = LC + LL
NT = LT // 128
EPS = 1e-6
GRID_W = 64
ROWS = LL // GRID_W


class KB:
    def __init__(self, nc, es, ndma=16):
        self.nc = nc
        self.eng = {'pe': nc.tensor, 'act': nc.scalar, 'dve': nc.vector, 'pool': nc.gpsimd, 'sp': nc.sync}
        self.sems = {}
        self.val = {}
        for e in ['pe', 'act', 'dve', 'pool']:
            self.sems[e] = es.enter_context(nc.semaphore('s_' + e))
            self.val[e] = 0
        self.dq = {}
        for q in ['sp', 'pool', 'act']:
            ids = []
            for i in range(ndma):
                sid = ('d', q, i)
                self.sems[sid] = es.enter_context(nc.semaphore(f'd_{q}_{i}'))
                self.val[sid] = 0
                ids.append(sid)
            self.dq[q] = [ids, 0]
        self.seen = {e: {} for e in self.eng}
        self.lastw = {}
        self.readers = {}
        self.n_inst = 0

    def _waits(self, eng, reads, writes, extra=()):
        need = {}

        def add(ev):
            if ev is None:
                return
            sid, v = ev
            if need.get(sid, 0) < v:
                need[sid] = v
        for r in reads:
            add(self.lastw.get(r))
        for w in writes:
            add(self.lastw.get(w))
            for ev in self.readers.get(w, {}).items():
                add(ev)
        for ev in extra:
            add(ev)
        e = self.eng[eng]
        for sid, v in need.items():
            if sid == 'pe' and eng == 'pe':
                continue
            if self.seen[eng].get(sid, 0) >= v:
                continue
            self.seen[eng][sid] = v
            e.wait_ge(self.sems[sid], v)
            self.n_inst += 1

    def _record(self, ev, reads, writes):
        sid, v = ev
        for w in writes:
            self.lastw[w] = ev
            self.readers[w] = {}
        for r in reads:
            d = self.readers.setdefault(r, {})
            if d.get(sid, 0) < v:
                d[sid] = v

    def I(self, eng, reads, writes, fn):
        self._waits(eng, reads, writes)
        ins = fn(self.eng[eng])
        self.val[eng] += 1
        ins.then_inc(self.sems[eng], 1)
        self._record((eng, self.val[eng]), reads, writes)
        self.n_inst += 1
        return ins

    def DMA(self, q, reads, writes, fn):
        ids, rr = self.dq[q]
        sid = ids[rr % len(ids)]
        self.dq[q][1] = rr + 1
        self._waits(q, reads, writes, extra=[(sid, self.val[sid])] if self.val[sid] else ())
        ins = fn(self.eng[q])
        self.val[sid] += 16
        ins.then_inc(self.sems[sid], 16)
        self._record((sid, self.val[sid]), reads, writes)
        self.n_inst += 1
        return ins

    def barrier(self):
        for eng in self.eng:
            e = self.eng[eng]
            for sid, v in self.val.items():
                if v == 0 or self.seen[eng].get(sid, 0) >= v:
                    continue
                if sid == eng:
                    continue
                self.seen[eng][sid] = v
                e.wait_ge(self.sems[sid], v)
                self.n_inst += 1

    def finish(self):
        e = self.eng['sp']
        for sid, v in self.val.items():
            if v and self.seen['sp'].get(sid, 0) < v:
                e.wait_ge(self.sems[sid], v)


class Ring:
    def __init__(self, items):
        self.items = items
        self.i = 0

    def next(self):
        it = self.items[self.i % len(self.items)]
        self.i += 1
        return it


def bcast_rows(ap_1d_row, nparts):
    return ap_1d_row.to_broadcast([nparts, ap_1d_row.shape[-1]])


def build_program(stop_after=None, dbg=None):
    nc = bass.Bass("TRN2", target_bir_lowering=False)
    es = ExitStack()
    kb = KB(nc, es)
    dbg = dbg or {}

    def din(name, shape, dt=F32):
        return nc.dram_tensor(name, list(shape), dt, kind="ExternalInput").ap()

    def dscr(name, shape, dt=F32):
        return nc.dram_tensor(name, list(shape), dt, kind="Internal").ap()

    def dout(name, shape, dt=F32):
        return nc.dram_tensor(name, list(shape), dt, kind="ExternalOutput").ap()

    uid = [0]

    def sb(name, shape, dt=F32, stack=es):
        uid[0] += 1
        return stack.enter_context(nc.sbuf_tensor(f"{name}_u{uid[0]}", list(shape), dt))

    xin = din("xin", [NB, LT, D])
    c3T = din("c3T", [128, KC, 3])
    ada_w = din("ada_w", [2, KC, 128, 6 * D])
    ada_b = din("ada_b", [2, 1, 6 * D])
    norms = din("norms", [5, 1, D])
    ident_d = din("ident", [128, 128])
    modsD = dscr("modsD", [2, 3, 6 * D])
    out_d = dout("out", [NB, LL, D])
    dbg_out = {}
    for k, shp in dbg.items():
        dbg_out[k] = dout("dbg_" + k, shp)

    ident = sb("ident_sb", [128, 128])
    ps = [es.enter_context(nc.psum_tensor(f"ps{i}", [128, 512], F32)) for i in range(8)]
    psk = [f"ps{i}" for i in range(8)]
    kb.DMA('sp', [], ['ident'], lambda e: e.dma_start(out=ident[:], in_=ident_d[:, :]))
    epsc = sb("epsc", [128, 1])
    kb.I('dve', [], ['epsc'], lambda e: e.memset(epsc[:], EPS))

    with ExitStack() as ph:
        sT = sb("sT", [128, KC, 3], stack=ph)
        kb.DMA('sp', [], ['sT'], lambda e: e.dma_start(out=sT[:], in_=c3T[:, :, :]))
        kb.I('act', ['sT'], ['sT'], lambda e: e.activation(out=sT[:], in_=sT[:], func=AF.Silu))
        wbuf = [sb(f"adaw{i}", [128, 3072], stack=ph) for i in range(3)]
        wring = Ring(list(range(3)))
        bias_t = sb("adab", [3, 6 * D], stack=ph)
        mods_t = sb("mods_t", [3, 6 * D], stack=ph)
        for l in range(2):
            kb.DMA('sp', ['mods_st'], ['adab'], lambda e: e.dma_start(out=bias_t[:], in_=ada_b[l].to_broadcast([3, 6 * D])))
            for half in range(2):
                for k in range(KC):
                    wi = wring.next()
                    kb.DMA('sp', [], [f'adaw{wi}'], lambda e: e.dma_start(out=wbuf[wi][:], in_=ada_w[l, k, :, half * 3072:(half + 1) * 3072]))
                    for j in range(6):
                        kb.I('pe', ['sT', f'adaw{wi}'], [psk[j]], lambda e: e.matmul(ps[j][0:3, :], lhsT=sT[:, k, :], rhs=wbuf[wi][:, j * 512:(j + 1) * 512], start=(k == 0), stop=(k == KC - 1)))
                for j in range(6):
                    c0 = half * 3072 + j * 512
                    kb.I('dve', [psk[j], 'adab'], ['mods_t'], lambda e: e.tensor_tensor(out=mods_t[:, c0:c0 + 512], in0=ps[j][0:3, :], in1=bias_t[:, c0:c0 + 512], op=ALU.add))
            kb.DMA('sp', ['mods_t'], [('modsD', l), 'mods_st'], lambda e: e.dma_start(out=modsD[l], in_=mods_t[:]))
    kb.barrier()
    if 'mods' in dbg_out:
        kb.DMA('sp', [('modsD', 0), ('modsD', 1)], ['dbg_mods'], lambda e: e.dma_start(out=dbg_out['mods'], in_=modsD))
    if stop_after == 'mods':
        kb.finish(); es.close(); return nc

    ev_w_in = din("ev_w_in", [128, KC, 3088])
    ev_w_out = din("ev_w_out", [128, KC, D])
    a_convT = din("a_convT", [1536, 4])
    a_gc = din("a_gc", [1, 16])
    a_norm = din("a_norm", [1, 128])
    lru_pc = din("lru_pc", [512, 11])
    lru_w = din("lru_w", [2, 2, 4, 128, 128])
    masks_d = din("masks", [6, 128, 128])
    mixT = dscr("mixT", [NB, KC, 128, LT])
    l1only = (stop_after or '').startswith('l1only')
    xs = din("xs_in", [NB, LT, D]) if l1only else dscr("xs", [NB, LT, D])

    ones = sb("ones_sb", [128, 128])
    kb.I('dve', [], ['ones'], lambda e: e.memset(ones[:], 1.0))
    onec = sb("onec", [128, 1])
    kb.I('dve', [], ['onec'], lambda e: e.memset(onec[:], 1.0))
    msk = sb("msk", [128, 6, 128])
    for i in range(6):
        kb.DMA('sp', [], ['msk'], lambda e: e.dma_start(out=msk[:, i, :], in_=masks_d[i]))
    psr = Ring(list(range(8)))

    def mm(out, lhsT, rhs, reads, writes, start=True, stop=True):
        kb.I('pe', reads, writes, lambda e: e.matmul(out, lhsT=lhsT, rhs=rhs, start=start, stop=stop))

    def tr(out, in_, reads, writes):
        kb.I('pe', reads + ['ident'], writes, lambda e: e.transpose(out=out, in_=in_, identity=ident[:]))

    TOKBLKS = [(0, 256)] + [(256 + i * 512, 512) for i in range(4)]

    def norm_mod_T(ph, l, b, src_ap_fn, norm_idx, slot_sh, slot_sc, hT, keep_h=None, tiles=range(NT)):
        with ExitStack() as st:
            nw = sb("nm_nw", [128, D], stack=st)
            kb.DMA('sp', [], ['nm_nw'], lambda e: e.dma_start(out=nw[:], in_=norms[norm_idx].to_broadcast([128, D])))
            md = {}
            for kind, row in (('c', 2), ('l', b)):
                A = sb("nm_A" + kind, [128, D], stack=st)
                S = sb("nm_S" + kind, [128, D], stack=st)
                kb.DMA('sp', [('modsD', l)], ['nm_A' + kind], lambda e: e.dma_start(out=A[:], in_=modsD[l, row:row + 1, slot_sc * D:(slot_sc + 1) * D].to_broadcast([128, D])))
                kb.DMA('sp', [('modsD', l)], ['nm_S' + kind], lambda e: e.dma_start(out=S[:], in_=modsD[l, row:row + 1, slot_sh * D:(slot_sh + 1) * D].to_broadcast([128, D])))
                kb.I('dve', ['nm_A' + kind, 'nm_nw'], ['nm_A' + kind], lambda e: e.scalar_tensor_tensor(out=A[:], in0=A[:], scalar=1.0, in1=nw[:], op0=ALU.add, op1=ALU.mult))
                md[kind] = (A, S)
            xt = [sb(f"nm_x{i}", [128, D], stack=st) for i in range(2)]
            ht = [sb(f"nm_h{i}", [128, D], stack=st) for i in range(2)]
            sq = sb("nm_sq", [128, D], stack=st)
            ss = [sb(f"nm_ss{i}", [128, 1], stack=st) for i in range(2)]
            for t in tiles:
                i = t % 2
                kind = 'c' if t < 2 else 'l'
                A, S = md[kind]
                src, srck = src_ap_fn(b, t)
                kb.DMA('sp', [srck], [f'nm_x{i}'], lambda e: e.dma_start(out=xt[i][:], in_=src))
                kb.I('act', [f'nm_x{i}'], ['nm_sq', f'nm_ss{i}'], lambda e: e.activation(out=sq[:], in_=xt[i][:], func=AF.Square, accum_out=ss[i][:]))
                kb.I('act', [f'nm_ss{i}', 'epsc'], [f'nm_ss{i}'], lambda e: e.activation(out=ss[i][:], in_=ss[i][:], func=AF.Sqrt, scale=1.0 / D, bias=epsc[:]))
                kb.I('dve', [f'nm_ss{i}'], [f'nm_ss{i}'], lambda e: e.reciprocal(out=ss[i][:], in_=ss[i][:]))
                kb.I('dve', [f'nm_x{i}', f'nm_ss{i}', 'nm_A' + kind], [f'nm_h{i}'], lambda e: e.scalar_tensor_tensor(out=ht[i][:], in0=xt[i][:], scalar=ss[i][:], in1=A[:], op0=ALU.mult, op1=ALU.mult))
                kb.I('dve', [f'nm_h{i}', 'nm_S' + kind], [f'nm_h{i}'], lambda e: e.tensor_tensor(out=ht[i][:], in0=ht[i][:], in1=S[:], op=ALU.add))
                if keep_h is not None:
                    keep_h(t, ht[i], f'nm_h{i}')
                for half in range(2):
                    pb = psr.next()
                    for kk in range(4):
                        k = half * 4 + kk
                        tr(ps[pb][:, kk * 128:(kk + 1) * 128], ht[i][:, k * 128:(k + 1) * 128], [f'nm_h{i}'], [psk[pb]])
                    kb.I('act', [psk[pb]], [('hT', t)], lambda e: e.activation(out=hT[:, half * 4:half * 4 + 4, t * 128:(t + 1) * 128], in_=ps[pb][:, :].rearrange("p (k t) -> p k t", k=4), func=AF.Copy))
            kb.barrier()

    def proj_fm(hT, w_sb, wkey, dst_fn, evac):
        for (t0, n) in TOKBLKS:
            pb = psr.next()
            for k in range(KC):
                mm(ps[pb][:, 0:n], w_sb[:, k, :], hT[:, k, t0:t0 + n], [wkey] + [('hT', tt) for tt in range(t0 // 128, (t0 + n) // 128)], [psk[pb]], start=(k == 0), stop=(k == KC - 1))
            evac(pb, t0, n)

    def layer0_mixer(b):
        with ExitStack() as ph:
            hT = sb("hT", [128, KC, LT], stack=ph)
            norm_mod_T(ph, 0, b, lambda b_, t: (xin[b_, t * 128:(t + 1) * 128, :], 'xin'), 0, 0, 1, hT)
            if 'hT0' in dbg_out and b == 0:
                kb.DMA('sp', [('hT', t) for t in range(NT)], ['dbg_hT0'], lambda e: e.dma_start(out=dbg_out['hT0'], in_=hT[:]))
            if stop_after == 'hT0':
                return
            with ExitStack() as st:
                wx = sb("l_wx", [128, KC, 128], stack=st)
                wg = sb("l_wg", [128, KC, 128], stack=st)
                pc = sb("l_pc", [128, 11], stack=st)
                cl = sb("l_cl", [128, 4], stack=st)
                gw = sb("l_gw", [128, 4, 128], stack=st)
                raw = sb("l_raw", [128, LT + 6], stack=st)
                xb = sb("l_xb", [128, LT], stack=st)
                gg = sb("l_gg", [128, LT], stack=st)
                t1 = sb("l_t1", [128, LT], stack=st)
                t2 = sb("l_t2", [128, LT], stack=st)
                av = sb("l_a", [128, LT], stack=st)
                bv = sb("l_b", [128, LT], stack=st)
                hf = sb("l_hf", [128, LT], stack=st)
                hb = sb("l_hb", [128, LT], stack=st)
                kb.I('dve', [], ['l_raw'], lambda e: e.memset(raw[:], 0.0))
                RC, RL = 0, LC + 3

                def rawpos(t0):
                    return (RC + 1 + t0) if t0 < LC else (RL + 1 + (t0 - LC))
                for ct in range(4):
                    c0 = 2064 + ct * 128
                    kb.DMA('sp', [], ['l_wx'], lambda e: e.dma_start(out=wx[:], in_=ev_w_in[:, :, c0:c0 + 128]))
                    kb.DMA('sp', [], ['l_wg'], lambda e: e.dma_start(out=wg[:], in_=ev_w_in[:, :, c0 + 512:c0 + 640]))
                    kb.DMA('sp', [], ['l_pc'], lambda e: e.dma_start(out=pc[:], in_=lru_pc[ct * 128:(ct + 1) * 128, :]))
                    for ax in range(2):
                        for d in range(2):
                            kb.DMA('sp', [], ['l_gw'], lambda e: e.dma_start(out=gw[:, ax * 2 + d, :], in_=lru_w[ax, d, ct]))
                    for d in range(2):
                        kb.I('act', ['l_pc'], ['l_cl'], lambda e: e.activation(out=cl[:, d:d + 1], in_=pc[:, 7 + 3 * d:8 + 3 * d], func=AF.Exp, scale=-1.0))
                    kb.I('act', ['l_cl', 'onec'], ['l_cl'], lambda e: e.activation(out=cl[:, 0:2], in_=cl[:, 0:2], func=AF.Ln, bias=onec[:]))
                    kb.I('dve', ['l_cl'], ['l_cl'], lambda e: e.tensor_scalar(out=cl[:, 2:4], in0=cl[:, 0:2], scalar1=-16.0, scalar2=None, op0=ALU.mult))
                    kb.I('dve', ['l_cl'], ['l_cl'], lambda e: e.tensor_scalar(out=cl[:, 0:2], in0=cl[:, 0:2], scalar1=-8.0, scalar2=None, op0=ALU.mult))
                    proj_fm(hT, wx, 'l_wx', None, lambda pb, t0, n: kb.I('act', [psk[pb]], ['l_raw'], lambda e: e.activation(out=raw[:, rawpos(t0):rawpos(t0) + n], in_=ps[pb][:, 0:n], func=AF.Copy)))
                    for (o0, r0, L) in ((0, RC, LC), (LC, RL, LL)):
                        kb.I('dve', ['l_raw', 'l_pc'], ['l_xb'], lambda e: e.tensor_scalar(out=xb[:, o0:o0 + L], in0=raw[:, r0:r0 + L], scalar1=pc[:, 0:1], scalar2=pc[:, 4:5], op0=ALU.mult, op1=ALU.add))
                        for j in range(1, 4):
                            kb.I('dve', ['l_raw', 'l_pc', 'l_xb'], ['l_xb'], lambda e: e.scalar_tensor_tensor(out=xb[:, o0:o0 + L], in0=raw[:, r0 + j:r0 + j + L], scalar=pc[:, j:j + 1], in1=xb[:, o0:o0 + L], op0=ALU.mult, op1=ALU.add))
                    proj_fm(hT, wg, 'l_wg', None, lambda pb, t0, n: kb.I('act', [psk[pb]], ['l_t1'], lambda e: e.activation(out=t1[:, t0:t0 + n], in_=ps[pb][:, 0:n], func=AF.Copy)))
                    kb.I('dve', ['l_t1'], ['l_t2'], lambda e: e.tensor_tensor(out=t2[:], in0=t1[:], in1=t1[:], op=ALU.mult))
                    kb.I('dve', ['l_t2'], ['l_t2'], lambda e: e.tensor_scalar(out=t2[:], in0=t2[:], scalar1=0.044715, scalar2=1.0, op0=ALU.mult, op1=ALU.add))
                    kb.I('dve', ['l_t2', 'l_t1'], ['l_t2'], lambda e: e.tensor_tensor(out=t2[:], in0=t2[:], in1=t1[:], op=ALU.mult))
                    kb.I('act', ['l_t2'], ['l_t2'], lambda e: e.activation(out=t2[:], in_=t2[:], func=AF.Sigmoid, scale=1.5957691216057308))
                    kb.I('dve', ['l_t2', 'l_t1'], ['l_gg'], lambda e: e.tensor_tensor(out=gg[:], in0=t2[:], in1=t1[:], op=ALU.mult))
                    for d in range(2):
                        for (t0, n) in TOKBLKS:
                            for ax, dst, dk_ in ((0, t1, 'l_t1'), (1, t2, 'l_t2')):
                                pb = psr.next()
                                mm(ps[pb][:, 0:n], gw[:, ax * 2 + d, :], xb[:, t0:t0 + n], ['l_gw', 'l_xb'], [psk[pb]])
                                bcol = 5 + 3 * d + ax
                                kb.I('act', [psk[pb], 'l_pc'], [dk_], lambda e: e.activation(out=dst[:, t0:t0 + n], in_=ps[pb][:, 0:n], func=AF.Sigmoid, bias=pc[:, bcol:bcol + 1]))
                        kb.I('act', ['l_t1', 'l_cl'], ['l_a'], lambda e: e.activation(out=av[:], in_=t1[:], func=AF.Exp, scale=cl[:, d:d + 1]))
                        kb.I('act', ['l_t1', 'l_cl'], ['l_b'], lambda e: e.activation(out=bv[:], in_=t1[:], func=AF.Exp, scale=cl[:, 2 + d:3 + d]))
                        kb.I('dve', ['l_b'], ['l_b'], lambda e: e.tensor_scalar(out=bv[:], in0=bv[:], scalar1=-1.0, scalar2=1.0, op0=ALU.mult, op1=ALU.add))
                        kb.I('dve', ['l_b'], ['l_b'], lambda e: e.tensor_scalar(out=bv[:], in0=bv[:], scalar1=0.0, scalar2=None, op0=ALU.max))
                        kb.I('act', ['l_b'], ['l_b'], lambda e: e.activation(out=bv[:], in_=bv[:], func=AF.Sqrt))
                        kb.I('dve', ['l_b', 'l_t2'], ['l_b'], lambda e: e.tensor_tensor(out=bv[:], in0=bv[:], in1=t2[:], op=ALU.mult))
                        kb.I('dve', ['l_b', 'l_xb'], ['l_b'], lambda e: e.tensor_tensor(out=bv[:], in0=bv[:], in1=xb[:], op=ALU.mult))
                        if d == 0:
                            kb.I('dve', ['l_a', 'l_b'], ['l_hf'], lambda e: e.tensor_tensor_scan(out=hf[:, :], data0=av[:, :], data1=bv[:, :], initial=0.0, op0=ALU.mult, op1=ALU.add))
                        else:
                            kb.I('dve', ['l_a', 'l_b'], ['l_hb'], lambda e: e.tensor_tensor_scan(out=hb[:, LC - 1::-1], data0=av[:, LC - 1::-1], data1=bv[:, LC - 1::-1], initial=0.0, op0=ALU.mult, op1=ALU.add))
                            kb.I('dve', ['l_a', 'l_b', 'l_hb'], ['l_hb'], lambda e: e.tensor_tensor_scan(out=hb[:, LT - 1:LC - 1:-1], data0=av[:, LT - 1:LC - 1:-1], data1=bv[:, LT - 1:LC - 1:-1], initial=hb[:, 0:1], op0=ALU.mult, op1=ALU.add))
                    kb.I('dve', ['l_hf', 'l_hb'], ['l_hf'], lambda e: e.tensor_tensor(out=hf[:], in0=hf[:], in1=hb[:], op=ALU.add))
                    kb.I('dve', ['l_hf', 'l_gg'], ['l_hf'], lambda e: e.tensor_tensor(out=hf[:], in0=hf[:], in1=gg[:], op=ALU.mult))
                    kb.DMA('sp', ['l_hf'], [('mixT', b)], lambda e: e.dma_start(out=mixT[b, 4 + ct], in_=hf[:]))
                kb.barrier()
            if stop_after == 'lru':
                return
            gdn(ph, b, hT)
            kb.barrier()

    def gdn(ph, b, hT):
        with ExitStack() as st:
            wh = sb("g_wh", [128, 4, KC, 128], stack=st)
            wg16 = sb("g_wg16", [128, KC, 16], stack=st)
            cw = sb("g_cw", [128, 3, 4], stack=st)
            gcst = sb("g_gcst", [128, 16], stack=st)
            anw = sb("g_anw", [128, 128], stack=st)
            raw = sb("g_raw", [128, LT + 6], stack=st)
            T3 = [sb(f"g_T{i}", [128, LT], stack=st) for i in range(3)]
            tmp = sb("g_tmp", [128, LT], stack=st)
            Ktm = sb("g_Ktm", [128, NT, 128], stack=st)
            Vtm = sb("g_Vtm", [128, NT, 128], stack=st)
            zs = sb("g_zs", [128, NT, 128], stack=st)
            O = sb("g_O", [128, NT, 128], stack=st)
            graw = sb("g_graw", [128, NT, 16], stack=st)
            gG = sb("g_g", [128, NT, 8], stack=st)
            gB = sb("g_bt", [128, NT, 8], stack=st)
            RC, RL = 0, LC + 3

            def rawpos(t0):
                return (RC + 1 + t0) if t0 < LC else (RL + 1 + (t0 - LC))
            kb.I('dve', [], ['g_raw'], lambda e: e.memset(raw[:], 0.0))
            kb.DMA('sp', [], ['g_gcst'], lambda e: e.dma_start(out=gcst[:], in_=a_gc[0:1, :].to_broadcast([128, 16])))
            kb.DMA('sp', [], ['g_anw'], lambda e: e.dma_start(out=anw[:], in_=a_norm[0:1, :].to_broadcast([128, 128])))
            kb.I('act', ['g_gcst'], ['g_gcst'], lambda e: e.activation(out=gcst[:, 0:8], in_=gcst[:, 0:8], func=AF.Exp))
            kb.I('dve', ['g_gcst'], ['g_gcst'], lambda e: e.tensor_scalar(out=gcst[:, 0:8], in0=gcst[:, 0:8], scalar1=-1.0, scalar2=None, op0=ALU.mult))
            kb.DMA('sp', [], ['g_wg16'], lambda e: e.dma_start(out=wg16[:], in_=ev_w_in[:, :, 2048:2064]))
            pb = psr.next()
            for n in range(NT):
                for k in range(KC):
                    mm(ps[pb][:, n * 16:(n + 1) * 16], hT[:, k, n * 128:(n + 1) * 128], wg16[:, k, :], ['g_wg16', ('hT', n)], [psk[pb]], start=(k == 0), stop=(k == KC - 1))
            kb.I('act', [psk[pb]], ['g_graw'], lambda e: e.activation(out=graw[:], in_=ps[pb][:, 0:NT * 16].rearrange("p (n c) -> p n c", c=16), func=AF.Copy))
            kb.I('dve', ['g_graw', 'g_gcst'], ['g_g'], lambda e: e.tensor_tensor(out=gG[:], in0=graw[:, :, 0:8], in1=gcst[:, 8:16].unsqueeze(1).to_broadcast([128, NT, 8]), op=ALU.add))
            kb.I('act', ['g_g'], ['g_g'], lambda e: e.activation(out=gG[:], in_=gG[:], func=AF.Exp))
            kb.I('act', ['g_g', 'onec'], ['g_g'], lambda e: e.activation(out=gG[:], in_=gG[:], func=AF.Ln, bias=onec[:]))
            kb.I('dve', ['g_g', 'g_gcst'], ['g_g'], lambda e: e.tensor_tensor(out=gG[:], in0=gG[:], in1=gcst[:, 0:8].unsqueeze(1).to_broadcast([128, NT, 8]), op=ALU.mult))
            kb.I('act', ['g_graw'], ['g_bt'], lambda e: e.activation(out=gB[:], in_=graw[:, :, 8:16], func=AF.Sigmoid))
            if 'gates' in dbg_out and b == 0:
                kb.DMA('sp', ['g_g'], ['dbg_gates'], lambda e: e.dma_start(out=dbg_out['gates'][:, :, 0:8], in_=gG[:]))
                kb.DMA('sp', ['g_bt'], ['dbg_gates'], lambda e: e.dma_start(out=dbg_out['gates'][:, :, 8:16], in_=gB[:]))

            for hd in range(4):
                for part in range(4):
                    c0 = part * 512 + hd * 128
                    kb.DMA('sp', [], ['g_wh'], lambda e: e.dma_start(out=wh[:, part], in_=ev_w_in[:, :, c0:c0 + 128]))
                for part in range(3):
                    c0 = part * 512 + hd * 128
                    kb.DMA('sp', [], ['g_cw'], lambda e: e.dma_start(out=cw[:, part, :], in_=a_convT[c0:c0 + 128, :]))
                for part in range(3):
                    Tp, tk = T3[part], f'g_T{part}'
                    proj_fm(hT, wh[:, part], 'g_wh', None, lambda pb, t0, n: kb.I('act', [psk[pb]], ['g_raw'], lambda e: e.activation(out=raw[:, rawpos(t0):rawpos(t0) + n], in_=ps[pb][:, 0:n], func=AF.Copy)))
                    for (o0, r0, L) in ((0, RC, LC), (LC, RL, LL)):
                        kb.I('dve', ['g_raw', 'g_cw'], ['g_tmp'], lambda e: e.tensor_scalar(out=tmp[:, o0:o0 + L], in0=raw[:, r0:r0 + L], scalar1=cw[:, part, 0:1], scalar2=None, op0=ALU.mult))
                        for j in range(1, 4):
                            kb.I('dve', ['g_raw', 'g_cw', 'g_tmp'], ['g_tmp'], lambda e: e.scalar_tensor_tensor(out=tmp[:, o0:o0 + L], in0=raw[:, r0 + j:r0 + j + L], scalar=cw[:, part, j:j + 1], in1=tmp[:, o0:o0 + L], op0=ALU.mult, op1=ALU.add))
                    kb.I('act', ['g_tmp'], [tk], lambda e: e.activation(out=Tp[:], in_=tmp[:], func=AF.Silu))
                    if part < 2:
                        kb.I('act', [tk], ['g_tmp'], lambda e: e.activation(out=tmp[:], in_=Tp[:], func=AF.Square))
                        for (t0, n) in TOKBLKS:
                            pb = psr.next()
                            mm(ps[pb][:, 0:n], ones[:], tmp[:, t0:t0 + n], ['ones', 'g_tmp'], [psk[pb]])
                            kb.I('act', [psk[pb], 'epsc', 'g_tmp'], ['g_tmp'], lambda e: e.activation(out=tmp[:, t0:t0 + n], in_=ps[pb][:, 0:n], func=AF.Sqrt, bias=epsc[:]))
                        kb.I('dve', ['g_tmp'], ['g_tmp'], lambda e: e.reciprocal(out=tmp[:], in_=tmp[:]))
                        sc_ = (128 ** -0.5) if part == 0 else 1.0
                        kb.I('dve', [tk, 'g_tmp'], [tk], lambda e: e.scalar_tensor_tensor(out=Tp[:], in0=Tp[:], scalar=sc_, in1=tmp[:], op0=ALU.mult, op1=ALU.mult))
                for src, dst, dk_ in ((T3[1], Ktm, 'g_Ktm'), (T3[2], Vtm, 'g_Vtm')):
                    sk = 'g_T1' if dst is Ktm else 'g_T2'
                    for n0 in range(0, NT, 4):
                        pb = psr.next()
                        nn = min(4, NT - n0)
                        for i in range(nn):
                            tr(ps[pb][:, i * 128:(i + 1) * 128], src[:, (n0 + i) * 128:(n0 + i + 1) * 128], [sk], [psk[pb]])
                        kb.I('act', [psk[pb]], [dk_], lambda e: e.activation(out=dst[:, n0:n0 + nn, :], in_=ps[pb][:, 0:nn * 128].rearrange("p (n c) -> p n c", c=128), func=AF.Copy))
                for n0 in range(0, NT, 4):
                    pb = psr.next()
                    nn = min(4, NT - n0)
                    for i in range(nn):
                        n = n0 + i
                        for k in range(KC):
                            mm(ps[pb][:, i * 128:(i + 1) * 128], hT[:, k, n * 128:(n + 1) * 128], wh[:, 3, k, :], ['g_wh', ('hT', n)], [psk[pb]], start=(k == 0), stop=(k == KC - 1))
                    kb.I('act', [psk[pb]], ['g_zs'], lambda e: e.activation(out=zs[:, n0:n0 + nn, :], in_=ps[pb][:, 0:nn * 128].rearrange("p (n c) -> p n c", c=128), func=AF.Silu))
                gdn_chains(st, b, hd, T3, Ktm, Vtm, gG, gB, O)
                ssq = sb(f"g_ssq{hd}", [128, NT], stack=st)
                O2 = tmp[:, :].rearrange("p (n c) -> p n c", c=128)
                kb.I('dve', ['g_O'], ['g_tmp'], lambda e: e.tensor_tensor(out=O2, in0=O[:], in1=O[:], op=ALU.mult))
                kb.I('dve', ['g_tmp'], ['g_ssq'], lambda e: e.tensor_reduce(out=ssq[:], in_=O2, axis=AX.X, op=ALU.add))
                kb.I('act', ['g_ssq', 'epsc'], ['g_ssq'], lambda e: e.activation(out=ssq[:], in_=ssq[:], func=AF.Sqrt, scale=1.0 / 128, bias=epsc[:]))
                kb.I('dve', ['g_ssq'], ['g_ssq'], lambda e: e.reciprocal(out=ssq[:], in_=ssq[:]))
                kb.I('dve', ['g_O', 'g_ssq'], ['g_O'], lambda e: e.tensor_tensor(out=O[:], in0=O[:], in1=ssq[:].unsqueeze(2).to_broadcast([128, NT, 128]), op=ALU.mult))
                kb.I('dve', ['g_O', 'g_anw'], ['g_O'], lambda e: e.tensor_tensor(out=O[:], in0=O[:], in1=anw[:].unsqueeze(1).to_broadcast([128, NT, 128]), op=ALU.mult))
                kb.I('dve', ['g_O', 'g_zs'], ['g_O'], lambda e: e.tensor_tensor(out=O[:], in0=O[:], in1=zs[:], op=ALU.mult))
                for n0 in range(0, NT, 4):
                    pb = psr.next()
                    nn = min(4, NT - n0)
                    for i in range(nn):
                        tr(ps[pb][:, i * 128:(i + 1) * 128], O[:, n0 + i, :], ['g_O'], [psk[pb]])
                    kb.I('act', [psk[pb]], ['g_tmp'], lambda e: e.activation(out=tmp[:, n0 * 128:(n0 + nn) * 128], in_=ps[pb][:, 0:nn * 128], func=AF.Copy))
                kb.DMA('sp', ['g_tmp'], [('mixT', b)], lambda e: e.dma_start(out=mixT[b, hd], in_=tmp[:]))
            kb.barrier()

    def gdn_chains(st0, b, hd, T3, Ktm, Vtm, gG, gB, O):
        QT, KT = T3[0], T3[1]
        with ExitStack() as st:
            NBUF = 2
            def mk(name, shape):
                return [sb(f"c_{name}{i}", shape, stack=st) for i in range(NBUF)]
            cs = mk("cs", [128, 8])
            Tg = mk("Tg", [128, 128]); dB = mk("dB", [128, 128])
            Ei = mk("Ei", [128, 128]); EmI = mk("EmI", [128, 128]); tt = mk("tt", [128, 128])
            Y = [mk("Ya", [128, 128]), mk("Yb", [128, 128])]
            YT = [mk("YTa", [128, 128]), mk("YTb", [128, 128])]
            R = [mk("Ra", [128, 128]), mk("Rb", [128, 128])]
            qkm = mk("qkm", [128, 128]); eg = mk("eg", [128, 128]); qd = mk("qd", [128, 128])
            rv = mk("rv", [128, 128]); rk = mk("rk", [128, 128]); U = mk("U", [128, 128]); WT = mk("WT", [128, 128])
            Kd = mk("Kd", [128, 128]); vn = mk("vn", [128, 128])
            S = [sb(f"c_S{i}", [128, 128], stack=st) for i in range(2)]
            it = 0
            for d in range(2):
                col = d * 4 + hd
                order = ([0, 1] + list(range(2, NT))) if d == 0 else ([1, 0] + list(range(NT - 1, 1, -1)))
                si = 0
                kb.I('dve', [], ['c_S0'], lambda e: e.memset(S[0][:], 0.0))
                for n in order:
                    i = it % NBUF
                    it += 1
                    K_ = lambda nm: f"c_{nm}{i}"
                    gcol = gG[:, n, col:col + 1]
                    bcol = gB[:, n, col:col + 1]
                    tok = slice(n * 128, (n + 1) * 128)
                    kb.I('dve', ['g_g', 'msk'], [K_('Tg')], lambda e: e.tensor_scalar(out=Tg[i][:], in0=msk[:, d, :], scalar1=gcol, scalar2=None, op0=ALU.mult))
                    kb.I('dve', ['g_bt', 'ident'], [K_('dB')], lambda e: e.tensor_scalar(out=dB[i][:], in0=ident[:], scalar1=bcol, scalar2=None, op0=ALU.mult))
                    pa = psr.next()
                    mm(ps[pa][:, 0:128], ones[:], Tg[i][:], ['ones', K_('Tg')], [psk[pa]])
                    mm(ps[pa][:, 128:256], msk[:, 4 + d, :], dB[i][:], ['msk', K_('dB')], [psk[pa]])
                    mm(ps[pa][:, 256:257], msk[:, d, :], gcol, ['msk', 'g_g'], [psk[pa]])
                    mm(ps[pa][:, 257:258], ones[:], gcol, ['ones', 'g_g'], [psk[pa]])
                    kb.I('act', [psk[pa]], [K_('cs')], lambda e: e.activation(out=cs[i][:, 0:2], in_=ps[pa][:, 256:258], func=AF.Copy))
                    kb.I('act', [K_('cs')], [K_('cs')], lambda e: e.activation(out=cs[i][:, 2:3], in_=cs[i][:, 0:1], func=AF.Exp))
                    kb.I('dve', [K_('cs'), 'g_bt'], [K_('cs')], lambda e: e.tensor_tensor(out=cs[i][:, 3:4], in0=cs[i][:, 2:3], in1=bcol, op=ALU.mult))
                    kb.I('act', [K_('cs')], [K_('cs')], lambda e: e.activation(out=cs[i][:, 4:5], in_=cs[i][:, 0:1], func=AF.Exp, scale=-1.0, bias=cs[i][:, 1:2]))
                    kb.I('act', [K_('cs')], [K_('cs')], lambda e: e.activation(out=cs[i][:, 5:6], in_=cs[i][:, 1:2], func=AF.Exp))
                    kb.I('dve', [psk[pa], K_('cs')], [K_('Ei')], lambda e: e.tensor_scalar(out=Ei[i][:], in0=ps[pa][:, 0:128], scalar1=cs[i][:, 0:1], scalar2=0.0, op0=ALU.subtract, op1=ALU.min))
                    kb.I('act', [K_('Ei')], [K_('Ei')], lambda e: e.activation(out=Ei[i][:], in_=Ei[i][:], func=AF.Exp))
                    kb.I('act', [psk[pa]], [K_('eg')], lambda e: e.activation(out=eg[i][:], in_=ps[pa][:, 0:128], func=AF.Exp))
                    kb.I('dve', [K_('Ei'), 'msk'], [K_('EmI')], lambda e: e.tensor_tensor(out=EmI[i][:], in0=Ei[i][:], in1=msk[:, 2 + d, :], op=ALU.mult))
                    kb.I('dve', [K_('Ei'), psk[pa]], [K_('tt')], lambda e: e.tensor_tensor(out=tt[i][:], in0=Ei[i][:], in1=ps[pa][:, 128:256], op=ALU.mult))
                    pq = psr.next()
                    mm(ps[pq][:, 0:128], KT[:, tok], KT[:, tok], ['g_T1'], [psk[pq]])
                    mm(ps[pq][:, 128:256], KT[:, tok], QT[:, tok], ['g_T1', 'g_T0'], [psk[pq]])
                    y0, yt0, r0 = Y[0][i], YT[0][i], R[0][i]
                    kb.I('dve', [psk[pq], K_('tt')], [K_('Ya')], lambda e: e.scalar_tensor_tensor(out=y0[:], in0=ps[pq][:, 0:128], scalar=-1.0, in1=tt[i][:], op0=ALU.mult, op1=ALU.mult))
                    kb.I('dve', [psk[pq], K_('EmI')], [K_('qkm')], lambda e: e.tensor_tensor(out=qkm[i][:], in0=ps[pq][:, 128:256], in1=EmI[i][:], op=ALU.mult))
                    kb.I('dve', [K_('eg'), 'g_T0'], [K_('qd')], lambda e: e.tensor_tensor(out=qd[i][:], in0=QT[:, tok], in1=eg[i][:], op=ALU.mult))
                    pz = psr.next()
                    tr(ps[pz][:, 0:128], y0[:], [K_('Ya')], [psk[pz]])
                    kb.I('act', [psk[pz]], [K_('YTa')], lambda e: e.activation(out=yt0[:], in_=ps[pz][:, 0:128], func=AF.Copy))
                    kb.I('dve', [K_('Ya'), 'ident'], [K_('Ra')], lambda e: e.tensor_tensor(out=r0[:], in0=y0[:], in1=ident[:], op=ALU.add))
                    cur = 0
                    names = ['a', 'b']
                    for lev in range(6):
                        nx = 1 - cur
                        yc, ytc, rc = Y[cur][i], YT[cur][i], R[cur][i]
                        yn, ytn, rn = Y[nx][i], YT[nx][i], R[nx][i]
                        kc_, kn_ = names[cur], names[nx]
                        pl = psr.next()
                        mm(ps[pl][:, 128:256], yc[:], ytc[:], [K_('Y' + kc_), K_('YT' + kc_)], [psk[pl]])
                        kb.I('act', [psk[pl]], [K_('YT' + kn_)], lambda e: e.activation(out=ytn[:], in_=ps[pl][:, 128:256], func=AF.Copy))
                        if lev < 5:
                            mm(ps[pl][:, 0:128], ytc[:], yc[:], [K_('Y' + kc_), K_('YT' + kc_)], [psk[pl]])
                            kb.I('act', [psk[pl]], [K_('Y' + kn_)], lambda e: e.activation(out=yn[:], in_=ps[pl][:, 0:128], func=AF.Copy))
                        mm(ps[pl][:, 256:384], ytn[:], rc[:], [K_('YT' + kn_), K_('R' + kc_)], [psk[pl]])
                        kb.I('dve', [psk[pl], K_('R' + kc_)], [K_('R' + kn_)], lambda e: e.tensor_tensor(out=rn[:], in0=rc[:], in1=ps[pl][:, 256:384], op=ALU.add))
                        cur = nx
                    Rf, Rk_ = R[cur][i], K_('R' + names[cur])
                    kb.I('dve', ['g_Vtm', 'g_bt'], [K_('rv')], lambda e: e.tensor_scalar(out=rv[i][:], in0=Vtm[:, n, :], scalar1=bcol, scalar2=None, op0=ALU.mult))
                    kb.I('dve', ['g_Ktm', K_('cs')], [K_('rk')], lambda e: e.tensor_scalar(out=rk[i][:], in0=Ktm[:, n, :], scalar1=cs[i][:, 3:4], scalar2=None, op0=ALU.mult))
                    kb.I('dve', ['g_Ktm', K_('cs')], [K_('Kd')], lambda e: e.tensor_scalar(out=Kd[i][:], in0=Ktm[:, n, :], scalar1=cs[i][:, 4:5], scalar2=None, op0=ALU.mult))
                    pu = psr.next()
                    mm(ps[pu][:, 0:128], Rf[:], rv[i][:], [Rk_, K_('rv')], [psk[pu]])
                    mm(ps[pu][:, 128:256], rk[i][:], Rf[:], [Rk_, K_('rk')], [psk[pu]])
                    kb.I('act', [psk[pu]], [K_('U')], lambda e: e.activation(out=U[i][:], in_=ps[pu][:, 0:128], func=AF.Copy))
                    kb.I('act', [psk[pu]], [K_('WT')], lambda e: e.activation(out=WT[i][:], in_=ps[pu][:, 128:256], func=AF.Copy))
                    Sc, Sn = S[si], S[1 - si]
                    skc, skn = f'c_S{si}', f'c_S{1 - si}'
                    p1 = psr.next()
                    mm(ps[p1][:, 0:128], WT[i][:], Sc[:], [K_('WT'), skc], [psk[p1]])
                    kb.I('dve', [psk[p1], K_('U')], [K_('vn')], lambda e: e.tensor_tensor(out=vn[i][:], in0=U[i][:], in1=ps[p1][:, 0:128], op=ALU.subtract))
                    mm(ps[p1][:, 128:256], qd[i][:], Sc[:], [K_('qd'), skc], [psk[p1]], start=True, stop=False)
                    mm(ps[p1][:, 128:256], qkm[i][:], vn[i][:], [K_('qkm'), K_('vn')], [psk[p1]], start=False, stop=True)
                    if d == 0:
                        kb.I('act', [psk[p1]], ['g_O'], lambda e: e.activation(out=O[:, n, :], in_=ps[p1][:, 128:256], func=AF.Copy))
                    else:
                        kb.I('dve', [psk[p1], 'g_O'], ['g_O'], lambda e: e.tensor_tensor(out=O[:, n, :], in0=O[:, n, :], in1=ps[p1][:, 128:256], op=ALU.add))
                    mm(ps[p1][:, 256:384], Kd[i][:], vn[i][:], [K_('Kd'), K_('vn')], [psk[p1]])
                    kb.I('dve', [psk[p1], skc, K_('cs')], [skn], lambda e: e.scalar_tensor_tensor(out=Sn[:], in0=Sc[:], scalar=cs[i][:, 5:6], in1=ps[p1][:, 256:384], op0=ALU.mult, op1=ALU.add))
                    si = 1 - si
            kb.barrier()

    def w_out_phase(b):
        with ExitStack() as st:
            wo = sb("o_w", [128, KC, D], stack=st)
            kb.DMA('sp', [], ['o_w'], lambda e: e.dma_start(out=wo[:], in_=ev_w_out[:, :, :]))
            gt = {}
            for kind, row in (('c', 2), ('l', b)):
                g = sb("o_g" + kind, [128, D], stack=st)
                kb.DMA('sp', [('modsD', 0)], ['o_g' + kind], lambda e: e.dma_start(out=g[:], in_=modsD[0, row:row + 1, 2 * D:3 * D].to_broadcast([128, D])))
                gt[kind] = g
            mt = [sb(f"o_m{i}", [128, KC, 128], stack=st) for i in range(2)]
            xt = [sb(f"o_x{i}", [128, D], stack=st) for i in range(2)]
            for t in range(NT):
                i = t % 2
                kind = 'c' if t < 2 else 'l'
                kb.DMA('sp', [('mixT', b)], [f'o_m{i}'], lambda e: e.dma_start(out=mt[i][:], in_=mixT[b, :, :, t * 128:(t + 1) * 128].rearrange("k p t -> p k t")))
                kb.DMA('sp', ['xin'], [f'o_x{i}'], lambda e: e.dma_start(out=xt[i][:], in_=xin[b, t * 128:(t + 1) * 128, :]))
                for hlf in range(2):
                    pb = psr.next()
                    for k in range(KC):
                        mm(ps[pb][:, :], mt[i][:, k, :], wo[:, k, hlf * 512:(hlf + 1) * 512], [f'o_m{i}', 'o_w'], [psk[pb]], start=(k == 0), stop=(k == KC - 1))
                    if 'yl0' in dbg_out and b == 0:
                        yb = sb(f"o_y{t}_{hlf}", [128, 512], stack=st)
                        kb.I('act', [psk[pb]], [f'o_y{t}_{hlf}'], lambda e: e.activation(out=yb[:], in_=ps[pb][:, :], func=AF.Copy))
                        kb.DMA('sp', [f'o_y{t}_{hlf}'], ['dbg_yl0'], lambda e: e.dma_start(out=dbg_out['yl0'][t * 128:(t + 1) * 128, hlf * 512:(hlf + 1) * 512], in_=yb[:]))
                    kb.I('dve', [psk[pb], 'o_g' + kind], [psk[pb]], lambda e: e.tensor_tensor(out=ps[pb][:, :], in0=ps[pb][:, :], in1=gt[kind][:, hlf * 512:(hlf + 1) * 512], op=ALU.mult))
                    kb.I('dve', [psk[pb], f'o_x{i}'], [f'o_x{i}'], lambda e: e.tensor_tensor(out=xt[i][:, hlf * 512:(hlf + 1) * 512], in0=xt[i][:, hlf * 512:(hlf + 1) * 512], in1=ps[pb][:, :], op=ALU.add))
                kb.DMA('sp', [f'o_x{i}'], [('xs', b)], lambda e: e.dma_start(out=xs[b, t * 128:(t + 1) * 128, :], in_=xt[i][:]))
            kb.barrier()

    peer_wq = din("peer_wq", [2, 128, KC, 2048])
    peer_kT = din("peer_kT", [2, 128, 16, 128])
    peer_u = [din(f"peer_u{i}", [16384, D]) for i in range(2)]
    peer_v = [din(f"peer_v{i}", [16384, D]) for i in range(2)]
    pconst = din("pconst", [1, 32])
    xs1 = dscr("xs1", [NB, LL, D])
    NEG = -1.0e30

    def peer_phase(l, b, tiles, src_fn, dst_fn, final=False):
        with ExitStack() as st:
            wqb = [sb(f"p_wq{i}", [128, KC, 512], stack=st) for i in range(2)]
            kT = sb("p_kT", [128, 16, 128], stack=st)
            kb.DMA('sp', [], ['p_kT'], lambda e: e.dma_start(out=kT[:], in_=peer_kT[l]))
            pc = sb("p_pc", [128, 32], stack=st)
            kb.DMA('sp', [], ['p_pc'], lambda e: e.dma_start(out=pc[:], in_=pconst[0:1, :].to_broadcast([128, 32])))
            nw = sb("p_nw", [128, D], stack=st)
            kb.DMA('sp', [], ['p_nw'], lambda e: e.dma_start(out=nw[:], in_=norms[2 + l].to_broadcast([128, D])))
            A = sb("p_A", [128, D], stack=st); S = sb("p_S", [128, D], stack=st); G = sb("p_G", [128, D], stack=st)
            fnw = None
            if final:
                fnw = sb("p_fnw", [128, D], stack=st)
                kb.DMA('sp', [], ['p_fnw'], lambda e: e.dma_start(out=fnw[:], in_=norms[4].to_broadcast([128, D])))
            xt = [sb(f"p_x{i}", [128, D], stack=st) for i in range(2)]
            h2 = sb("p_h2", [128, D], stack=st)
            sq = sb("p_sq", [128, D], stack=st)
            ss = sb("p_ss", [128, 1], stack=st)
            h2T = sb("p_h2T", [128, KC, 128], stack=st)
            qT = sb("p_qT", [128, 16, 128], stack=st)
            s1 = sb("p_s1", [128, 16, 128], stack=st)
            s2 = sb("p_s2", [128, 16, 128], stack=st)
            m8 = sb("p_m8", [128, 16, 16], stack=st)
            i8 = sb("p_i8", [128, 16, 16], U32, stack=st)
            i8f = sb("p_i8f", [128, 16, 16], stack=st)
            cand = sb("p_cand", [128, 8, 256], stack=st)
            cand2 = sb("p_cand2", [128, 8, 256], stack=st)
            sc = sb("p_sc", [128, 8, 16], stack=st)
            ci = sb("p_ci", [128, 8, 16], U32, stack=st)
            cf = sb("p_cf", [128, 8, 16], stack=st)
            fi = sb("p_fi", [128, 8, 16], stack=st)
            fj = sb("p_fj", [128, 8, 16], stack=st)
            oh = sb("p_oh", [128, 8, 16, 16], stack=st)
            g1 = sb("p_g1", [128, 8, 16], stack=st)
            g2_ = sb("p_g2", [128, 8, 16], stack=st)
            eid = sb("p_eid", [128, 128], I32, stack=st)
            gate = sb("p_gate", [128, 8, 16], stack=st)
            gsum = sb("p_gsum", [128, 8], stack=st)
            act = sb("p_act", [128, 128], stack=st)
            t1 = sb("p_t1", [128, 128], stack=st)
            wgt = sb("p_wgt", [128, 128], stack=st)
            NG = 12
            gb = [sb(f"p_gb{i}", [128, D], stack=st) for i in range(NG)]
            gring = Ring(list(range(NG)))
            acc = sb("p_acc", [128, D], stack=st)
            junk = sb("p_junk", [128, D], stack=st)
            NDG = 4
            dg = [sb(f"p_dg{i}", [128, 128], stack=st) for i in range(NDG)]
            dring = Ring(list(range(NDG)))
            psr = Ring(list(range(6)))
            cur_kind = None
            for ti, t in enumerate(tiles):
                i = ti % 2
                kind = 'c' if t < 2 else 'l'
                row = 2 if kind == 'c' else b
                if kind != cur_kind:
                    cur_kind = kind
                    kb.DMA('sp', [('modsD', l)], ['p_A'], lambda e: e.dma_start(out=A[:], in_=modsD[l, row:row + 1, 4 * D:5 * D].to_broadcast([128, D])))
                    kb.DMA('sp', [('modsD', l)], ['p_S'], lambda e: e.dma_start(out=S[:], in_=modsD[l, row:row + 1, 3 * D:4 * D].to_broadcast([128, D])))
                    kb.DMA('sp', [('modsD', l)], ['p_G'], lambda e: e.dma_start(out=G[:], in_=modsD[l, row:row + 1, 5 * D:6 * D].to_broadcast([128, D])))
                    kb.I('dve', ['p_A', 'p_nw'], ['p_A'], lambda e: e.scalar_tensor_tensor(out=A[:], in0=A[:], scalar=1.0, in1=nw[:], op0=ALU.add, op1=ALU.mult))
                sap, skey = src_fn(t)
                kb.DMA('sp', [skey], [f'p_x{i}'], lambda e: e.dma_start(out=xt[i][:], in_=sap))
                kb.I('act', [f'p_x{i}'], ['p_sq', 'p_ss'], lambda e: e.activation(out=sq[:], in_=xt[i][:], func=AF.Square, accum_out=ss[:]))
                kb.I('act', ['p_ss', 'epsc'], ['p_ss'], lambda e: e.activation(out=ss[:], in_=ss[:], func=AF.Sqrt, scale=1.0 / D, bias=epsc[:]))
                kb.I('dve', ['p_ss'], ['p_ss'], lambda e: e.reciprocal(out=ss[:], in_=ss[:]))
                kb.I('dve', [f'p_x{i}', 'p_ss', 'p_A'], ['p_h2'], lambda e: e.scalar_tensor_tensor(out=h2[:], in0=xt[i][:], scalar=ss[:], in1=A[:], op0=ALU.mult, op1=ALU.mult))
                kb.I('dve', ['p_h2', 'p_S'], ['p_h2'], lambda e: e.tensor_tensor(out=h2[:], in0=h2[:], in1=S[:], op=ALU.add))
                for half in range(2):
                    pb = psr.next()
                    for kk in range(4):
                        k = half * 4 + kk
                        tr(ps[pb][:, kk * 128:(kk + 1) * 128], h2[:, k * 128:(k + 1) * 128], ['p_h2'], [psk[pb]])
                    kb.I('act', [psk[pb]], ['p_h2T'], lambda e: e.activation(out=h2T[:, half * 4:half * 4 + 4, :], in_=ps[pb][:, :].rearrange("p (k t) -> p k t", k=4), func=AF.Copy))
                for q0 in range(0, 16, 4):
                    pb = psr.next()
                    wi_ = (q0 // 4) % 2
                    wq = wqb[wi_]
                    kb.DMA('sp', [], [f'p_wq{wi_}'], lambda e: e.dma_start(out=wq[:], in_=peer_wq[l, :, :, q0 * 128:(q0 + 4) * 128]))
                    for qq in range(4):
                        for k in range(KC):
                            mm(ps[pb][:, qq * 128:(qq + 1) * 128], wq[:, k, qq * 128:(qq + 1) * 128], h2T[:, k, :], [f'p_wq{wi_}', 'p_h2T'], [psk[pb]], start=(k == 0), stop=(k == KC - 1))
                    kb.I('act', [psk[pb]], ['p_qT'], lambda e: e.activation(out=qT[:, q0:q0 + 4, :], in_=ps[pb][:, :].rearrange("p (k t) -> p k t", k=4), func=AF.Copy))
                for q0 in range(0, 16, 4):
                    pb = psr.next()
                    for qq in range(4):
                        hp = q0 + qq
                        mm(ps[pb][:, qq * 128:(qq + 1) * 128], qT[:, hp, :], kT[:, hp, :], ['p_qT', 'p_kT'], [psk[pb]])
                    kb.I('act', [psk[pb]], ['p_s1'], lambda e: e.activation(out=s1[:, q0:q0 + 4, :], in_=ps[pb][:, :].rearrange("p (k t) -> p k t", k=4), func=AF.Copy))
                for hp in range(16):
                    kb.I('dve', ['p_s1'], ['p_m8'], lambda e: e.max(out=m8[:, hp, 0:8], in_=s1[:, hp, :]))
                    kb.I('dve', ['p_s1', 'p_m8'], ['p_i8'], lambda e: e.max_index(out=i8[:, hp, 0:8], in_max=m8[:, hp, 0:8], in_values=s1[:, hp, :]))
                    kb.I('dve', ['p_s1', 'p_m8'], ['p_s2'], lambda e: e.match_replace(out=s2[:, hp, :], in_to_replace=m8[:, hp, 0:8], in_values=s1[:, hp, :], imm_value=NEG))
                    kb.I('dve', ['p_s2'], ['p_m8'], lambda e: e.max(out=m8[:, hp, 8:16], in_=s2[:, hp, :]))
                    kb.I('dve', ['p_s2', 'p_m8'], ['p_i8'], lambda e: e.max_index(out=i8[:, hp, 8:16], in_max=m8[:, hp, 8:16], in_values=s2[:, hp, :]))
                kb.I('dve', ['p_i8'], ['p_i8f'], lambda e: e.tensor_copy(out=i8f[:], in_=i8[:]))
                m8v = m8[:, :, :].rearrange("p (h two) k -> p h two k", two=2)
                i8v = i8f[:, :, :].rearrange("p (h two) k -> p h two k", two=2)
                candv = cand[:, :, :].rearrange("p h (i j) -> p h i j", j=16)
                kb.I('dve', ['p_m8'], ['p_cand'], lambda e: e.tensor_tensor(out=candv, in0=m8v[:, :, 0, :].unsqueeze(3).to_broadcast([128, 8, 16, 16]), in1=m8v[:, :, 1, :].unsqueeze(2).to_broadcast([128, 8, 16, 16]), op=ALU.add))
                for h in range(8):
                    kb.I('dve', ['p_cand'], ['p_sc'], lambda e: e.max(out=sc[:, h, 0:8], in_=cand[:, h, :]))
                    kb.I('dve', ['p_cand', 'p_sc'], ['p_ci'], lambda e: e.max_index(out=ci[:, h, 0:8], in_max=sc[:, h, 0:8], in_values=cand[:, h, :]))
                    kb.I('dve', ['p_cand', 'p_sc'], ['p_cand2'], lambda e: e.match_replace(out=cand2[:, h, :], in_to_replace=sc[:, h, 0:8], in_values=cand[:, h, :], imm_value=NEG))
                    kb.I('dve', ['p_cand2'], ['p_sc'], lambda e: e.max(out=sc[:, h, 8:16], in_=cand2[:, h, :]))
                    kb.I('dve', ['p_cand2', 'p_sc'], ['p_ci'], lambda e: e.max_index(out=ci[:, h, 8:16], in_max=sc[:, h, 8:16], in_values=cand2[:, h, :]))
                kb.I('dve', ['p_ci'], ['p_cf'], lambda e: e.tensor_copy(out=cf[:], in_=ci[:]))
                thr_b = pc[:, 0:16].unsqueeze(1).unsqueeze(1).to_broadcast([128, 8, 16, 16])
                iot_b = pc[:, 16:32].unsqueeze(1).unsqueeze(1).to_broadcast([128, 8, 16, 16])
                kb.I('dve', ['p_cf', 'p_pc'], ['p_oh'], lambda e: e.tensor_tensor(out=oh[:], in0=cf[:].unsqueeze(3).to_broadcast([128, 8, 16, 16]), in1=thr_b, op=ALU.is_ge))
                kb.I('dve', ['p_oh'], ['p_fi'], lambda e: e.tensor_reduce(out=fi[:], in_=oh[:], axis=AX.X, op=ALU.add))
                kb.I('dve', ['p_fi', 'p_cf'], ['p_fj'], lambda e: e.scalar_tensor_tensor(out=fj[:], in0=fi[:], scalar=-16.0, in1=cf[:], op0=ALU.mult, op1=ALU.add))
                for (fx, half_, gdst, gk) in ((fi, 0, g1, 'p_g1'), (fj, 1, g2_, 'p_g2')):
                    fk = 'p_fi' if half_ == 0 else 'p_fj'
                    kb.I('dve', [fk, 'p_pc'], ['p_oh'], lambda e: e.tensor_tensor(out=oh[:], in0=fx[:].unsqueeze(3).to_broadcast([128, 8, 16, 16]), in1=iot_b, op=ALU.is_equal))
                    kb.I('dve', ['p_oh', 'p_i8f'], ['p_oh'], lambda e: e.tensor_tensor(out=oh[:], in0=oh[:], in1=i8v[:, :, half_, :].unsqueeze(2).to_broadcast([128, 8, 16, 16]), op=ALU.mult))
                    kb.I('dve', ['p_oh'], [gk], lambda e: e.tensor_reduce(out=gdst[:], in_=oh[:], axis=AX.X, op=ALU.add))
                kb.I('dve', ['p_g1', 'p_g2'], ['p_g1'], lambda e: e.scalar_tensor_tensor(out=g1[:], in0=g1[:], scalar=128.0, in1=g2_[:], op0=ALU.mult, op1=ALU.add))
                kb.I('dve', ['p_g1'], ['p_eid'], lambda e: e.tensor_copy(out=eid[:, :].rearrange("p (h k) -> p h k", k=16), in_=g1[:]))
                kb.I('dve', ['p_sc'], ['p_gate'], lambda e: e.tensor_tensor(out=gate[:], in0=sc[:], in1=sc[:, :, 0:1].to_broadcast([128, 8, 16]), op=ALU.subtract))
                kb.I('act', ['p_gate'], ['p_gate'], lambda e: e.activation(out=gate[:], in_=gate[:], func=AF.Exp))
                kb.I('dve', ['p_gate'], ['p_gsum'], lambda e: e.tensor_reduce(out=gsum[:], in_=gate[:], axis=AX.X, op=ALU.add))
                kb.I('dve', ['p_gsum'], ['p_gsum'], lambda e: e.reciprocal(out=gsum[:], in_=gsum[:]))
                kb.I('dve', ['p_gate', 'p_gsum'], ['p_gate'], lambda e: e.tensor_tensor(out=gate[:], in0=gate[:], in1=gsum[:].unsqueeze(2).to_broadcast([128, 8, 16]), op=ALU.mult))
                for slot in range(128):
                    gi = gring.next()
                    kb.DMA('pool', ['p_eid'], [f'p_gb{gi}'], lambda e: e.indirect_dma_start(out=gb[gi][:, :], out_offset=None, in_=peer_u[l][:, :], in_offset=bass.IndirectOffsetOnAxis(ap=eid[:, slot:slot + 1], axis=0)))
                    kb.I('dve', [f'p_gb{gi}', 'p_h2'], [('p_act', slot)], lambda e: e.scalar_tensor_tensor(out=junk[:], in0=gb[gi][:], scalar=1.0, in1=h2[:], op0=ALU.mult, op1=ALU.mult, accum_out=act[:, slot:slot + 1]))
                kb.I('dve', [('p_act', s_) for s_ in range(128)], ['p_t1', 'p_act'], lambda e: e.tensor_tensor(out=t1[:], in0=act[:], in1=act[:], op=ALU.mult))
                kb.I('dve', ['p_t1'], ['p_t1'], lambda e: e.tensor_scalar(out=t1[:], in0=t1[:], scalar1=0.044715, scalar2=1.0, op0=ALU.mult, op1=ALU.add))
                kb.I('dve', ['p_t1', 'p_act'], ['p_t1'], lambda e: e.tensor_tensor(out=t1[:], in0=t1[:], in1=act[:], op=ALU.mult))
                kb.I('act', ['p_t1'], ['p_t1'], lambda e: e.activation(out=t1[:], in_=t1[:], func=AF.Sigmoid, scale=1.5957691216057308))
                kb.I('dve', ['p_t1', 'p_act'], ['p_t1'], lambda e: e.tensor_tensor(out=t1[:], in0=t1[:], in1=act[:], op=ALU.mult))
                kb.I('dve', ['p_t1', 'p_gate'], ['p_wgt'], lambda e: e.tensor_tensor(out=wgt[:], in0=t1[:], in1=gate[:, :, :].rearrange("p h k -> p (h k)"), op=ALU.mult))
                for slot in range(128):
                    gi = gring.next()
                    kb.DMA('pool', ['p_eid'], [f'p_gb{gi}'], lambda e: e.indirect_dma_start(out=gb[gi][:, :], out_offset=None, in_=peer_v[l][:, :], in_offset=bass.IndirectOffsetOnAxis(ap=eid[:, slot:slot + 1], axis=0)))
                    di = dring.next()
                    kb.I('dve', ['ident', 'p_wgt'], [f'p_dg{di}'], lambda e: e.tensor_scalar(out=dg[di][:], in0=ident[:], scalar1=wgt[:, slot:slot + 1], scalar2=None, op0=ALU.mult))
                    for hf in range(2):
                        mm(ps[6 + hf][:, :], dg[di][:], gb[gi][:, hf * 512:(hf + 1) * 512], [f'p_dg{di}', f'p_gb{gi}'], [psk[6 + hf]], start=(slot == 0), stop=(slot == 127))
                for hf in range(2):
                    kb.I('dve', [psk[6 + hf], 'p_G'], ['p_acc'], lambda e: e.tensor_tensor(out=acc[:, hf * 512:(hf + 1) * 512], in0=ps[6 + hf][:, :], in1=G[:, hf * 512:(hf + 1) * 512], op=ALU.mult))
                kb.I('dve', ['p_acc', f'p_x{i}'], [f'p_x{i}'], lambda e: e.tensor_tensor(out=xt[i][:], in0=xt[i][:], in1=acc[:], op=ALU.add))
                if final:
                    kb.I('act', [f'p_x{i}'], ['p_sq', 'p_ss'], lambda e: e.activation(out=sq[:], in_=xt[i][:], func=AF.Square, accum_out=ss[:]))
                    kb.I('act', ['p_ss', 'epsc'], ['p_ss'], lambda e: e.activation(out=ss[:], in_=ss[:], func=AF.Sqrt, scale=1.0 / D, bias=epsc[:]))
                    kb.I('dve', ['p_ss'], ['p_ss'], lambda e: e.reciprocal(out=ss[:], in_=ss[:]))
                    kb.I('dve', [f'p_x{i}', 'p_ss', 'p_fnw'], [f'p_x{i}'], lambda e: e.scalar_tensor_tensor(out=xt[i][:], in0=xt[i][:], scalar=ss[:], in1=fnw[:], op0=ALU.mult, op1=ALU.mult))
                dst_fn(t, xt[i], f'p_x{i}')
            kb.barrier()

    od_w_in = din("od_w_in", [128, KC, 2064])
    od_w_out = din("od_w_out", [128, KC, D])
    c_gc = din("c_gc", [1, 16])
    c_norm = din("c_norm", [1, 512])
    d_w = din("d_w", [4, 128, 128])
    d_scale = din("d_scale", [512, 1])
    poolm = din("poolm", [4, 128, 128])
    mixT1 = dscr("mixT1", [NB, KC, 128, LT])

    def xs_cm_src(b, t):
        if t < 2:
            return [(slice(0, 128), xs[b, t * 128:(t + 1) * 128, :])]
        v = xs[b, LC:LT, :].rearrange("(r w) d -> r w d", w=GRID_W)
        return [(slice(wi * 32, (wi + 1) * 32), v[:, (t - 2) * 4 + wi, :]) for wi in range(4)]

    def layer1_mixer(b):
        with ExitStack() as ph:
            hT = sb("hT1", [128, KC, LT], stack=ph)
            with ExitStack() as st:
                nw = sb("n1_nw", [128, D], stack=st)
                kb.DMA('sp', [], ['n1_nw'], lambda e: e.dma_start(out=nw[:], in_=norms[1].to_broadcast([128, D])))
                md = {}
                for kind, row in (('c', 2), ('l', b)):
                    A = sb("n1_A" + kind, [128, D], stack=st)
                    S = sb("n1_S" + kind, [128, D], stack=st)
                    kb.DMA('sp', [('modsD', 1)], ['n1_A' + kind], lambda e: e.dma_start(out=A[:], in_=modsD[1, row:row + 1, D:2 * D].to_broadcast([128, D])))
                    kb.DMA('sp', [('modsD', 1)], ['n1_S' + kind], lambda e: e.dma_start(out=S[:], in_=modsD[1, row:row + 1, 0:D].to_broadcast([128, D])))
                    kb.I('dve', ['n1_A' + kind, 'n1_nw'], ['n1_A' + kind], lambda e: e.scalar_tensor_tensor(out=A[:], in0=A[:], scalar=1.0, in1=nw[:], op0=ALU.add, op1=ALU.mult))
                    md[kind] = (A, S)
                xt = [sb(f"n1_x{i}", [128, D], stack=st) for i in range(2)]
                ht = [sb(f"n1_h{i}", [128, D], stack=st) for i in range(2)]
                sq = sb("n1_sq", [128, D], stack=st)
                ss = [sb(f"n1_ss{i}", [128, 1], stack=st) for i in range(2)]
                for t in range(NT):
                    i = t % 2
                    kind = 'c' if t < 2 else 'l'
                    A, S = md[kind]
                    for (psl, sap) in xs_cm_src(b, t):
                        kb.DMA('sp', [('xs', b)], [f'n1_x{i}'], lambda e: e.dma_start(out=xt[i][psl, :], in_=sap))
                    kb.I('act', [f'n1_x{i}'], ['n1_sq', f'n1_ss{i}'], lambda e: e.activation(out=sq[:], in_=xt[i][:], func=AF.Square, accum_out=ss[i][:]))
                    kb.I('act', [f'n1_ss{i}', 'epsc'], [f'n1_ss{i}'], lambda e: e.activation(out=ss[i][:], in_=ss[i][:], func=AF.Sqrt, scale=1.0 / D, bias=epsc[:]))
                    kb.I('dve', [f'n1_ss{i}'], [f'n1_ss{i}'], lambda e: e.reciprocal(out=ss[i][:], in_=ss[i][:]))
                    kb.I('dve', [f'n1_x{i}', f'n1_ss{i}', 'n1_A' + kind], [f'n1_h{i}'], lambda e: e.scalar_tensor_tensor(out=ht[i][:], in0=xt[i][:], scalar=ss[i][:], in1=A[:], op0=ALU.mult, op1=ALU.mult))
                    kb.I('dve', [f'n1_h{i}', 'n1_S' + kind], [f'n1_h{i}'], lambda e: e.tensor_tensor(out=ht[i][:], in0=ht[i][:], in1=S[:], op=ALU.add))
                    for half in range(2):
                        pb = psr.next()
                        for kk in range(4):
                            k = half * 4 + kk
                            tr(ps[pb][:, kk * 128:(kk + 1) * 128], ht[i][:, k * 128:(k + 1) * 128], [f'n1_h{i}'], [psk[pb]])
                        kb.I('act', [psk[pb]], [('hT', t)], lambda e: e.activation(out=hT[:, half * 4:half * 4 + 4, t * 128:(t + 1) * 128], in_=ps[pb][:, :].rearrange("p (k t) -> p k t", k=4), func=AF.Copy))
                kb.barrier()
            with ExitStack() as st:
                wd = sb("q_wd", [128, KC, 512], stack=st)
                kb.DMA('sp', [], ['q_wd'], lambda e: e.dma_start(out=wd[:], in_=od_w_in[:, :, 1552:2064]))
                pm = sb("q_pm", [128, 4, 128], stack=st)
                dw = sb("q_dw", [128, 4, 128], stack=st)
                dsc = sb("q_dsc", [128, 4], stack=st)
                for g in range(4):
                    kb.DMA('sp', [], ['q_pm'], lambda e: e.dma_start(out=pm[:, g, :], in_=poolm[g]))
                    kb.DMA('sp', [], ['q_dw'], lambda e: e.dma_start(out=dw[:, g, :], in_=d_w[g]))
                    kb.DMA('sp', [], ['q_dsc'], lambda e: e.dma_start(out=dsc[:, g:g + 1], in_=d_scale[g * 128:(g + 1) * 128, :]))
                dtm = sb("q_dtm", [128, 512], stack=st)
                pT = sb("q_pT", [128, 128], stack=st)
                yT = sb("q_yT", [128, 4, LT], stack=st)
                for t in range(2, NT):
                    pb = psr.next()
                    for k in range(KC):
                        mm(ps[pb][:, :], hT[:, k, t * 128:(t + 1) * 128], wd[:, k, :], ['q_wd', ('hT', t)], [psk[pb]], start=(k == 0), stop=(k == KC - 1))
                    kb.I('act', [psk[pb]], ['q_dtm'], lambda e: e.activation(out=dtm[:], in_=ps[pb][:, :], func=AF.Copy))
                    for g in range(4):
                        p2 = psr.next()
                        mm(ps[p2][:, 0:128], dtm[:, g * 128:(g + 1) * 128], pm[:, g, :], ['q_dtm', 'q_pm'], [psk[p2]])
                        kb.I('act', [psk[p2]], ['q_pT'], lambda e: e.activation(out=pT[:], in_=ps[p2][:, 0:128], func=AF.Copy))
                        mm(ps[p2][:, 128:256], dw[:, g, :], pT[:], ['q_dw', 'q_pT'], [psk[p2]])
                        kb.I('dve', [psk[p2], 'q_dsc'], ['q_yT'], lambda e: e.tensor_scalar(out=yT[:, g, t * 128:(t + 1) * 128], in0=ps[p2][:, 128:256], scalar1=dsc[:, g:g + 1], scalar2=None, op0=ALU.mult))
                for g in range(4):
                    kb.DMA('sp', ['q_yT'], [('mixT1', b)], lambda e: e.dma_start(out=mixT1[b, 4 + g, :, LC:LT], in_=yT[:, g, LC:LT]))
                kb.barrier()
            mlstm(b, hT)
            kb.barrier()

    def mlstm(b, hT):
        with ExitStack() as st:
            wh = sb("m_wh", [128, KC, 64 + 64 + 128 + 128], stack=st)
            wg16 = sb("m_wg16", [128, KC, 16], stack=st)
            gcst = sb("m_gcst", [128, 16], stack=st)
            cnw = sb("m_cnw", [128, 512], stack=st)
            QT = sb("m_QT", [64, LT], stack=st)
            KT = sb("m_KT", [64, LT], stack=st)
            Ktm = sb("m_Ktm", [128, NT, 64], stack=st)
            V1 = sb("m_V1", [128, NT, 132], stack=st)
            og = sb("m_og", [128, NT, 128], stack=st)
            O = sb("m_O", [128, NT, 128], stack=st)
            tmp = sb("m_tmp", [128, LT], stack=st)
            graw = sb("m_graw", [128, NT, 16], stack=st)
            LI = sb("m_li", [128, NT, 8], stack=st)
            LF = sb("m_lf", [128, NT, 8], stack=st)
            kb.DMA('sp', [], ['m_gcst'], lambda e: e.dma_start(out=gcst[:], in_=c_gc[0:1, :].to_broadcast([128, 16])))
            kb.DMA('sp', [], ['m_cnw'], lambda e: e.dma_start(out=cnw[:], in_=c_norm[0:1, :].to_broadcast([128, 512])))
            kb.DMA('sp', [], ['m_wg16'], lambda e: e.dma_start(out=wg16[:], in_=od_w_in[:, :, 1024:1040]))
            pb = psr.next()
            for n in range(NT):
                for k in range(KC):
                    mm(ps[pb][:, n * 16:(n + 1) * 16], hT[:, k, n * 128:(n + 1) * 128], wg16[:, k, :], ['m_wg16', ('hT', n)], [psk[pb]], start=(k == 0), stop=(k == KC - 1))
            kb.I('act', [psk[pb]], ['m_graw'], lambda e: e.activation(out=graw[:], in_=ps[pb][:, 0:NT * 16].rearrange("p (n c) -> p n c", c=16), func=AF.Copy))
            kb.I('dve', ['m_graw', 'm_gcst'], ['m_li'], lambda e: e.tensor_tensor(out=LI[:], in0=graw[:, :, 0:8], in1=gcst[:, 0:8].unsqueeze(1).to_broadcast([128, NT, 8]), op=ALU.add))
            kb.I('dve', ['m_graw', 'm_gcst'], ['m_lf'], lambda e: e.tensor_tensor(out=LF[:], in0=graw[:, :, 8:16], in1=gcst[:, 8:16].unsqueeze(1).to_broadcast([128, NT, 8]), op=ALU.add))
            kb.I('act', ['m_lf'], ['m_lf'], lambda e: e.activation(out=LF[:], in_=LF[:], func=AF.Exp, scale=-1.0))
            kb.I('act', ['m_lf', 'onec'], ['m_lf'], lambda e: e.activation(out=LF[:], in_=LF[:], func=AF.Ln, bias=onec[:]))
            kb.I('dve', ['m_lf'], ['m_lf'], lambda e: e.tensor_scalar(out=LF[:], in0=LF[:], scalar1=-1.0, scalar2=None, op0=ALU.mult))
            kb.I('dve', [], ['m_V1'], lambda e: e.memset(V1[:], 1.0))
            for hd in range(4):
                for (o0, c0, w_) in ((0, hd * 64, 64), (64, 256 + hd * 64, 64), (128, 512 + hd * 128, 128), (256, 1040 + hd * 128, 128)):
                    kb.DMA('sp', [], ['m_wh'], lambda e: e.dma_start(out=wh[:, :, o0:o0 + w_], in_=od_w_in[:, :, c0:c0 + w_]))
                for (dst, dk_, o0, scl) in ((QT, 'm_QT', 0, 1.0), (KT, 'm_KT', 64, 0.125)):
                    for (t0, n) in TOKBLKS:
                        pb = psr.next()
                        for k in range(KC):
                            mm(ps[pb][0:64, 0:n], wh[:, k, o0:o0 + 64], hT[:, k, t0:t0 + n], ['m_wh'] + [('hT', tt) for tt in range(t0 // 128, (t0 + n) // 128)], [psk[pb]], start=(k == 0), stop=(k == KC - 1))
                        kb.I('act', [psk[pb]], [dk_], lambda e: e.activation(out=dst[:, t0:t0 + n], in_=ps[pb][0:64, 0:n], func=AF.Copy, scale=scl))
                for n0 in range(0, NT, 8):
                    pb = psr.next()
                    nn = min(8, NT - n0)
                    for i in range(nn):
                        kb.I('pe', ['m_KT', 'ident'], [psk[pb]], lambda e: e.transpose(out=ps[pb][:, i * 64:(i + 1) * 64], in_=KT[:, (n0 + i) * 128:(n0 + i + 1) * 128], identity=ident[0:64, 0:64]))
                    kb.I('act', [psk[pb]], ['m_Ktm'], lambda e: e.activation(out=Ktm[:, n0:n0 + nn, :], in_=ps[pb][:, 0:nn * 64].rearrange("p (n c) -> p n c", c=64), func=AF.Copy))
                for (o0, dst, dk_, fn_, wdt) in ((128, V1, 'm_V1', AF.Copy, 128), (256, og, 'm_og', AF.Sigmoid, 128)):
                    for n0 in range(0, NT, 4):
                        pb = psr.next()
                        nn = min(4, NT - n0)
                        for i in range(nn):
                            n = n0 + i
                            for k in range(KC):
                                mm(ps[pb][:, i * 128:(i + 1) * 128], hT[:, k, n * 128:(n + 1) * 128], wh[:, k, o0:o0 + 128], ['m_wh', ('hT', n)], [psk[pb]], start=(k == 0), stop=(k == KC - 1))
                        kb.I('act', [psk[pb]], [dk_], lambda e: e.activation(out=dst[:, n0:n0 + nn, 0:128], in_=ps[pb][:, 0:nn * 128].rearrange("p (n c) -> p n c", c=128), func=fn_))
                mlstm_chains(b, hd, QT, KT, Ktm, V1, LI, LF, O)
                ssq = sb(f"m_ssq{hd}", [128, NT], stack=st)
                O2 = tmp[:, :].rearrange("p (n c) -> p n c", c=128)
                kb.I('dve', ['m_O'], ['m_tmp'], lambda e: e.tensor_tensor(out=O2, in0=O[:], in1=O[:], op=ALU.mult))
                kb.I('dve', ['m_tmp'], ['m_ssq'], lambda e: e.tensor_reduce(out=ssq[:], in_=O2, axis=AX.X, op=ALU.add))
                kb.I('act', ['m_ssq', 'epsc'], ['m_ssq'], lambda e: e.activation(out=ssq[:], in_=ssq[:], func=AF.Sqrt, scale=1.0 / 128, bias=epsc[:]))
                kb.I('dve', ['m_ssq'], ['m_ssq'], lambda e: e.reciprocal(out=ssq[:], in_=ssq[:]))
                kb.I('dve', ['m_O', 'm_ssq'], ['m_O'], lambda e: e.tensor_tensor(out=O[:], in0=O[:], in1=ssq[:].unsqueeze(2).to_broadcast([128, NT, 128]), op=ALU.mult))
                kb.I('dve', ['m_O', 'm_cnw'], ['m_O'], lambda e: e.tensor_tensor(out=O[:], in0=O[:], in1=cnw[:, hd * 128:(hd + 1) * 128].unsqueeze(1).to_broadcast([128, NT, 128]), op=ALU.mult))
                kb.I('dve', ['m_O', 'm_og'], ['m_O'], lambda e: e.tensor_tensor(out=O[:], in0=O[:], in1=og[:], op=ALU.mult))
                for n0 in range(0, NT, 4):
                    pb = psr.next()
                    nn = min(4, NT - n0)
                    for i in range(nn):
                        tr(ps[pb][:, i * 128:(i + 1) * 128], O[:, n0 + i, :], ['m_O'], [psk[pb]])
                    kb.I('act', [psk[pb]], ['m_tmp'], lambda e: e.activation(out=tmp[:, n0 * 128:(n0 + nn) * 128], in_=ps[pb][:, 0:nn * 128], func=AF.Copy))
                kb.DMA('sp', ['m_tmp'], [('mixT1', b)], lambda e: e.dma_start(out=mixT1[b, hd, :, LC:LT], in_=tmp[:, LC:LT]))
            kb.barrier()

    def mlstm_chains(b, hd, QT, KT, Ktm, V1, LI, LF, O):
        with ExitStack() as st:
            NBUF = 2

            def mk(name, shape):
                return [sb(f"k_{name}{i}", shape, stack=st) for i in range(NBUF)]
            cs = mk("cs", [128, 8])
            Tg = mk("Tg", [128, 128]); Ei = mk("Ei", [128, 128]); eg = mk("eg", [64, 128]); qd = mk("qd", [64, 128])
            ST = mk("ST", [128, 128]); Kd = mk("Kd", [128, 64])
            C = [sb(f"k_C{i}", [64, 132], stack=st) for i in range(2)]
            kb.I('dve', [], ['m_O'], lambda e: e.memset(O[:, 0:2, :], 0.0))
            it = 0
            for d in range(2):
                col = d * 4 + hd
                order = ([0, 1] + list(range(2, NT))) if d == 0 else ([1, 0] + list(range(NT - 1, 1, -1)))
                si = 0
                kb.I('dve', [], ['k_C0'], lambda e: e.memset(C[0][:], 0.0))
                for n in order:
                    i = it % NBUF
                    it += 1
                    K_ = lambda nm: f"k_{nm}{i}"
                    fcol = LF[:, n, col:col + 1]
                    icol = LI[:, n, col:col + 1]
                    tok = slice(n * 128, (n + 1) * 128)
                    Cc, Cn = C[si], C[1 - si]
                    ckc, ckn = f'k_C{si}', f'k_C{1 - si}'
                    kb.I('dve', ['m_lf', 'msk'], [K_('Tg')], lambda e: e.tensor_scalar(out=Tg[i][:], in0=msk[:, d, :], scalar1=fcol, scalar2=None, op0=ALU.mult))
                    pa = psr.next()
                    mm(ps[pa][:, 0:128], ones[:], Tg[i][:], ['ones', K_('Tg')], [psk[pa]])
                    mm(ps[pa][:, 256:257], msk[:, d, :], fcol, ['msk', 'm_lf'], [psk[pa]])
                    mm(ps[pa][:, 257:258], ones[:], fcol, ['ones', 'm_lf'], [psk[pa]])
                    kb.I('act', [psk[pa]], [K_('cs')], lambda e: e.activation(out=cs[i][:, 0:2], in_=ps[pa][:, 256:258], func=AF.Copy))
                    kb.I('dve', [K_('cs')], [K_('cs')], lambda e: e.tensor_tensor(out=cs[i][:, 2:3], in0=cs[i][:, 1:2], in1=cs[i][:, 0:1], op=ALU.subtract))
                    kb.I('dve', [K_('cs'), 'm_li'], [K_('cs')], lambda e: e.tensor_tensor(out=cs[i][:, 2:3], in0=cs[i][:, 2:3], in1=icol, op=ALU.add))
                    kb.I('act', [K_('cs')], [K_('cs')], lambda e: e.activation(out=cs[i][:, 3:5], in_=cs[i][:, 1:3], func=AF.Exp))
                    kb.I('dve', ['m_Ktm', K_('cs')], [K_('Kd')], lambda e: e.tensor_scalar(out=Kd[i][:], in0=Ktm[:, n, :], scalar1=cs[i][:, 4:5], scalar2=None, op0=ALU.mult))
                    if n >= 2:
                        kb.I('dve', [psk[pa], K_('cs')], [K_('Ei')], lambda e: e.tensor_scalar(out=Ei[i][:], in0=ps[pa][:, 0:128], scalar1=cs[i][:, 0:1], scalar2=0.0, op0=ALU.subtract, op1=ALU.min))
                        kb.I('act', [K_('Ei'), 'm_li'], [K_('Ei')], lambda e: e.activation(out=Ei[i][:], in_=Ei[i][:], func=AF.Exp, bias=icol))
                        kb.I('dve', [K_('Ei'), 'msk'], [K_('Ei')], lambda e: e.tensor_tensor(out=Ei[i][:], in0=Ei[i][:], in1=msk[:, 2 + d, :], op=ALU.mult))
                        kb.I('act', [psk[pa]], [K_('eg')], lambda e: e.activation(out=eg[i][:], in_=ps[pa][0:64, 0:128], func=AF.Exp))
                        kb.I('dve', [K_('eg'), 'm_QT'], [K_('qd')], lambda e: e.tensor_tensor(out=qd[i][:], in0=QT[:, tok], in1=eg[i][:], op=ALU.mult))
                        pq = psr.next()
                        mm(ps[pq][:, 0:128], KT[:, tok], QT[:, tok], ['m_KT', 'm_QT'], [psk[pq]])
                        kb.I('dve', [psk[pq], K_('Ei')], [K_('ST')], lambda e: e.tensor_tensor(out=ST[i][:], in0=ps[pq][:, 0:128], in1=Ei[i][:], op=ALU.mult))
                        mm(ps[pq][:, 128:257], qd[i][:], Cc[:, 0:129], [K_('qd'), ckc], [psk[pq]], start=True, stop=False)
                        mm(ps[pq][:, 128:257], ST[i][:], V1[:, n, 0:129], [K_('ST'), 'm_V1'], [psk[pq]], start=False, stop=True)
                        kb.I('act', [psk[pq]], [K_('cs')], lambda e: e.activation(out=cs[i][:, 5:6], in_=ps[pq][:, 256:257], func=AF.Abs))
                        kb.I('dve', [K_('cs')], [K_('cs')], lambda e: e.tensor_scalar(out=cs[i][:, 5:6], in0=cs[i][:, 5:6], scalar1=1.0, scalar2=None, op0=ALU.max))
                        kb.I('dve', [K_('cs')], [K_('cs')], lambda e: e.reciprocal(out=cs[i][:, 5:6], in_=cs[i][:, 5:6]))
                        if d == 0:
                            kb.I('dve', [psk[pq], K_('cs')], ['m_O'], lambda e: e.tensor_scalar(out=O[:, n, :], in0=ps[pq][:, 128:256], scalar1=cs[i][:, 5:6], scalar2=None, op0=ALU.mult))
                        else:
                            kb.I('dve', [psk[pq], K_('cs'), 'm_O'], ['m_O'], lambda e: e.scalar_tensor_tensor(out=O[:, n, :], in0=ps[pq][:, 128:256], scalar=cs[i][:, 5:6], in1=O[:, n, :], op0=ALU.mult, op1=ALU.add))
                    p3 = psr.next()
                    mm(ps[p3][0:64, 0:129], Kd[i][:], V1[:, n, 0:129], [K_('Kd'), 'm_V1'], [psk[p3]])
                    kb.I('dve', [psk[p3], ckc, K_('cs')], [ckn], lambda e: e.scalar_tensor_tensor(out=Cn[:, 0:129], in0=Cc[:, 0:129], scalar=cs[i][0:64, 3:4], in1=ps[p3][0:64, 0:129], op0=ALU.mult, op1=ALU.add))
                    si = 1 - si
            kb.barrier()

    def w_out1_phase(b):
        with ExitStack() as st:
            wo = sb("o1_w", [128, KC, D], stack=st)
            kb.DMA('sp', [], ['o1_w'], lambda e: e.dma_start(out=wo[:], in_=od_w_out[:, :, :]))
            g = sb("o1_g", [128, D], stack=st)
            kb.DMA('sp', [('modsD', 1)], ['o1_g'], lambda e: e.dma_start(out=g[:], in_=modsD[1, b:b + 1, 2 * D:3 * D].to_broadcast([128, D])))
            mt = [sb(f"o1_m{i}", [128, KC, 128], stack=st) for i in range(2)]
            xt = [sb(f"o1_x{i}", [128, D], stack=st) for i in range(2)]
            for t in range(2, NT):
                i = t % 2
                kb.DMA('sp', [('mixT1', b)], [f'o1_m{i}'], lambda e: e.dma_start(out=mt[i][:], in_=mixT1[b, :, :, t * 128:(t + 1) * 128].rearrange("k p t -> p k t")))
                for (psl, sap) in xs_cm_src(b, t):
                    kb.DMA('sp', [('xs', b)], [f'o1_x{i}'], lambda e: e.dma_start(out=xt[i][psl, :], in_=sap))
                for hlf in range(2):
                    pb = psr.next()
                    for k in range(KC):
                        mm(ps[pb][:, :], mt[i][:, k, :], wo[:, k, hlf * 512:(hlf + 1) * 512], [f'o1_m{i}', 'o1_w'], [psk[pb]], start=(k == 0), stop=(k == KC - 1))
                    kb.I('dve', [psk[pb], 'o1_g'], [psk[pb]], lambda e: e.tensor_tensor(out=ps[pb][:, :], in0=ps[pb][:, :], in1=g[:, hlf * 512:(hlf + 1) * 512], op=ALU.mult))
                    kb.I('dve', [psk[pb], f'o1_x{i}'], [f'o1_x{i}'], lambda e: e.tensor_tensor(out=xt[i][:, hlf * 512:(hlf + 1) * 512], in0=xt[i][:, hlf * 512:(hlf + 1) * 512], in1=ps[pb][:, :], op=ALU.add))
                kb.DMA('sp', [f'o1_x{i}'], [('xs1', b)], lambda e: e.dma_start(out=xs1[b, (t - 2) * 128:(t - 1) * 128, :], in_=xt[i][:]))
            kb.barrier()

    def layer1(b, tiles=None):
        layer1_mixer(b)
        w_out1_phase(b)
        ov = out_d[b].rearrange("(r w) d -> r w d", w=GRID_W)

        def dst1(t, tile, key):
            for wi in range(4):
                kb.DMA('sp', [key], [('out', b)], lambda e: e.dma_start(out=ov[:, (t - 2) * 4 + wi, :], in_=tile[wi * 32:(wi + 1) * 32, :]))
        peer_phase(1, b, tiles or list(range(2, NT)), lambda t: (xs1[b, (t - 2) * 128:(t - 1) * 128, :], ('xs1', b)), dst1, final=True)

    nb_run = 1 if (stop_after or '').endswith('_b0') else NB
    for b in range(0 if l1only else nb_run):
        layer0_mixer(b)
        if stop_after in ('hT0', 'lru'):
            break
        w_out_phase(b)
        if stop_after == 'mix0_b0':
            continue

        def dst0(t, tile, key, b=b):
            kb.DMA('sp', [key], [('xs', b)], lambda e: e.dma_start(out=xs[b, t * 128:(t + 1) * 128, :], in_=tile[:]))
        peer_tiles = list(range(NT)) if stop_after != 'peer0_b0' else [0, 2]
        peer_phase(0, b, peer_tiles, lambda t, b=b: (xs[b, t * 128:(t + 1) * 128, :], ('xs', b)), dst0)
    if stop_after is None or stop_after.startswith('l1'):
        for b in range(nb_run):
            if stop_after == 'l1mix_b0':
                layer1_mixer(b)
                w_out1_phase(b)
            elif l1only:
                layer1(b, tiles=[2, 17])
            else:
                layer1(b)
    if 'xs' in dbg_out:
        kb.barrier()
        kb.DMA('sp', [('xs', 0), ('xs', 1)], ['dbg_xs'], lambda e: e.dma_start(out=dbg_out['xs'], in_=xs))
    if 'xs1' in dbg_out:
        kb.barrier()
        kb.DMA('sp', [('xs1', 0), ('xs1', 1)], ['dbg_xs1'], lambda e: e.dma_start(out=dbg_out['xs1'], in_=xs1))

    kb.finish()
    es.close()
    return nc


def make_in_maps(inputs):
    x = np.asarray(inputs['x'], np.float32)
    ctx = np.asarray(inputs['ctx'], np.float32)
    c = np.asarray(inputs['c'], np.float32)
    c_ctx = np.asarray(inputs['c_ctx'], np.float32)
    ada_w = np.ascontiguousarray(np.asarray(inputs['ada_w'], np.float32).reshape(2, KC, 128, 6 * D))
    ada_b = np.ascontiguousarray(np.asarray(inputs['ada_b'], np.float32).reshape(2, 1, 6 * D))
    norms = np.stack([inputs['norm_mix'][0], inputs['norm_mix'][1], inputs['norm_ffn'][0], inputs['norm_ffn'][1],
                      inputs['final_norm']]).astype(np.float32).reshape(5, 1, D)
    ident = np.eye(128, dtype=np.float32)
    ev_w_in = np.ascontiguousarray(np.asarray(inputs['ev_w_in'][0], np.float32).reshape(KC, 128, 3088).transpose(1, 0, 2))
    ev_w_out = np.ascontiguousarray(np.asarray(inputs['ev_w_out'][0], np.float32).reshape(KC, 128, D).transpose(1, 0, 2))
    a_convT = np.ascontiguousarray(np.asarray(inputs['a_conv'][0], np.float32).T)
    a_gc = np.concatenate([np.asarray(inputs['a_alog'][0]).reshape(8), np.asarray(inputs['a_dtb'][0]).reshape(8)]).astype(np.float32).reshape(1, 16)
    a_norm = np.asarray(inputs['a_norm'][0], np.float32).reshape(1, 128)
    lru_pc = np.zeros((512, 11), np.float32)
    lru_pc[:, 0:4] = np.asarray(inputs['b_conv_w'][0]).T
    lru_pc[:, 4] = np.asarray(inputs['b_conv_b'][0])
    for d in range(2):
        lru_pc[:, 5 + 3 * d] = np.asarray(inputs['b_ba'][0, d]).reshape(512)
        lru_pc[:, 6 + 3 * d] = np.asarray(inputs['b_bx'][0, d]).reshape(512)
        lru_pc[:, 7 + 3 * d] = np.asarray(inputs['b_lam'][0, d]).reshape(512)
    lru_w = np.zeros((2, 2, 4, 128, 128), np.float32)
    for ax, nm in enumerate(('b_wa', 'b_wx')):
        w = np.asarray(inputs[nm][0], np.float32)
        for d in range(2):
            for ct in range(4):
                lru_w[ax, d, ct, 0:64, 0:64] = w[d, 2 * ct]
                lru_w[ax, d, ct, 64:128, 64:128] = w[d, 2 * ct + 1]
    ii = np.arange(128)
    kk_, aa_ = ii[:, None], ii[None, :]
    masks = np.stack([(kk_ <= aa_), (kk_ >= aa_), (aa_ >= kk_), (aa_ <= kk_), (kk_ > aa_), (kk_ < aa_)]).astype(np.float32)
    peer_wq = np.ascontiguousarray(np.asarray(inputs['peer_wq'], np.float32).reshape(2, KC, 128, 2048).transpose(0, 2, 1, 3))
    peer_kT = np.ascontiguousarray(np.asarray(inputs['peer_keys'], np.float32).reshape(2, 16, 128, 128).transpose(0, 3, 1, 2))
    peer_u = np.asarray(inputs['peer_u'], np.float32)
    peer_v = np.asarray(inputs['peer_v'], np.float32)
    peer_u0, peer_u1, peer_v0, peer_v1 = peer_u[0], peer_u[1], peer_v[0], peer_v[1]
    pconst = np.concatenate([16.0 * (np.arange(16) + 1), np.arange(16)]).astype(np.float32).reshape(1, 32)
    od_w_in = np.ascontiguousarray(np.asarray(inputs['od_w_in'][0], np.float32).reshape(KC, 128, 2064).transpose(1, 0, 2))
    od_w_out = np.ascontiguousarray(np.asarray(inputs['od_w_out'][0], np.float32).reshape(KC, 128, D).transpose(1, 0, 2))
    c_gc = np.concatenate([np.asarray(inputs['c_ibias'][0]).reshape(8), np.asarray(inputs['c_fbias'][0]).reshape(8)]).astype(np.float32).reshape(1, 16)
    c_norm = np.asarray(inputs['c_norm'][0], np.float32).reshape(1, 512)
    d_w = np.asarray(inputs['d_w'][0], np.float32)
    d_scale = np.asarray(inputs['d_scale'][0], np.float32).reshape(512, 1)
    poolm = np.zeros((4, 128, 128), np.float32)
    seg = ROWS
    for gi, w in enumerate((2, 4, 8, 16)):
        P = np.zeros((seg, seg), np.float32)
        for t_ in range(seg):
            lo = max(t_ - w // 2, 0)
            hi = min(t_ - w // 2 + w, seg)
            P[t_, lo:hi] = 1.0 / (hi - lo)
        P = P - np.eye(seg, dtype=np.float32)
        for sgi in range(128 // seg):
            poolm[gi, sgi * seg:(sgi + 1) * seg, sgi * seg:(sgi + 1) * seg] = P.T
    shared = dict(od_w_in=od_w_in, od_w_out=od_w_out, c_gc=c_gc, c_norm=c_norm, d_w=d_w, d_scale=d_scale, poolm=poolm, peer_wq=peer_wq, peer_kT=peer_kT, peer_u0=peer_u0, peer_u1=peer_u1, peer_v0=peer_v0, peer_v1=peer_v1, pconst=pconst, ada_w=ada_w, ada_b=ada_b, norms=norms, ident=ident, ev_w_in=ev_w_in, ev_w_out=ev_w_out, a_convT=a_convT,
                  a_gc=a_gc, a_norm=a_norm, lru_pc=lru_pc, lru_w=lru_w, masks=masks)
    maps = []
    for ci in range(NCORES):
        b0 = ci * NB
        xin = np.concatenate([ctx[b0:b0 + NB], x[b0:b0 + NB]], axis=1)
        c3 = np.stack([c[b0], c[b0 + 1], c_ctx], axis=1)
        c3T = np.ascontiguousarray(c3.reshape(KC, 128, 3).transpose(1, 0, 2))
        maps.append(dict(xin=np.ascontiguousarray(xin), c3T=c3T, **shared))
    return maps


def kernel(**inputs):
    nc = build_program()
    maps = make_in_maps(inputs)
    res = run_bass_kernel_spmd(nc, maps, core_ids=list(range(NCORES)))
    outs = [r["out"] for r in res.results]
    return np.concatenate(outs, axis=0).astype(np.float32)
```

```python
import numpy as np
from contextlib import ExitStack
import concourse.bass as bass
import concourse.mybir as mybir
from concourse.bass_utils import run_bass_kernel_spmd

F32 = mybir.dt.float32
U32 = mybir.dt.uint32
I32 = mybir.dt.int32
AF = mybir.ActivationFunctionType
ALU = mybir.AluOpType
AX = mybir.AxisListType

NCORES = 8
NB = 2
D = 1024
KC = 8
LC = 256
LL = 2048
LT = LC + LL
NT = LT // 128
EPS = 1e-6
GRID_W = 64
ROWS = LL // GRID_W


class KB:
    def __init__(self, nc, es, ndma=16):
        self.nc = nc
        self.eng = {'pe': nc.tensor, 'act': nc.scalar, 'dve': nc.vector, 'pool': nc.gpsimd, 'sp': nc.sync}
        self.sems = {}
        self.val = {}
        for e in ['pe', 'act', 'dve', 'pool']:
            self.sems[e] = es.enter_context(nc.semaphore('s_' + e))
            self.val[e] = 0
        self.dq = {}
        for q in ['sp', 'pool', 'act']:
            ids = []
            for i in range(ndma):
                sid = ('d', q, i)
                self.sems[sid] = es.enter_context(nc.semaphore(f'd_{q}_{i}'))
                self.val[sid] = 0
                ids.append(sid)
            self.dq[q] = [ids, 0]
        self.seen = {e: {} for e in self.eng}
        self.lastw = {}
        self.readers = {}
        self.n_inst = 0

    def _waits(self, eng, reads, writes, extra=()):
        need = {}

        def add(ev):
            if ev is None:
                return
            sid, v = ev
            if need.get(sid, 0) < v:
                need[sid] = v
        for r in reads:
            add(self.lastw.get(r))
        for w in writes:
            add(self.lastw.get(w))
            for ev in self.readers.get(w, {}).items():
                add(ev)
        for ev in extra:
            add(ev)
        e = self.eng[eng]
        for sid, v in need.items():
            if sid == 'pe' and eng == 'pe':
                continue
            if self.seen[eng].get(sid, 0) >= v:
                continue
            self.seen[eng][sid] = v
            e.wait_ge(self.sems[sid], v)
            self.n_inst += 1

    def _record(self, ev, reads, writes):
        sid, v = ev
        for w in writes:
            self.lastw[w] = ev
            self.readers[w] = {}
        for r in reads:
            d = self.readers.setdefault(r, {})
            if d.get(sid, 0) < v:
                d[sid] = v

    def I(self, eng, reads, writes, fn):
        self._waits(eng, reads, writes)
        ins = fn(self.eng[eng])
        self.val[eng] += 1
        ins.then_inc(self.sems[eng], 1)
        self._record((eng, self.val[eng]), reads, writes)
        self.n_inst += 1
        return ins

    def DMA(self, q, reads, writes, fn):
        ids, rr = self.dq[q]
        sid = ids[rr % len(ids)]
        self.dq[q][1] = rr + 1
        self._waits(q, reads, writes, extra=[(sid, self.val[sid])] if self.val[sid] else ())
        ins = fn(self.eng[q])
        self.val[sid] += 16
        ins.then_inc(self.sems[sid], 16)
        self._record((sid, self.val[sid]), reads, writes)
        self.n_inst += 1
        return ins

    def barrier(self):
        for eng in self.eng:
            e = self.eng[eng]
            for sid, v in self.val.items():
                if v == 0 or self.seen[eng].get(sid, 0) >= v:
                    continue
                if sid == eng:
                    continue
                self.seen[eng][sid] = v
                e.wait_ge(self.sems[sid], v)
                self.n_inst += 1

    def finish(self):
        e = self.eng['sp']
        for sid, v in self.val.items():
            if v and self.seen['sp'].get(sid, 0) < v:
                e.wait_ge(self.sems[sid], v)


class Ring:
    def __init__(self, items):
        self.items = items
        self.i = 0

    def next(self):
        it = self.items[self.i % len(self.items)]
        self.i += 1
        return it


def bcast_rows(ap_1d_row, nparts):
    return ap_1d_row.to_broadcast([nparts, ap_1d_row.shape[-1]])


def build_program(stop_after=None, dbg=None):
    nc = bass.Bass("TRN2", target_bir_lowering=False)
    es = ExitStack()
    kb = KB(nc, es)
    dbg = dbg or {}

    def din(name, shape, dt=F32):
        return nc.dram_tensor(name, list(shape), dt, kind="ExternalInput").ap()

    def dscr(name, shape, dt=F32):
        return nc.dram_tensor(name, list(shape), dt, kind="Internal").ap()

    def dout(name, shape, dt=F32):
        return nc.dram_tensor(name, list(shape), dt, kind="ExternalOutput").ap()

    uid = [0]

    def sb(name, shape, dt=F32, stack=es):
        uid[0] += 1
        return stack.enter_context(nc.sbuf_tensor(f"{name}_u{uid[0]}", list(shape), dt))

    xin = din("xin", [NB, LT, D])
    c3T = din("c3T", [128, KC, 3])
    ada_w = din("ada_w", [2, KC, 128, 6 * D])
    ada_b = din("ada_b", [2, 1, 6 * D])
    norms = din("norms", [5, 1, D])
    ident_d = din("ident", [128, 128])
    modsD = dscr("modsD", [2, 3, 6 * D])
    out_d = dout("out", [NB, LL, D])
    dbg_out = {}
    for k, shp in dbg.items():
        dbg_out[k] = dout("dbg_" + k, shp)

    ident = sb("ident_sb", [128, 128])
    ps = [es.enter_context(nc.psum_tensor(f"ps{i}", [128, 512], F32)) for i in range(8)]
    psk = [f"ps{i}" for i in range(8)]
    kb.DMA('sp', [], ['ident'], lambda e: e.dma_start(out=ident[:], in_=ident_d[:, :]))
    epsc = sb("epsc", [128, 1])
    kb.I('dve', [], ['epsc'], lambda e: e.memset(epsc[:], EPS))

    with ExitStack() as ph:
        sT = sb("sT", [128, KC, 3], stack=ph)
        kb.DMA('sp', [], ['sT'], lambda e: e.dma_start(out=sT[:], in_=c3T[:, :, :]))
        kb.I('act', ['sT'], ['sT'], lambda e: e.activation(out=sT[:], in_=sT[:], func=AF.Silu))
        wbuf = [sb(f"adaw{i}", [128, 3072], stack=ph) for i in range(3)]
        wring = Ring(list(range(3)))
        bias_t = sb("adab", [3, 6 * D], stack=ph)
        mods_t = sb("mods_t", [3, 6 * D], stack=ph)
        for l in range(2):
            kb.DMA('sp', ['mods_st'], ['adab'], lambda e: e.dma_start(out=bias_t[:], in_=ada_b[l].to_broadcast([3, 6 * D])))
            for half in range(2):
                for k in range(KC):
                    wi = wring.next()
                    kb.DMA('sp', [], [f'adaw{wi}'], lambda e: e.dma_start(out=wbuf[wi][:], in_=ada_w[l, k, :, half * 3072:(half + 1) * 3072]))
                    for j in range(6):
                        kb.I('pe', ['sT', f'adaw{wi}'], [psk[j]], lambda e: e.matmul(ps[j][0:3, :], lhsT=sT[:, k, :], rhs=wbuf[wi][:, j * 512:(j + 1) * 512], start=(k == 0), stop=(k == KC - 1)))
                for j in range(6):
                    c0 = half * 3072 + j * 512
                    kb.I('dve', [psk[j], 'adab'], ['mods_t'], lambda e: e.tensor_tensor(out=mods_t[:, c0:c0 + 512], in0=ps[j][0:3, :], in1=bias_t[:, c0:c0 + 512], op=ALU.add))
            kb.DMA('sp', ['mods_t'], [('modsD', l), 'mods_st'], lambda e: e.dma_start(out=modsD[l], in_=mods_t[:]))
    kb.barrier()
    if 'mods' in dbg_out:
        kb.DMA('sp', [('modsD', 0), ('modsD', 1)], ['dbg_mods'], lambda e: e.dma_start(out=dbg_out['mods'], in_=modsD))
    if stop_after == 'mods':
        kb.finish(); es.close(); return nc

    ev_w_in = din("ev_w_in", [128, KC, 3088])
    ev_w_out = din("ev_w_out", [128, KC, D])
    a_convT = din("a_convT", [1536, 4])
    a_gc = din("a_gc", [1, 16])
    a_norm = din("a_norm", [1, 128])
    lru_pc = din("lru_pc", [512, 11])
    lru_w = din("lru_w", [2, 2, 4, 128, 128])
    masks_d = din("masks", [6, 128, 128])
    mixT = dscr("mixT", [NB, KC, 128, LT])
    l1only = (stop_after or '').startswith('l1only')
    xs = din("xs_in", [NB, LT, D]) if l1only else dscr("xs", [NB, LT, D])

    ones = sb("ones_sb", [128, 128])
    kb.I('dve', [], ['ones'], lambda e: e.memset(ones[:], 1.0))
    onec = sb("onec", [128, 1])
    kb.I('dve', [], ['onec'], lambda e: e.memset(onec[:], 1.0))
    msk = sb("msk", [128, 6, 128])
    for i in range(6):
        kb.DMA('sp', [], ['msk'], lambda e: e.dma_start(out=msk[:, i, :], in_=masks_d[i]))
    psr = Ring(list(range(8)))

    def mm(out, lhsT, rhs, reads, writes, start=True, stop=True):
        kb.I('pe', reads, writes, lambda e: e.matmul(out, lhsT=lhsT, rhs=rhs, start=start, stop=stop))

    def tr(out, in_, reads, writes):
        kb.I('pe', reads + ['ident'], writes, lambda e: e.transpose(out=out, in_=in_, identity=ident[:]))

    TOKBLKS = [(0, 256)] + [(256 + i * 512, 512) for i in range(4)]

    def norm_mod_T(ph, l, b, src_ap_fn, norm_idx, slot_sh, slot_sc, hT, keep_h=None, tiles=range(NT)):
        with ExitStack() as st:
            nw = sb("nm_nw", [128, D], stack=st)
            kb.DMA('sp', [], ['nm_nw'], lambda e: e.dma_start(out=nw[:], in_=norms[norm_idx].to_broadcast([128, D])))
            md = {}
            for kind, row in (('c', 2), ('l', b)):
                A = sb("nm_A" + kind, [128, D], stack=st)
                S = sb("nm_S" + kind, [128, D], stack=st)
                kb.DMA('sp', [('modsD', l)], ['nm_A' + kind], lambda e: e.dma_start(out=A[:], in_=modsD[l, row:row + 1, slot_sc * D:(slot_sc + 1) * D].to_broadcast([128, D])))
                kb.DMA('sp', [('modsD', l)], ['nm_S' + kind], lambda e: e.dma_start(out=S[:], in_=modsD[l, row:row + 1, slot_sh * D:(slot_sh + 1) * D].to_broadcast([128, D])))
                kb.I('dve', ['nm_A' + kind, 'nm_nw'], ['nm_A' + kind], lambda e: e.scalar_tensor_tensor(out=A[:], in0=A[:], scalar=1.0, in1=nw[:], op0=ALU.add, op1=ALU.mult))
                md[kind] = (A, S)
            xt = [sb(f"nm_x{i}", [128, D], stack=st) for i in range(2)]
            ht = [sb(f"nm_h{i}", [128, D], stack=st) for i in range(2)]
            sq = sb("nm_sq", [128, D], stack=st)
            ss = [sb(f"nm_ss{i}", [128, 1], stack=st) for i in range(2)]
            for t in tiles:
                i = t % 2
                kind = 'c' if t < 2 else 'l'
                A, S = md[kind]
                src, srck = src_ap_fn(b, t)
                kb.DMA('sp', [srck], [f'nm_x{i}'], lambda e: e.dma_start(out=xt[i][:], in_=src))
                kb.I('act', [f'nm_x{i}'], ['nm_sq', f'nm_ss{i}'], lambda e: e.activation(out=sq[:], in_=xt[i][:], func=AF.Square, accum_out=ss[i][:]))
                kb.I('act', [f'nm_ss{i}', 'epsc'], [f'nm_ss{i}'], lambda e: e.activation(out=ss[i][:], in_=ss[i][:], func=AF.Sqrt, scale=1.0 / D, bias=epsc[:]))
                kb.I('dve', [f'nm_ss{i}'], [f'nm_ss{i}'], lambda e: e.reciprocal(out=ss[i][:], in_=ss[i][:]))
                kb.I('dve', [f'nm_x{i}', f'nm_ss{i}', 'nm_A' + kind], [f'nm_h{i}'], lambda e: e.scalar_tensor_tensor(out=ht[i][:], in0=xt[i][:], scalar=ss[i][:], in1=A[:], op0=ALU.mult, op1=ALU.mult))
                kb.I('dve', [f'nm_h{i}', 'nm_S' + kind], [f'nm_h{i}'], lambda e: e.tensor_tensor(out=ht[i][:], in0=ht[i][:], in1=S[:], op=ALU.add))
                if keep_h is not None:
                    keep_h(t, ht[i], f'nm_h{i}')
                for half in range(2):
                    pb = psr.next()
                    for kk in range(4):
                        k = half * 4 + kk
                        tr(ps[pb][:, kk * 128:(kk + 1) * 128], ht[i][:, k * 128:(k + 1) * 128], [f'nm_h{i}'], [psk[pb]])
                    kb.I('act', [psk[pb]], [('hT', t)], lambda e: e.activation(out=hT[:, half * 4:half * 4 + 4, t * 128:(t + 1) * 128], in_=ps[pb][:, :].rearrange("p (k t) -> p k t", k=4), func=AF.Copy))
            kb.barrier()

    def proj_fm(hT, w_sb, wkey, dst_fn, evac):
        for (t0, n) in TOKBLKS:
            pb = psr.next()
            for k in range(KC):
                mm(ps[pb][:, 0:n], w_sb[:, k, :], hT[:, k, t0:t0 + n], [wkey] + [('hT', tt) for tt in range(t0 // 128, (t0 + n) // 128)], [psk[pb]], start=(k == 0), stop=(k == KC - 1))
            evac(pb, t0, n)

    def layer0_mixer(b):
        with ExitStack() as ph:
            hT = sb("hT", [128, KC, LT], stack=ph)
            norm_mod_T(ph, 0, b, lambda b_, t: (xin[b_, t * 128:(t + 1) * 128, :], 'xin'), 0, 0, 1, hT)
            if 'hT0' in dbg_out and b == 0:
                kb.DMA('sp', [('hT', t) for t in range(NT)], ['dbg_hT0'], lambda e: e.dma_start(out=dbg_out['hT0'], in_=hT[:]))
            if stop_after == 'hT0':
                return
            with ExitStack() as st:
                wx = sb("l_wx", [128, KC, 128], stack=st)
                wg = sb("l_wg", [128, KC, 128], stack=st)
                pc = sb("l_pc", [128, 11], stack=st)
                cl = sb("l_cl", [128, 4], stack=st)
                gw = sb("l_gw", [128, 4, 128], stack=st)
                raw = sb("l_raw", [128, LT + 6], stack=st)
                xb = sb("l_xb", [128, LT], stack=st)
                gg = sb("l_gg", [128, LT], stack=st)
                t1 = sb("l_t1", [128, LT], stack=st)
                t2 = sb("l_t2", [128, LT], stack=st)
                av = sb("l_a", [128, LT], stack=st)
                bv = sb("l_b", [128, LT], stack=st)
                hf = sb("l_hf", [128, LT], stack=st)
                hb = sb("l_hb", [128, LT], stack=st)
                kb.I('dve', [], ['l_raw'], lambda e: e.memset(raw[:], 0.0))
                RC, RL = 0, LC + 3

                def rawpos(t0):
                    return (RC + 1 + t0) if t0 < LC else (RL + 1 + (t0 - LC))
                for ct in range(4):
                    c0 = 2064 + ct * 128
                    kb.DMA('sp', [], ['l_wx'], lambda e: e.dma_start(out=wx[:], in_=ev_w_in[:, :, c0:c0 + 128]))
                    kb.DMA('sp', [], ['l_wg'], lambda e: e.dma_start(out=wg[:], in_=ev_w_in[:, :, c0 + 512:c0 + 640]))
                    kb.DMA('sp', [], ['l_pc'], lambda e: e.dma_start(out=pc[:], in_=lru_pc[ct * 128:(ct + 1) * 128, :]))
                    for ax in range(2):
                        for d in range(2):
                            kb.DMA('sp', [], ['l_gw'], lambda e: e.dma_start(out=gw[:, ax * 2 + d, :], in_=lru_w[ax, d, ct]))
                    for d in range(2):
                        kb.I('act', ['l_pc'], ['l_cl'], lambda e: e.activation(out=cl[:, d:d + 1], in_=pc[:, 7 + 3 * d:8 + 3 * d], func=AF.Exp, scale=-1.0))
                    kb.I('act', ['l_cl', 'onec'], ['l_cl'], lambda e: e.activation(out=cl[:, 0:2], in_=cl[:, 0:2], func=AF.Ln, bias=onec[:]))
                    kb.I('dve', ['l_cl'], ['l_cl'], lambda e: e.tensor_scalar(out=cl[:, 2:4], in0=cl[:, 0:2], scalar1=-16.0, scalar2=None, op0=ALU.mult))
                    kb.I('dve', ['l_cl'], ['l_cl'], lambda e: e.tensor_scalar(out=cl[:, 0:2], in0=cl[:, 0:2], scalar1=-8.0, scalar2=None, op0=ALU.mult))
                    proj_fm(hT, wx, 'l_wx', None, lambda pb, t0, n: kb.I('act', [psk[pb]], ['l_raw'], lambda e: e.activation(out=raw[:, rawpos(t0):rawpos(t0) + n], in_=ps[pb][:, 0:n], func=AF.Copy)))
                    for (o0, r0, L) in ((0, RC, LC), (LC, RL, LL)):
                        kb.I('dve', ['l_raw', 'l_pc'], ['l_xb'], lambda e: e.tensor_scalar(out=xb[:, o0:o0 + L], in0=raw[:, r0:r0 + L], scalar1=pc[:, 0:1], scalar2=pc[:, 4:5], op0=ALU.mult, op1=ALU.add))
                        for j in range(1, 4):
                            kb.I('dve', ['l_raw', 'l_pc', 'l_xb'], ['l_xb'], lambda e: e.scalar_tensor_tensor(out=xb[:, o0:o0 + L], in0=raw[:, r0 + j:r0 + j + L], scalar=pc[:, j:j + 1], in1=xb[:, o0:o0 + L], op0=ALU.mult, op1=ALU.add))
                    proj_fm(hT, wg, 'l_wg', None, lambda pb, t0, n: kb.I('act', [psk[pb]], ['l_t1'], lambda e: e.activation(out=t1[:, t0:t0 + n], in_=ps[pb][:, 0:n], func=AF.Copy)))
                    kb.I('dve', ['l_t1'], ['l_t2'], lambda e: e.tensor_tensor(out=t2[:], in0=t1[:], in1=t1[:], op=ALU.mult))
                    kb.I('dve', ['l_t2'], ['l_t2'], lambda e: e.tensor_scalar(out=t2[:], in0=t2[:], scalar1=0.044715, scalar2=1.0, op0=ALU.mult, op1=ALU.add))
                    kb.I('dve', ['l_t2', 'l_t1'], ['l_t2'], lambda e: e.tensor_tensor(out=t2[:], in0=t2[:], in1=t1[:], op=ALU.mult))
                    kb.I('act', ['l_t2'], ['l_t2'], lambda e: e.activation(out=t2[:], in_=t2[:], func=AF.Sigmoid, scale=1.5957691216057308))
                    kb.I('dve', ['l_t2', 'l_t1'], ['l_gg'], lambda e: e.tensor_tensor(out=gg[:], in0=t2[:], in1=t1[:], op=ALU.mult))
                    for d in range(2):
                        for (t0, n) in TOKBLKS:
                            for ax, dst, dk_ in ((0, t1, 'l_t1'), (1, t2, 'l_t2')):
                                pb = psr.next()
                                mm(ps[pb][:, 0:n], gw[:, ax * 2 + d, :], xb[:, t0:t0 + n], ['l_gw', 'l_xb'], [psk[pb]])
                                bcol = 5 + 3 * d + ax
                                kb.I('act', [psk[pb], 'l_pc'], [dk_], lambda e: e.activation(out=dst[:, t0:t0 + n], in_=ps[pb][:, 0:n], func=AF.Sigmoid, bias=pc[:, bcol:bcol + 1]))
                        kb.I('act', ['l_t1', 'l_cl'], ['l_a'], lambda e: e.activation(out=av[:], in_=t1[:], func=AF.Exp, scale=cl[:, d:d + 1]))
                        kb.I('act', ['l_t1', 'l_cl'], ['l_b'], lambda e: e.activation(out=bv[:], in_=t1[:], func=AF.Exp, scale=cl[:, 2 + d:3 + d]))
                        kb.I('dve', ['l_b'], ['l_b'], lambda e: e.tensor_scalar(out=bv[:], in0=bv[:], scalar1=-1.0, scalar2=1.0, op0=ALU.mult, op1=ALU.add))
                        kb.I('dve', ['l_b'], ['l_b'], lambda e: e.tensor_scalar(out=bv[:], in0=bv[:], scalar1=0.0, scalar2=None, op0=ALU.max))
                        kb.I('act', ['l_b'], ['l_b'], lambda e: e.activation(out=bv[:], in_=bv[:], func=AF.Sqrt))
                        kb.I('dve', ['l_b', 'l_t2'], ['l_b'], lambda e: e.tensor_tensor(out=bv[:], in0=bv[:], in1=t2[:], op=ALU.mult))
                        kb.I('dve', ['l_b', 'l_xb'], ['l_b'], lambda e: e.tensor_tensor(out=bv[:], in0=bv[:], in1=xb[:], op=ALU.mult))
                        if d == 0:
                            kb.I('dve', ['l_a', 'l_b'], ['l_hf'], lambda e: e.tensor_tensor_scan(out=hf[:, :], data0=av[:, :], data1=bv[:, :], initial=0.0, op0=ALU.mult, op1=ALU.add))
                        else:
                            kb.I('dve', ['l_a', 'l_b'], ['l_hb'], lambda e: e.tensor_tensor_scan(out=hb[:, LC - 1::-1], data0=av[:, LC - 1::-1], data1=bv[:, LC - 1::-1], initial=0.0, op0=ALU.mult, op1=ALU.add))
                            kb.I('dve', ['l_a', 'l_b', 'l_hb'], ['l_hb'], lambda e: e.tensor_tensor_scan(out=hb[:, LT - 1:LC - 1:-1], data0=av[:, LT - 1:LC - 1:-1], data1=bv[:, LT - 1:LC - 1:-1], initial=hb[:, 0:1], op0=ALU.mult, op1=ALU.add))
                    kb.I('dve', ['l_hf', 'l_hb'], ['l_hf'], lambda e: e.tensor_tensor(out=hf[:], in0=hf[:], in1=hb[:], op=ALU.add))
                    kb.I('dve', ['l_hf', 'l_gg'], ['l_hf'], lambda e: e.tensor_tensor(out=hf[:], in0=hf[:], in1=gg[:], op=ALU.mult))
                    kb.DMA('sp', ['l_hf'], [('mixT', b)], lambda e: e.dma_start(out=mixT[b, 4 + ct], in_=hf[:]))
                kb.barrier()
            if stop_after == 'lru':
                return
            gdn(ph, b, hT)
            kb.barrier()

    def gdn(ph, b, hT):
        with ExitStack() as st:
            wh = sb("g_wh", [128, 4, KC, 128], stack=st)
            wg16 = sb("g_wg16", [128, KC, 16], stack=st)
            cw = sb("g_cw", [128, 3, 4], stack=st)
            gcst = sb("g_gcst", [128, 16], stack=st)
            anw = sb("g_anw", [128, 128], stack=st)
            raw = sb("g_raw", [128, LT + 6], stack=st)
            T3 = [sb(f"g_T{i}", [128, LT], stack=st) for i in range(3)]
            tmp = sb("g_tmp", [128, LT], stack=st)
            Ktm = sb("g_Ktm", [128, NT, 128], stack=st)
            Vtm = sb("g_Vtm", [128, NT, 128], stack=st)
            zs = sb("g_zs", [128, NT, 128], stack=st)
            O = sb("g_O", [128, NT, 128], stack=st)
            graw = sb("g_graw", [128, NT, 16], stack=st)
            gG = sb("g_g", [128, NT, 8], stack=st)
            gB = sb("g_bt", [128, NT, 8], stack=st)
            RC, RL = 0, LC + 3

            def rawpos(t0):
                return (RC + 1 + t0) if t0 < LC else (RL + 1 + (t0 - LC))
            kb.I('dve', [], ['g_raw'], lambda e: e.memset(raw[:], 0.0))
            kb.DMA('sp', [], ['g_gcst'], lambda e: e.dma_start(out=gcst[:], in_=a_gc[0:1, :].to_broadcast([128, 16])))
            kb.DMA('sp', [], ['g_anw'], lambda e: e.dma_start(out=anw[:], in_=a_norm[0:1, :].to_broadcast([128, 128])))
            kb.I('act', ['g_gcst'], ['g_gcst'], lambda e: e.activation(out=gcst[:, 0:8], in_=gcst[:, 0:8], func=AF.Exp))
            kb.I('dve', ['g_gcst'], ['g_gcst'], lambda e: e.tensor_scalar(out=gcst[:, 0:8], in0=gcst[:, 0:8], scalar1=-1.0, scalar2=None, op0=ALU.mult))
            kb.DMA('sp', [], ['g_wg16'], lambda e: e.dma_start(out=wg16[:], in_=ev_w_in[:, :, 2048:2064]))
            pb = psr.next()
            for n in range(NT):
                for k in range(KC):
                    mm(ps[pb][:, n * 16:(n + 1) * 16], hT[:, k, n * 128:(n + 1) * 128], wg16[:, k, :], ['g_wg16', ('hT', n)], [psk[pb]], start=(k == 0), stop=(k == KC - 1))
            kb.I('act', [psk[pb]], ['g_graw'], lambda e: e.activation(out=graw[:], in_=ps[pb][:, 0:NT * 16].rearrange("p (n c) -> p n c", c=16), func=AF.Copy))
            kb.I('dve', ['g_graw', 'g_gcst'], ['g_g'], lambda e: e.tensor_tensor(out=gG[:], in0=graw[:, :, 0:8], in1=gcst[:, 8:16].unsqueeze(1).to_broadcast([128, NT, 8]), op=ALU.add))
            kb.I('act', ['g_g'], ['g_g'], lambda e: e.activation(out=gG[:], in_=gG[:], func=AF.Exp))
            kb.I('act', ['g_g', 'onec'], ['g_g'], lambda e: e.activation(out=gG[:], in_=gG[:], func=AF.Ln, bias=onec[:]))
            kb.I('dve', ['g_g', 'g_gcst'], ['g_g'], lambda e: e.tensor_tensor(out=gG[:], in0=gG[:], in1=gcst[:, 0:8].unsqueeze(1).to_broadcast([128, NT, 8]), op=ALU.mult))
            kb.I('act', ['g_graw'], ['g_bt'], lambda e: e.activation(out=gB[:], in_=graw[:, :, 8:16], func=AF.Sigmoid))
            if 'gates' in dbg_out and b == 0:
                kb.DMA('sp', ['g_g'], ['dbg_gates'], lambda e: e.dma_start(out=dbg_out['gates'][:, :, 0:8], in_=gG[:]))
                kb.DMA('sp', ['g_bt'], ['dbg_gates'], lambda e: e.dma_start(out=dbg_out['gates'][:, :, 8:16], in_=gB[:]))

            for hd in range(4):
                for part in range(4):
                    c0 = part * 512 + hd * 128
                    kb.DMA('sp', [], ['g_wh'], lambda e: e.dma_start(out=wh[:, part], in_=ev_w_in[:, :, c0:c0 + 128]))
                for part in range(3):
                    c0 = part * 512 + hd * 128
                    kb.DMA('sp', [], ['g_cw'], lambda e: e.dma_start(out=cw[:, part, :], in_=a_convT[c0:c0 + 128, :]))
                for part in range(3):
                    Tp, tk = T3[part], f'g_T{part}'
                    proj_fm(hT, wh[:, part], 'g_wh', None, lambda pb, t0, n: kb.I('act', [psk[pb]], ['g_raw'], lambda e: e.activation(out=raw[:, rawpos(t0):rawpos(t0) + n], in_=ps[pb][:, 0:n], func=AF.Copy)))
                    for (o0, r0, L) in ((0, RC, LC), (LC, RL, LL)):
                        kb.I('dve', ['g_raw', 'g_cw'], ['g_tmp'], lambda e: e.tensor_scalar(out=tmp[:, o0:o0 + L], in0=raw[:, r0:r0 + L], scalar1=cw[:, part, 0:1], scalar2=None, op0=ALU.mult))
                        for j in range(1, 4):
                            kb.I('dve', ['g_raw', 'g_cw', 'g_tmp'], ['g_tmp'], lambda e: e.scalar_tensor_tensor(out=tmp[:, o0:o0 + L], in0=raw[:, r0 + j:r0 + j + L], scalar=cw[:, part, j:j + 1], in1=tmp[:, o0:o0 + L], op0=ALU.mult, op1=ALU.add))
                    kb.I('act', ['g_tmp'], [tk], lambda e: e.activation(out=Tp[:], in_=tmp[:], func=AF.Silu))
                    if part < 2:
                        kb.I('act', [tk], ['g_tmp'], lambda e: e.activation(out=tmp[:], in_=Tp[:], func=AF.Square))
                        for (t0, n) in TOKBLKS:
                            pb = psr.next()
                            mm(ps[pb][:, 0:n], ones[:], tmp[:, t0:t0 + n], ['ones', 'g_tmp'], [psk[pb]])
                            kb.I('act', [psk[pb], 'epsc', 'g_tmp'], ['g_tmp'], lambda e: e.activation(out=tmp[:, t0:t0 + n], in_=ps[pb][:, 0:n], func=AF.Sqrt, bias=epsc[:]))
                        kb.I('dve', ['g_tmp'], ['g_tmp'], lambda e: e.reciprocal(out=tmp[:], in_=tmp[:]))
                        sc_ = (128 ** -0.5) if part == 0 else 1.0
                        kb.I('dve', [tk, 'g_tmp'], [tk], lambda e: e.scalar_tensor_tensor(out=Tp[:], in0=Tp[:], scalar=sc_, in1=tmp[:], op0=ALU.mult, op1=ALU.mult))
                for src, dst, dk_ in ((T3[1], Ktm, 'g_Ktm'), (T3[2], Vtm, 'g_Vtm')):
                    sk = 'g_T1' if dst is Ktm else 'g_T2'
                    for n0 in range(0, NT, 4):
                        pb = psr.next()
                        nn = min(4, NT - n0)
                        for i in range(nn):
                            tr(ps[pb][:, i * 128:(i + 1) * 128], src[:, (n0 + i) * 128:(n0 + i + 1) * 128], [sk], [psk[pb]])
                        kb.I('act', [psk[pb]], [dk_], lambda e: e.activation(out=dst[:, n0:n0 + nn, :], in_=ps[pb][:, 0:nn * 128].rearrange("p (n c) -> p n c", c=128), func=AF.Copy))
                for n0 in range(0, NT, 4):
                    pb = psr.next()
                    nn = min(4, NT - n0)
                    for i in range(nn):
                        n = n0 + i
                        for k in range(KC):
                            mm(ps[pb][:, i * 128:(i + 1) * 128], hT[:, k, n * 128:(n + 1) * 128], wh[:, 3, k, :], ['g_wh', ('hT', n)], [psk[pb]], start=(k == 0), stop=(k == KC - 1))
                    kb.I('act', [psk[pb]], ['g_zs'], lambda e: e.activation(out=zs[:, n0:n0 + nn, :], in_=ps[pb][:, 0:nn * 128].rearrange("p (n c) -> p n c", c=128), func=AF.Silu))
                gdn_chains(st, b, hd, T3, Ktm, Vtm, gG, gB, O)
                ssq = sb(f"g_ssq{hd}", [128, NT], stack=st)
                O2 = tmp[:, :].rearrange("p (n c) -> p n c", c=128)
                kb.I('dve', ['g_O'], ['g_tmp'], lambda e: e.tensor_tensor(out=O2, in0=O[:], in1=O[:], op=ALU.mult))
                kb.I('dve', ['g_tmp'], ['g_ssq'], lambda e: e.tensor_reduce(out=ssq[:], in_=O2, axis=AX.X, op=ALU.add))
                kb.I('act', ['g_ssq', 'epsc'], ['g_ssq'], lambda e: e.activation(out=ssq[:], in_=ssq[:], func=AF.Sqrt, scale=1.0 / 128, bias=epsc[:]))
                kb.I('dve', ['g_ssq'], ['g_ssq'], lambda e: e.reciprocal(out=ssq[:], in_=ssq[:]))
                kb.I('dve', ['g_O', 'g_ssq'], ['g_O'], lambda e: e.tensor_tensor(out=O[:], in0=O[:], in1=ssq[:].unsqueeze(2).to_broadcast([128, NT, 128]), op=ALU.mult))
                kb.I('dve', ['g_O', 'g_anw'], ['g_O'], lambda e: e.tensor_tensor(out=O[:], in0=O[:], in1=anw[:].unsqueeze(1).to_broadcast([128, NT, 128]), op=ALU.mult))
                kb.I('dve', ['g_O', 'g_zs'], ['g_O'], lambda e: e.tensor_tensor(out=O[:], in0=O[:], in1=zs[:], op=ALU.mult))
                for n0 in range(0, NT, 4):
                    pb = psr.next()
                    nn = min(4, NT - n0)
                    for i in range(nn):
                        tr(ps[pb][:, i * 128:(i + 1) * 128], O[:, n0 + i, :], ['g_O'], [psk[pb]])
                    kb.I('act', [psk[pb]], ['g_tmp'], lambda e: e.activation(out=tmp[:, n0 * 128:(n0 + nn) * 128], in_=ps[pb][:, 0:nn * 128], func=AF.Copy))
                kb.DMA('sp', ['g_tmp'], [('mixT', b)], lambda e: e.dma_start(out=mixT[b, hd], in_=tmp[:]))
            kb.barrier()

    def gdn_chains(st0, b, hd, T3, Ktm, Vtm, gG, gB, O):
        QT, KT = T3[0], T3[1]
        with ExitStack() as st:
            NBUF = 2
            def mk(name, shape):
                return [sb(f"c_{name}{i}", shape, stack=st) for i in range(NBUF)]
            cs = mk("cs", [128, 8])
            Tg = mk("Tg", [128, 128]); dB = mk("dB", [128, 128])
            Ei = mk("Ei", [128, 128]); EmI = mk("EmI", [128, 128]); tt = mk("tt", [128, 128])
            Y = [mk("Ya", [128, 128]), mk("Yb", [128, 128])]
            YT = [mk("YTa", [128, 128]), mk("YTb", [128, 128])]
            R = [mk("Ra", [128, 128]), mk("Rb", [128, 128])]
            qkm = mk("qkm", [128, 128]); eg = mk("eg", [128, 128]); qd = mk("qd", [128, 128])
            rv = mk("rv", [128, 128]); rk = mk("rk", [128, 128]); U = mk("U", [128, 128]); WT = mk("WT", [128, 128])
            Kd = mk("Kd", [128, 128]); vn = mk("vn", [128, 128])
            S = [sb(f"c_S{i}", [128, 128], stack=st) for i in range(2)]
            it = 0
            for d in range(2):
                col = d * 4 + hd
                order = ([0, 1] + list(range(2, NT))) if d == 0 else ([1, 0] + list(range(NT - 1, 1, -1)))
                si = 0
                kb.I('dve', [], ['c_S0'], lambda e: e.memset(S[0][:], 0.0))
                for n in order:
                    i = it % NBUF
                    it += 1
                    K_ = lambda nm: f"c_{nm}{i}"
                    gcol = gG[:, n, col:col + 1]
                    bcol = gB[:, n, col:col + 1]
                    tok = slice(n * 128, (n + 1) * 128)
                    kb.I('dve', ['g_g', 'msk'], [K_('Tg')], lambda e: e.tensor_scalar(out=Tg[i][:], in0=msk[:, d, :], scalar1=gcol, scalar2=None, op0=ALU.mult))
                    kb.I('dve', ['g_bt', 'ident'], [K_('dB')], lambda e: e.tensor_scalar(out=dB[i][:], in0=ident[:], scalar1=bcol, scalar2=None, op0=ALU.mult))
                    pa = psr.next()
                    mm(ps[pa][:, 0:128], ones[:], Tg[i][:], ['ones', K_('Tg')], [psk[pa]])
                    mm(ps[pa][:, 128:256], msk[:, 4 + d, :], dB[i][:], ['msk', K_('dB')], [psk[pa]])
                    mm(ps[pa][:, 256:257], msk[:, d, :], gcol, ['msk', 'g_g'], [psk[pa]])
                    mm(ps[pa][:, 257:258], ones[:], gcol, ['ones', 'g_g'], [psk[pa]])
                    kb.I('act', [psk[pa]], [K_('cs')], lambda e: e.activation(out=cs[i][:, 0:2], in_=ps[pa][:, 256:258], func=AF.Copy))
                    kb.I('act', [K_('cs')], [K_('cs')], lambda e: e.activation(out=cs[i][:, 2:3], in_=cs[i][:, 0:1], func=AF.Exp))
                    kb.I('dve', [K_('cs'), 'g_bt'], [K_('cs')], lambda e: e.tensor_tensor(out=cs[i][:, 3:4], in0=cs[i][:, 2:3], in1=bcol, op=ALU.mult))
                    kb.I('act', [K_('cs')], [K_('cs')], lambda e: e.activation(out=cs[i][:, 4:5], in_=cs[i][:, 0:1], func=AF.Exp, scale=-1.0, bias=cs[i][:, 1:2]))
                    kb.I('act', [K_('cs')], [K_('cs')], lambda e: e.activation(out=cs[i][:, 5:6], in_=cs[i][:, 1:2], func=AF.Exp))
                    kb.I('dve', [psk[pa], K_('cs')], [K_('Ei')], lambda e: e.tensor_scalar(out=Ei[i][:], in0=ps[pa][:, 0:128], scalar1=cs[i][:, 0:1], scalar2=0.0, op0=ALU.subtract, op1=ALU.min))
                    kb.I('act', [K_('Ei')], [K_('Ei')], lambda e: e.activation(out=Ei[i][:], in_=Ei[i][:], func=AF.Exp))
                    kb.I('act', [psk[pa]], [K_('eg')], lambda e: e.activation(out=eg[i][:], in_=ps[pa][:, 0:128], func=AF.Exp))
                    kb.I('dve', [K_('Ei'), 'msk'], [K_('EmI')], lambda e: e.tensor_tensor(out=EmI[i][:], in0=Ei[i][:], in1=msk[:, 2 + d, :], op=ALU.mult))
                    kb.I('dve', [K_('Ei'), psk[pa]], [K_('tt')], lambda e: e.tensor_tensor(out=tt[i][:], in0=Ei[i][:], in1=ps[pa][:, 128:256], op=ALU.mult))
                    pq = psr.next()
                    mm(ps[pq][:, 0:128], KT[:, tok], KT[:, tok], ['g_T1'], [psk[pq]])
                    mm(ps[pq][:, 128:256], KT[:, tok], QT[:, tok], ['g_T1', 'g_T0'], [psk[pq]])
                    y0, yt0, r0 = Y[0][i], YT[0][i], R[0][i]
                    kb.I('dve', [psk[pq], K_('tt')], [K_('Ya')], lambda e: e.scalar_tensor_tensor(out=y0[:], in0=ps[pq][:, 0:128], scalar=-1.0, in1=tt[i][:], op0=ALU.mult, op1=ALU.mult))
                    kb.I('dve', [psk[pq], K_('EmI')], [K_('qkm')], lambda e: e.tensor_tensor(out=qkm[i][:], in0=ps[pq][:, 128:256], in1=EmI[i][:], op=ALU.mult))
                    kb.I('dve', [K_('eg'), 'g_T0'], [K_('qd')], lambda e: e.tensor_tensor(out=qd[i][:], in0=QT[:, tok], in1=eg[i][:], op=ALU.mult))
                    pz = psr.next()
                    tr(ps[pz][:, 0:128], y0[:], [K_('Ya')], [psk[pz]])
                    kb.I('act', [psk[pz]], [K_('YTa')], lambda e: e.activation(out=yt0[:], in_=ps[pz][:, 0:128], func=AF.Copy))
                    kb.I('dve', [K_('Ya'), 'ident'], [K_('Ra')], lambda e: e.tensor_tensor(out=r0[:], in0=y0[:], in1=ident[:], op=ALU.add))
                    cur = 0
                    names = ['a', 'b']
                    for lev in range(6):
                        nx = 1 - cur
                        yc, ytc, rc = Y[cur][i], YT[cur][i], R[cur][i]
                        yn, ytn, rn = Y[nx][i], YT[nx][i], R[nx][i]
                        kc_, kn_ = names[cur], names[nx]
                        pl = psr.next()
                        mm(ps[pl][:, 128:256], yc[:], ytc[:], [K_('Y' + kc_), K_('YT' + kc_)], [psk[pl]])
                        kb.I('act', [psk[pl]], [K_('YT' + kn_)], lambda e: e.activation(out=ytn[:], in_=ps[pl][:, 128:256], func=AF.Copy))
                        if lev < 5:
                            mm(ps[pl][:, 0:128], ytc[:], yc[:], [K_('Y' + kc_), K_('YT' + kc_)], [psk[pl]])
                            kb.I('act', [psk[pl]], [K_('Y' + kn_)], lambda e: e.activation(out=yn[:], in_=ps[pl][:, 0:128], func=AF.Copy))
                        mm(ps[pl][:, 256:384], ytn[:], rc[:], [K_('YT' + kn_), K_('R' + kc_)], [psk[pl]])
                        kb.I('dve', [psk[pl], K_('R' + kc_)], [K_('R' + kn_)], lambda e: e.tensor_tensor(out=rn[:], in0=rc[:], in1=ps[pl][:, 256:384], op=ALU.add))
                        cur = nx
                    Rf, Rk_ = R[cur][i], K_('R' + names[cur])
                    kb.I('dve', ['g_Vtm', 'g_bt'], [K_('rv')], lambda e: e.tensor_scalar(out=rv[i][:], in0=Vtm[:, n, :], scalar1=bcol, scalar2=None, op0=ALU.mult))
                    kb.I('dve', ['g_Ktm', K_('cs')], [K_('rk')], lambda e: e.tensor_scalar(out=rk[i][:], in0=Ktm[:, n, :], scalar1=cs[i][:, 3:4], scalar2=None, op0=ALU.mult))
                    kb.I('dve', ['g_Ktm', K_('cs')], [K_('Kd')], lambda e: e.tensor_scalar(out=Kd[i][:], in0=Ktm[:, n, :], scalar1=cs[i][:, 4:5], scalar2=None, op0=ALU.mult))
                    pu = psr.next()
                    mm(ps[pu][:, 0:128], Rf[:], rv[i][:], [Rk_, K_('rv')], [psk[pu]])
                    mm(ps[pu][:, 128:256], rk[i][:], Rf[:], [Rk_, K_('rk')], [psk[pu]])
                    kb.I('act', [psk[pu]], [K_('U')], lambda e: e.activation(out=U[i][:], in_=ps[pu][:, 0:128], func=AF.Copy))
                    kb.I('act', [psk[pu]], [K_('WT')], lambda e: e.activation(out=WT[i][:], in_=ps[pu][:, 128:256], func=AF.Copy))
                    Sc, Sn = S[si], S[1 - si]
                    skc, skn = f'c_S{si}', f'c_S{1 - si}'
                    p1 = psr.next()
                    mm(ps[p1][:, 0:128], WT[i][:], Sc[:], [K_('WT'), skc], [psk[p1]])
                    kb.I('dve', [psk[p1], K_('U')], [K_('vn')], lambda e: e.tensor_tensor(out=vn[i][:], in0=U[i][:], in1=ps[p1][:, 0:128], op=ALU.subtract))
                    mm(ps[p1][:, 128:256], qd[i][:], Sc[:], [K_('qd'), skc], [psk[p1]], start=True, stop=False)
                    mm(ps[p1][:, 128:256], qkm[i][:], vn[i][:], [K_('qkm'), K_('vn')], [psk[p1]], start=False, stop=True)
                    if d == 0:
                        kb.I('act', [psk[p1]], ['g_O'], lambda e: e.activation(out=O[:, n, :], in_=ps[p1][:, 128:256], func=AF.Copy))
                    else:
                        kb.I('dve', [psk[p1], 'g_O'], ['g_O'], lambda e: e.tensor_tensor(out=O[:, n, :], in0=O[:, n, :], in1=ps[p1][:, 128:256], op=ALU.add))
                    mm(ps[p1][:, 256:384], Kd[i][:], vn[i][:], [K_('Kd'), K_('vn')], [psk[p1]])
                    kb.I('dve', [psk[p1], skc, K_('cs')], [skn], lambda e: e.scalar_tensor_tensor(out=Sn[:], in0=Sc[:], scalar=cs[i][:, 5:6], in1=ps[p1][:, 256:384], op0=ALU.mult, op1=ALU.add))
                    si = 1 - si
            kb.barrier()

    def w_out_phase(b):
        with ExitStack() as st:
            wo = sb("o_w", [128, KC, D], stack=st)
            kb.DMA('sp', [], ['o_w'], lambda e: e.dma_start(out=wo[:], in_=ev_w_out[:, :, :]))
            gt = {}
            for kind, row in (('c', 2), ('l', b)):
                g = sb("o_g" + kind, [128, D], stack=st)
                kb.DMA('sp', [('modsD', 0)], ['o_g' + kind], lambda e: e.dma_start(out=g[:], in_=modsD[0, row:row + 1, 2 * D:3 * D].to_broadcast([128, D])))
                gt[kind] = g
            mt = [sb(f"o_m{i}", [128, KC, 128], stack=st) for i in range(2)]
            xt = [sb(f"o_x{i}", [128, D], stack=st) for i in range(2)]
            for t in range(NT):
                i = t % 2
                kind = 'c' if t < 2 else 'l'
                kb.DMA('sp', [('mixT', b)], [f'o_m{i}'], lambda e: e.dma_start(out=mt[i][:], in_=mixT[b, :, :, t * 128:(t + 1) * 128].rearrange("k p t -> p k t")))
                kb.DMA('sp', ['xin'], [f'o_x{i}'], lambda e: e.dma_start(out=xt[i][:], in_=xin[b, t * 128:(t + 1) * 128, :]))
                for hlf in range(2):
                    pb = psr.next()
                    for k in range(KC):
                        mm(ps[pb][:, :], mt[i][:, k, :], wo[:, k, hlf * 512:(hlf + 1) * 512], [f'o_m{i}', 'o_w'], [psk[pb]], start=(k == 0), stop=(k == KC - 1))
                    if 'yl0' in dbg_out and b == 0:
                        yb = sb(f"o_y{t}_{hlf}", [128, 512], stack=st)
                        kb.I('act', [psk[pb]], [f'o_y{t}_{hlf}'], lambda e: e.activation(out=yb[:], in_=ps[pb][:, :], func=AF.Copy))
                        kb.DMA('sp', [f'o_y{t}_{hlf}'], ['dbg_yl0'], lambda e: e.dma_start(out=dbg_out['yl0'][t * 128:(t + 1) * 128, hlf * 512:(hlf + 1) * 512], in_=yb[:]))
                    kb.I('dve', [psk[pb], 'o_g' + kind], [psk[pb]], lambda e: e.tensor_tensor(out=ps[pb][:, :], in0=ps[pb][:, :], in1=gt[kind][:, hlf * 512:(hlf + 1) * 512], op=ALU.mult))
                    kb.I('dve', [psk[pb], f'o_x{i}'], [f'o_x{i}'], lambda e: e.tensor_tensor(out=xt[i][:, hlf * 512:(hlf + 1) * 512], in0=xt[i][:, hlf * 512:(hlf + 1) * 512], in1=ps[pb][:, :], op=ALU.add))
                kb.DMA('sp', [f'o_x{i}'], [('xs', b)], lambda e: e.dma_start(out=xs[b, t * 128:(t + 1) * 128, :], in_=xt[i][:]))
            kb.barrier()

    peer_wq = din("peer_wq", [2, 128, KC, 2048])
    peer_kT = din("peer_kT", [2, 128, 16, 128])
    peer_u = [din(f"peer_u{i}", [16384, D]) for i in range(2)]
    peer_v = [din(f"peer_v{i}", [16384, D]) for i in range(2)]
    pconst = din("pconst", [1, 32])
    xs1 = dscr("xs1", [NB, LL, D])
    NEG = -1.0e30

    def peer_phase(l, b, tiles, src_fn, dst_fn, final=False):
        with ExitStack() as st:
            wqb = [sb(f"p_wq{i}", [128, KC, 512], stack=st) for i in range(2)]
            kT = sb("p_kT", [128, 16, 128], stack=st)
            kb.DMA('sp', [], ['p_kT'], lambda e: e.dma_start(out=kT[:], in_=peer_kT[l]))
            pc = sb("p_pc", [128, 32], stack=st)
            kb.DMA('sp', [], ['p_pc'], lambda e: e.dma_start(out=pc[:], in_=pconst[0:1, :].to_broadcast([128, 32])))
            nw = sb("p_nw", [128, D], stack=st)
            kb.DMA('sp', [], ['p_nw'], lambda e: e.dma_start(out=nw[:], in_=norms[2 + l].to_broadcast([128, D])))
            A = sb("p_A", [128, D], stack=st); S = sb("p_S", [128, D], stack=st)
            Gk = {}
            for kind_ in sorted(set('c' if t_ < 2 else 'l' for t_ in tiles)):
                Gk[kind_] = sb("p_G" + kind_, [128, D], stack=st)
                row_ = 2 if kind_ == 'c' else b
                kb.DMA('sp', [('modsD', l)], ['p_G' + kind_], lambda e: e.dma_start(out=Gk[kind_][:], in_=modsD[l, row_:row_ + 1, 5 * D:6 * D].to_broadcast([128, D])))
            fnw = None
            if final:
                fnw = sb("p_fnw", [128, D], stack=st)
                kb.DMA('sp', [], ['p_fnw'], lambda e: e.dma_start(out=fnw[:], in_=norms[4].to_broadcast([128, D])))
            xt = [sb(f"p_x{i}", [128, D], stack=st) for i in range(2)]
            h2b = [sb(f"p_h2{i}", [128, D], stack=st) for i in range(2)]
            ssf = sb("p_ssf", [128, 1], stack=st)
            sq = sb("p_sq", [128, D], stack=st)
            ss = sb("p_ss", [128, 1], stack=st)
            h2T = sb("p_h2T", [128, KC, 128], stack=st)
            qT = sb("p_qT", [128, 16, 128], stack=st)
            s1 = sb("p_s1", [128, 16, 128], stack=st)
            s2 = sb("p_s2", [128, 16, 128], stack=st)
            m8 = sb("p_m8", [128, 16, 16], stack=st)
            i8 = sb("p_i8", [128, 16, 16], U32, stack=st)
            i8f = sb("p_i8f", [128, 16, 16], stack=st)
            cand = sb("p_cand", [128, 8, 256], stack=st)
            cand2 = sb("p_cand2", [128, 8, 256], stack=st)
            sc = sb("p_sc", [128, 8, 16], stack=st)
            ci = sb("p_ci", [128, 8, 16], U32, stack=st)
            cf = sb("p_cf", [128, 8, 16], stack=st)
            fi = sb("p_fi", [128, 8, 16], stack=st)
            fj = sb("p_fj", [128, 8, 16], stack=st)
            oh = sb("p_oh", [128, 8, 16, 16], stack=st)
            g1 = sb("p_g1", [128, 8, 16], stack=st)
            g2_ = sb("p_g2", [128, 8, 16], stack=st)
            eidb = [sb(f"p_eid{i}", [128, 128], I32, stack=st) for i in range(2)]
            gateb = [sb(f"p_gate{i}", [128, 8, 16], stack=st) for i in range(2)]
            gsum = sb("p_gsum", [128, 8], stack=st)
            act = sb("p_act", [128, 128], stack=st)
            t1 = sb("p_t1", [128, 128], stack=st)
            wgt = sb("p_wgt", [128, 128], stack=st)
            NG = 10
            gb = [sb(f"p_gb{i}", [128, D], stack=st) for i in range(NG)]
            gring = Ring(list(range(NG)))
            acc = sb("p_acc", [128, D], stack=st)
            junk = sb("p_junk", [128, D], stack=st)
            NDG = 4
            dg = [sb(f"p_dg{i}", [128, 128], stack=st) for i in range(NDG)]
            dring = Ring(list(range(NDG)))
            psr = Ring(list(range(6)))
            cur_kind = [None]

            def front(ti, t):
                i = ti % 2
                kind = 'c' if t < 2 else 'l'
                row = 2 if kind == 'c' else b
                if kind != cur_kind[0]:
                    cur_kind[0] = kind
                    kb.DMA('sp', [('modsD', l)], ['p_A'], lambda e: e.dma_start(out=A[:], in_=modsD[l, row:row + 1, 4 * D:5 * D].to_broadcast([128, D])))
                    yield
                    kb.DMA('sp', [('modsD', l)], ['p_S'], lambda e: e.dma_start(out=S[:], in_=modsD[l, row:row + 1, 3 * D:4 * D].to_broadcast([128, D])))
                    yield
                    kb.I('dve', ['p_A', 'p_nw'], ['p_A'], lambda e: e.scalar_tensor_tensor(out=A[:], in0=A[:], scalar=1.0, in1=nw[:], op0=ALU.add, op1=ALU.mult))
                    yield
                sap, skey = src_fn(t)
                kb.DMA('sp', [skey], [f'p_x{i}'], lambda e: e.dma_start(out=xt[i][:], in_=sap))
                yield
                kb.I('act', [f'p_x{i}'], ['p_sq', 'p_ss'], lambda e: e.activation(out=sq[:], in_=xt[i][:], func=AF.Square, accum_out=ss[:]))
                yield
                kb.I('act', ['p_ss', 'epsc'], ['p_ss'], lambda e: e.activation(out=ss[:], in_=ss[:], func=AF.Sqrt, scale=1.0 / D, bias=epsc[:]))
                yield
                kb.I('dve', ['p_ss'], ['p_ss'], lambda e: e.reciprocal(out=ss[:], in_=ss[:]))
                yield
                kb.I('dve', [f'p_x{i}', 'p_ss', 'p_A'], [f'p_h2{i}'], lambda e: e.scalar_tensor_tensor(out=h2b[i][:], in0=xt[i][:], scalar=ss[:], in1=A[:], op0=ALU.mult, op1=ALU.mult))
                yield
                kb.I('dve', [f'p_h2{i}', 'p_S'], [f'p_h2{i}'], lambda e: e.tensor_tensor(out=h2b[i][:], in0=h2b[i][:], in1=S[:], op=ALU.add))
                yield
                for half in range(2):
                    pb = psr.next()
                    for kk in range(4):
                        k = half * 4 + kk
                        tr(ps[pb][:, kk * 128:(kk + 1) * 128], h2b[i][:, k * 128:(k + 1) * 128], [f'p_h2{i}'], [psk[pb]])
                        yield
                    kb.I('act', [psk[pb]], ['p_h2T'], lambda e: e.activation(out=h2T[:, half * 4:half * 4 + 4, :], in_=ps[pb][:, :].rearrange("p (k t) -> p k t", k=4), func=AF.Copy))
                    yield
                for q0 in range(0, 16, 4):
                    pb = psr.next()
                    wi_ = (q0 // 4) % 2
                    wq = wqb[wi_]
                    kb.DMA('sp', [], [f'p_wq{wi_}'], lambda e: e.dma_start(out=wq[:], in_=peer_wq[l, :, :, q0 * 128:(q0 + 4) * 128]))
                    yield
                    for qq in range(4):
                        for k in range(KC):
                            mm(ps[pb][:, qq * 128:(qq + 1) * 128], wq[:, k, qq * 128:(qq + 1) * 128], h2T[:, k, :], [f'p_wq{wi_}', 'p_h2T'], [psk[pb]], start=(k == 0), stop=(k == KC - 1))
                            yield
                    kb.I('act', [psk[pb]], ['p_qT'], lambda e: e.activation(out=qT[:, q0:q0 + 4, :], in_=ps[pb][:, :].rearrange("p (k t) -> p k t", k=4), func=AF.Copy))
                    yield
                for q0 in range(0, 16, 4):
                    pb = psr.next()
                    for qq in range(4):
                        hp = q0 + qq
                        mm(ps[pb][:, qq * 128:(qq + 1) * 128], qT[:, hp, :], kT[:, hp, :], ['p_qT', 'p_kT'], [psk[pb]])
                        yield
                    kb.I('act', [psk[pb]], ['p_s1'], lambda e: e.activation(out=s1[:, q0:q0 + 4, :], in_=ps[pb][:, :].rearrange("p (k t) -> p k t", k=4), func=AF.Copy))
                    yield
                for hp in range(16):
                    kb.I('dve', ['p_s1'], ['p_m8'], lambda e: e.max(out=m8[:, hp, 0:8], in_=s1[:, hp, :]))
                    yield
                    kb.I('dve', ['p_s1', 'p_m8'], ['p_i8'], lambda e: e.max_index(out=i8[:, hp, 0:8], in_max=m8[:, hp, 0:8], in_values=s1[:, hp, :]))
                    yield
                    kb.I('dve', ['p_s1', 'p_m8'], ['p_s2'], lambda e: e.match_replace(out=s2[:, hp, :], in_to_replace=m8[:, hp, 0:8], in_values=s1[:, hp, :], imm_value=NEG))
                    yield
                    kb.I('dve', ['p_s2'], ['p_m8'], lambda e: e.max(out=m8[:, hp, 8:16], in_=s2[:, hp, :]))
                    yield
                    kb.I('dve', ['p_s2', 'p_m8'], ['p_i8'], lambda e: e.max_index(out=i8[:, hp, 8:16], in_max=m8[:, hp, 8:16], in_values=s2[:, hp, :]))
                    yield
                kb.I('dve', ['p_i8'], ['p_i8f'], lambda e: e.tensor_copy(out=i8f[:], in_=i8[:]))
                yield
                m8v = m8[:, :, :].rearrange("p (h two) k -> p h two k", two=2)
                i8v = i8f[:, :, :].rearrange("p (h two) k -> p h two k", two=2)
                candv = cand[:, :, :].rearrange("p h (i j) -> p h i j", j=16)
                kb.I('dve', ['p_m8'], ['p_cand'], lambda e: e.tensor_tensor(out=candv, in0=m8v[:, :, 0, :].unsqueeze(3).to_broadcast([128, 8, 16, 16]), in1=m8v[:, :, 1, :].unsqueeze(2).to_broadcast([128, 8, 16, 16]), op=ALU.add))
                yield
                for h in range(8):
                    kb.I('dve', ['p_cand'], ['p_sc'], lambda e: e.max(out=sc[:, h, 0:8], in_=cand[:, h, :]))
                    yield
                    kb.I('dve', ['p_cand', 'p_sc'], ['p_ci'], lambda e: e.max_index(out=ci[:, h, 0:8], in_max=sc[:, h, 0:8], in_values=cand[:, h, :]))
                    yield
                    kb.I('dve', ['p_cand', 'p_sc'], ['p_cand2'], lambda e: e.match_replace(out=cand2[:, h, :], in_to_replace=sc[:, h, 0:8], in_values=cand[:, h, :], imm_value=NEG))
                    yield
                    kb.I('dve', ['p_cand2'], ['p_sc'], lambda e: e.max(out=sc[:, h, 8:16], in_=cand2[:, h, :]))
                    yield
                    kb.I('dve', ['p_cand2', 'p_sc'], ['p_ci'], lambda e: e.max_index(out=ci[:, h, 8:16], in_max=sc[:, h, 8:16], in_values=cand2[:, h, :]))
                    yield
                kb.I('dve', ['p_ci'], ['p_cf'], lambda e: e.tensor_copy(out=cf[:], in_=ci[:]))
                yield
                thr_b = pc[:, 0:16].unsqueeze(1).unsqueeze(1).to_broadcast([128, 8, 16, 16])
                iot_b = pc[:, 16:32].unsqueeze(1).unsqueeze(1).to_broadcast([128, 8, 16, 16])
                kb.I('dve', ['p_cf', 'p_pc'], ['p_oh'], lambda e: e.tensor_tensor(out=oh[:], in0=cf[:].unsqueeze(3).to_broadcast([128, 8, 16, 16]), in1=thr_b, op=ALU.is_ge))
                yield
                kb.I('dve', ['p_oh'], ['p_fi'], lambda e: e.tensor_reduce(out=fi[:], in_=oh[:], axis=AX.X, op=ALU.add))
                yield
                kb.I('dve', ['p_fi', 'p_cf'], ['p_fj'], lambda e: e.scalar_tensor_tensor(out=fj[:], in0=fi[:], scalar=-16.0, in1=cf[:], op0=ALU.mult, op1=ALU.add))
                yield
                for (fx, half_, gdst, gk) in ((fi, 0, g1, 'p_g1'), (fj, 1, g2_, 'p_g2')):
                    fk = 'p_fi' if half_ == 0 else 'p_fj'
                    kb.I('dve', [fk, 'p_pc'], ['p_oh'], lambda e: e.tensor_tensor(out=oh[:], in0=fx[:].unsqueeze(3).to_broadcast([128, 8, 16, 16]), in1=iot_b, op=ALU.is_equal))
                    yield
                    kb.I('dve', ['p_oh', 'p_i8f'], ['p_oh'], lambda e: e.tensor_tensor(out=oh[:], in0=oh[:], in1=i8v[:, :, half_, :].unsqueeze(2).to_broadcast([128, 8, 16, 16]), op=ALU.mult))
                    yield
                    kb.I('dve', ['p_oh'], [gk], lambda e: e.tensor_reduce(out=gdst[:], in_=oh[:], axis=AX.X, op=ALU.add))
                    yield
                kb.I('dve', ['p_g1', 'p_g2'], ['p_g1'], lambda e: e.scalar_tensor_tensor(out=g1[:], in0=g1[:], scalar=128.0, in1=g2_[:], op0=ALU.mult, op1=ALU.add))
                yield
                kb.I('dve', ['p_g1'], [f'p_eid{i}'], lambda e: e.tensor_copy(out=eidb[i][:, :].rearrange("p (h k) -> p h k", k=16), in_=g1[:]))
                yield
                kb.I('dve', ['p_sc'], [f'p_gate{i}'], lambda e: e.tensor_tensor(out=gateb[i][:], in0=sc[:], in1=sc[:, :, 0:1].to_broadcast([128, 8, 16]), op=ALU.subtract))
                yield
                kb.I('act', [f'p_gate{i}'], [f'p_gate{i}'], lambda e: e.activation(out=gateb[i][:], in_=gateb[i][:], func=AF.Exp))
                yield
                kb.I('dve', [f'p_gate{i}'], ['p_gsum'], lambda e: e.tensor_reduce(out=gsum[:], in_=gateb[i][:], axis=AX.X, op=ALU.add))
                yield
                kb.I('dve', ['p_gsum'], ['p_gsum'], lambda e: e.reciprocal(out=gsum[:], in_=gsum[:]))
                yield
                kb.I('dve', [f'p_gate{i}', 'p_gsum'], [f'p_gate{i}'], lambda e: e.tensor_tensor(out=gateb[i][:], in0=gateb[i][:], in1=gsum[:].unsqueeze(2).to_broadcast([128, 8, 16]), op=ALU.mult))
                yield

            def pump(gen, n):
                if gen is None:
                    return
                k = 0
                while n is None or k < n:
                    try:
                        next(gen)
                    except StopIteration:
                        return
                    k += 1

            def back(ti, t, nxt):
                i = ti % 2
                kind = 'c' if t < 2 else 'l'
                for slot in range(128):
                    gi = gring.next()
                    kb.DMA('pool', [f'p_eid{i}'], [f'p_gb{gi}'], lambda e: e.indirect_dma_start(out=gb[gi][:, :], out_offset=None, in_=peer_u[l][:, :], in_offset=bass.IndirectOffsetOnAxis(ap=eidb[i][:, slot:slot + 1], axis=0)))
                    kb.I('dve', [f'p_gb{gi}', f'p_h2{i}'], [('p_act', slot)], lambda e: e.scalar_tensor_tensor(out=junk[:], in0=gb[gi][:], scalar=1.0, in1=h2b[i][:], op0=ALU.mult, op1=ALU.mult, accum_out=act[:, slot:slot + 1]))
                    pump(nxt, 2)
                kb.I('dve', [('p_act', s_) for s_ in range(128)], ['p_t1', 'p_act'], lambda e: e.tensor_tensor(out=t1[:], in0=act[:], in1=act[:], op=ALU.mult))
                kb.I('dve', ['p_t1'], ['p_t1'], lambda e: e.tensor_scalar(out=t1[:], in0=t1[:], scalar1=0.044715, scalar2=1.0, op0=ALU.mult, op1=ALU.add))
                kb.I('dve', ['p_t1', 'p_act'], ['p_t1'], lambda e: e.tensor_tensor(out=t1[:], in0=t1[:], in1=act[:], op=ALU.mult))
                kb.I('act', ['p_t1'], ['p_t1'], lambda e: e.activation(out=t1[:], in_=t1[:], func=AF.Sigmoid, scale=1.5957691216057308))
                kb.I('dve', ['p_t1', 'p_act'], ['p_t1'], lambda e: e.tensor_tensor(out=t1[:], in0=t1[:], in1=act[:], op=ALU.mult))
                kb.I('dve', ['p_t1', f'p_gate{i}'], ['p_wgt'], lambda e: e.tensor_tensor(out=wgt[:], in0=t1[:], in1=gateb[i][:, :, :].rearrange("p h k -> p (h k)"), op=ALU.mult))
                for slot in range(128):
                    gi = gring.next()
                    kb.DMA('pool', [f'p_eid{i}'], [f'p_gb{gi}'], lambda e: e.indirect_dma_start(out=gb[gi][:, :], out_offset=None, in_=peer_v[l][:, :], in_offset=bass.IndirectOffsetOnAxis(ap=eidb[i][:, slot:slot + 1], axis=0)))
                    di = dring.next()
                    kb.I('dve', ['ident', 'p_wgt'], [f'p_dg{di}'], lambda e: e.tensor_scalar(out=dg[di][:], in0=ident[:], scalar1=wgt[:, slot:slot + 1], scalar2=None, op0=ALU.mult))
                    for hf in range(2):
                        mm(ps[6 + hf][:, :], dg[di][:], gb[gi][:, hf * 512:(hf + 1) * 512], [f'p_dg{di}', f'p_gb{gi}'], [psk[6 + hf]], start=(slot == 0), stop=(slot == 127))
                    pump(nxt, 2)
                for hf in range(2):
                    kb.I('dve', [psk[6 + hf], 'p_G' + kind], ['p_acc'], lambda e: e.tensor_tensor(out=acc[:, hf * 512:(hf + 1) * 512], in0=ps[6 + hf][:, :], in1=Gk[kind][:, hf * 512:(hf + 1) * 512], op=ALU.mult))
                kb.I('dve', ['p_acc', f'p_x{i}'], [f'p_x{i}'], lambda e: e.tensor_tensor(out=xt[i][:], in0=xt[i][:], in1=acc[:], op=ALU.add))
                if final:
                    kb.I('act', [f'p_x{i}'], ['p_ssf'], lambda e: e.activation(out=junk[:], in_=xt[i][:], func=AF.Square, accum_out=ssf[:]))
                    kb.I('act', ['p_ssf', 'epsc'], ['p_ssf'], lambda e: e.activation(out=ssf[:], in_=ssf[:], func=AF.Sqrt, scale=1.0 / D, bias=epsc[:]))
                    kb.I('dve', ['p_ssf'], ['p_ssf'], lambda e: e.reciprocal(out=ssf[:], in_=ssf[:]))
                    kb.I('dve', [f'p_x{i}', 'p_ssf', 'p_fnw'], [f'p_x{i}'], lambda e: e.scalar_tensor_tensor(out=xt[i][:], in0=xt[i][:], scalar=ssf[:], in1=fnw[:], op0=ALU.mult, op1=ALU.mult))
                dst_fn(t, xt[i], f'p_x{i}')
                pump(nxt, None)

            gens = [front(ti, t) for ti, t in enumerate(tiles)]
            pump(gens[0], None)
            for ti, t in enumerate(tiles):
                back(ti, t, gens[ti + 1] if ti + 1 < len(tiles) else None)
            kb.barrier()

    od_w_in = din("od_w_in", [128, KC, 2064])
    od_w_out = din("od_w_out", [128, KC, D])
    c_gc = din("c_gc", [1, 16])
    c_norm = din("c_norm", [1, 512])
    d_w = din("d_w", [4, 128, 128])
    d_scale = din("d_scale", [512, 1])
    poolm = din("poolm", [4, 128, 128])
    mixT1 = dscr("mixT1", [NB, KC, 128, LT])

    def xs_cm_src(b, t):
        if t < 2:
            return [(slice(0, 128), xs[b, t * 128:(t + 1) * 128, :])]
        v = xs[b, LC:LT, :].rearrange("(r w) d -> r w d", w=GRID_W)
        return [(slice(wi * 32, (wi + 1) * 32), v[:, (t - 2) * 4 + wi, :]) for wi in range(4)]

    def layer1_mixer(b):
        with ExitStack() as ph:
            hT = sb("hT1", [128, KC, LT], stack=ph)
            with ExitStack() as st:
                nw = sb("n1_nw", [128, D], stack=st)
                kb.DMA('sp', [], ['n1_nw'], lambda e: e.dma_start(out=nw[:], in_=norms[1].to_broadcast([128, D])))
                md = {}
                for kind, row in (('c', 2), ('l', b)):
                    A = sb("n1_A" + kind, [128, D], stack=st)
                    S = sb("n1_S" + kind, [128, D], stack=st)
                    kb.DMA('sp', [('modsD', 1)], ['n1_A' + kind], lambda e: e.dma_start(out=A[:], in_=modsD[1, row:row + 1, D:2 * D].to_broadcast([128, D])))
                    kb.DMA('sp', [('modsD', 1)], ['n1_S' + kind], lambda e: e.dma_start(out=S[:], in_=modsD[1, row:row + 1, 0:D].to_broadcast([128, D])))
                    kb.I('dve', ['n1_A' + kind, 'n1_nw'], ['n1_A' + kind], lambda e: e.scalar_tensor_tensor(out=A[:], in0=A[:], scalar=1.0, in1=nw[:], op0=ALU.add, op1=ALU.mult))
                    md[kind] = (A, S)
                xt = [sb(f"n1_x{i}", [128, D], stack=st) for i in range(2)]
                ht = [sb(f"n1_h{i}", [128, D], stack=st) for i in range(2)]
                sq = sb("n1_sq", [128, D], stack=st)
                ss = [sb(f"n1_ss{i}", [128, 1], stack=st) for i in range(2)]
                for t in range(NT):
                    i = t % 2
                    kind = 'c' if t < 2 else 'l'
                    A, S = md[kind]
                    for (psl, sap) in xs_cm_src(b, t):
                        kb.DMA('sp', [('xs', b)], [f'n1_x{i}'], lambda e: e.dma_start(out=xt[i][psl, :], in_=sap))
                    kb.I('act', [f'n1_x{i}'], ['n1_sq', f'n1_ss{i}'], lambda e: e.activation(out=sq[:], in_=xt[i][:], func=AF.Square, accum_out=ss[i][:]))
                    kb.I('act', [f'n1_ss{i}', 'epsc'], [f'n1_ss{i}'], lambda e: e.activation(out=ss[i][:], in_=ss[i][:], func=AF.Sqrt, scale=1.0 / D, bias=epsc[:]))
                    kb.I('dve', [f'n1_ss{i}'], [f'n1_ss{i}'], lambda e: e.reciprocal(out=ss[i][:], in_=ss[i][:]))
                    kb.I('dve', [f'n1_x{i}', f'n1_ss{i}', 'n1_A' + kind], [f'n1_h{i}'], lambda e: e.scalar_tensor_tensor(out=ht[i][:], in0=xt[i][:], scalar=ss[i][:], in1=A[:], op0=ALU.mult, op1=ALU.mult))
                    kb.I('dve', [f'n1_h{i}', 'n1_S' + kind], [f'n1_h{i}'], lambda e: e.tensor_tensor(out=ht[i][:], in0=ht[i][:], in1=S[:], op=ALU.add))
                    for half in range(2):
                        pb = psr.next()
                        for kk in range(4):
                            k = half * 4 + kk
                            tr(ps[pb][:, kk * 128:(kk + 1) * 128], ht[i][:, k * 128:(k + 1) * 128], [f'n1_h{i}'], [psk[pb]])
                        kb.I('act', [psk[pb]], [('hT', t)], lambda e: e.activation(out=hT[:, half * 4:half * 4 + 4, t * 128:(t + 1) * 128], in_=ps[pb][:, :].rearrange("p (k t) -> p k t", k=4), func=AF.Copy))
                kb.barrier()
            with ExitStack() as st:
                wd = sb("q_wd", [128, KC, 512], stack=st)
                kb.DMA('sp', [], ['q_wd'], lambda e: e.dma_start(out=wd[:], in_=od_w_in[:, :, 1552:2064]))
                pm = sb("q_pm", [128, 4, 128], stack=st)
                dw = sb("q_dw", [128, 4, 128], stack=st)
                dsc = sb("q_dsc", [128, 4], stack=st)
                for g in range(4):
                    kb.DMA('sp', [], ['q_pm'], lambda e: e.dma_start(out=pm[:, g, :], in_=poolm[g]))
                    kb.DMA('sp', [], ['q_dw'], lambda e: e.dma_start(out=dw[:, g, :], in_=d_w[g]))
                    kb.DMA('sp', [], ['q_dsc'], lambda e: e.dma_start(out=dsc[:, g:g + 1], in_=d_scale[g * 128:(g + 1) * 128, :]))
                dtm = sb("q_dtm", [128, 512], stack=st)
                pT = sb("q_pT", [128, 128], stack=st)
                yT = sb("q_yT", [128, 4, LT], stack=st)
                for t in range(2, NT):
                    pb = psr.next()
                    for k in range(KC):
                        mm(ps[pb][:, :], hT[:, k, t * 128:(t + 1) * 128], wd[:, k, :], ['q_wd', ('hT', t)], [psk[pb]], start=(k == 0), stop=(k == KC - 1))
                    kb.I('act', [psk[pb]], ['q_dtm'], lambda e: e.activation(out=dtm[:], in_=ps[pb][:, :], func=AF.Copy))
                    for g in range(4):
                        p2 = psr.next()
                        mm(ps[p2][:, 0:128], dtm[:, g * 128:(g + 1) * 128], pm[:, g, :], ['q_dtm', 'q_pm'], [psk[p2]])
                        kb.I('act', [psk[p2]], ['q_pT'], lambda e: e.activation(out=pT[:], in_=ps[p2][:, 0:128], func=AF.Copy))
                        mm(ps[p2][:, 128:256], dw[:, g, :], pT[:], ['q_dw', 'q_pT'], [psk[p2]])
                        kb.I('dve', [psk[p2], 'q_dsc'], ['q_yT'], lambda e: e.tensor_scalar(out=yT[:, g, t * 128:(t + 1) * 128], in0=ps[p2][:, 128:256], scalar1=dsc[:, g:g + 1], scalar2=None, op0=ALU.mult))
                for g in range(4):
                    kb.DMA('sp', ['q_yT'], [('mixT1', b)], lambda e: e.dma_start(out=mixT1[b, 4 + g, :, LC:LT], in_=yT[:, g, LC:LT]))
                kb.barrier()
            mlstm(b, hT)
            kb.barrier()

    def mlstm(b, hT):
        with ExitStack() as st:
            wh = sb("m_wh", [128, KC, 64 + 64 + 128 + 128], stack=st)
            wg16 = sb("m_wg16", [128, KC, 16], stack=st)
            gcst = sb("m_gcst", [128, 16], stack=st)
            cnw = sb("m_cnw", [128, 512], stack=st)
            QT = sb("m_QT", [64, LT], stack=st)
            KT = sb("m_KT", [64, LT], stack=st)
            Ktm = sb("m_Ktm", [128, NT, 64], stack=st)
            V1 = sb("m_V1", [128, NT, 132], stack=st)
            og = sb("m_og", [128, NT, 128], stack=st)
            O = sb("m_O", [128, NT, 128], stack=st)
            tmp = sb("m_tmp", [128, LT], stack=st)
            graw = sb("m_graw", [128, NT, 16], stack=st)
            LI = sb("m_li", [128, NT, 8], stack=st)
            LF = sb("m_lf", [128, NT, 8], stack=st)
            kb.DMA('sp', [], ['m_gcst'], lambda e: e.dma_start(out=gcst[:], in_=c_gc[0:1, :].to_broadcast([128, 16])))
            kb.DMA('sp', [], ['m_cnw'], lambda e: e.dma_start(out=cnw[:], in_=c_norm[0:1, :].to_broadcast([128, 512])))
            kb.DMA('sp', [], ['m_wg16'], lambda e: e.dma_start(out=wg16[:], in_=od_w_in[:, :, 1024:1040]))
            pb = psr.next()
            for n in range(NT):
                for k in range(KC):
                    mm(ps[pb][:, n * 16:(n + 1) * 16], hT[:, k, n * 128:(n + 1) * 128], wg16[:, k, :], ['m_wg16', ('hT', n)], [psk[pb]], start=(k == 0), stop=(k == KC - 1))
            kb.I('act', [psk[pb]], ['m_graw'], lambda e: e.activation(out=graw[:], in_=ps[pb][:, 0:NT * 16].rearrange("p (n c) -> p n c", c=16), func=AF.Copy))
            kb.I('dve', ['m_graw', 'm_gcst'], ['m_li'], lambda e: e.tensor_tensor(out=LI[:], in0=graw[:, :, 0:8], in1=gcst[:, 0:8].unsqueeze(1).to_broadcast([128, NT, 8]), op=ALU.add))
            kb.I('dve', ['m_graw', 'm_gcst'], ['m_lf'], lambda e: e.tensor_tensor(out=LF[:], in0=graw[:, :, 8:16], in1=gcst[:, 8:16].unsqueeze(1).to_broadcast([128, NT, 8]), op=ALU.add))
            kb.I('act', ['m_lf'], ['m_lf'], lambda e: e.activation(out=LF[:], in_=LF[:], func=AF.Exp, scale=-1.0))
            kb.I('act', ['m_lf', 'onec'], ['m_lf'], lambda e: e.activation(out=LF[:], in_=LF[:], func=AF.Ln, bias=onec[:]))
            kb.I('dve', ['m_lf'], ['m_lf'], lambda e: e.tensor_scalar(out=LF[:], in0=LF[:], scalar1=-1.0, scalar2=None, op0=ALU.mult))
            kb.I('dve', [], ['m_V1'], lambda e: e.memset(V1[:], 1.0))
            for hd in range(4):
                for (o0, c0, w_) in ((0, hd * 64, 64), (64, 256 + hd * 64, 64), (128, 512 + hd * 128, 128), (256, 1040 + hd * 128, 128)):
                    kb.DMA('sp', [], ['m_wh'], lambda e: e.dma_start(out=wh[:, :, o0:o0 + w_], in_=od_w_in[:, :, c0:c0 + w_]))
                for (dst, dk_, o0, scl) in ((QT, 'm_QT', 0, 1.0), (KT, 'm_KT', 64, 0.125)):
                    for (t0, n) in TOKBLKS:
                        pb = psr.next()
                        for k in range(KC):
                            mm(ps[pb][0:64, 0:n], wh[:, k, o0:o0 + 64], hT[:, k, t0:t0 + n], ['m_wh'] + [('hT', tt) for tt in range(t0 // 128, (t0 + n) // 128)], [psk[pb]], start=(k == 0), stop=(k == KC - 1))
                        kb.I('act', [psk[pb]], [dk_], lambda e: e.activation(out=dst[:, t0:t0 + n], in_=ps[pb][0:64, 0:n], func=AF.Copy, scale=scl))
                for n0 in range(0, NT, 8):
                    pb = psr.next()
                    nn = min(8, NT - n0)
                    for i in range(nn):
                        kb.I('pe', ['m_KT', 'ident'], [psk[pb]], lambda e: e.transpose(out=ps[pb][:, i * 64:(i + 1) * 64], in_=KT[:, (n0 + i) * 128:(n0 + i + 1) * 128], identity=ident[0:64, 0:64]))
                    kb.I('act', [psk[pb]], ['m_Ktm'], lambda e: e.activation(out=Ktm[:, n0:n0 + nn, :], in_=ps[pb][:, 0:nn * 64].rearrange("p (n c) -> p n c", c=64), func=AF.Copy))
                for (o0, dst, dk_, fn_, wdt) in ((128, V1, 'm_V1', AF.Copy, 128), (256, og, 'm_og', AF.Sigmoid, 128)):
                    for n0 in range(0, NT, 4):
                        pb = psr.next()
                        nn = min(4, NT - n0)
                        for i in range(nn):
                            n = n0 + i
                            for k in range(KC):
                                mm(ps[pb][:, i * 128:(i + 1) * 128], hT[:, k, n * 128:(n + 1) * 128], wh[:, k, o0:o0 + 128], ['m_wh', ('hT', n)], [psk[pb]], start=(k == 0), stop=(k == KC - 1))
                        kb.I('act', [psk[pb]], [dk_], lambda e: e.activation(out=dst[:, n0:n0 + nn, 0:128], in_=ps[pb][:, 0:nn * 128].rearrange("p (n c) -> p n c", c=128), func=fn_))
                mlstm_chains(b, hd, QT, KT, Ktm, V1, LI, LF, O)
                ssq = sb(f"m_ssq{hd}", [128, NT], stack=st)
                O2 = tmp[:, :].rearrange("p (n c) -> p n c", c=128)
                kb.I('dve', ['m_O'], ['m_tmp'], lambda e: e.tensor_tensor(out=O2, in0=O[:], in1=O[:], op=ALU.mult))
                kb.I('dve', ['m_tmp'], ['m_ssq'], lambda e: e.tensor_reduce(out=ssq[:], in_=O2, axis=AX.X, op=ALU.add))
                kb.I('act', ['m_ssq', 'epsc'], ['m_ssq'], lambda e: e.activation(out=ssq[:], in_=ssq[:], func=AF.Sqrt, scale=1.0 / 128, bias=epsc[:]))
                kb.I('dve', ['m_ssq'], ['m_ssq'], lambda e: e.reciprocal(out=ssq[:], in_=ssq[:]))
                kb.I('dve', ['m_O', 'm_ssq'], ['m_O'], lambda e: e.tensor_tensor(out=O[:], in0=O[:], in1=ssq[:].unsqueeze(2).to_broadcast([128, NT, 128]), op=ALU.mult))
                kb.I('dve', ['m_O', 'm_cnw'], ['m_O'], lambda e: e.tensor_tensor(out=O[:], in0=O[:], in1=cnw[:, hd * 128:(hd + 1) * 128].unsqueeze(1).to_broadcast([128, NT, 128]), op=ALU.mult))
                kb.I('dve', ['m_O', 'm_og'], ['m_O'], lambda e: e.tensor_tensor(out=O[:], in0=O[:], in1=og[:], op=ALU.mult))
                for n0 in range(0, NT, 4):
                    pb = psr.next()
                    nn = min(4, NT - n0)
                    for i in range(nn):
                        tr(ps[pb][:, i * 128:(i + 1) * 128], O[:, n0 + i, :], ['m_O'], [psk[pb]])
                    kb.I('act', [psk[pb]], ['m_tmp'], lambda e: e.activation(out=tmp[:, n0 * 128:(n0 + nn) * 128], in_=ps[pb][:, 0:nn * 128], func=AF.Copy))
                kb.DMA('sp', ['m_tmp'], [('mixT1', b)], lambda e: e.dma_start(out=mixT1[b, hd, :, LC:LT], in_=tmp[:, LC:LT]))
            kb.barrier()

    def mlstm_chains(b, hd, QT, KT, Ktm, V1, LI, LF, O):
        with ExitStack() as st:
            NBUF = 2

            def mk(name, shape):
                return [sb(f"k_{name}{i}", shape, stack=st) for i in range(NBUF)]
            cs = mk("cs", [128, 8])
            Tg = mk("Tg", [128, 128]); Ei = mk("Ei", [128, 128]); eg = mk("eg", [64, 128]); qd = mk("qd", [64, 128])
            ST = mk("ST", [128, 128]); Kd = mk("Kd", [128, 64])
            C = [sb(f"k_C{i}", [64, 132], stack=st) for i in range(2)]
            kb.I('dve', [], ['m_O'], lambda e: e.memset(O[:, 0:2, :], 0.0))
            it = 0
            for d in range(2):
                col = d * 4 + hd
                order = ([0, 1] + list(range(2, NT))) if d == 0 else ([1, 0] + list(range(NT - 1, 1, -1)))
                si = 0
                kb.I('dve', [], ['k_C0'], lambda e: e.memset(C[0][:], 0.0))
                for n in order:
                    i = it % NBUF
                    it += 1
                    K_ = lambda nm: f"k_{nm}{i}"
                    fcol = LF[:, n, col:col + 1]
                    icol = LI[:, n, col:col + 1]
                    tok = slice(n * 128, (n + 1) * 128)
                    Cc, Cn = C[si], C[1 - si]
                    ckc, ckn = f'k_C{si}', f'k_C{1 - si}'
                    kb.I('dve', ['m_lf', 'msk'], [K_('Tg')], lambda e: e.tensor_scalar(out=Tg[i][:], in0=msk[:, d, :], scalar1=fcol, scalar2=None, op0=ALU.mult))
                    pa = psr.next()
                    mm(ps[pa][:, 0:128], ones[:], Tg[i][:], ['ones', K_('Tg')], [psk[pa]])
                    mm(ps[pa][:, 256:257], msk[:, d, :], fcol, ['msk', 'm_lf'], [psk[pa]])
                    mm(ps[pa][:, 257:258], ones[:], fcol, ['ones', 'm_lf'], [psk[pa]])
                    kb.I('act', [psk[pa]], [K_('cs')], lambda e: e.activation(out=cs[i][:, 0:2], in_=ps[pa][:, 256:258], func=AF.Copy))
                    kb.I('dve', [K_('cs')], [K_('cs')], lambda e: e.tensor_tensor(out=cs[i][:, 2:3], in0=cs[i][:, 1:2], in1=cs[i][:, 0:1], op=ALU.subtract))
                    kb.I('dve', [K_('cs'), 'm_li'], [K_('cs')], lambda e: e.tensor_tensor(out=cs[i][:, 2:3], in0=cs[i][:, 2:3], in1=icol, op=ALU.add))
                    kb.I('act', [K_('cs')], [K_('cs')], lambda e: e.activation(out=cs[i][:, 3:5], in_=cs[i][:, 1:3], func=AF.Exp))
                    kb.I('dve', ['m_Ktm', K_('cs')], [K_('Kd')], lambda e: e.tensor_scalar(out=Kd[i][:], in0=Ktm[:, n, :], scalar1=cs[i][:, 4:5], scalar2=None, op0=ALU.mult))
                    if n >= 2:
                        kb.I('dve', [psk[pa], K_('cs')], [K_('Ei')], lambda e: e.tensor_scalar(out=Ei[i][:], in0=ps[pa][:, 0:128], scalar1=cs[i][:, 0:1], scalar2=0.0, op0=ALU.subtract, op1=ALU.min))
                        kb.I('act', [K_('Ei'), 'm_li'], [K_('Ei')], lambda e: e.activation(out=Ei[i][:], in_=Ei[i][:], func=AF.Exp, bias=icol))
                        kb.I('dve', [K_('Ei'), 'msk'], [K_('Ei')], lambda e: e.tensor_tensor(out=Ei[i][:], in0=Ei[i][:], in1=msk[:, 2 + d, :], op=ALU.mult))
                        kb.I('act', [psk[pa]], [K_('eg')], lambda e: e.activation(out=eg[i][:], in_=ps[pa][0:64, 0:128], func=AF.Exp))
                        kb.I('dve', [K_('eg'), 'm_QT'], [K_('qd')], lambda e: e.tensor_tensor(out=qd[i][:], in0=QT[:, tok], in1=eg[i][:], op=ALU.mult))
                        pq = psr.next()
                        mm(ps[pq][:, 0:128], KT[:, tok], QT[:, tok], ['m_KT', 'm_QT'], [psk[pq]])
                        kb.I('dve', [psk[pq], K_('Ei')], [K_('ST')], lambda e: e.tensor_tensor(out=ST[i][:], in0=ps[pq][:, 0:128], in1=Ei[i][:], op=ALU.mult))
                        mm(ps[pq][:, 128:257], qd[i][:], Cc[:, 0:129], [K_('qd'), ckc], [psk[pq]], start=True, stop=False)
                        mm(ps[pq][:, 128:257], ST[i][:], V1[:, n, 0:129], [K_('ST'), 'm_V1'], [psk[pq]], start=False, stop=True)
                        kb.I('act', [psk[pq]], [K_('cs')], lambda e: e.activation(out=cs[i][:, 5:6], in_=ps[pq][:, 256:257], func=AF.Abs))
                        kb.I('dve', [K_('cs')], [K_('cs')], lambda e: e.tensor_scalar(out=cs[i][:, 5:6], in0=cs[i][:, 5:6], scalar1=1.0, scalar2=None, op0=ALU.max))
                        kb.I('dve', [K_('cs')], [K_('cs')], lambda e: e.reciprocal(out=cs[i][:, 5:6], in_=cs[i][:, 5:6]))
                        if d == 0:
                            kb.I('dve', [psk[pq], K_('cs')], ['m_O'], lambda e: e.tensor_scalar(out=O[:, n, :], in0=ps[pq][:, 128:256], scalar1=cs[i][:, 5:6], scalar2=None, op0=ALU.mult))
                        else:
                            kb.I('dve', [psk[pq], K_('cs'), 'm_O'], ['m_O'], lambda e: e.scalar_tensor_tensor(out=O[:, n, :], in0=ps[pq][:, 128:256], scalar=cs[i][:, 5:6], in1=O[:, n, :], op0=ALU.mult, op1=ALU.add))
                    p3 = psr.next()
                    mm(ps[p3][0:64, 0:129], Kd[i][:], V1[:, n, 0:129], [K_('Kd'), 'm_V1'], [psk[p3]])
                    kb.I('dve', [psk[p3], ckc, K_('cs')], [ckn], lambda e: e.scalar_tensor_tensor(out=Cn[:, 0:129], in0=Cc[:, 0:129], scalar=cs[i][0:64, 3:4], in1=ps[p3][0:64, 0:129], op0=ALU.mult, op1=ALU.add))
                    si = 1 - si
            kb.barrier()

    def w_out1_phase(b):
        with ExitStack() as st:
            wo = sb("o1_w", [128, KC, D], stack=st)
            kb.DMA('sp', [], ['o1_w'], lambda e: e.dma_start(out=wo[:], in_=od_w_out[:, :, :]))
            g = sb("o1_g", [128, D], stack=st)
            kb.DMA('sp', [('modsD', 1)], ['o1_g'], lambda e: e.dma_start(out=g[:], in_=modsD[1, b:b + 1, 2 * D:3 * D].to_broadcast([128, D])))
            mt = [sb(f"o1_m{i}", [128, KC, 128], stack=st) for i in range(2)]
            xt = [sb(f"o1_x{i}", [128, D], stack=st) for i in range(2)]
            for t in range(2, NT):
                i = t % 2
                kb.DMA('sp', [('mixT1', b)], [f'o1_m{i}'], lambda e: e.dma_start(out=mt[i][:], in_=mixT1[b, :, :, t * 128:(t + 1) * 128].rearrange("k p t -> p k t")))
                for (psl, sap) in xs_cm_src(b, t):
                    kb.DMA('sp', [('xs', b)], [f'o1_x{i}'], lambda e: e.dma_start(out=xt[i][psl, :], in_=sap))
                for hlf in range(2):
                    pb = psr.next()
                    for k in range(KC):
                        mm(ps[pb][:, :], mt[i][:, k, :], wo[:, k, hlf * 512:(hlf + 1) * 512], [f'o1_m{i}', 'o1_w'], [psk[pb]], start=(k == 0), stop=(k == KC - 1))
                    kb.I('dve', [psk[pb], 'o1_g'], [psk[pb]], lambda e: e.tensor_tensor(out=ps[pb][:, :], in0=ps[pb][:, :], in1=g[:, hlf * 512:(hlf + 1) * 512], op=ALU.mult))
                    kb.I('dve', [psk[pb], f'o1_x{i}'], [f'o1_x{i}'], lambda e: e.tensor_tensor(out=xt[i][:, hlf * 512:(hlf + 1) * 512], in0=xt[i][:, hlf * 512:(hlf + 1) * 512], in1=ps[pb][:, :], op=ALU.add))
                kb.DMA('sp', [f'o1_x{i}'], [('xs1', b)], lambda e: e.dma_start(out=xs1[b, (t - 2) * 128:(t - 1) * 128, :], in_=xt[i][:]))
            kb.barrier()

    def layer1(b, tiles=None):
        layer1_mixer(b)
        w_out1_phase(b)
        ov = out_d[b].rearrange("(r w) d -> r w d", w=GRID_W)

        def dst1(t, tile, key):
            for wi in range(4):
                kb.DMA('sp', [key], [('out', b)], lambda e: e.dma_start(out=ov[:, (t - 2) * 4 + wi, :], in_=tile[wi * 32:(wi + 1) * 32, :]))
        peer_phase(1, b, tiles or list(range(2, NT)), lambda t: (xs1[b, (t - 2) * 128:(t - 1) * 128, :], ('xs1', b)), dst1, final=True)

    nb_run = 1 if (stop_after or '').endswith('_b0') else NB
    for b in range(0 if l1only else nb_run):
        layer0_mixer(b)
        if stop_after in ('hT0', 'lru'):
            break
        w_out_phase(b)
        if stop_after == 'mix0_b0':
            continue

        def dst0(t, tile, key, b=b):
            kb.DMA('sp', [key], [('xs', b)], lambda e: e.dma_start(out=xs[b, t * 128:(t + 1) * 128, :], in_=tile[:]))
        peer_tiles = list(range(NT)) if stop_after != 'peer0_b0' else [0, 2]
        peer_phase(0, b, peer_tiles, lambda t, b=b: (xs[b, t * 128:(t + 1) * 128, :], ('xs', b)), dst0)
    if stop_after is None or stop_after.startswith('l1'):
        for b in range(nb_run):
            if stop_after == 'l1mix_b0':
                layer1_mixer(b)
                w_out1_phase(b)
            elif l1only:
                layer1(b, tiles=[2, 17])
            else:
                layer1(b)
    if 'xs' in dbg_out:
        kb.barrier()
        kb.DMA('sp', [('xs', 0), ('xs', 1)], ['dbg_xs'], lambda e: e.dma_start(out=dbg_out['xs'], in_=xs))
    if 'xs1' in dbg_out:
        kb.barrier()
        kb.DMA('sp', [('xs1', 0), ('xs1', 1)], ['dbg_xs1'], lambda e: e.dma_start(out=dbg_out['xs1'], in_=xs1))

    kb.finish()
    es.close()
    return nc


def make_in_maps(inputs):
    x = np.asarray(inputs['x'], np.float32)
    ctx = np.asarray(inputs['ctx'], np.float32)
    c = np.asarray(inputs['c'], np.float32)
    c_ctx = np.asarray(inputs['c_ctx'], np.float32)
    ada_w = np.ascontiguousarray(np.asarray(inputs['ada_w'], np.float32).reshape(2, KC, 128, 6 * D))
    ada_b = np.ascontiguousarray(np.asarray(inputs['ada_b'], np.float32).reshape(2, 1, 6 * D))
    norms = np.stack([inputs['norm_mix'][0], inputs['norm_mix'][1], inputs['norm_ffn'][0], inputs['norm_ffn'][1],
                      inputs['final_norm']]).astype(np.float32).reshape(5, 1, D)
    ident = np.eye(128, dtype=np.float32)
    ev_w_in = np.ascontiguousarray(np.asarray(inputs['ev_w_in'][0], np.float32).reshape(KC, 128, 3088).transpose(1, 0, 2))
    ev_w_out = np.ascontiguousarray(np.asarray(inputs['ev_w_out'][0], np.float32).reshape(KC, 128, D).transpose(1, 0, 2))
    a_convT = np.ascontiguousarray(np.asarray(inputs['a_conv'][0], np.float32).T)
    a_gc = np.concatenate([np.asarray(inputs['a_alog'][0]).reshape(8), np.asarray(inputs['a_dtb'][0]).reshape(8)]).astype(np.float32).reshape(1, 16)
    a_norm = np.asarray(inputs['a_norm'][0], np.float32).reshape(1, 128)
    lru_pc = np.zeros((512, 11), np.float32)
    lru_pc[:, 0:4] = np.asarray(inputs['b_conv_w'][0]).T
    lru_pc[:, 4] = np.asarray(inputs['b_conv_b'][0])
    for d in range(2):
        lru_pc[:, 5 + 3 * d] = np.asarray(inputs['b_ba'][0, d]).reshape(512)
        lru_pc[:, 6 + 3 * d] = np.asarray(inputs['b_bx'][0, d]).reshape(512)
        lru_pc[:, 7 + 3 * d] = np.asarray(inputs['b_lam'][0, d]).reshape(512)
    lru_w = np.zeros((2, 2, 4, 128, 128), np.float32)
    for ax, nm in enumerate(('b_wa', 'b_wx')):
        w = np.asarray(inputs[nm][0], np.float32)
        for d in range(2):
            for ct in range(4):
                lru_w[ax, d, ct, 0:64, 0:64] = w[d, 2 * ct]
                lru_w[ax, d, ct, 64:128, 64:128] = w[d, 2 * ct + 1]
    ii = np.arange(128)
    kk_, aa_ = ii[:, None], ii[None, :]
    masks = np.stack([(kk_ <= aa_), (kk_ >= aa_), (aa_ >= kk_), (aa_ <= kk_), (kk_ > aa_), (kk_ < aa_)]).astype(np.float32)
    peer_wq = np.ascontiguousarray(np.asarray(inputs['peer_wq'], np.float32).reshape(2, KC, 128, 2048).transpose(0, 2, 1, 3))
    peer_kT = np.ascontiguousarray(np.asarray(inputs['peer_keys'], np.float32).reshape(2, 16, 128, 128).transpose(0, 3, 1, 2))
    peer_u = np.asarray(inputs['peer_u'], np.float32)
    peer_v = np.asarray(inputs['peer_v'], np.float32)
    peer_u0, peer_u1, peer_v0, peer_v1 = peer_u[0], peer_u[1], peer_v[0], peer_v[1]
    pconst = np.concatenate([16.0 * (np.arange(16) + 1), np.arange(16)]).astype(np.float32).reshape(1, 32)
    od_w_in = np.ascontiguousarray(np.asarray(inputs['od_w_in'][0], np.float32).reshape(KC, 128, 2064).transpose(1, 0, 2))
    od_w_out = np.ascontiguousarray(np.asarray(inputs['od_w_out'][0], np.float32).reshape(KC, 128, D).transpose(1, 0, 2))
    c_gc = np.concatenate([np.asarray(inputs['c_ibias'][0]).reshape(8), np.asarray(inputs['c_fbias'][0]).reshape(8)]).astype(np.float32).reshape(1, 16)
    c_norm = np.asarray(inputs['c_norm'][0], np.float32).reshape(1, 512)
    d_w = np.asarray(inputs['d_w'][0], np.float32)
    d_scale = np.asarray(inputs['d_scale'][0], np.float32).reshape(512, 1)
    poolm = np.zeros((4, 128, 128), np.float32)
    seg = ROWS
    for gi, w in enumerate((2, 4, 8, 16)):
        P = np.zeros((seg, seg), np.float32)
        for t_ in range(seg):
            lo = max(t_ - w // 2, 0)
            hi = min(t_ - w // 2 + w, seg)
            P[t_, lo:hi] = 1.0 / (hi - lo)
        P = P - np.eye(seg, dtype=np.float32)
        for sgi in range(128 // seg):
            poolm[gi, sgi * seg:(sgi + 1) * seg, sgi * seg:(sgi + 1) * seg] = P.T
    shared = dict(od_w_in=od_w_in, od_w_out=od_w_out, c_gc=c_gc, c_norm=c_norm, d_w=d_w, d_scale=d_scale, poolm=poolm, peer_wq=peer_wq, peer_kT=peer_kT, peer_u0=peer_u0, peer_u1=peer_u1, peer_v0=peer_v0, peer_v1=peer_v1, pconst=pconst, ada_w=ada_w, ada_b=ada_b, norms=norms, ident=ident, ev_w_in=ev_w_in, ev_w_out=ev_w_out, a_convT=a_convT,
                  a_gc=a_gc, a_norm=a_norm, lru_pc=lru_pc, lru_w=lru_w, masks=masks)
    maps = []
    for ci in range(NCORES):
        b0 = ci * NB
        xin = np.concatenate([ctx[b0:b0 + NB], x[b0:b0 + NB]], axis=1)
        c3 = np.stack([c[b0], c[b0 + 1], c_ctx], axis=1)
        c3T = np.ascontiguousarray(c3.reshape(KC, 128, 3).transpose(1, 0, 2))
        maps.append(dict(xin=np.ascontiguousarray(xin), c3T=c3T, **shared))
    return maps


def kernel(**inputs):
    nc = build_program()
    maps = make_in_maps(inputs)
    res = run_bass_kernel_spmd(nc, maps, core_ids=list(range(NCORES)))
    outs = [r["out"] for r in res.results]
    return np.concatenate(outs, axis=0).astype(np.float32)
```

```python
import numpy as np
from contextlib import ExitStack
import concourse.bass as bass
import concourse.mybir as mybir
from concourse.bass_utils import run_bass_kernel_spmd

F32 = mybir.dt.float32
U32 = mybir.dt.uint32
I32 = mybir.dt.int32
AF = mybir.ActivationFunctionType
ALU = mybir.AluOpType
AX = mybir.AxisListType

NCORES = 8
NB = 2
D = 1024
KC = 8
LC = 256
LL = 2048
LT = LC + LL
NT = LT // 128
EPS = 1e-6
GRID_W = 64
ROWS = LL // GRID_W


class KB:
    def __init__(self, nc, es, ndma=16):
        self.nc = nc
        self.eng = {'pe': nc.tensor, 'act': nc.scalar, 'dve': nc.vector, 'pool': nc.gpsimd, 'sp': nc.sync}
        self.sems = {}
        self.val = {}
        for e in ['pe', 'act', 'dve', 'pool']:
            self.sems[e] = es.enter_context(nc.semaphore('s_' + e))
            self.val[e] = 0
        self.dq = {}
        for q in ['sp', 'pool', 'act']:
            ids = []
            for i in range(ndma):
                sid = ('d', q, i)
                self.sems[sid] = es.enter_context(nc.semaphore(f'd_{q}_{i}'))
                self.val[sid] = 0
                ids.append(sid)
            self.dq[q] = [ids, 0]
        self.seen = {e: {} for e in self.eng}
        self.lastw = {}
        self.readers = {}
        self.n_inst = 0

    def _waits(self, eng, reads, writes, extra=()):
        need = {}

        def add(ev):
            if ev is None:
                return
            sid, v = ev
            if need.get(sid, 0) < v:
                need[sid] = v
        for r in reads:
            add(self.lastw.get(r))
        for w in writes:
            add(self.lastw.get(w))
            for ev in self.readers.get(w, {}).items():
                add(ev)
        for ev in extra:
            add(ev)
        e = self.eng[eng]
        for sid, v in need.items():
            if sid == 'pe' and eng == 'pe':
                continue
            if self.seen[eng].get(sid, 0) >= v:
                continue
            self.seen[eng][sid] = v
            e.wait_ge(self.sems[sid], v)
            self.n_inst += 1

    def _record(self, ev, reads, writes):
        sid, v = ev
        for w in writes:
            self.lastw[w] = ev
            self.readers[w] = {}
        for r in reads:
            d = self.readers.setdefault(r, {})
            if d.get(sid, 0) < v:
                d[sid] = v

    def I(self, eng, reads, writes, fn):
        self._waits(eng, reads, writes)
        ins = fn(self.eng[eng])
        self.val[eng] += 1
        ins.then_inc(self.sems[eng], 1)
        self._record((eng, self.val[eng]), reads, writes)
        self.n_inst += 1
        return ins

    def DMA(self, q, reads, writes, fn):
        ids, rr = self.dq[q]
        sid = ids[rr % len(ids)]
        self.dq[q][1] = rr + 1
        self._waits(q, reads, writes, extra=[(sid, self.val[sid])] if self.val[sid] else ())
        ins = fn(self.eng[q])
        self.val[sid] += 16
        ins.then_inc(self.sems[sid], 16)
        self._record((sid, self.val[sid]), reads, writes)
        self.n_inst += 1
        return ins

    def barrier(self):
        for eng in self.eng:
            e = self.eng[eng]
            for sid, v in self.val.items():
                if v == 0 or self.seen[eng].get(sid, 0) >= v:
                    continue
                if sid == eng:
                    continue
                self.seen[eng][sid] = v
                e.wait_ge(self.sems[sid], v)
                self.n_inst += 1

    def finish(self):
        e = self.eng['sp']
        for sid, v in self.val.items():
            if v and self.seen['sp'].get(sid, 0) < v:
                e.wait_ge(self.sems[sid], v)


class Ring:
    def __init__(self, items):
        self.items = items
        self.i = 0

    def next(self):
        it = self.items[self.i % len(self.items)]
        self.i += 1
        return it


def bcast_rows(ap_1d_row, nparts):
    return ap_1d_row.to_broadcast([nparts, ap_1d_row.shape[-1]])


def build_program(stop_after=None, dbg=None):
    nc = bass.Bass("TRN2", target_bir_lowering=False)
    es = ExitStack()
    kb = KB(nc, es)
    dbg = dbg or {}

    def din(name, shape, dt=F32):
        return nc.dram_tensor(name, list(shape), dt, kind="ExternalInput").ap()

    def dscr(name, shape, dt=F32):
        return nc.dram_tensor(name, list(shape), dt, kind="Internal").ap()

    def dout(name, shape, dt=F32):
        return nc.dram_tensor(name, list(shape), dt, kind="ExternalOutput").ap()

    uid = [0]

    def sb(name, shape, dt=F32, stack=es):
        uid[0] += 1
        return stack.enter_context(nc.sbuf_tensor(f"{name}_u{uid[0]}", list(shape), dt))

    xin = din("xin", [NB, LT, D])
    c3T = din("c3T", [128, KC, 3])
    ada_w = din("ada_w", [2, KC, 128, 6 * D])
    ada_b = din("ada_b", [2, 1, 6 * D])
    norms = din("norms", [5, 1, D])
    ident_d = din("ident", [128, 128])
    modsD = dscr("modsD", [2, 3, 6 * D])
    out_d = dout("out", [NB, LL, D])
    dbg_out = {}
    for k, shp in dbg.items():
        dbg_out[k] = dout("dbg_" + k, shp)

    ident = sb("ident_sb", [128, 128])
    ps = [es.enter_context(nc.psum_tensor(f"ps{i}", [128, 512], F32)) for i in range(8)]
    psk = [f"ps{i}" for i in range(8)]
    kb.DMA('sp', [], ['ident'], lambda e: e.dma_start(out=ident[:], in_=ident_d[:, :]))
    epsc = sb("epsc", [128, 1])
    kb.I('dve', [], ['epsc'], lambda e: e.memset(epsc[:], EPS))

    with ExitStack() as ph:
        sT = sb("sT", [128, KC, 3], stack=ph)
        kb.DMA('sp', [], ['sT'], lambda e: e.dma_start(out=sT[:], in_=c3T[:, :, :]))
        kb.I('act', ['sT'], ['sT'], lambda e: e.activation(out=sT[:], in_=sT[:], func=AF.Silu))
        wbuf = [sb(f"adaw{i}", [128, 3072], stack=ph) for i in range(3)]
        wring = Ring(list(range(3)))
        bias_t = sb("adab", [3, 6 * D], stack=ph)
        mods_t = sb("mods_t", [3, 6 * D], stack=ph)
        for l in range(2):
            kb.DMA('sp', ['mods_st'], ['adab'], lambda e: e.dma_start(out=bias_t[:], in_=ada_b[l].to_broadcast([3, 6 * D])))
            for half in range(2):
                for k in range(KC):
                    wi = wring.next()
                    kb.DMA('sp', [], [f'adaw{wi}'], lambda e: e.dma_start(out=wbuf[wi][:], in_=ada_w[l, k, :, half * 3072:(half + 1) * 3072]))
                    for j in range(6):
                        kb.I('pe', ['sT', f'adaw{wi}'], [psk[j]], lambda e: e.matmul(ps[j][0:3, :], lhsT=sT[:, k, :], rhs=wbuf[wi][:, j * 512:(j + 1) * 512], start=(k == 0), stop=(k == KC - 1)))
                for j in range(6):
                    c0 = half * 3072 + j * 512
                    kb.I('dve', [psk[j], 'adab'], ['mods_t'], lambda e: e.tensor_tensor(out=mods_t[:, c0:c0 + 512], in0=ps[j][0:3, :], in1=bias_t[:, c0:c0 + 512], op=ALU.add))
            kb.DMA('sp', ['mods_t'], [('modsD', l), 'mods_st'], lambda e: e.dma_start(out=modsD[l], in_=mods_t[:]))
    kb.barrier()
    if 'mods' in dbg_out:
        kb.DMA('sp', [('modsD', 0), ('modsD', 1)], ['dbg_mods'], lambda e: e.dma_start(out=dbg_out['mods'], in_=modsD))
    if stop_after == 'mods':
        kb.finish(); es.close(); return nc

    ev_w_in = din("ev_w_in", [128, KC, 3088])
    ev_w_out = din("ev_w_out", [128, KC, D])
    a_convT = din("a_convT", [1536, 4])
    a_gc = din("a_gc", [1, 16])
    a_norm = din("a_norm", [1, 128])
    lru_pc = din("lru_pc", [512, 11])
    lru_w = din("lru_w", [2, 2, 4, 128, 128])
    masks_d = din("masks", [6, 128, 128])
    mixT = dscr("mixT", [NB, KC, 128, LT])
    l1only = (stop_after or '').startswith('l1only')
    xs = din("xs_in", [NB, LT, D]) if l1only else dscr("xs", [NB, LT, D])

    ones = sb("ones_sb", [128, 128])
    kb.I('dve', [], ['ones'], lambda e: e.memset(ones[:], 1.0))
    onec = sb("onec", [128, 1])
    kb.I('dve', [], ['onec'], lambda e: e.memset(onec[:], 1.0))
    msk = sb("msk", [128, 6, 128])
    for i in range(6):
        kb.DMA('sp', [], ['msk'], lambda e: e.dma_start(out=msk[:, i, :], in_=masks_d[i]))
    psr = Ring(list(range(8)))

    def mm(out, lhsT, rhs, reads, writes, start=True, stop=True):
        kb.I('pe', reads, writes, lambda e: e.matmul(out, lhsT=lhsT, rhs=rhs, start=start, stop=stop))

    def tr(out, in_, reads, writes):
        kb.I('pe', reads + ['ident'], writes, lambda e: e.transpose(out=out, in_=in_, identity=ident[:]))

    TOKBLKS = [(0, 256)] + [(256 + i * 512, 512) for i in range(4)]

    def norm_mod_T(ph, l, b, src_ap_fn, norm_idx, slot_sh, slot_sc, hT, keep_h=None, tiles=range(NT)):
        with ExitStack() as st:
            nw = sb("nm_nw", [128, D], stack=st)
            kb.DMA('sp', [], ['nm_nw'], lambda e: e.dma_start(out=nw[:], in_=norms[norm_idx].to_broadcast([128, D])))
            md = {}
            for kind, row in (('c', 2), ('l', b)):
                A = sb("nm_A" + kind, [128, D], stack=st)
                S = sb("nm_S" + kind, [128, D], stack=st)
                kb.DMA('sp', [('modsD', l)], ['nm_A' + kind], lambda e: e.dma_start(out=A[:], in_=modsD[l, row:row + 1, slot_sc * D:(slot_sc + 1) * D].to_broadcast([128, D])))
                kb.DMA('sp', [('modsD', l)], ['nm_S' + kind], lambda e: e.dma_start(out=S[:], in_=modsD[l, row:row + 1, slot_sh * D:(slot_sh + 1) * D].to_broadcast([128, D])))
                kb.I('dve', ['nm_A' + kind, 'nm_nw'], ['nm_A' + kind], lambda e: e.scalar_tensor_tensor(out=A[:], in0=A[:], scalar=1.0, in1=nw[:], op0=ALU.add, op1=ALU.mult))
                md[kind] = (A, S)
            xt = [sb(f"nm_x{i}", [128, D], stack=st) for i in range(2)]
            ht = [sb(f"nm_h{i}", [128, D], stack=st) for i in range(2)]
            sq = sb("nm_sq", [128, D], stack=st)
            ss = [sb(f"nm_ss{i}", [128, 1], stack=st) for i in range(2)]
            for t in tiles:
                i = t % 2
                kind = 'c' if t < 2 else 'l'
                A, S = md[kind]
                src, srck = src_ap_fn(b, t)
                kb.DMA('sp', [srck], [f'nm_x{i}'], lambda e: e.dma_start(out=xt[i][:], in_=src))
                kb.I('act', [f'nm_x{i}'], ['nm_sq', f'nm_ss{i}'], lambda e: e.activation(out=sq[:], in_=xt[i][:], func=AF.Square, accum_out=ss[i][:]))
                kb.I('act', [f'nm_ss{i}', 'epsc'], [f'nm_ss{i}'], lambda e: e.activation(out=ss[i][:], in_=ss[i][:], func=AF.Sqrt, scale=1.0 / D, bias=epsc[:]))
                kb.I('dve', [f'nm_ss{i}'], [f'nm_ss{i}'], lambda e: e.reciprocal(out=ss[i][:], in_=ss[i][:]))
                kb.I('dve', [f'nm_x{i}', f'nm_ss{i}', 'nm_A' + kind], [f'nm_h{i}'], lambda e: e.scalar_tensor_tensor(out=ht[i][:], in0=xt[i][:], scalar=ss[i][:], in1=A[:], op0=ALU.mult, op1=ALU.mult))
                kb.I('dve', [f'nm_h{i}', 'nm_S' + kind], [f'nm_h{i}'], lambda e: e.tensor_tensor(out=ht[i][:], in0=ht[i][:], in1=S[:], op=ALU.add))
                if keep_h is not None:
                    keep_h(t, ht[i], f'nm_h{i}')
                for half in range(2):
                    pb = psr.next()
                    for kk in range(4):
                        k = half * 4 + kk
                        tr(ps[pb][:, kk * 128:(kk + 1) * 128], ht[i][:, k * 128:(k + 1) * 128], [f'nm_h{i}'], [psk[pb]])
                    kb.I('act', [psk[pb]], [('hT', t)], lambda e: e.activation(out=hT[:, half * 4:half * 4 + 4, t * 128:(t + 1) * 128], in_=ps[pb][:, :].rearrange("p (k t) -> p k t", k=4), func=AF.Copy))
            kb.barrier()

    def proj_fm(hT, w_sb, wkey, dst_fn, evac):
        for (t0, n) in TOKBLKS:
            pb = psr.next()
            for k in range(KC):
                mm(ps[pb][:, 0:n], w_sb[:, k, :], hT[:, k, t0:t0 + n], [wkey] + [('hT', tt) for tt in range(t0 // 128, (t0 + n) // 128)], [psk[pb]], start=(k == 0), stop=(k == KC - 1))
            evac(pb, t0, n)

    def layer0_mixer(b):
        with ExitStack() as ph:
            hT = sb("hT", [128, KC, LT], stack=ph)
            norm_mod_T(ph, 0, b, lambda b_, t: (xin[b_, t * 128:(t + 1) * 128, :], 'xin'), 0, 0, 1, hT)
            if 'hT0' in dbg_out and b == 0:
                kb.DMA('sp', [('hT', t) for t in range(NT)], ['dbg_hT0'], lambda e: e.dma_start(out=dbg_out['hT0'], in_=hT[:]))
            if stop_after == 'hT0':
                return
            with ExitStack() as st:
                wx = sb("l_wx", [128, KC, 128], stack=st)
                wg = sb("l_wg", [128, KC, 128], stack=st)
                pc = sb("l_pc", [128, 11], stack=st)
                cl = sb("l_cl", [128, 4], stack=st)
                gw = sb("l_gw", [128, 4, 128], stack=st)
                raw = sb("l_raw", [128, LT + 6], stack=st)
                xb = sb("l_xb", [128, LT], stack=st)
                gg = sb("l_gg", [128, LT], stack=st)
                t1 = sb("l_t1", [128, LT], stack=st)
                t2 = sb("l_t2", [128, LT], stack=st)
                av = sb("l_a", [128, LT], stack=st)
                bv = sb("l_b", [128, LT], stack=st)
                hf = sb("l_hf", [128, LT], stack=st)
                hb = sb("l_hb", [128, LT], stack=st)
                kb.I('dve', [], ['l_raw'], lambda e: e.memset(raw[:], 0.0))
                RC, RL = 0, LC + 3

                def rawpos(t0):
                    return (RC + 1 + t0) if t0 < LC else (RL + 1 + (t0 - LC))
                for ct in range(4):
                    c0 = 2064 + ct * 128
                    kb.DMA('sp', [], ['l_wx'], lambda e: e.dma_start(out=wx[:], in_=ev_w_in[:, :, c0:c0 + 128]))
                    kb.DMA('sp', [], ['l_wg'], lambda e: e.dma_start(out=wg[:], in_=ev_w_in[:, :, c0 + 512:c0 + 640]))
                    kb.DMA('sp', [], ['l_pc'], lambda e: e.dma_start(out=pc[:], in_=lru_pc[ct * 128:(ct + 1) * 128, :]))
                    for ax in range(2):
                        for d in range(2):
                            kb.DMA('sp', [], ['l_gw'], lambda e: e.dma_start(out=gw[:, ax * 2 + d, :], in_=lru_w[ax, d, ct]))
                    for d in range(2):
                        kb.I('act', ['l_pc'], ['l_cl'], lambda e: e.activation(out=cl[:, d:d + 1], in_=pc[:, 7 + 3 * d:8 + 3 * d], func=AF.Exp, scale=-1.0))
                    kb.I('act', ['l_cl', 'onec'], ['l_cl'], lambda e: e.activation(out=cl[:, 0:2], in_=cl[:, 0:2], func=AF.Ln, bias=onec[:]))
                    kb.I('dve', ['l_cl'], ['l_cl'], lambda e: e.tensor_scalar(out=cl[:, 2:4], in0=cl[:, 0:2], scalar1=-16.0, scalar2=None, op0=ALU.mult))
                    kb.I('dve', ['l_cl'], ['l_cl'], lambda e: e.tensor_scalar(out=cl[:, 0:2], in0=cl[:, 0:2], scalar1=-8.0, scalar2=None, op0=ALU.mult))
                    proj_fm(hT, wx, 'l_wx', None, lambda pb, t0, n: kb.I('act', [psk[pb]], ['l_raw'], lambda e: e.activation(out=raw[:, rawpos(t0):rawpos(t0) + n], in_=ps[pb][:, 0:n], func=AF.Copy)))
                    for (o0, r0, L) in ((0, RC, LC), (LC, RL, LL)):
                        kb.I('dve', ['l_raw', 'l_pc'], ['l_xb'], lambda e: e.tensor_scalar(out=xb[:, o0:o0 + L], in0=raw[:, r0:r0 + L], scalar1=pc[:, 0:1], scalar2=pc[:, 4:5], op0=ALU.mult, op1=ALU.add))
                        for j in range(1, 4):
                            kb.I('dve', ['l_raw', 'l_pc', 'l_xb'], ['l_xb'], lambda e: e.scalar_tensor_tensor(out=xb[:, o0:o0 + L], in0=raw[:, r0 + j:r0 + j + L], scalar=pc[:, j:j + 1], in1=xb[:, o0:o0 + L], op0=ALU.mult, op1=ALU.add))
                    proj_fm(hT, wg, 'l_wg', None, lambda pb, t0, n: kb.I('act', [psk[pb]], ['l_t1'], lambda e: e.activation(out=t1[:, t0:t0 + n], in_=ps[pb][:, 0:n], func=AF.Copy)))
                    kb.I('dve', ['l_t1'], ['l_t2'], lambda e: e.tensor_tensor(out=t2[:], in0=t1[:], in1=t1[:], op=ALU.mult))
                    kb.I('dve', ['l_t2'], ['l_t2'], lambda e: e.tensor_scalar(out=t2[:], in0=t2[:], scalar1=0.044715, scalar2=1.0, op0=ALU.mult, op1=ALU.add))
                    kb.I('dve', ['l_t2', 'l_t1'], ['l_t2'], lambda e: e.tensor_tensor(out=t2[:], in0=t2[:], in1=t1[:], op=ALU.mult))
                    kb.I('act', ['l_t2'], ['l_t2'], lambda e: e.activation(out=t2[:], in_=t2[:], func=AF.Sigmoid, scale=1.5957691216057308))
                    kb.I('dve', ['l_t2', 'l_t1'], ['l_gg'], lambda e: e.tensor_tensor(out=gg[:], in0=t2[:], in1=t1[:], op=ALU.mult))
                    for d in range(2):
                        for (t0, n) in TOKBLKS:
                            for ax, dst, dk_ in ((0, t1, 'l_t1'), (1, t2, 'l_t2')):
                                pb = psr.next()
                                mm(ps[pb][:, 0:n], gw[:, ax * 2 + d, :], xb[:, t0:t0 + n], ['l_gw', 'l_xb'], [psk[pb]])
                                bcol = 5 + 3 * d + ax
                                kb.I('act', [psk[pb], 'l_pc'], [dk_], lambda e: e.activation(out=dst[:, t0:t0 + n], in_=ps[pb][:, 0:n], func=AF.Sigmoid, bias=pc[:, bcol:bcol + 1]))
                        kb.I('act', ['l_t1', 'l_cl'], ['l_a'], lambda e: e.activation(out=av[:], in_=t1[:], func=AF.Exp, scale=cl[:, d:d + 1]))
                        kb.I('act', ['l_t1', 'l_cl'], ['l_b'], lambda e: e.activation(out=bv[:], in_=t1[:], func=AF.Exp, scale=cl[:, 2 + d:3 + d]))
                        kb.I('dve', ['l_b'], ['l_b'], lambda e: e.tensor_scalar(out=bv[:], in0=bv[:], scalar1=-1.0, scalar2=1.0, op0=ALU.mult, op1=ALU.add))
                        kb.I('dve', ['l_b'], ['l_b'], lambda e: e.tensor_scalar(out=bv[:], in0=bv[:], scalar1=0.0, scalar2=None, op0=ALU.max))
                        kb.I('act', ['l_b'], ['l_b'], lambda e: e.activation(out=bv[:], in_=bv[:], func=AF.Sqrt))
                        kb.I('dve', ['l_b', 'l_t2'], ['l_b'], lambda e: e.tensor_tensor(out=bv[:], in0=bv[:], in1=t2[:], op=ALU.mult))
                        kb.I('dve', ['l_b', 'l_xb'], ['l_b'], lambda e: e.tensor_tensor(out=bv[:], in0=bv[:], in1=xb[:], op=ALU.mult))
                        if d == 0:
                            kb.I('dve', ['l_a', 'l_b'], ['l_hf'], lambda e: e.tensor_tensor_scan(out=hf[:, :], data0=av[:, :], data1=bv[:, :], initial=0.0, op0=ALU.mult, op1=ALU.add))
                        else:
                            kb.I('dve', ['l_a', 'l_b'], ['l_hb'], lambda e: e.tensor_tensor_scan(out=hb[:, LC - 1::-1], data0=av[:, LC - 1::-1], data1=bv[:, LC - 1::-1], initial=0.0, op0=ALU.mult, op1=ALU.add))
                            kb.I('dve', ['l_a', 'l_b', 'l_hb'], ['l_hb'], lambda e: e.tensor_tensor_scan(out=hb[:, LT - 1:LC - 1:-1], data0=av[:, LT - 1:LC - 1:-1], data1=bv[:, LT - 1:LC - 1:-1], initial=hb[:, 0:1], op0=ALU.mult, op1=ALU.add))
                    kb.I('dve', ['l_hf', 'l_hb'], ['l_hf'], lambda e: e.tensor_tensor(out=hf[:], in0=hf[:], in1=hb[:], op=ALU.add))
                    kb.I('dve', ['l_hf', 'l_gg'], ['l_hf'], lambda e: e.tensor_tensor(out=hf[:], in0=hf[:], in1=gg[:], op=ALU.mult))
                    kb.DMA('sp', ['l_hf'], [('mixT', b)], lambda e: e.dma_start(out=mixT[b, 4 + ct], in_=hf[:]))
                kb.barrier()
            if stop_after == 'lru':
                return
            gdn(ph, b, hT)
            kb.barrier()

    def gdn(ph, b, hT):
        with ExitStack() as st:
            wg16 = sb("g_wg16", [128, KC, 16], stack=st)
            cw = sb("g_cw", [128, 3, 4], stack=st)
            gcst = sb("g_gcst", [128, 16], stack=st)
            anw = sb("g_anw", [128, 128], stack=st)
            T3 = [sb(f"g_T{i}", [128, LT], stack=st) for i in range(3)]
            tmp = sb("g_tmp", [128, LT], stack=st)
            Ktm = sb("g_Ktm", [128, NT, 128], stack=st)
            Vtm = sb("g_Vtm", [128, NT, 128], stack=st)
            OKEYS = [('g_O', n_) for n_ in range(NT)]
            O = sb("g_O", [128, NT, 128], stack=st)
            graw = sb("g_graw", [128, NT, 16], stack=st)
            gG = sb("g_g", [128, NT, 8], stack=st)
            gB = sb("g_bt", [128, NT, 8], stack=st)
            RC, RL = 0, LC + 3

            def rawpos(t0):
                return (RC + 1 + t0) if t0 < LC else (RL + 1 + (t0 - LC))
            kb.DMA('sp', [], ['g_gcst'], lambda e: e.dma_start(out=gcst[:], in_=a_gc[0:1, :].to_broadcast([128, 16])))
            kb.DMA('sp', [], ['g_anw'], lambda e: e.dma_start(out=anw[:], in_=a_norm[0:1, :].to_broadcast([128, 128])))
            kb.I('act', ['g_gcst'], ['g_gcst'], lambda e: e.activation(out=gcst[:, 0:8], in_=gcst[:, 0:8], func=AF.Exp))
            kb.I('dve', ['g_gcst'], ['g_gcst'], lambda e: e.tensor_scalar(out=gcst[:, 0:8], in0=gcst[:, 0:8], scalar1=-1.0, scalar2=None, op0=ALU.mult))
            kb.DMA('sp', [], ['g_wg16'], lambda e: e.dma_start(out=wg16[:], in_=ev_w_in[:, :, 2048:2064]))
            pb = psr.next()
            for n in range(NT):
                for k in range(KC):
                    mm(ps[pb][:, n * 16:(n + 1) * 16], hT[:, k, n * 128:(n + 1) * 128], wg16[:, k, :], ['g_wg16', ('hT', n)], [psk[pb]], start=(k == 0), stop=(k == KC - 1))
            kb.I('act', [psk[pb]], ['g_graw'], lambda e: e.activation(out=graw[:], in_=ps[pb][:, 0:NT * 16].rearrange("p (n c) -> p n c", c=16), func=AF.Copy))
            kb.I('dve', ['g_graw', 'g_gcst'], ['g_g'], lambda e: e.tensor_tensor(out=gG[:], in0=graw[:, :, 0:8], in1=gcst[:, 8:16].unsqueeze(1).to_broadcast([128, NT, 8]), op=ALU.add))
            kb.I('act', ['g_g'], ['g_g'], lambda e: e.activation(out=gG[:], in_=gG[:], func=AF.Exp))
            kb.I('act', ['g_g', 'onec'], ['g_g'], lambda e: e.activation(out=gG[:], in_=gG[:], func=AF.Ln, bias=onec[:]))
            kb.I('dve', ['g_g', 'g_gcst'], ['g_g'], lambda e: e.tensor_tensor(out=gG[:], in0=gG[:], in1=gcst[:, 0:8].unsqueeze(1).to_broadcast([128, NT, 8]), op=ALU.mult))
            kb.I('act', ['g_graw'], ['g_bt'], lambda e: e.activation(out=gB[:], in_=graw[:, :, 8:16], func=AF.Sigmoid))
            if 'gates' in dbg_out and b == 0:
                kb.DMA('sp', ['g_g'], ['dbg_gates'], lambda e: e.dma_start(out=dbg_out['gates'][:, :, 0:8], in_=gG[:]))
                kb.DMA('sp', ['g_bt'], ['dbg_gates'], lambda e: e.dma_start(out=dbg_out['gates'][:, :, 8:16], in_=gB[:]))

            for hd in range(4):
                with ExitStack() as hs:
                    wh = sb("g_wh", [128, 4, KC, 128], stack=hs)
                    raw = sb("g_raw", [128, LT + 6], stack=hs)
                    kb.I('dve', [], ['g_raw'], lambda e: e.memset(raw[:], 0.0))
                    for part in range(4):
                        c0 = part * 512 + hd * 128
                        kb.DMA('sp', [], ['g_wh'], lambda e: e.dma_start(out=wh[:, part], in_=ev_w_in[:, :, c0:c0 + 128]))
                    for part in range(3):
                        c0 = part * 512 + hd * 128
                        kb.DMA('sp', [], ['g_cw'], lambda e: e.dma_start(out=cw[:, part, :], in_=a_convT[c0:c0 + 128, :]))
                    for part in range(3):
                        Tp, tk = T3[part], f'g_T{part}'
                        proj_fm(hT, wh[:, part], 'g_wh', None, lambda pb, t0, n: kb.I('act', [psk[pb]], ['g_raw'], lambda e: e.activation(out=raw[:, rawpos(t0):rawpos(t0) + n], in_=ps[pb][:, 0:n], func=AF.Copy)))
                        for (o0, r0, L) in ((0, RC, LC), (LC, RL, LL)):
                            kb.I('dve', ['g_raw', 'g_cw'], ['g_tmp'], lambda e: e.tensor_scalar(out=tmp[:, o0:o0 + L], in0=raw[:, r0:r0 + L], scalar1=cw[:, part, 0:1], scalar2=None, op0=ALU.mult))
                            for j in range(1, 4):
                                kb.I('dve', ['g_raw', 'g_cw', 'g_tmp'], ['g_tmp'], lambda e: e.scalar_tensor_tensor(out=tmp[:, o0:o0 + L], in0=raw[:, r0 + j:r0 + j + L], scalar=cw[:, part, j:j + 1], in1=tmp[:, o0:o0 + L], op0=ALU.mult, op1=ALU.add))
                        kb.I('act', ['g_tmp'], [tk], lambda e: e.activation(out=Tp[:], in_=tmp[:], func=AF.Silu))
                        if part < 2:
                            kb.I('act', [tk], ['g_tmp'], lambda e: e.activation(out=tmp[:], in_=Tp[:], func=AF.Square))
                            for (t0, n) in TOKBLKS:
                                pb = psr.next()
                                mm(ps[pb][:, 0:n], ones[:], tmp[:, t0:t0 + n], ['ones', 'g_tmp'], [psk[pb]])
                                kb.I('act', [psk[pb], 'epsc', 'g_tmp'], ['g_tmp'], lambda e: e.activation(out=tmp[:, t0:t0 + n], in_=ps[pb][:, 0:n], func=AF.Sqrt, bias=epsc[:]))
                            kb.I('dve', ['g_tmp'], ['g_tmp'], lambda e: e.reciprocal(out=tmp[:], in_=tmp[:]))
                            sc_ = (128 ** -0.5) if part == 0 else 1.0
                            kb.I('dve', [tk, 'g_tmp'], [tk], lambda e: e.scalar_tensor_tensor(out=Tp[:], in0=Tp[:], scalar=sc_, in1=tmp[:], op0=ALU.mult, op1=ALU.mult))
                    for src, dst, dk_ in ((T3[1], Ktm, 'g_Ktm'), (T3[2], Vtm, 'g_Vtm')):
                        sk = 'g_T1' if dst is Ktm else 'g_T2'
                        for n0 in range(0, NT, 4):
                            pb = psr.next()
                            nn = min(4, NT - n0)
                            for i in range(nn):
                                tr(ps[pb][:, i * 128:(i + 1) * 128], src[:, (n0 + i) * 128:(n0 + i + 1) * 128], [sk], [psk[pb]])
                            kb.I('act', [psk[pb]], [dk_], lambda e: e.activation(out=dst[:, n0:n0 + nn, :], in_=ps[pb][:, 0:nn * 128].rearrange("p (n c) -> p n c", c=128), func=AF.Copy))
                    zs = T3[2][:, :].rearrange("p (n c) -> p n c", c=128)
                    for n0 in range(0, NT, 4):
                        pb = psr.next()
                        nn = min(4, NT - n0)
                        for i in range(nn):
                            n = n0 + i
                            for k in range(KC):
                                mm(ps[pb][:, i * 128:(i + 1) * 128], hT[:, k, n * 128:(n + 1) * 128], wh[:, 3, k, :], ['g_wh', ('hT', n)], [psk[pb]], start=(k == 0), stop=(k == KC - 1))
                        kb.I('act', [psk[pb]], ['g_T2'], lambda e: e.activation(out=zs[:, n0:n0 + nn, :], in_=ps[pb][:, 0:nn * 128].rearrange("p (n c) -> p n c", c=128), func=AF.Silu))
                    kb.barrier()
                gdn_chains(st, b, hd, T3, Ktm, Vtm, gG, gB, O)
                ssq = sb(f"g_ssq{hd}", [128, NT], stack=st)
                O2 = tmp[:, :].rearrange("p (n c) -> p n c", c=128)
                kb.I('dve', OKEYS, ['g_tmp'], lambda e: e.tensor_tensor(out=O2, in0=O[:], in1=O[:], op=ALU.mult))
                kb.I('dve', ['g_tmp'], ['g_ssq'], lambda e: e.tensor_reduce(out=ssq[:], in_=O2, axis=AX.X, op=ALU.add))
                kb.I('act', ['g_ssq', 'epsc'], ['g_ssq'], lambda e: e.activation(out=ssq[:], in_=ssq[:], func=AF.Sqrt, scale=1.0 / 128, bias=epsc[:]))
                kb.I('dve', ['g_ssq'], ['g_ssq'], lambda e: e.reciprocal(out=ssq[:], in_=ssq[:]))
                kb.I('dve', OKEYS + ['g_ssq'], OKEYS, lambda e: e.tensor_tensor(out=O[:], in0=O[:], in1=ssq[:].unsqueeze(2).to_broadcast([128, NT, 128]), op=ALU.mult))
                kb.I('dve', OKEYS + ['g_anw'], OKEYS, lambda e: e.tensor_tensor(out=O[:], in0=O[:], in1=anw[:].unsqueeze(1).to_broadcast([128, NT, 128]), op=ALU.mult))
                kb.I('dve', OKEYS + ['g_T2'], OKEYS, lambda e: e.tensor_tensor(out=O[:], in0=O[:], in1=zs, op=ALU.mult))
                for n0 in range(0, NT, 4):
                    pb = psr.next()
                    nn = min(4, NT - n0)
                    for i in range(nn):
                        tr(ps[pb][:, i * 128:(i + 1) * 128], O[:, n0 + i, :], OKEYS, [psk[pb]])
                    kb.I('act', [psk[pb]], ['g_tmp'], lambda e: e.activation(out=tmp[:, n0 * 128:(n0 + nn) * 128], in_=ps[pb][:, 0:nn * 128], func=AF.Copy))
                kb.DMA('sp', ['g_tmp'], [('mixT', b)], lambda e: e.dma_start(out=mixT[b, hd], in_=tmp[:]))
            kb.barrier()

    def gdn_chains(st0, b, hd, T3, Ktm, Vtm, gG, gB, O):
        QT, KT = T3[0], T3[1]
        OK = [('g_O', n_) for n_ in range(NT)]
        with ExitStack() as st:
            names = ["Tg", "dB", "Ei", "EmI", "tt", "Ya", "Yb", "YTa", "YTb", "Ra", "Rb", "qkm", "eg", "qd", "rv", "rk", "U", "WT", "Kd", "vn"]
            sets = {}
            for d_ in range(2):
                for p_ in range(2):
                    B = {nm: sb(f"c_{nm}{d_}{p_}", [128, 128], stack=st) for nm in names}
                    B["cs"] = sb(f"c_cs{d_}{p_}", [128, 8], stack=st)
                    sets[(d_, p_)] = B
            S = {d_: [sb(f"c_S{d_}{i}", [128, 128], stack=st) for i in range(2)] for d_ in range(2)}
            kb.I('dve', [], OK, lambda e: e.memset(O[:], 0.0))
            for d_ in range(2):
                kb.I('dve', [], [f'c_S{d_}0'], lambda e: e.memset(S[d_][0][:], 0.0))
            done = {0: 0, 1: 0}

            def gen(d, p):
                col = d * 4 + hd
                order = ([0, 1] + list(range(2, NT))) if d == 0 else ([1, 0] + list(range(NT - 1, 1, -1)))
                B = sets[(d, p)]
                tag = f"{d}{p}"
                K_ = lambda nm: f"c_{nm}{tag}"
                cs = B["cs"]
                banks = [2 * (2 * d + p), 2 * (2 * d + p) + 1]
                bi = [0]

                def nb():
                    x = banks[bi[0] % 2]
                    bi[0] += 1
                    return x
                for k in range(p, NT, 2):
                    n = order[k]
                    gcol = gG[:, n, col:col + 1]
                    bcol = gB[:, n, col:col + 1]
                    tok = slice(n * 128, (n + 1) * 128)
                    kb.I('dve', ['g_g', 'msk'], [K_('Tg')], lambda e: e.tensor_scalar(out=B["Tg"][:], in0=msk[:, d, :], scalar1=gcol, scalar2=None, op0=ALU.mult)); yield
                    kb.I('dve', ['g_bt', 'ident'], [K_('dB')], lambda e: e.tensor_scalar(out=B["dB"][:], in0=ident[:], scalar1=bcol, scalar2=None, op0=ALU.mult)); yield
                    pa = nb()
                    mm(ps[pa][:, 0:128], ones[:], B["Tg"][:], ['ones', K_('Tg')], [psk[pa]]); yield
                    mm(ps[pa][:, 128:256], msk[:, 4 + d, :], B["dB"][:], ['msk', K_('dB')], [psk[pa]]); yield
                    mm(ps[pa][:, 256:257], msk[:, d, :], gcol, ['msk', 'g_g'], [psk[pa]]); yield
                    mm(ps[pa][:, 257:258], ones[:], gcol, ['ones', 'g_g'], [psk[pa]]); yield
                    kb.I('act', [psk[pa]], [K_('cs')], lambda e: e.activation(out=cs[:, 0:2], in_=ps[pa][:, 256:258], func=AF.Copy)); yield
                    kb.I('act', [K_('cs')], [K_('cs')], lambda e: e.activation(out=cs[:, 2:3], in_=cs[:, 0:1], func=AF.Exp)); yield
                    kb.I('dve', [K_('cs'), 'g_bt'], [K_('cs')], lambda e: e.tensor_tensor(out=cs[:, 3:4], in0=cs[:, 2:3], in1=bcol, op=ALU.mult)); yield
                    kb.I('act', [K_('cs')], [K_('cs')], lambda e: e.activation(out=cs[:, 4:5], in_=cs[:, 0:1], func=AF.Exp, scale=-1.0, bias=cs[:, 1:2])); yield
                    kb.I('act', [K_('cs')], [K_('cs')], lambda e: e.activation(out=cs[:, 5:6], in_=cs[:, 1:2], func=AF.Exp)); yield
                    kb.I('dve', [psk[pa], K_('cs')], [K_('Ei')], lambda e: e.tensor_scalar(out=B["Ei"][:], in0=ps[pa][:, 0:128], scalar1=cs[:, 0:1], scalar2=0.0, op0=ALU.subtract, op1=ALU.min)); yield
                    kb.I('act', [K_('Ei')], [K_('Ei')], lambda e: e.activation(out=B["Ei"][:], in_=B["Ei"][:], func=AF.Exp)); yield
                    kb.I('act', [psk[pa]], [K_('eg')], lambda e: e.activation(out=B["eg"][:], in_=ps[pa][:, 0:128], func=AF.Exp)); yield
                    kb.I('dve', [K_('Ei'), 'msk'], [K_('EmI')], lambda e: e.tensor_tensor(out=B["EmI"][:], in0=B["Ei"][:], in1=msk[:, 2 + d, :], op=ALU.mult)); yield
                    kb.I('dve', [K_('Ei'), psk[pa]], [K_('tt')], lambda e: e.tensor_tensor(out=B["tt"][:], in0=B["Ei"][:], in1=ps[pa][:, 128:256], op=ALU.mult)); yield
                    pq = nb()
                    mm(ps[pq][:, 0:128], KT[:, tok], KT[:, tok], ['g_T1'], [psk[pq]]); yield
                    mm(ps[pq][:, 128:256], KT[:, tok], QT[:, tok], ['g_T1', 'g_T0'], [psk[pq]]); yield
                    kb.I('dve', [psk[pq], K_('tt')], [K_('Ya')], lambda e: e.scalar_tensor_tensor(out=B["Ya"][:], in0=ps[pq][:, 0:128], scalar=-1.0, in1=B["tt"][:], op0=ALU.mult, op1=ALU.mult)); yield
                    kb.I('dve', [psk[pq], K_('EmI')], [K_('qkm')], lambda e: e.tensor_tensor(out=B["qkm"][:], in0=ps[pq][:, 128:256], in1=B["EmI"][:], op=ALU.mult)); yield
                    kb.I('dve', [K_('eg'), 'g_T0'], [K_('qd')], lambda e: e.tensor_tensor(out=B["qd"][:], in0=QT[:, tok], in1=B["eg"][:], op=ALU.mult)); yield
                    pz = nb()
                    tr(ps[pz][:, 0:128], B["Ya"][:], [K_('Ya')], [psk[pz]]); yield
                    kb.I('act', [psk[pz]], [K_('YTa')], lambda e: e.activation(out=B["YTa"][:], in_=ps[pz][:, 0:128], func=AF.Copy)); yield
                    kb.I('dve', [K_('Ya'), 'ident'], [K_('Ra')], lambda e: e.tensor_tensor(out=B["Ra"][:], in0=B["Ya"][:], in1=ident[:], op=ALU.add)); yield
                    cur = 'a'
                    for lev in range(6):
                        nx = 'b' if cur == 'a' else 'a'
                        yc, ytc, rc = B["Y" + cur], B["YT" + cur], B["R" + cur]
                        yn, ytn, rn = B["Y" + nx], B["YT" + nx], B["R" + nx]
                        pl = nb()
                        mm(ps[pl][:, 128:256], yc[:], ytc[:], [K_('Y' + cur), K_('YT' + cur)], [psk[pl]]); yield
                        kb.I('act', [psk[pl]], [K_('YT' + nx)], lambda e: e.activation(out=ytn[:], in_=ps[pl][:, 128:256], func=AF.Copy)); yield
                        if lev < 5:
                            mm(ps[pl][:, 0:128], ytc[:], yc[:], [K_('Y' + cur), K_('YT' + cur)], [psk[pl]]); yield
                            kb.I('act', [psk[pl]], [K_('Y' + nx)], lambda e: e.activation(out=yn[:], in_=ps[pl][:, 0:128], func=AF.Copy)); yield
                        mm(ps[pl][:, 256:384], ytn[:], rc[:], [K_('YT' + nx), K_('R' + cur)], [psk[pl]]); yield
                        kb.I('dve', [psk[pl], K_('R' + cur)], [K_('R' + nx)], lambda e: e.tensor_tensor(out=rn[:], in0=rc[:], in1=ps[pl][:, 256:384], op=ALU.add)); yield
                        cur = nx
                    Rf, Rk_ = B["R" + cur], K_('R' + cur)
                    kb.I('dve', ['g_Vtm', 'g_bt'], [K_('rv')], lambda e: e.tensor_scalar(out=B["rv"][:], in0=Vtm[:, n, :], scalar1=bcol, scalar2=None, op0=ALU.mult)); yield
                    kb.I('dve', ['g_Ktm', K_('cs')], [K_('rk')], lambda e: e.tensor_scalar(out=B["rk"][:], in0=Ktm[:, n, :], scalar1=cs[:, 3:4], scalar2=None, op0=ALU.mult)); yield
                    kb.I('dve', ['g_Ktm', K_('cs')], [K_('Kd')], lambda e: e.tensor_scalar(out=B["Kd"][:], in0=Ktm[:, n, :], scalar1=cs[:, 4:5], scalar2=None, op0=ALU.mult)); yield
                    pu = nb()
                    mm(ps[pu][:, 0:128], Rf[:], B["rv"][:], [Rk_, K_('rv')], [psk[pu]]); yield
                    mm(ps[pu][:, 128:256], B["rk"][:], Rf[:], [Rk_, K_('rk')], [psk[pu]]); yield
                    kb.I('act', [psk[pu]], [K_('U')], lambda e: e.activation(out=B["U"][:], in_=ps[pu][:, 0:128], func=AF.Copy)); yield
                    kb.I('act', [psk[pu]], [K_('WT')], lambda e: e.activation(out=B["WT"][:], in_=ps[pu][:, 128:256], func=AF.Copy)); yield
                    while done[d] != k:
                        yield
                    Sc, Sn = S[d][k % 2], S[d][(k + 1) % 2]
                    skc, skn = f'c_S{d}{k % 2}', f'c_S{d}{(k + 1) % 2}'
                    p1 = nb()
                    mm(ps[p1][:, 0:128], B["WT"][:], Sc[:], [K_('WT'), skc], [psk[p1]]); yield
                    kb.I('dve', [psk[p1], K_('U')], [K_('vn')], lambda e: e.tensor_tensor(out=B["vn"][:], in0=B["U"][:], in1=ps[p1][:, 0:128], op=ALU.subtract)); yield
                    mm(ps[p1][:, 128:256], B["qd"][:], Sc[:], [K_('qd'), skc], [psk[p1]], start=True, stop=False); yield
                    mm(ps[p1][:, 128:256], B["qkm"][:], B["vn"][:], [K_('qkm'), K_('vn')], [psk[p1]], start=False, stop=True); yield
                    kb.I('dve', [psk[p1], ('g_O', n)], [('g_O', n)], lambda e: e.tensor_tensor(out=O[:, n, :], in0=O[:, n, :], in1=ps[p1][:, 128:256], op=ALU.add)); yield
                    mm(ps[p1][:, 256:384], B["Kd"][:], B["vn"][:], [K_('Kd'), K_('vn')], [psk[p1]]); yield
                    kb.I('dve', [psk[p1], skc, K_('cs')], [skn], lambda e: e.scalar_tensor_tensor(out=Sn[:], in0=Sc[:], scalar=cs[:, 5:6], in1=ps[p1][:, 256:384], op0=ALU.mult, op1=ALU.add))
                    done[d] = k + 1
                    yield

            active = [gen(0, 0), gen(1, 0), gen(0, 1), gen(1, 1)]
            while active:
                for g_ in list(active):
                    try:
                        next(g_)
                    except StopIteration:
                        active.remove(g_)
            kb.barrier()

    def w_out_phase(b):
        with ExitStack() as st:
            wo = sb("o_w", [128, KC, D], stack=st)
            kb.DMA('sp', [], ['o_w'], lambda e: e.dma_start(out=wo[:], in_=ev_w_out[:, :, :]))
            gt = {}
            for kind, row in (('c', 2), ('l', b)):
                g = sb("o_g" + kind, [128, D], stack=st)
                kb.DMA('sp', [('modsD', 0)], ['o_g' + kind], lambda e: e.dma_start(out=g[:], in_=modsD[0, row:row + 1, 2 * D:3 * D].to_broadcast([128, D])))
                gt[kind] = g
            mt = [sb(f"o_m{i}", [128, KC, 128], stack=st) for i in range(2)]
            xt = [sb(f"o_x{i}", [128, D], stack=st) for i in range(2)]
            for t in range(NT):
                i = t % 2
                kind = 'c' if t < 2 else 'l'
                kb.DMA('sp', [('mixT', b)], [f'o_m{i}'], lambda e: e.dma_start(out=mt[i][:], in_=mixT[b, :, :, t * 128:(t + 1) * 128].rearrange("k p t -> p k t")))
                kb.DMA('sp', ['xin'], [f'o_x{i}'], lambda e: e.dma_start(out=xt[i][:], in_=xin[b, t * 128:(t + 1) * 128, :]))
                for hlf in range(2):
                    pb = psr.next()
                    for k in range(KC):
                        mm(ps[pb][:, :], mt[i][:, k, :], wo[:, k, hlf * 512:(hlf + 1) * 512], [f'o_m{i}', 'o_w'], [psk[pb]], start=(k == 0), stop=(k == KC - 1))
                    if 'yl0' in dbg_out and b == 0:
                        yb = sb(f"o_y{t}_{hlf}", [128, 512], stack=st)
                        kb.I('act', [psk[pb]], [f'o_y{t}_{hlf}'], lambda e: e.activation(out=yb[:], in_=ps[pb][:, :], func=AF.Copy))
                        kb.DMA('sp', [f'o_y{t}_{hlf}'], ['dbg_yl0'], lambda e: e.dma_start(out=dbg_out['yl0'][t * 128:(t + 1) * 128, hlf * 512:(hlf + 1) * 512], in_=yb[:]))
                    kb.I('dve', [psk[pb], 'o_g' + kind], [psk[pb]], lambda e: e.tensor_tensor(out=ps[pb][:, :], in0=ps[pb][:, :], in1=gt[kind][:, hlf * 512:(hlf + 1) * 512], op=ALU.mult))
                    kb.I('dve', [psk[pb], f'o_x{i}'], [f'o_x{i}'], lambda e: e.tensor_tensor(out=xt[i][:, hlf * 512:(hlf + 1) * 512], in0=xt[i][:, hlf * 512:(hlf + 1) * 512], in1=ps[pb][:, :], op=ALU.add))
                kb.DMA('sp', [f'o_x{i}'], [('xs', b)], lambda e: e.dma_start(out=xs[b, t * 128:(t + 1) * 128, :], in_=xt[i][:]))
            kb.barrier()

    peer_wq = din("peer_wq", [2, 128, KC, 2048])
    peer_kT = din("peer_kT", [2, 128, 16, 128])
    peer_u = [din(f"peer_u{i}", [16384, D]) for i in range(2)]
    peer_v = [din(f"peer_v{i}", [16384, D]) for i in range(2)]
    pconst = din("pconst", [1, 32])
    xs1 = dscr("xs1", [NB, LL, D])
    NEG = -1.0e30

    def peer_phase(l, b, tiles, src_fn, dst_fn, final=False):
        with ExitStack() as st:
            wqb = [sb(f"p_wq{i}", [128, KC, 512], stack=st) for i in range(2)]
            kT = sb("p_kT", [128, 16, 128], stack=st)
            kb.DMA('sp', [], ['p_kT'], lambda e: e.dma_start(out=kT[:], in_=peer_kT[l]))
            pc = sb("p_pc", [128, 32], stack=st)
            kb.DMA('sp', [], ['p_pc'], lambda e: e.dma_start(out=pc[:], in_=pconst[0:1, :].to_broadcast([128, 32])))
            nw = sb("p_nw", [128, D], stack=st)
            kb.DMA('sp', [], ['p_nw'], lambda e: e.dma_start(out=nw[:], in_=norms[2 + l].to_broadcast([128, D])))
            A = sb("p_A", [128, D], stack=st); S = sb("p_S", [128, D], stack=st)
            Gk = {}
            for kind_ in sorted(set('c' if t_ < 2 else 'l' for t_ in tiles)):
                Gk[kind_] = sb("p_G" + kind_, [128, D], stack=st)
                row_ = 2 if kind_ == 'c' else b
                kb.DMA('sp', [('modsD', l)], ['p_G' + kind_], lambda e: e.dma_start(out=Gk[kind_][:], in_=modsD[l, row_:row_ + 1, 5 * D:6 * D].to_broadcast([128, D])))
            fnw = None
            if final:
                fnw = sb("p_fnw", [128, D], stack=st)
                kb.DMA('sp', [], ['p_fnw'], lambda e: e.dma_start(out=fnw[:], in_=norms[4].to_broadcast([128, D])))
            xt = [sb(f"p_x{i}", [128, D], stack=st) for i in range(2)]
            h2b = [sb(f"p_h2{i}", [128, D], stack=st) for i in range(2)]
            ssf = sb("p_ssf", [128, 1], stack=st)
            sq = sb("p_sq", [128, D], stack=st)
            ss = sb("p_ss", [128, 1], stack=st)
            h2T = sb("p_h2T", [128, KC, 128], stack=st)
            qT = sb("p_qT", [128, 16, 128], stack=st)
            s1 = sb("p_s1", [128, 16, 128], stack=st)
            s2 = sb("p_s2", [128, 16, 128], stack=st)
            m8 = sb("p_m8", [128, 16, 16], stack=st)
            i8 = sb("p_i8", [128, 16, 16], U32, stack=st)
            i8f = sb("p_i8f", [128, 16, 16], stack=st)
            cand = sb("p_cand", [128, 8, 256], stack=st)
            cand2 = sb("p_cand2", [128, 8, 256], stack=st)
            sc = sb("p_sc", [128, 8, 16], stack=st)
            ci = sb("p_ci", [128, 8, 16], U32, stack=st)
            cf = sb("p_cf", [128, 8, 16], stack=st)
            fi = sb("p_fi", [128, 8, 16], stack=st)
            fj = sb("p_fj", [128, 8, 16], stack=st)
            oh = sb("p_oh", [128, 8, 16, 16], stack=st)
            g1 = sb("p_g1", [128, 8, 16], stack=st)
            g2_ = sb("p_g2", [128, 8, 16], stack=st)
            eidb = [sb(f"p_eid{i}", [128, 128], I32, stack=st) for i in range(2)]
            gateb = [sb(f"p_gate{i}", [128, 8, 16], stack=st) for i in range(2)]
            gsum = sb("p_gsum", [128, 8], stack=st)
            act = sb("p_act", [128, 128], stack=st)
            t1 = sb("p_t1", [128, 128], stack=st)
            wgt = sb("p_wgt", [128, 128], stack=st)
            NG = 10
            gb = [sb(f"p_gb{i}", [128, D], stack=st) for i in range(NG)]
            gring = Ring(list(range(NG)))
            acc = sb("p_acc", [128, D], stack=st)
            junk = sb("p_junk", [128, D], stack=st)
            NDG = 4
            dg = [sb(f"p_dg{i}", [128, 128], stack=st) for i in range(NDG)]
            dring = Ring(list(range(NDG)))
            psr = Ring(list(range(6)))
            cur_kind = [None]

            def front(ti, t):
                i = ti % 2
                kind = 'c' if t < 2 else 'l'
                row = 2 if kind == 'c' else b
                if kind != cur_kind[0]:
                    cur_kind[0] = kind
                    kb.DMA('sp', [('modsD', l)], ['p_A'], lambda e: e.dma_start(out=A[:], in_=modsD[l, row:row + 1, 4 * D:5 * D].to_broadcast([128, D])))
                    yield
                    kb.DMA('sp', [('modsD', l)], ['p_S'], lambda e: e.dma_start(out=S[:], in_=modsD[l, row:row + 1, 3 * D:4 * D].to_broadcast([128, D])))
                    yield
                    kb.I('dve', ['p_A', 'p_nw'], ['p_A'], lambda e: e.scalar_tensor_tensor(out=A[:], in0=A[:], scalar=1.0, in1=nw[:], op0=ALU.add, op1=ALU.mult))
                    yield
                sap, skey = src_fn(t)
                kb.DMA('sp', [skey], [f'p_x{i}'], lambda e: e.dma_start(out=xt[i][:], in_=sap))
                yield
                kb.I('act', [f'p_x{i}'], ['p_sq', 'p_ss'], lambda e: e.activation(out=sq[:], in_=xt[i][:], func=AF.Square, accum_out=ss[:]))
                yield
                kb.I('act', ['p_ss', 'epsc'], ['p_ss'], lambda e: e.activation(out=ss[:], in_=ss[:], func=AF.Sqrt, scale=1.0 / D, bias=epsc[:]))
                yield
                kb.I('dve', ['p_ss'], ['p_ss'], lambda e: e.reciprocal(out=ss[:], in_=ss[:]))
                yield
                kb.I('dve', [f'p_x{i}', 'p_ss', 'p_A'], [f'p_h2{i}'], lambda e: e.scalar_tensor_tensor(out=h2b[i][:], in0=xt[i][:], scalar=ss[:], in1=A[:], op0=ALU.mult, op1=ALU.mult))
                yield
                kb.I('dve', [f'p_h2{i}', 'p_S'], [f'p_h2{i}'], lambda e: e.tensor_tensor(out=h2b[i][:], in0=h2b[i][:], in1=S[:], op=ALU.add))
                yield
                for half in range(2):
                    pb = psr.next()
                    for kk in range(4):
                        k = half * 4 + kk
                        tr(ps[pb][:, kk * 128:(kk + 1) * 128], h2b[i][:, k * 128:(k + 1) * 128], [f'p_h2{i}'], [psk[pb]])
                        yield
                    kb.I('act', [psk[pb]], ['p_h2T'], lambda e: e.activation(out=h2T[:, half * 4:half * 4 + 4, :], in_=ps[pb][:, :].rearrange("p (k t) -> p k t", k=4), func=AF.Copy))
                    yield
                for q0 in range(0, 16, 4):
                    pb = psr.next()
                    wi_ = (q0 // 4) % 2
                    wq = wqb[wi_]
                    kb.DMA('sp', [], [f'p_wq{wi_}'], lambda e: e.dma_start(out=wq[:], in_=peer_wq[l, :, :, q0 * 128:(q0 + 4) * 128]))
                    yield
                    for qq in range(4):
                        for k in range(KC):
                            mm(ps[pb][:, qq * 128:(qq + 1) * 128], wq[:, k, qq * 128:(qq + 1) * 128], h2T[:, k, :], [f'p_wq{wi_}', 'p_h2T'], [psk[pb]], start=(k == 0), stop=(k == KC - 1))
                            yield
                    kb.I('act', [psk[pb]], ['p_qT'], lambda e: e.activation(out=qT[:, q0:q0 + 4, :], in_=ps[pb][:, :].rearrange("p (k t) -> p k t", k=4), func=AF.Copy))
                    yield
                for q0 in range(0, 16, 4):
                    pb = psr.next()
                    for qq in range(4):
                        hp = q0 + qq
                        mm(ps[pb][:, qq * 128:(qq + 1) * 128], qT[:, hp, :], kT[:, hp, :], ['p_qT', 'p_kT'], [psk[pb]])
                        yield
                    kb.I('act', [psk[pb]], ['p_s1'], lambda e: e.activation(out=s1[:, q0:q0 + 4, :], in_=ps[pb][:, :].rearrange("p (k t) -> p k t", k=4), func=AF.Copy))
                    yield
                for hp in range(16):
                    kb.I('dve', ['p_s1'], ['p_m8'], lambda e: e.max(out=m8[:, hp, 0:8], in_=s1[:, hp, :]))
                    yield
                    kb.I('dve', ['p_s1', 'p_m8'], ['p_i8'], lambda e: e.max_index(out=i8[:, hp, 0:8], in_max=m8[:, hp, 0:8], in_values=s1[:, hp, :]))
                    yield
                    kb.I('dve', ['p_s1', 'p_m8'], ['p_s2'], lambda e: e.match_replace(out=s2[:, hp, :], in_to_replace=m8[:, hp, 0:8], in_values=s1[:, hp, :], imm_value=NEG))
                    yield
                    kb.I('dve', ['p_s2'], ['p_m8'], lambda e: e.max(out=m8[:, hp, 8:16], in_=s2[:, hp, :]))
                    yield
                    kb.I('dve', ['p_s2', 'p_m8'], ['p_i8'], lambda e: e.max_index(out=i8[:, hp, 8:16], in_max=m8[:, hp, 8:16], in_values=s2[:, hp, :]))
                    yield
                kb.I('dve', ['p_i8'], ['p_i8f'], lambda e: e.tensor_copy(out=i8f[:], in_=i8[:]))
                yield
                m8v = m8[:, :, :].rearrange("p (h two) k -> p h two k", two=2)
                i8v = i8f[:, :, :].rearrange("p (h two) k -> p h two k", two=2)
                candv = cand[:, :, :].rearrange("p h (i j) -> p h i j", j=16)
                kb.I('dve', ['p_m8'], ['p_cand'], lambda e: e.tensor_tensor(out=candv, in0=m8v[:, :, 0, :].unsqueeze(3).to_broadcast([128, 8, 16, 16]), in1=m8v[:, :, 1, :].unsqueeze(2).to_broadcast([128, 8, 16, 16]), op=ALU.add))
                yield
                for h in range(8):
                    kb.I('dve', ['p_cand'], ['p_sc'], lambda e: e.max(out=sc[:, h, 0:8], in_=cand[:, h, :]))
                    yield
                    kb.I('dve', ['p_cand', 'p_sc'], ['p_ci'], lambda e: e.max_index(out=ci[:, h, 0:8], in_max=sc[:, h, 0:8], in_values=cand[:, h, :]))
                    yield
                    kb.I('dve', ['p_cand', 'p_sc'], ['p_cand2'], lambda e: e.match_replace(out=cand2[:, h, :], in_to_replace=sc[:, h, 0:8], in_values=cand[:, h, :], imm_value=NEG))
                    yield
                    kb.I('dve', ['p_cand2'], ['p_sc'], lambda e: e.max(out=sc[:, h, 8:16], in_=cand2[:, h, :]))
                    yield
                    kb.I('dve', ['p_cand2', 'p_sc'], ['p_ci'], lambda e: e.max_index(out=ci[:, h, 8:16], in_max=sc[:, h, 8:16], in_values=cand2[:, h, :]))
                    yield
                kb.I('dve', ['p_ci'], ['p_cf'], lambda e: e.tensor_copy(out=cf[:], in_=ci[:]))
                yield
                thr_b = pc[:, 0:16].unsqueeze(1).unsqueeze(1).to_broadcast([128, 8, 16, 16])
                iot_b = pc[:, 16:32].unsqueeze(1).unsqueeze(1).to_broadcast([128, 8, 16, 16])
                kb.I('dve', ['p_cf', 'p_pc'], ['p_oh'], lambda e: e.tensor_tensor(out=oh[:], in0=cf[:].unsqueeze(3).to_broadcast([128, 8, 16, 16]), in1=thr_b, op=ALU.is_ge))
                yield
                kb.I('dve', ['p_oh'], ['p_fi'], lambda e: e.tensor_reduce(out=fi[:], in_=oh[:], axis=AX.X, op=ALU.add))
                yield
                kb.I('dve', ['p_fi', 'p_cf'], ['p_fj'], lambda e: e.scalar_tensor_tensor(out=fj[:], in0=fi[:], scalar=-16.0, in1=cf[:], op0=ALU.mult, op1=ALU.add))
                yield
                for (fx, half_, gdst, gk) in ((fi, 0, g1, 'p_g1'), (fj, 1, g2_, 'p_g2')):
                    fk = 'p_fi' if half_ == 0 else 'p_fj'
                    kb.I('dve', [fk, 'p_pc'], ['p_oh'], lambda e: e.tensor_tensor(out=oh[:], in0=fx[:].unsqueeze(3).to_broadcast([128, 8, 16, 16]), in1=iot_b, op=ALU.is_equal))
                    yield
                    kb.I('dve', ['p_oh', 'p_i8f'], ['p_oh'], lambda e: e.tensor_tensor(out=oh[:], in0=oh[:], in1=i8v[:, :, half_, :].unsqueeze(2).to_broadcast([128, 8, 16, 16]), op=ALU.mult))
                    yield
                    kb.I('dve', ['p_oh'], [gk], lambda e: e.tensor_reduce(out=gdst[:], in_=oh[:], axis=AX.X, op=ALU.add))
                    yield
                kb.I('dve', ['p_g1', 'p_g2'], ['p_g1'], lambda e: e.scalar_tensor_tensor(out=g1[:], in0=g1[:], scalar=128.0, in1=g2_[:], op0=ALU.mult, op1=ALU.add))
                yield
                kb.I('dve', ['p_g1'], [f'p_eid{i}'], lambda e: e.tensor_copy(out=eidb[i][:, :].rearrange("p (h k) -> p h k", k=16), in_=g1[:]))
                yield
                kb.I('dve', ['p_sc'], [f'p_gate{i}'], lambda e: e.tensor_tensor(out=gateb[i][:], in0=sc[:], in1=sc[:, :, 0:1].to_broadcast([128, 8, 16]), op=ALU.subtract))
                yield
                kb.I('act', [f'p_gate{i}'], [f'p_gate{i}'], lambda e: e.activation(out=gateb[i][:], in_=gateb[i][:], func=AF.Exp))
                yield
                kb.I('dve', [f'p_gate{i}'], ['p_gsum'], lambda e: e.tensor_reduce(out=gsum[:], in_=gateb[i][:], axis=AX.X, op=ALU.add))
                yield
                kb.I('dve', ['p_gsum'], ['p_gsum'], lambda e: e.reciprocal(out=gsum[:], in_=gsum[:]))
                yield
                kb.I('dve', [f'p_gate{i}', 'p_gsum'], [f'p_gate{i}'], lambda e: e.tensor_tensor(out=gateb[i][:], in0=gateb[i][:], in1=gsum[:].unsqueeze(2).to_broadcast([128, 8, 16]), op=ALU.mult))
                yield

            def pump(gen, n):
                if gen is None:
                    return
                k = 0
                while n is None or k < n:
                    try:
                        next(gen)
                    except StopIteration:
                        return
                    k += 1

            def back(ti, t, nxt):
                i = ti % 2
                kind = 'c' if t < 2 else 'l'
                for slot in range(128):
                    gi = gring.next()
                    kb.DMA('pool', [f'p_eid{i}'], [f'p_gb{gi}'], lambda e: e.indirect_dma_start(out=gb[gi][:, :], out_offset=None, in_=peer_u[l][:, :], in_offset=bass.IndirectOffsetOnAxis(ap=eidb[i][:, slot:slot + 1], axis=0)))
                    kb.I('dve', [f'p_gb{gi}', f'p_h2{i}'], [('p_act', slot)], lambda e: e.scalar_tensor_tensor(out=junk[:], in0=gb[gi][:], scalar=1.0, in1=h2b[i][:], op0=ALU.mult, op1=ALU.mult, accum_out=act[:, slot:slot + 1]))
                    pump(nxt, 2)
                kb.I('dve', [('p_act', s_) for s_ in range(128)], ['p_t1', 'p_act'], lambda e: e.tensor_tensor(out=t1[:], in0=act[:], in1=act[:], op=ALU.mult))
                kb.I('dve', ['p_t1'], ['p_t1'], lambda e: e.tensor_scalar(out=t1[:], in0=t1[:], scalar1=0.044715, scalar2=1.0, op0=ALU.mult, op1=ALU.add))
                kb.I('dve', ['p_t1', 'p_act'], ['p_t1'], lambda e: e.tensor_tensor(out=t1[:], in0=t1[:], in1=act[:], op=ALU.mult))
                kb.I('act', ['p_t1'], ['p_t1'], lambda e: e.activation(out=t1[:], in_=t1[:], func=AF.Sigmoid, scale=1.5957691216057308))
                kb.I('dve', ['p_t1', 'p_act'], ['p_t1'], lambda e: e.tensor_tensor(out=t1[:], in0=t1[:], in1=act[:], op=ALU.mult))
                kb.I('dve', ['p_t1', f'p_gate{i}'], ['p_wgt'], lambda e: e.tensor_tensor(out=wgt[:], in0=t1[:], in1=gateb[i][:, :, :].rearrange("p h k -> p (h k)"), op=ALU.mult))
                for slot in range(128):
                    gi = gring.next()
                    kb.DMA('pool', [f'p_eid{i}'], [f'p_gb{gi}'], lambda e: e.indirect_dma_start(out=gb[gi][:, :], out_offset=None, in_=peer_v[l][:, :], in_offset=bass.IndirectOffsetOnAxis(ap=eidb[i][:, slot:slot + 1], axis=0)))
                    di = dring.next()
                    kb.I('dve', ['ident', 'p_wgt'], [f'p_dg{di}'], lambda e: e.tensor_scalar(out=dg[di][:], in0=ident[:], scalar1=wgt[:, slot:slot + 1], scalar2=None, op0=ALU.mult))
                    for hf in range(2):
                        mm(ps[6 + hf][:, :], dg[di][:], gb[gi][:, hf * 512:(hf + 1) * 512], [f'p_dg{di}', f'p_gb{gi}'], [psk[6 + hf]], start=(slot == 0), stop=(slot == 127))
                    pump(nxt, 2)
                for hf in range(2):
                    kb.I('dve', [psk[6 + hf], 'p_G' + kind], ['p_acc'], lambda e: e.tensor_tensor(out=acc[:, hf * 512:(hf + 1) * 512], in0=ps[6 + hf][:, :], in1=Gk[kind][:, hf * 512:(hf + 1) * 512], op=ALU.mult))
                kb.I('dve', ['p_acc', f'p_x{i}'], [f'p_x{i}'], lambda e: e.tensor_tensor(out=xt[i][:], in0=xt[i][:], in1=acc[:], op=ALU.add))
                if final:
                    kb.I('act', [f'p_x{i}'], ['p_ssf'], lambda e: e.activation(out=junk[:], in_=xt[i][:], func=AF.Square, accum_out=ssf[:]))
                    kb.I('act', ['p_ssf', 'epsc'], ['p_ssf'], lambda e: e.activation(out=ssf[:], in_=ssf[:], func=AF.Sqrt, scale=1.0 / D, bias=epsc[:]))
                    kb.I('dve', ['p_ssf'], ['p_ssf'], lambda e: e.reciprocal(out=ssf[:], in_=ssf[:]))
                    kb.I('dve', [f'p_x{i}', 'p_ssf', 'p_fnw'], [f'p_x{i}'], lambda e: e.scalar_tensor_tensor(out=xt[i][:], in0=xt[i][:], scalar=ssf[:], in1=fnw[:], op0=ALU.mult, op1=ALU.mult))
                dst_fn(t, xt[i], f'p_x{i}')
                pump(nxt, None)

            gens = [front(ti, t) for ti, t in enumerate(tiles)]
            pump(gens[0], None)
            for ti, t in enumerate(tiles):
                back(ti, t, gens[ti + 1] if ti + 1 < len(tiles) else None)
            kb.barrier()

    od_w_in = din("od_w_in", [128, KC, 2064])
    od_w_out = din("od_w_out", [128, KC, D])
    c_gc = din("c_gc", [1, 16])
    c_norm = din("c_norm", [1, 512])
    d_w = din("d_w", [4, 128, 128])
    d_scale = din("d_scale", [512, 1])
    poolm = din("poolm", [4, 128, 128])
    mixT1 = dscr("mixT1", [NB, KC, 128, LT])

    def xs_cm_src(b, t):
        if t < 2:
            return [(slice(0, 128), xs[b, t * 128:(t + 1) * 128, :])]
        v = xs[b, LC:LT, :].rearrange("(r w) d -> r w d", w=GRID_W)
        return [(slice(wi * 32, (wi + 1) * 32), v[:, (t - 2) * 4 + wi, :]) for wi in range(4)]

    def layer1_mixer(b):
        with ExitStack() as ph:
            hT = sb("hT1", [128, KC, LT], stack=ph)
            with ExitStack() as st:
                nw = sb("n1_nw", [128, D], stack=st)
                kb.DMA('sp', [], ['n1_nw'], lambda e: e.dma_start(out=nw[:], in_=norms[1].to_broadcast([128, D])))
                md = {}
                for kind, row in (('c', 2), ('l', b)):
                    A = sb("n1_A" + kind, [128, D], stack=st)
                    S = sb("n1_S" + kind, [128, D], stack=st)
                    kb.DMA('sp', [('modsD', 1)], ['n1_A' + kind], lambda e: e.dma_start(out=A[:], in_=modsD[1, row:row + 1, D:2 * D].to_broadcast([128, D])))
                    kb.DMA('sp', [('modsD', 1)], ['n1_S' + kind], lambda e: e.dma_start(out=S[:], in_=modsD[1, row:row + 1, 0:D].to_broadcast([128, D])))
                    kb.I('dve', ['n1_A' + kind, 'n1_nw'], ['n1_A' + kind], lambda e: e.scalar_tensor_tensor(out=A[:], in0=A[:], scalar=1.0, in1=nw[:], op0=ALU.add, op1=ALU.mult))
                    md[kind] = (A, S)
                xt = [sb(f"n1_x{i}", [128, D], stack=st) for i in range(2)]
                ht = [sb(f"n1_h{i}", [128, D], stack=st) for i in range(2)]
                sq = sb("n1_sq", [128, D], stack=st)
                ss = [sb(f"n1_ss{i}", [128, 1], stack=st) for i in range(2)]
                for t in range(NT):
                    i = t % 2
                    kind = 'c' if t < 2 else 'l'
                    A, S = md[kind]
                    for (psl, sap) in xs_cm_src(b, t):
                        kb.DMA('sp', [('xs', b)], [f'n1_x{i}'], lambda e: e.dma_start(out=xt[i][psl, :], in_=sap))
                    kb.I('act', [f'n1_x{i}'], ['n1_sq', f'n1_ss{i}'], lambda e: e.activation(out=sq[:], in_=xt[i][:], func=AF.Square, accum_out=ss[i][:]))
                    kb.I('act', [f'n1_ss{i}', 'epsc'], [f'n1_ss{i}'], lambda e: e.activation(out=ss[i][:], in_=ss[i][:], func=AF.Sqrt, scale=1.0 / D, bias=epsc[:]))
                    kb.I('dve', [f'n1_ss{i}'], [f'n1_ss{i}'], lambda e: e.reciprocal(out=ss[i][:], in_=ss[i][:]))
                    kb.I('dve', [f'n1_x{i}', f'n1_ss{i}', 'n1_A' + kind], [f'n1_h{i}'], lambda e: e.scalar_tensor_tensor(out=ht[i][:], in0=xt[i][:], scalar=ss[i][:], in1=A[:], op0=ALU.mult, op1=ALU.mult))
                    kb.I('dve', [f'n1_h{i}', 'n1_S' + kind], [f'n1_h{i}'], lambda e: e.tensor_tensor(out=ht[i][:], in0=ht[i][:], in1=S[:], op=ALU.add))
                    for half in range(2):
                        pb = psr.next()
                        for kk in range(4):
                            k = half * 4 + kk
                            tr(ps[pb][:, kk * 128:(kk + 1) * 128], ht[i][:, k * 128:(k + 1) * 128], [f'n1_h{i}'], [psk[pb]])
                        kb.I('act', [psk[pb]], [('hT', t)], lambda e: e.activation(out=hT[:, half * 4:half * 4 + 4, t * 128:(t + 1) * 128], in_=ps[pb][:, :].rearrange("p (k t) -> p k t", k=4), func=AF.Copy))
                kb.barrier()
            with ExitStack() as st:
                wd = sb("q_wd", [128, KC, 512], stack=st)
                kb.DMA('sp', [], ['q_wd'], lambda e: e.dma_start(out=wd[:], in_=od_w_in[:, :, 1552:2064]))
                pm = sb("q_pm", [128, 4, 128], stack=st)
                dw = sb("q_dw", [128, 4, 128], stack=st)
                dsc = sb("q_dsc", [128, 4], stack=st)
                for g in range(4):
                    kb.DMA('sp', [], ['q_pm'], lambda e: e.dma_start(out=pm[:, g, :], in_=poolm[g]))
                    kb.DMA('sp', [], ['q_dw'], lambda e: e.dma_start(out=dw[:, g, :], in_=d_w[g]))
                    kb.DMA('sp', [], ['q_dsc'], lambda e: e.dma_start(out=dsc[:, g:g + 1], in_=d_scale[g * 128:(g + 1) * 128, :]))
                dtm = sb("q_dtm", [128, 512], stack=st)
                pT = sb("q_pT", [128, 128], stack=st)
                yT = sb("q_yT", [128, 4, LT], stack=st)
                for t in range(2, NT):
                    pb = psr.next()
                    for k in range(KC):
                        mm(ps[pb][:, :], hT[:, k, t * 128:(t + 1) * 128], wd[:, k, :], ['q_wd', ('hT', t)], [psk[pb]], start=(k == 0), stop=(k == KC - 1))
                    kb.I('act', [psk[pb]], ['q_dtm'], lambda e: e.activation(out=dtm[:], in_=ps[pb][:, :], func=AF.Copy))
                    for g in range(4):
                        p2 = psr.next()
                        mm(ps[p2][:, 0:128], dtm[:, g * 128:(g + 1) * 128], pm[:, g, :], ['q_dtm', 'q_pm'], [psk[p2]])
                        kb.I('act', [psk[p2]], ['q_pT'], lambda e: e.activation(out=pT[:], in_=ps[p2][:, 0:128], func=AF.Copy))
                        mm(ps[p2][:, 128:256], dw[:, g, :], pT[:], ['q_dw', 'q_pT'], [psk[p2]])
                        kb.I('dve', [psk[p2], 'q_dsc'], ['q_yT'], lambda e: e.tensor_scalar(out=yT[:, g, t * 128:(t + 1) * 128], in0=ps[p2][:, 128:256], scalar1=dsc[:, g:g + 1], scalar2=None, op0=ALU.mult))
                for g in range(4):
                    kb.DMA('sp', ['q_yT'], [('mixT1', b)], lambda e: e.dma_start(out=mixT1[b, 4 + g, :, LC:LT], in_=yT[:, g, LC:LT]))
                kb.barrier()
            mlstm(b, hT)
            kb.barrier()

    def mlstm(b, hT):
        with ExitStack() as st:
            wh = sb("m_wh", [128, KC, 64 + 64 + 128 + 128], stack=st)
            wg16 = sb("m_wg16", [128, KC, 16], stack=st)
            gcst = sb("m_gcst", [128, 16], stack=st)
            cnw = sb("m_cnw", [128, 512], stack=st)
            QT = sb("m_QT", [64, LT], stack=st)
            KT = sb("m_KT", [64, LT], stack=st)
            Ktm = sb("m_Ktm", [128, NT, 64], stack=st)
            V1 = sb("m_V1", [128, NT, 132], stack=st)
            og = sb("m_og", [128, NT, 128], stack=st)
            O = sb("m_O", [128, NT, 128], stack=st)
            tmp = sb("m_tmp", [128, LT], stack=st)
            graw = sb("m_graw", [128, NT, 16], stack=st)
            LI = sb("m_li", [128, NT, 8], stack=st)
            LF = sb("m_lf", [128, NT, 8], stack=st)
            kb.DMA('sp', [], ['m_gcst'], lambda e: e.dma_start(out=gcst[:], in_=c_gc[0:1, :].to_broadcast([128, 16])))
            kb.DMA('sp', [], ['m_cnw'], lambda e: e.dma_start(out=cnw[:], in_=c_norm[0:1, :].to_broadcast([128, 512])))
            kb.DMA('sp', [], ['m_wg16'], lambda e: e.dma_start(out=wg16[:], in_=od_w_in[:, :, 1024:1040]))
            pb = psr.next()
            for n in range(NT):
                for k in range(KC):
                    mm(ps[pb][:, n * 16:(n + 1) * 16], hT[:, k, n * 128:(n + 1) * 128], wg16[:, k, :], ['m_wg16', ('hT', n)], [psk[pb]], start=(k == 0), stop=(k == KC - 1))
            kb.I('act', [psk[pb]], ['m_graw'], lambda e: e.activation(out=graw[:], in_=ps[pb][:, 0:NT * 16].rearrange("p (n c) -> p n c", c=16), func=AF.Copy))
            kb.I('dve', ['m_graw', 'm_gcst'], ['m_li'], lambda e: e.tensor_tensor(out=LI[:], in0=graw[:, :, 0:8], in1=gcst[:, 0:8].unsqueeze(1).to_broadcast([128, NT, 8]), op=ALU.add))
            kb.I('dve', ['m_graw', 'm_gcst'], ['m_lf'], lambda e: e.tensor_tensor(out=LF[:], in0=graw[:, :, 8:16], in1=gcst[:, 8:16].unsqueeze(1).to_broadcast([128, NT, 8]), op=ALU.add))
            kb.I('act', ['m_lf'], ['m_lf'], lambda e: e.activation(out=LF[:], in_=LF[:], func=AF.Exp, scale=-1.0))
            kb.I('act', ['m_lf', 'onec'], ['m_lf'], lambda e: e.activation(out=LF[:], in_=LF[:], func=AF.Ln, bias=onec[:]))
            kb.I('dve', ['m_lf'], ['m_lf'], lambda e: e.tensor_scalar(out=LF[:], in0=LF[:], scalar1=-1.0, scalar2=None, op0=ALU.mult))
            kb.I('dve', [], ['m_V1'], lambda e: e.memset(V1[:], 1.0))
            for hd in range(4):
                for (o0, c0, w_) in ((0, hd * 64, 64), (64, 256 + hd * 64, 64), (128, 512 + hd * 128, 128), (256, 1040 + hd * 128, 128)):
                    kb.DMA('sp', [], ['m_wh'], lambda e: e.dma_start(out=wh[:, :, o0:o0 + w_], in_=od_w_in[:, :, c0:c0 + w_]))
                for (dst, dk_, o0, scl) in ((QT, 'm_QT', 0, 1.0), (KT, 'm_KT', 64, 0.125)):
                    for (t0, n) in TOKBLKS:
                        pb = psr.next()
                        for k in range(KC):
                            mm(ps[pb][0:64, 0:n], wh[:, k, o0:o0 + 64], hT[:, k, t0:t0 + n], ['m_wh'] + [('hT', tt) for tt in range(t0 // 128, (t0 + n) // 128)], [psk[pb]], start=(k == 0), stop=(k == KC - 1))
                        kb.I('act', [psk[pb]], [dk_], lambda e: e.activation(out=dst[:, t0:t0 + n], in_=ps[pb][0:64, 0:n], func=AF.Copy, scale=scl))
                for n0 in range(0, NT, 8):
                    pb = psr.next()
                    nn = min(8, NT - n0)
                    for i in range(nn):
                        kb.I('pe', ['m_KT', 'ident'], [psk[pb]], lambda e: e.transpose(out=ps[pb][:, i * 64:(i + 1) * 64], in_=KT[:, (n0 + i) * 128:(n0 + i + 1) * 128], identity=ident[0:64, 0:64]))
                    kb.I('act', [psk[pb]], ['m_Ktm'], lambda e: e.activation(out=Ktm[:, n0:n0 + nn, :], in_=ps[pb][:, 0:nn * 64].rearrange("p (n c) -> p n c", c=64), func=AF.Copy))
                for (o0, dst, dk_, fn_, wdt) in ((128, V1, 'm_V1', AF.Copy, 128), (256, og, 'm_og', AF.Sigmoid, 128)):
                    for n0 in range(0, NT, 4):
                        pb = psr.next()
                        nn = min(4, NT - n0)
                        for i in range(nn):
                            n = n0 + i
                            for k in range(KC):
                                mm(ps[pb][:, i * 128:(i + 1) * 128], hT[:, k, n * 128:(n + 1) * 128], wh[:, k, o0:o0 + 128], ['m_wh', ('hT', n)], [psk[pb]], start=(k == 0), stop=(k == KC - 1))
                        kb.I('act', [psk[pb]], [dk_], lambda e: e.activation(out=dst[:, n0:n0 + nn, 0:128], in_=ps[pb][:, 0:nn * 128].rearrange("p (n c) -> p n c", c=128), func=fn_))
                mlstm_chains(b, hd, QT, KT, Ktm, V1, LI, LF, O)
                ssq = sb(f"m_ssq{hd}", [128, NT], stack=st)
                O2 = tmp[:, :].rearrange("p (n c) -> p n c", c=128)
                kb.I('dve', ['m_O'], ['m_tmp'], lambda e: e.tensor_tensor(out=O2, in0=O[:], in1=O[:], op=ALU.mult))
                kb.I('dve', ['m_tmp'], ['m_ssq'], lambda e: e.tensor_reduce(out=ssq[:], in_=O2, axis=AX.X, op=ALU.add))
                kb.I('act', ['m_ssq', 'epsc'], ['m_ssq'], lambda e: e.activation(out=ssq[:], in_=ssq[:], func=AF.Sqrt, scale=1.0 / 128, bias=epsc[:]))
                kb.I('dve', ['m_ssq'], ['m_ssq'], lambda e: e.reciprocal(out=ssq[:], in_=ssq[:]))
                kb.I('dve', ['m_O', 'm_ssq'], ['m_O'], lambda e: e.tensor_tensor(out=O[:], in0=O[:], in1=ssq[:].unsqueeze(2).to_broadcast([128, NT, 128]), op=ALU.mult))
                kb.I('dve', ['m_O', 'm_cnw'], ['m_O'], lambda e: e.tensor_tensor(out=O[:], in0=O[:], in1=cnw[:, hd * 128:(hd + 1) * 128].unsqueeze(1).to_broadcast([128, NT, 128]), op=ALU.mult))
                kb.I('dve', ['m_O', 'm_og'], ['m_O'], lambda e: e.tensor_tensor(out=O[:], in0=O[:], in1=og[:], op=ALU.mult))
                for n0 in range(0, NT, 4):
                    pb = psr.next()
                    nn = min(4, NT - n0)
                    for i in range(nn):
                        tr(ps[pb][:, i * 128:(i + 1) * 128], O[:, n0 + i, :], ['m_O'], [psk[pb]])
                    kb.I('act', [psk[pb]], ['m_tmp'], lambda e: e.activation(out=tmp[:, n0 * 128:(n0 + nn) * 128], in_=ps[pb][:, 0:nn * 128], func=AF.Copy))
                kb.DMA('sp', ['m_tmp'], [('mixT1', b)], lambda e: e.dma_start(out=mixT1[b, hd, :, LC:LT], in_=tmp[:, LC:LT]))
            kb.barrier()

    def mlstm_chains(b, hd, QT, KT, Ktm, V1, LI, LF, O):
        with ExitStack() as st:
            NBUF = 2

            def mk(name, shape):
                return [sb(f"k_{name}{i}", shape, stack=st) for i in range(NBUF)]
            cs = mk("cs", [128, 8])
            Tg = mk("Tg", [128, 128]); Ei = mk("Ei", [128, 128]); eg = mk("eg", [64, 128]); qd = mk("qd", [64, 128])
            ST = mk("ST", [128, 128]); Kd = mk("Kd", [128, 64])
            C = [sb(f"k_C{i}", [64, 132], stack=st) for i in range(2)]
            kb.I('dve', [], ['m_O'], lambda e: e.memset(O[:, 0:2, :], 0.0))
            it = 0
            for d in range(2):
                col = d * 4 + hd
                order = ([0, 1] + list(range(2, NT))) if d == 0 else ([1, 0] + list(range(NT - 1, 1, -1)))
                si = 0
                kb.I('dve', [], ['k_C0'], lambda e: e.memset(C[0][:], 0.0))
                for n in order:
                    i = it % NBUF
                    it += 1
                    K_ = lambda nm: f"k_{nm}{i}"
                    fcol = LF[:, n, col:col + 1]
                    icol = LI[:, n, col:col + 1]
                    tok = slice(n * 128, (n + 1) * 128)
                    Cc, Cn = C[si], C[1 - si]
                    ckc, ckn = f'k_C{si}', f'k_C{1 - si}'
                    kb.I('dve', ['m_lf', 'msk'], [K_('Tg')], lambda e: e.tensor_scalar(out=Tg[i][:], in0=msk[:, d, :], scalar1=fcol, scalar2=None, op0=ALU.mult))
                    pa = psr.next()
                    mm(ps[pa][:, 0:128], ones[:], Tg[i][:], ['ones', K_('Tg')], [psk[pa]])
                    mm(ps[pa][:, 256:257], msk[:, d, :], fcol, ['msk', 'm_lf'], [psk[pa]])
                    mm(ps[pa][:, 257:258], ones[:], fcol, ['ones', 'm_lf'], [psk[pa]])
                    kb.I('act', [psk[pa]], [K_('cs')], lambda e: e.activation(out=cs[i][:, 0:2], in_=ps[pa][:, 256:258], func=AF.Copy))
                    kb.I('dve', [K_('cs')], [K_('cs')], lambda e: e.tensor_tensor(out=cs[i][:, 2:3], in0=cs[i][:, 1:2], in1=cs[i][:, 0:1], op=ALU.subtract))
                    kb.I('dve', [K_('cs'), 'm_li'], [K_('cs')], lambda e: e.tensor_tensor(out=cs[i][:, 2:3], in0=cs[i][:, 2:3], in1=icol, op=ALU.add))
                    kb.I('act', [K_('cs')], [K_('cs')], lambda e: e.activation(out=cs[i][:, 3:5], in_=cs[i][:, 1:3], func=AF.Exp))
                    kb.I('dve', ['m_Ktm', K_('cs')], [K_('Kd')], lambda e: e.tensor_scalar(out=Kd[i][:], in0=Ktm[:, n, :], scalar1=cs[i][:, 4:5], scalar2=None, op0=ALU.mult))
                    if n >= 2:
                        kb.I('dve', [psk[pa], K_('cs')], [K_('Ei')], lambda e: e.tensor_scalar(out=Ei[i][:], in0=ps[pa][:, 0:128], scalar1=cs[i][:, 0:1], scalar2=0.0, op0=ALU.subtract, op1=ALU.min))
                        kb.I('act', [K_('Ei'), 'm_li'], [K_('Ei')], lambda e: e.activation(out=Ei[i][:], in_=Ei[i][:], func=AF.Exp, bias=icol))
                        kb.I('dve', [K_('Ei'), 'msk'], [K_('Ei')], lambda e: e.tensor_tensor(out=Ei[i][:], in0=Ei[i][:], in1=msk[:, 2 + d, :], op=ALU.mult))
                        kb.I('act', [psk[pa]], [K_('eg')], lambda e: e.activation(out=eg[i][:], in_=ps[pa][0:64, 0:128], func=AF.Exp))
                        kb.I('dve', [K_('eg'), 'm_QT'], [K_('qd')], lambda e: e.tensor_tensor(out=qd[i][:], in0=QT[:, tok], in1=eg[i][:], op=ALU.mult))
                        pq = psr.next()
                        mm(ps[pq][:, 0:128], KT[:, tok], QT[:, tok], ['m_KT', 'm_QT'], [psk[pq]])
                        kb.I('dve', [psk[pq], K_('Ei')], [K_('ST')], lambda e: e.tensor_tensor(out=ST[i][:], in0=ps[pq][:, 0:128], in1=Ei[i][:], op=ALU.mult))
                        mm(ps[pq][:, 128:257], qd[i][:], Cc[:, 0:129], [K_('qd'), ckc], [psk[pq]], start=True, stop=False)
                        mm(ps[pq][:, 128:257], ST[i][:], V1[:, n, 0:129], [K_('ST'), 'm_V1'], [psk[pq]], start=False, stop=True)
                        kb.I('act', [psk[pq]], [K_('cs')], lambda e: e.activation(out=cs[i][:, 5:6], in_=ps[pq][:, 256:257], func=AF.Abs))
                        kb.I('dve', [K_('cs')], [K_('cs')], lambda e: e.tensor_scalar(out=cs[i][:, 5:6], in0=cs[i][:, 5:6], scalar1=1.0, scalar2=None, op0=ALU.max))
                        kb.I('dve', [K_('cs')], [K_('cs')], lambda e: e.reciprocal(out=cs[i][:, 5:6], in_=cs[i][:, 5:6]))
                        if d == 0:
                            kb.I('dve', [psk[pq], K_('cs')], ['m_O'], lambda e: e.tensor_scalar(out=O[:, n, :], in0=ps[pq][:, 128:256], scalar1=cs[i][:, 5:6], scalar2=None, op0=ALU.mult))
                        else:
                            kb.I('dve', [psk[pq], K_('cs'), 'm_O'], ['m_O'], lambda e: e.scalar_tensor_tensor(out=O[:, n, :], in0=ps[pq][:, 128:256], scalar=cs[i][:, 5:6], in1=O[:, n, :], op0=ALU.mult, op1=ALU.add))
                    p3 = psr.next()
                    mm(ps[p3][0:64, 0:129], Kd[i][:], V1[:, n, 0:129], [K_('Kd'), 'm_V1'], [psk[p3]])
                    kb.I('dve', [psk[p3], ckc, K_('cs')], [ckn], lambda e: e.scalar_tensor_tensor(out=Cn[:, 0:129], in0=Cc[:, 0:129], scalar=cs[i][0:64, 3:4], in1=ps[p3][0:64, 0:129], op0=ALU.mult, op1=ALU.add))
                    si = 1 - si
            kb.barrier()

    def w_out1_phase(b):
        with ExitStack() as st:
            wo = sb("o1_w", [128, KC, D], stack=st)
            kb.DMA('sp', [], ['o1_w'], lambda e: e.dma_start(out=wo[:], in_=od_w_out[:, :, :]))
            g = sb("o1_g", [128, D], stack=st)
            kb.DMA('sp', [('modsD', 1)], ['o1_g'], lambda e: e.dma_start(out=g[:], in_=modsD[1, b:b + 1, 2 * D:3 * D].to_broadcast([128, D])))
            mt = [sb(f"o1_m{i}", [128, KC, 128], stack=st) for i in range(2)]
            xt = [sb(f"o1_x{i}", [128, D], stack=st) for i in range(2)]
            for t in range(2, NT):
                i = t % 2
                kb.DMA('sp', [('mixT1', b)], [f'o1_m{i}'], lambda e: e.dma_start(out=mt[i][:], in_=mixT1[b, :, :, t * 128:(t + 1) * 128].rearrange("k p t -> p k t")))
                for (psl, sap) in xs_cm_src(b, t):
                    kb.DMA('sp', [('xs', b)], [f'o1_x{i}'], lambda e: e.dma_start(out=xt[i][psl, :], in_=sap))
                for hlf in range(2):
                    pb = psr.next()
                    for k in range(KC):
                        mm(ps[pb][:, :], mt[i][:, k, :], wo[:, k, hlf * 512:(hlf + 1) * 512], [f'o1_m{i}', 'o1_w'], [psk[pb]], start=(k == 0), stop=(k == KC - 1))
                    kb.I('dve', [psk[pb], 'o1_g'], [psk[pb]], lambda e: e.tensor_tensor(out=ps[pb][:, :], in0=ps[pb][:, :], in1=g[:, hlf * 512:(hlf + 1) * 512], op=ALU.mult))
                    kb.I('dve', [psk[pb], f'o1_x{i}'], [f'o1_x{i}'], lambda e: e.tensor_tensor(out=xt[i][:, hlf * 512:(hlf + 1) * 512], in0=xt[i][:, hlf * 512:(hlf + 1) * 512], in1=ps[pb][:, :], op=ALU.add))
                kb.DMA('sp', [f'o1_x{i}'], [('xs1', b)], lambda e: e.dma_start(out=xs1[b, (t - 2) * 128:(t - 1) * 128, :], in_=xt[i][:]))
            kb.barrier()

    def layer1(b, tiles=None):
        layer1_mixer(b)
        w_out1_phase(b)
        ov = out_d[b].rearrange("(r w) d -> r w d", w=GRID_W)

        def dst1(t, tile, key):
            for wi in range(4):
                kb.DMA('sp', [key], [('out', b)], lambda e: e.dma_start(out=ov[:, (t - 2) * 4 + wi, :], in_=tile[wi * 32:(wi + 1) * 32, :]))
        peer_phase(1, b, tiles or list(range(2, NT)), lambda t: (xs1[b, (t - 2) * 128:(t - 1) * 128, :], ('xs1', b)), dst1, final=True)

    nb_run = 1 if (stop_after or '').endswith('_b0') else NB
    for b in range(0 if l1only else nb_run):
        layer0_mixer(b)
        if stop_after in ('hT0', 'lru'):
            break
        w_out_phase(b)
        if stop_after == 'mix0_b0':
            continue

        def dst0(t, tile, key, b=b):
            kb.DMA('sp', [key], [('xs', b)], lambda e: e.dma_start(out=xs[b, t * 128:(t + 1) * 128, :], in_=tile[:]))
        peer_tiles = list(range(NT)) if stop_after != 'peer0_b0' else [0, 2]
        peer_phase(0, b, peer_tiles, lambda t, b=b: (xs[b, t * 128:(t + 1) * 128, :], ('xs', b)), dst0)
    if stop_after is None or stop_after.startswith('l1'):
        for b in range(nb_run):
            if stop_after == 'l1mix_b0':
                layer1_mixer(b)
                w_out1_phase(b)
            elif l1only:
                layer1(b, tiles=[2, 17])
            else:
                layer1(b)
    if 'xs' in dbg_out:
        kb.barrier()
        kb.DMA('sp', [('xs', 0), ('xs', 1)], ['dbg_xs'], lambda e: e.dma_start(out=dbg_out['xs'], in_=xs))
    if 'xs1' in dbg_out:
        kb.barrier()
        kb.DMA('sp', [('xs1', 0), ('xs1', 1)], ['dbg_xs1'], lambda e: e.dma_start(out=dbg_out['xs1'], in_=xs1))

    kb.finish()
    es.close()
    return nc


def make_in_maps(inputs):
    x = np.asarray(inputs['x'], np.float32)
    ctx = np.asarray(inputs['ctx'], np.float32)
    c = np.asarray(inputs['c'], np.float32)
    c_ctx = np.asarray(inputs['c_ctx'], np.float32)
    ada_w = np.ascontiguousarray(np.asarray(inputs['ada_w'], np.float32).reshape(2, KC, 128, 6 * D))
    ada_b = np.ascontiguousarray(np.asarray(inputs['ada_b'], np.float32).reshape(2, 1, 6 * D))
    norms = np.stack([inputs['norm_mix'][0], inputs['norm_mix'][1], inputs['norm_ffn'][0], inputs['norm_ffn'][1],
                      inputs['final_norm']]).astype(np.float32).reshape(5, 1, D)
    ident = np.eye(128, dtype=np.float32)
    ev_w_in = np.ascontiguousarray(np.asarray(inputs['ev_w_in'][0], np.float32).reshape(KC, 128, 3088).transpose(1, 0, 2))
    ev_w_out = np.ascontiguousarray(np.asarray(inputs['ev_w_out'][0], np.float32).reshape(KC, 128, D).transpose(1, 0, 2))
    a_convT = np.ascontiguousarray(np.asarray(inputs['a_conv'][0], np.float32).T)
    a_gc = np.concatenate([np.asarray(inputs['a_alog'][0]).reshape(8), np.asarray(inputs['a_dtb'][0]).reshape(8)]).astype(np.float32).reshape(1, 16)
    a_norm = np.asarray(inputs['a_norm'][0], np.float32).reshape(1, 128)
    lru_pc = np.zeros((512, 11), np.float32)
    lru_pc[:, 0:4] = np.asarray(inputs['b_conv_w'][0]).T
    lru_pc[:, 4] = np.asarray(inputs['b_conv_b'][0])
    for d in range(2):
        lru_pc[:, 5 + 3 * d] = np.asarray(inputs['b_ba'][0, d]).reshape(512)
        lru_pc[:, 6 + 3 * d] = np.asarray(inputs['b_bx'][0, d]).reshape(512)
        lru_pc[:, 7 + 3 * d] = np.asarray(inputs['b_lam'][0, d]).reshape(512)
    lru_w = np.zeros((2, 2, 4, 128, 128), np.float32)
    for ax, nm in enumerate(('b_wa', 'b_wx')):
        w = np.asarray(inputs[nm][0], np.float32)
        for d in range(2):
            for ct in range(4):
                lru_w[ax, d, ct, 0:64, 0:64] = w[d, 2 * ct]
                lru_w[ax, d, ct, 64:128, 64:128] = w[d, 2 * ct + 1]
    ii = np.arange(128)
    kk_, aa_ = ii[:, None], ii[None, :]
    masks = np.stack([(kk_ <= aa_), (kk_ >= aa_), (aa_ >= kk_), (aa_ <= kk_), (kk_ > aa_), (kk_ < aa_)]).astype(np.float32)
    peer_wq = np.ascontiguousarray(np.asarray(inputs['peer_wq'], np.float32).reshape(2, KC, 128, 2048).transpose(0, 2, 1, 3))
    peer_kT = np.ascontiguousarray(np.asarray(inputs['peer_keys'], np.float32).reshape(2, 16, 128, 128).transpose(0, 3, 1, 2))
    peer_u = np.asarray(inputs['peer_u'], np.float32)
    peer_v = np.asarray(inputs['peer_v'], np.float32)
    peer_u0, peer_u1, peer_v0, peer_v1 = peer_u[0], peer_u[1], peer_v[0], peer_v[1]
    pconst = np.concatenate([16.0 * (np.arange(16) + 1), np.arange(16)]).astype(np.float32).reshape(1, 32)
    od_w_in = np.ascontiguousarray(np.asarray(inputs['od_w_in'][0], np.float32).reshape(KC, 128, 2064).transpose(1, 0, 2))
    od_w_out = np.ascontiguousarray(np.asarray(inputs['od_w_out'][0], np.float32).reshape(KC, 128, D).transpose(1, 0, 2))
    c_gc = np.concatenate([np.asarray(inputs['c_ibias'][0]).reshape(8), np.asarray(inputs['c_fbias'][0]).reshape(8)]).astype(np.float32).reshape(1, 16)
    c_norm = np.asarray(inputs['c_norm'][0], np.float32).reshape(1, 512)
    d_w = np.asarray(inputs['d_w'][0], np.float32)
    d_scale = np.asarray(inputs['d_scale'][0], np.float32).reshape(512, 1)
    poolm = np.zeros((4, 128, 128), np.float32)
    seg = ROWS
    for gi, w in enumerate((2, 4, 8, 16)):
        P = np.zeros((seg, seg), np.float32)
        for t_ in range(seg):
            lo = max(t_ - w // 2, 0)
            hi = min(t_ - w // 2 + w, seg)
            P[t_, lo:hi] = 1.0 / (hi - lo)
        P = P - np.eye(seg, dtype=np.float32)
        for sgi in range(128 // seg):
            poolm[gi, sgi * seg:(sgi + 1) * seg, sgi * seg:(sgi + 1) * seg] = P.T
    shared = dict(od_w_in=od_w_in, od_w_out=od_w_out, c_gc=c_gc, c_norm=c_norm, d_w=d_w, d_scale=d_scale, poolm=poolm, peer_wq=peer_wq, peer_kT=peer_kT, peer_u0=peer_u0, peer_u1=peer_u1, peer_v0=peer_v0, peer_v1=peer_v1, pconst=pconst, ada_w=ada_w, ada_b=ada_b, norms=norms, ident=ident, ev_w_in=ev_w_in, ev_w_out=ev_w_out, a_convT=a_convT,
                  a_gc=a_gc, a_norm=a_norm, lru_pc=lru_pc, lru_w=lru_w, masks=masks)
    maps = []
    for ci in range(NCORES):
        b0 = ci * NB
        xin = np.concatenate([ctx[b0:b0 + NB], x[b0:b0 + NB]], axis=1)
        c3 = np.stack([c[b0], c[b0 + 1], c_ctx], axis=1)
        c3T = np.ascontiguousarray(c3.reshape(KC, 128, 3).transpose(1, 0, 2))
        maps.append(dict(xin=np.ascontiguousarray(xin), c3T=c3T, **shared))
    return maps


def kernel(**inputs):
    nc = build_program()
    maps = make_in_maps(inputs)
    res = run_bass_kernel_spmd(nc, maps, core_ids=list(range(NCORES)))
    outs = [r["out"] for r in res.results]
    return np.concatenate(outs, axis=0).astype(np.float32)
```

```python
import numpy as np
from contextlib import ExitStack
import concourse.bass as bass
import concourse.mybir as mybir
from concourse.bass_utils import run_bass_kernel_spmd

F32 = mybir.dt.float32
U32 = mybir.dt.uint32
I32 = mybir.dt.int32
AF = mybir.ActivationFunctionType
ALU = mybir.AluOpType
AX = mybir.AxisListType

NCORES = 8
NB = 2
D = 1024
KC = 8
LC = 256
LL = 2048
LT = LC + LL
NT = LT // 128
EPS = 1e-6
GRID_W = 64
ROWS = LL // GRID_W


class KB:
    def __init__(self, nc, es, ndma=16):
        self.nc = nc
        self.eng = {'pe': nc.tensor, 'act': nc.scalar, 'dve': nc.vector, 'pool': nc.gpsimd, 'sp': nc.sync}
        self.sems = {}
        self.val = {}
        for e in ['pe', 'act', 'dve', 'pool']:
            self.sems[e] = es.enter_context(nc.semaphore('s_' + e))
            self.val[e] = 0
        self.dq = {}
        for q in ['sp', 'pool', 'act']:
            ids = []
            for i in range(ndma):
                sid = ('d', q, i)
                self.sems[sid] = es.enter_context(nc.semaphore(f'd_{q}_{i}'))
                self.val[sid] = 0
                ids.append(sid)
            self.dq[q] = [ids, 0]
        self.seen = {e: {} for e in self.eng}
        self.lastw = {}
        self.readers = {}
        self.n_inst = 0

    def _waits(self, eng, reads, writes, extra=()):
        need = {}

        def add(ev):
            if ev is None:
                return
            sid, v = ev
            if need.get(sid, 0) < v:
                need[sid] = v
        for r in reads:
            add(self.lastw.get(r))
        for w in writes:
            add(self.lastw.get(w))
            for ev in self.readers.get(w, {}).items():
                add(ev)
        for ev in extra:
            add(ev)
        e = self.eng[eng]
        for sid, v in need.items():
            if sid == 'pe' and eng == 'pe':
                continue
            if self.seen[eng].get(sid, 0) >= v:
                continue
            self.seen[eng][sid] = v
            e.wait_ge(self.sems[sid], v)
            self.n_inst += 1

    def _record(self, ev, reads, writes):
        sid, v = ev
        for w in writes:
            self.lastw[w] = ev
            self.readers[w] = {}
        for r in reads:
            d = self.readers.setdefault(r, {})
            if d.get(sid, 0) < v:
                d[sid] = v

    def I(self, eng, reads, writes, fn):
        self._waits(eng, reads, writes)
        ins = fn(self.eng[eng])
        self.val[eng] += 1
        ins.then_inc(self.sems[eng], 1)
        self._record((eng, self.val[eng]), reads, writes)
        self.n_inst += 1
        return ins

    def DMA(self, q, reads, writes, fn):
        ids, rr = self.dq[q]
        sid = ids[rr % len(ids)]
        self.dq[q][1] = rr + 1
        self._waits(q, reads, writes, extra=[(sid, self.val[sid])] if self.val[sid] else ())
        ins = fn(self.eng[q])
        self.val[sid] += 16
        ins.then_inc(self.sems[sid], 16)
        self._record((sid, self.val[sid]), reads, writes)
        self.n_inst += 1
        return ins

    def barrier(self):
        for eng in self.eng:
            e = self.eng[eng]
            for sid, v in self.val.items():
                if v == 0 or self.seen[eng].get(sid, 0) >= v:
                    continue
                if sid == eng:
                    continue
                self.seen[eng][sid] = v
                e.wait_ge(self.sems[sid], v)
                self.n_inst += 1

    def finish(self):
        e = self.eng['sp']
        for sid, v in self.val.items():
            if v and self.seen['sp'].get(sid, 0) < v:
                e.wait_ge(self.sems[sid], v)


class Ring:
    def __init__(self, items):
        self.items = items
        self.i = 0

    def next(self):
        it = self.items[self.i % len(self.items)]
        self.i += 1
        return it


def bcast_rows(ap_1d_row, nparts):
    return ap_1d_row.to_broadcast([nparts, ap_1d_row.shape[-1]])


def build_program(stop_after=None, dbg=None):
    nc = bass.Bass("TRN2", target_bir_lowering=False)
    es = ExitStack()
    kb = KB(nc, es)
    dbg = dbg or {}

    def din(name, shape, dt=F32):
        return nc.dram_tensor(name, list(shape), dt, kind="ExternalInput").ap()

    def dscr(name, shape, dt=F32):
        return nc.dram_tensor(name, list(shape), dt, kind="Internal").ap()

    def dout(name, shape, dt=F32):
        return nc.dram_tensor(name, list(shape), dt, kind="ExternalOutput").ap()

    uid = [0]

    def sb(name, shape, dt=F32, stack=es):
        uid[0] += 1
        return stack.enter_context(nc.sbuf_tensor(f"{name}_u{uid[0]}", list(shape), dt))

    xin = din("xin", [NB, LT, D])
    c3T = din("c3T", [128, KC, 3])
    ada_w = din("ada_w", [2, KC, 128, 6 * D])
    ada_b = din("ada_b", [2, 1, 6 * D])
    norms = din("norms", [5, 1, D])
    ident_d = din("ident", [128, 128])
    modsD = dscr("modsD", [2, 3, 6 * D])
    out_d = dout("out", [NB, LL, D])
    dbg_out = {}
    for k, shp in dbg.items():
        dbg_out[k] = dout("dbg_" + k, shp)

    ident = sb("ident_sb", [128, 128])
    ps = [es.enter_context(nc.psum_tensor(f"ps{i}", [128, 512], F32)) for i in range(8)]
    psk = [f"ps{i}" for i in range(8)]
    kb.DMA('sp', [], ['ident'], lambda e: e.dma_start(out=ident[:], in_=ident_d[:, :]))
    epsc = sb("epsc", [128, 1])
    kb.I('dve', [], ['epsc'], lambda e: e.memset(epsc[:], EPS))

    with ExitStack() as ph:
        sT = sb("sT", [128, KC, 3], stack=ph)
        kb.DMA('sp', [], ['sT'], lambda e: e.dma_start(out=sT[:], in_=c3T[:, :, :]))
        kb.I('act', ['sT'], ['sT'], lambda e: e.activation(out=sT[:], in_=sT[:], func=AF.Silu))
        wbuf = [sb(f"adaw{i}", [128, 3072], stack=ph) for i in range(3)]
        wring = Ring(list(range(3)))
        bias_t = sb("adab", [3, 6 * D], stack=ph)
        mods_t = sb("mods_t", [3, 6 * D], stack=ph)
        for l in range(2):
            kb.DMA('sp', ['mods_st'], ['adab'], lambda e: e.dma_start(out=bias_t[:], in_=ada_b[l].to_broadcast([3, 6 * D])))
            for half in range(2):
                for k in range(KC):
                    wi = wring.next()
                    kb.DMA('sp', [], [f'adaw{wi}'], lambda e: e.dma_start(out=wbuf[wi][:], in_=ada_w[l, k, :, half * 3072:(half + 1) * 3072]))
                    for j in range(6):
                        kb.I('pe', ['sT', f'adaw{wi}'], [psk[j]], lambda e: e.matmul(ps[j][0:3, :], lhsT=sT[:, k, :], rhs=wbuf[wi][:, j * 512:(j + 1) * 512], start=(k == 0), stop=(k == KC - 1)))
                for j in range(6):
                    c0 = half * 3072 + j * 512
                    kb.I('dve', [psk[j], 'adab'], ['mods_t'], lambda e: e.tensor_tensor(out=mods_t[:, c0:c0 + 512], in0=ps[j][0:3, :], in1=bias_t[:, c0:c0 + 512], op=ALU.add))
            kb.DMA('sp', ['mods_t'], [('modsD', l), 'mods_st'], lambda e: e.dma_start(out=modsD[l], in_=mods_t[:]))
    kb.barrier()
    if 'mods' in dbg_out:
        kb.DMA('sp', [('modsD', 0), ('modsD', 1)], ['dbg_mods'], lambda e: e.dma_start(out=dbg_out['mods'], in_=modsD))
    if stop_after == 'mods':
        kb.finish(); es.close(); return nc

    ev_w_in = din("ev_w_in", [128, KC, 3088])
    ev_w_out = din("ev_w_out", [128, KC, D])
    a_convT = din("a_convT", [1536, 4])
    a_gc = din("a_gc", [1, 16])
    a_norm = din("a_norm", [1, 128])
    lru_pc = din("lru_pc", [512, 11])
    lru_w = din("lru_w", [2, 2, 4, 128, 128])
    masks_d = din("masks", [6, 128, 128])
    mixT = dscr("mixT", [NB, KC, 128, LT])
    l1only = (stop_after or '').startswith('l1only')
    xs = din("xs_in", [NB, LT, D]) if l1only else dscr("xs", [NB, LT, D])

    ones = sb("ones_sb", [128, 128])
    kb.I('dve', [], ['ones'], lambda e: e.memset(ones[:], 1.0))
    onec = sb("onec", [128, 1])
    kb.I('dve', [], ['onec'], lambda e: e.memset(onec[:], 1.0))
    msk = sb("msk", [128, 6, 128])
    for i in range(6):
        kb.DMA('sp', [], ['msk'], lambda e: e.dma_start(out=msk[:, i, :], in_=masks_d[i]))
    psr = Ring(list(range(8)))

    def mm(out, lhsT, rhs, reads, writes, start=True, stop=True):
        kb.I('pe', reads, writes, lambda e: e.matmul(out, lhsT=lhsT, rhs=rhs, start=start, stop=stop))

    def tr(out, in_, reads, writes):
        kb.I('pe', reads + ['ident'], writes, lambda e: e.transpose(out=out, in_=in_, identity=ident[:]))

    TOKBLKS = [(0, 256)] + [(256 + i * 512, 512) for i in range(4)]

    def norm_mod_T(ph, l, b, src_ap_fn, norm_idx, slot_sh, slot_sc, hT, keep_h=None, tiles=range(NT)):
        with ExitStack() as st:
            nw = sb("nm_nw", [128, D], stack=st)
            kb.DMA('sp', [], ['nm_nw'], lambda e: e.dma_start(out=nw[:], in_=norms[norm_idx].to_broadcast([128, D])))
            md = {}
            for kind, row in (('c', 2), ('l', b)):
                A = sb("nm_A" + kind, [128, D], stack=st)
                S = sb("nm_S" + kind, [128, D], stack=st)
                kb.DMA('sp', [('modsD', l)], ['nm_A' + kind], lambda e: e.dma_start(out=A[:], in_=modsD[l, row:row + 1, slot_sc * D:(slot_sc + 1) * D].to_broadcast([128, D])))
                kb.DMA('sp', [('modsD', l)], ['nm_S' + kind], lambda e: e.dma_start(out=S[:], in_=modsD[l, row:row + 1, slot_sh * D:(slot_sh + 1) * D].to_broadcast([128, D])))
                kb.I('dve', ['nm_A' + kind, 'nm_nw'], ['nm_A' + kind], lambda e: e.scalar_tensor_tensor(out=A[:], in0=A[:], scalar=1.0, in1=nw[:], op0=ALU.add, op1=ALU.mult))
                md[kind] = (A, S)
            xt = [sb(f"nm_x{i}", [128, D], stack=st) for i in range(2)]
            ht = [sb(f"nm_h{i}", [128, D], stack=st) for i in range(2)]
            sq = sb("nm_sq", [128, D], stack=st)
            ss = [sb(f"nm_ss{i}", [128, 1], stack=st) for i in range(2)]
            for t in tiles:
                i = t % 2
                kind = 'c' if t < 2 else 'l'
                A, S = md[kind]
                src, srck = src_ap_fn(b, t)
                kb.DMA('sp', [srck], [f'nm_x{i}'], lambda e: e.dma_start(out=xt[i][:], in_=src))
                kb.I('act', [f'nm_x{i}'], ['nm_sq', f'nm_ss{i}'], lambda e: e.activation(out=sq[:], in_=xt[i][:], func=AF.Square, accum_out=ss[i][:]))
                kb.I('act', [f'nm_ss{i}', 'epsc'], [f'nm_ss{i}'], lambda e: e.activation(out=ss[i][:], in_=ss[i][:], func=AF.Sqrt, scale=1.0 / D, bias=epsc[:]))
                kb.I('dve', [f'nm_ss{i}'], [f'nm_ss{i}'], lambda e: e.reciprocal(out=ss[i][:], in_=ss[i][:]))
                kb.I('dve', [f'nm_x{i}', f'nm_ss{i}', 'nm_A' + kind], [f'nm_h{i}'], lambda e: e.scalar_tensor_tensor(out=ht[i][:], in0=xt[i][:], scalar=ss[i][:], in1=A[:], op0=ALU.mult, op1=ALU.mult))
                kb.I('dve', [f'nm_h{i}', 'nm_S' + kind], [f'nm_h{i}'], lambda e: e.tensor_tensor(out=ht[i][:], in0=ht[i][:], in1=S[:], op=ALU.add))
                if keep_h is not None:
                    keep_h(t, ht[i], f'nm_h{i}')
                for half in range(2):
                    pb = psr.next()
                    for kk in range(4):
                        k = half * 4 + kk
                        tr(ps[pb][:, kk * 128:(kk + 1) * 128], ht[i][:, k * 128:(k + 1) * 128], [f'nm_h{i}'], [psk[pb]])
                    kb.I('act', [psk[pb]], [('hT', t)], lambda e: e.activation(out=hT[:, half * 4:half * 4 + 4, t * 128:(t + 1) * 128], in_=ps[pb][:, :].rearrange("p (k t) -> p k t", k=4), func=AF.Copy))
            kb.barrier()

    def proj_fm(hT, w_sb, wkey, dst_fn, evac):
        for (t0, n) in TOKBLKS:
            pb = psr.next()
            for k in range(KC):
                mm(ps[pb][:, 0:n], w_sb[:, k, :], hT[:, k, t0:t0 + n], [wkey] + [('hT', tt) for tt in range(t0 // 128, (t0 + n) // 128)], [psk[pb]], start=(k == 0), stop=(k == KC - 1))
            evac(pb, t0, n)

    def layer0_mixer(b):
        with ExitStack() as ph:
            hT = sb("hT", [128, KC, LT], stack=ph)
            norm_mod_T(ph, 0, b, lambda b_, t: (xin[b_, t * 128:(t + 1) * 128, :], 'xin'), 0, 0, 1, hT)
            if 'hT0' in dbg_out and b == 0:
                kb.DMA('sp', [('hT', t) for t in range(NT)], ['dbg_hT0'], lambda e: e.dma_start(out=dbg_out['hT0'], in_=hT[:]))
            if stop_after == 'hT0':
                return
            with ExitStack() as st:
                wx = sb("l_wx", [128, KC, 128], stack=st)
                wg = sb("l_wg", [128, KC, 128], stack=st)
                pc = sb("l_pc", [128, 11], stack=st)
                cl = sb("l_cl", [128, 4], stack=st)
                gw = sb("l_gw", [128, 4, 128], stack=st)
                raw = sb("l_raw", [128, LT + 6], stack=st)
                xb = sb("l_xb", [128, LT], stack=st)
                gg = sb("l_gg", [128, LT], stack=st)
                t1 = sb("l_t1", [128, LT], stack=st)
                t2 = sb("l_t2", [128, LT], stack=st)
                av = sb("l_a", [128, LT], stack=st)
                bv = sb("l_b", [128, LT], stack=st)
                hf = sb("l_hf", [128, LT], stack=st)
                hb = sb("l_hb", [128, LT], stack=st)
                kb.I('dve', [], ['l_raw'], lambda e: e.memset(raw[:], 0.0))
                RC, RL = 0, LC + 3

                def rawpos(t0):
                    return (RC + 1 + t0) if t0 < LC else (RL + 1 + (t0 - LC))
                for ct in range(4):
                    c0 = 2064 + ct * 128
                    kb.DMA('sp', [], ['l_wx'], lambda e: e.dma_start(out=wx[:], in_=ev_w_in[:, :, c0:c0 + 128]))
                    kb.DMA('sp', [], ['l_wg'], lambda e: e.dma_start(out=wg[:], in_=ev_w_in[:, :, c0 + 512:c0 + 640]))
                    kb.DMA('sp', [], ['l_pc'], lambda e: e.dma_start(out=pc[:], in_=lru_pc[ct * 128:(ct + 1) * 128, :]))
                    for ax in range(2):
                        for d in range(2):
                            kb.DMA('sp', [], ['l_gw'], lambda e: e.dma_start(out=gw[:, ax * 2 + d, :], in_=lru_w[ax, d, ct]))
                    for d in range(2):
                        kb.I('act', ['l_pc'], ['l_cl'], lambda e: e.activation(out=cl[:, d:d + 1], in_=pc[:, 7 + 3 * d:8 + 3 * d], func=AF.Exp, scale=-1.0))
                    kb.I('act', ['l_cl', 'onec'], ['l_cl'], lambda e: e.activation(out=cl[:, 0:2], in_=cl[:, 0:2], func=AF.Ln, bias=onec[:]))
                    kb.I('dve', ['l_cl'], ['l_cl'], lambda e: e.tensor_scalar(out=cl[:, 2:4], in0=cl[:, 0:2], scalar1=-16.0, scalar2=None, op0=ALU.mult))
                    kb.I('dve', ['l_cl'], ['l_cl'], lambda e: e.tensor_scalar(out=cl[:, 0:2], in0=cl[:, 0:2], scalar1=-8.0, scalar2=None, op0=ALU.mult))
                    proj_fm(hT, wx, 'l_wx', None, lambda pb, t0, n: kb.I('act', [psk[pb]], ['l_raw'], lambda e: e.activation(out=raw[:, rawpos(t0):rawpos(t0) + n], in_=ps[pb][:, 0:n], func=AF.Copy)))
                    for (o0, r0, L) in ((0, RC, LC), (LC, RL, LL)):
                        kb.I('dve', ['l_raw', 'l_pc'], ['l_xb'], lambda e: e.tensor_scalar(out=xb[:, o0:o0 + L], in0=raw[:, r0:r0 + L], scalar1=pc[:, 0:1], scalar2=pc[:, 4:5], op0=ALU.mult, op1=ALU.add))
                        for j in range(1, 4):
                            kb.I('dve', ['l_raw', 'l_pc', 'l_xb'], ['l_xb'], lambda e: e.scalar_tensor_tensor(out=xb[:, o0:o0 + L], in0=raw[:, r0 + j:r0 + j + L], scalar=pc[:, j:j + 1], in1=xb[:, o0:o0 + L], op0=ALU.mult, op1=ALU.add))
                    proj_fm(hT, wg, 'l_wg', None, lambda pb, t0, n: kb.I('act', [psk[pb]], ['l_t1'], lambda e: e.activation(out=t1[:, t0:t0 + n], in_=ps[pb][:, 0:n], func=AF.Copy)))
                    kb.I('dve', ['l_t1'], ['l_t2'], lambda e: e.tensor_tensor(out=t2[:], in0=t1[:], in1=t1[:], op=ALU.mult))
                    kb.I('dve', ['l_t2'], ['l_t2'], lambda e: e.tensor_scalar(out=t2[:], in0=t2[:], scalar1=0.044715, scalar2=1.0, op0=ALU.mult, op1=ALU.add))
                    kb.I('dve', ['l_t2', 'l_t1'], ['l_t2'], lambda e: e.tensor_tensor(out=t2[:], in0=t2[:], in1=t1[:], op=ALU.mult))
                    kb.I('act', ['l_t2'], ['l_t2'], lambda e: e.activation(out=t2[:], in_=t2[:], func=AF.Sigmoid, scale=1.5957691216057308))
                    kb.I('dve', ['l_t2', 'l_t1'], ['l_gg'], lambda e: e.tensor_tensor(out=gg[:], in0=t2[:], in1=t1[:], op=ALU.mult))
                    for d in range(2):
                        for (t0, n) in TOKBLKS:
                            for ax, dst, dk_ in ((0, t1, 'l_t1'), (1, t2, 'l_t2')):
                                pb = psr.next()
                                mm(ps[pb][:, 0:n], gw[:, ax * 2 + d, :], xb[:, t0:t0 + n], ['l_gw', 'l_xb'], [psk[pb]])
                                bcol = 5 + 3 * d + ax
                                kb.I('act', [psk[pb], 'l_pc'], [dk_], lambda e: e.activation(out=dst[:, t0:t0 + n], in_=ps[pb][:, 0:n], func=AF.Sigmoid, bias=pc[:, bcol:bcol + 1]))
                        kb.I('act', ['l_t1', 'l_cl'], ['l_a'], lambda e: e.activation(out=av[:], in_=t1[:], func=AF.Exp, scale=cl[:, d:d + 1]))
                        kb.I('act', ['l_t1', 'l_cl'], ['l_b'], lambda e: e.activation(out=bv[:], in_=t1[:], func=AF.Exp, scale=cl[:, 2 + d:3 + d]))
                        kb.I('dve', ['l_b'], ['l_b'], lambda e: e.tensor_scalar(out=bv[:], in0=bv[:], scalar1=-1.0, scalar2=1.0, op0=ALU.mult, op1=ALU.add))
                        kb.I('dve', ['l_b'], ['l_b'], lambda e: e.tensor_scalar(out=bv[:], in0=bv[:], scalar1=0.0, scalar2=None, op0=ALU.max))
                        kb.I('act', ['l_b'], ['l_b'], lambda e: e.activation(out=bv[:], in_=bv[:], func=AF.Sqrt))
                        kb.I('dve', ['l_b', 'l_t2'], ['l_b'], lambda e: e.tensor_tensor(out=bv[:], in0=bv[:], in1=t2[:], op=ALU.mult))
                        kb.I('dve', ['l_b', 'l_xb'], ['l_b'], lambda e: e.tensor_tensor(out=bv[:], in0=bv[:], in1=xb[:], op=ALU.mult))
                        if d == 0:
                            kb.I('dve', ['l_a', 'l_b'], ['l_hf'], lambda e: e.tensor_tensor_scan(out=hf[:, :], data0=av[:, :], data1=bv[:, :], initial=0.0, op0=ALU.mult, op1=ALU.add))
                        else:
                            kb.I('dve', ['l_a', 'l_b'], ['l_hb'], lambda e: e.tensor_tensor_scan(out=hb[:, LC - 1::-1], data0=av[:, LC - 1::-1], data1=bv[:, LC - 1::-1], initial=0.0, op0=ALU.mult, op1=ALU.add))
                            kb.I('dve', ['l_a', 'l_b', 'l_hb'], ['l_hb'], lambda e: e.tensor_tensor_scan(out=hb[:, LT - 1:LC - 1:-1], data0=av[:, LT - 1:LC - 1:-1], data1=bv[:, LT - 1:LC - 1:-1], initial=hb[:, 0:1], op0=ALU.mult, op1=ALU.add))
                    kb.I('dve', ['l_hf', 'l_hb'], ['l_hf'], lambda e: e.tensor_tensor(out=hf[:], in0=hf[:], in1=hb[:], op=ALU.add))
                    kb.I('dve', ['l_hf', 'l_gg'], ['l_hf'], lambda e: e.tensor_tensor(out=hf[:], in0=hf[:], in1=gg[:], op=ALU.mult))
                    kb.DMA('sp', ['l_hf'], [('mixT', b)], lambda e: e.dma_start(out=mixT[b, 4 + ct], in_=hf[:]))
                kb.barrier()
            if stop_after == 'lru':
                return
            gdn(ph, b, hT)
            kb.barrier()

    def gdn(ph, b, hT):
        with ExitStack() as st:
            wg16 = sb("g_wg16", [128, KC, 16], stack=st)
            cw = sb("g_cw", [128, 3, 4], stack=st)
            gcst = sb("g_gcst", [128, 16], stack=st)
            anw = sb("g_anw", [128, 128], stack=st)
            T3 = [sb(f"g_T{i}", [128, LT], stack=st) for i in range(3)]
            tmp = sb("g_tmp", [128, LT], stack=st)
            Ktm = sb("g_Ktm", [128, NT, 128], stack=st)
            Vtm = sb("g_Vtm", [128, NT, 128], stack=st)
            OKEYS = [('g_O', n_) for n_ in range(NT)]
            O = sb("g_O", [128, NT, 128], stack=st)
            graw = sb("g_graw", [128, NT, 16], stack=st)
            gG = sb("g_g", [128, NT, 8], stack=st)
            gB = sb("g_bt", [128, NT, 8], stack=st)
            RC, RL = 0, LC + 3

            def rawpos(t0):
                return (RC + 1 + t0) if t0 < LC else (RL + 1 + (t0 - LC))
            kb.DMA('sp', [], ['g_gcst'], lambda e: e.dma_start(out=gcst[:], in_=a_gc[0:1, :].to_broadcast([128, 16])))
            kb.DMA('sp', [], ['g_anw'], lambda e: e.dma_start(out=anw[:], in_=a_norm[0:1, :].to_broadcast([128, 128])))
            kb.I('act', ['g_gcst'], ['g_gcst'], lambda e: e.activation(out=gcst[:, 0:8], in_=gcst[:, 0:8], func=AF.Exp))
            kb.I('dve', ['g_gcst'], ['g_gcst'], lambda e: e.tensor_scalar(out=gcst[:, 0:8], in0=gcst[:, 0:8], scalar1=-1.0, scalar2=None, op0=ALU.mult))
            kb.DMA('sp', [], ['g_wg16'], lambda e: e.dma_start(out=wg16[:], in_=ev_w_in[:, :, 2048:2064]))
            pb = psr.next()
            for n in range(NT):
                for k in range(KC):
                    mm(ps[pb][:, n * 16:(n + 1) * 16], hT[:, k, n * 128:(n + 1) * 128], wg16[:, k, :], ['g_wg16', ('hT', n)], [psk[pb]], start=(k == 0), stop=(k == KC - 1))
            kb.I('act', [psk[pb]], ['g_graw'], lambda e: e.activation(out=graw[:], in_=ps[pb][:, 0:NT * 16].rearrange("p (n c) -> p n c", c=16), func=AF.Copy))
            kb.I('dve', ['g_graw', 'g_gcst'], ['g_g'], lambda e: e.tensor_tensor(out=gG[:], in0=graw[:, :, 0:8], in1=gcst[:, 8:16].unsqueeze(1).to_broadcast([128, NT, 8]), op=ALU.add))
            kb.I('act', ['g_g'], ['g_g'], lambda e: e.activation(out=gG[:], in_=gG[:], func=AF.Exp))
            kb.I('act', ['g_g', 'onec'], ['g_g'], lambda e: e.activation(out=gG[:], in_=gG[:], func=AF.Ln, bias=onec[:]))
            kb.I('dve', ['g_g', 'g_gcst'], ['g_g'], lambda e: e.tensor_tensor(out=gG[:], in0=gG[:], in1=gcst[:, 0:8].unsqueeze(1).to_broadcast([128, NT, 8]), op=ALU.mult))
            kb.I('act', ['g_graw'], ['g_bt'], lambda e: e.activation(out=gB[:], in_=graw[:, :, 8:16], func=AF.Sigmoid))
            if 'gates' in dbg_out and b == 0:
                kb.DMA('sp', ['g_g'], ['dbg_gates'], lambda e: e.dma_start(out=dbg_out['gates'][:, :, 0:8], in_=gG[:]))
                kb.DMA('sp', ['g_bt'], ['dbg_gates'], lambda e: e.dma_start(out=dbg_out['gates'][:, :, 8:16], in_=gB[:]))

            for hd in range(4):
                with ExitStack() as hs:
                    wh = sb("g_wh", [128, 4, KC, 128], stack=hs)
                    raw = sb("g_raw", [128, LT + 6], stack=hs)
                    kb.I('dve', [], ['g_raw'], lambda e: e.memset(raw[:], 0.0))
                    for part in range(4):
                        c0 = part * 512 + hd * 128
                        kb.DMA('sp', [], ['g_wh'], lambda e: e.dma_start(out=wh[:, part], in_=ev_w_in[:, :, c0:c0 + 128]))
                    for part in range(3):
                        c0 = part * 512 + hd * 128
                        kb.DMA('sp', [], ['g_cw'], lambda e: e.dma_start(out=cw[:, part, :], in_=a_convT[c0:c0 + 128, :]))
                    for part in range(3):
                        Tp, tk = T3[part], f'g_T{part}'
                        proj_fm(hT, wh[:, part], 'g_wh', None, lambda pb, t0, n: kb.I('act', [psk[pb]], ['g_raw'], lambda e: e.activation(out=raw[:, rawpos(t0):rawpos(t0) + n], in_=ps[pb][:, 0:n], func=AF.Copy)))
                        for (o0, r0, L) in ((0, RC, LC), (LC, RL, LL)):
                            kb.I('dve', ['g_raw', 'g_cw'], ['g_tmp'], lambda e: e.tensor_scalar(out=tmp[:, o0:o0 + L], in0=raw[:, r0:r0 + L], scalar1=cw[:, part, 0:1], scalar2=None, op0=ALU.mult))
                            for j in range(1, 4):
                                kb.I('dve', ['g_raw', 'g_cw', 'g_tmp'], ['g_tmp'], lambda e: e.scalar_tensor_tensor(out=tmp[:, o0:o0 + L], in0=raw[:, r0 + j:r0 + j + L], scalar=cw[:, part, j:j + 1], in1=tmp[:, o0:o0 + L], op0=ALU.mult, op1=ALU.add))
                        kb.I('act', ['g_tmp'], [tk], lambda e: e.activation(out=Tp[:], in_=tmp[:], func=AF.Silu))
                        if part < 2:
                            kb.I('act', [tk], ['g_tmp'], lambda e: e.activation(out=tmp[:], in_=Tp[:], func=AF.Square))
                            for (t0, n) in TOKBLKS:
                                pb = psr.next()
                                mm(ps[pb][:, 0:n], ones[:], tmp[:, t0:t0 + n], ['ones', 'g_tmp'], [psk[pb]])
                                kb.I('act', [psk[pb], 'epsc', 'g_tmp'], ['g_tmp'], lambda e: e.activation(out=tmp[:, t0:t0 + n], in_=ps[pb][:, 0:n], func=AF.Sqrt, bias=epsc[:]))
                            kb.I('dve', ['g_tmp'], ['g_tmp'], lambda e: e.reciprocal(out=tmp[:], in_=tmp[:]))
                            sc_ = (128 ** -0.5) if part == 0 else 1.0
                            kb.I('dve', [tk, 'g_tmp'], [tk], lambda e: e.scalar_tensor_tensor(out=Tp[:], in0=Tp[:], scalar=sc_, in1=tmp[:], op0=ALU.mult, op1=ALU.mult))
                    for src, dst, dk_ in ((T3[1], Ktm, 'g_Ktm'), (T3[2], Vtm, 'g_Vtm')):
                        sk = 'g_T1' if dst is Ktm else 'g_T2'
                        for n0 in range(0, NT, 4):
                            pb = psr.next()
                            nn = min(4, NT - n0)
                            for i in range(nn):
                                tr(ps[pb][:, i * 128:(i + 1) * 128], src[:, (n0 + i) * 128:(n0 + i + 1) * 128], [sk], [psk[pb]])
                            kb.I('act', [psk[pb]], [dk_], lambda e: e.activation(out=dst[:, n0:n0 + nn, :], in_=ps[pb][:, 0:nn * 128].rearrange("p (n c) -> p n c", c=128), func=AF.Copy))
                    zs = T3[2][:, :].rearrange("p (n c) -> p n c", c=128)
                    for n0 in range(0, NT, 4):
                        pb = psr.next()
                        nn = min(4, NT - n0)
                        for i in range(nn):
                            n = n0 + i
                            for k in range(KC):
                                mm(ps[pb][:, i * 128:(i + 1) * 128], hT[:, k, n * 128:(n + 1) * 128], wh[:, 3, k, :], ['g_wh', ('hT', n)], [psk[pb]], start=(k == 0), stop=(k == KC - 1))
                        kb.I('act', [psk[pb]], ['g_T2'], lambda e: e.activation(out=zs[:, n0:n0 + nn, :], in_=ps[pb][:, 0:nn * 128].rearrange("p (n c) -> p n c", c=128), func=AF.Silu))
                    kb.barrier()
                gdn_chains(st, b, hd, T3, Ktm, Vtm, gG, gB, O)
                ssq = sb(f"g_ssq{hd}", [128, NT], stack=st)
                O2 = tmp[:, :].rearrange("p (n c) -> p n c", c=128)
                kb.I('dve', OKEYS, ['g_tmp'], lambda e: e.tensor_tensor(out=O2, in0=O[:], in1=O[:], op=ALU.mult))
                kb.I('dve', ['g_tmp'], ['g_ssq'], lambda e: e.tensor_reduce(out=ssq[:], in_=O2, axis=AX.X, op=ALU.add))
                kb.I('act', ['g_ssq', 'epsc'], ['g_ssq'], lambda e: e.activation(out=ssq[:], in_=ssq[:], func=AF.Sqrt, scale=1.0 / 128, bias=epsc[:]))
                kb.I('dve', ['g_ssq'], ['g_ssq'], lambda e: e.reciprocal(out=ssq[:], in_=ssq[:]))
                kb.I('dve', OKEYS + ['g_ssq'], OKEYS, lambda e: e.tensor_tensor(out=O[:], in0=O[:], in1=ssq[:].unsqueeze(2).to_broadcast([128, NT, 128]), op=ALU.mult))
                kb.I('dve', OKEYS + ['g_anw'], OKEYS, lambda e: e.tensor_tensor(out=O[:], in0=O[:], in1=anw[:].unsqueeze(1).to_broadcast([128, NT, 128]), op=ALU.mult))
                kb.I('dve', OKEYS + ['g_T2'], OKEYS, lambda e: e.tensor_tensor(out=O[:], in0=O[:], in1=zs, op=ALU.mult))
                for n0 in range(0, NT, 4):
                    pb = psr.next()
                    nn = min(4, NT - n0)
                    for i in range(nn):
                        tr(ps[pb][:, i * 128:(i + 1) * 128], O[:, n0 + i, :], OKEYS, [psk[pb]])
                    kb.I('act', [psk[pb]], ['g_tmp'], lambda e: e.activation(out=tmp[:, n0 * 128:(n0 + nn) * 128], in_=ps[pb][:, 0:nn * 128], func=AF.Copy))
                kb.DMA('sp', ['g_tmp'], [('mixT', b)], lambda e: e.dma_start(out=mixT[b, hd], in_=tmp[:]))
            kb.barrier()

    def gdn_chains(st0, b, hd, T3, Ktm, Vtm, gG, gB, O):
        QT, KT = T3[0], T3[1]
        OK = [('g_O', n_) for n_ in range(NT)]
        with ExitStack() as st:
            names = ["Tg", "dB", "Ei", "EmI", "tt", "Ya", "Yb", "YTa", "YTb", "Ra", "Rb", "qkm", "eg", "qd", "rv", "rk", "U", "WT", "Kd", "vn"]
            sets = {}
            for d_ in range(2):
                for p_ in range(2):
                    B = {nm: sb(f"c_{nm}{d_}{p_}", [128, 128], stack=st) for nm in names}
                    B["cs"] = sb(f"c_cs{d_}{p_}", [128, 8], stack=st)
                    sets[(d_, p_)] = B
            S = {d_: [sb(f"c_S{d_}{i}", [128, 128], stack=st) for i in range(2)] for d_ in range(2)}
            kb.I('dve', [], OK, lambda e: e.memset(O[:], 0.0))
            for d_ in range(2):
                kb.I('dve', [], [f'c_S{d_}0'], lambda e: e.memset(S[d_][0][:], 0.0))
            done = {0: 0, 1: 0}

            def gen(d, p):
                col = d * 4 + hd
                order = ([0, 1] + list(range(2, NT))) if d == 0 else ([1, 0] + list(range(NT - 1, 1, -1)))
                B = sets[(d, p)]
                tag = f"{d}{p}"
                K_ = lambda nm: f"c_{nm}{tag}"
                cs = B["cs"]
                banks = [2 * (2 * d + p), 2 * (2 * d + p) + 1]
                bi = [0]

                def nb():
                    x = banks[bi[0] % 2]
                    bi[0] += 1
                    return x
                for k in range(p, NT, 2):
                    n = order[k]
                    gcol = gG[:, n, col:col + 1]
                    bcol = gB[:, n, col:col + 1]
                    tok = slice(n * 128, (n + 1) * 128)
                    kb.I('dve', ['g_g', 'msk'], [K_('Tg')], lambda e: e.tensor_scalar(out=B["Tg"][:], in0=msk[:, d, :], scalar1=gcol, scalar2=None, op0=ALU.mult)); yield
                    kb.I('dve', ['g_bt', 'ident'], [K_('dB')], lambda e: e.tensor_scalar(out=B["dB"][:], in0=ident[:], scalar1=bcol, scalar2=None, op0=ALU.mult)); yield
                    pa = nb()
                    mm(ps[pa][:, 0:128], ones[:], B["Tg"][:], ['ones', K_('Tg')], [psk[pa]]); yield
                    mm(ps[pa][:, 128:256], msk[:, 4 + d, :], B["dB"][:], ['msk', K_('dB')], [psk[pa]]); yield
                    mm(ps[pa][:, 256:257], msk[:, d, :], gcol, ['msk', 'g_g'], [psk[pa]]); yield
                    mm(ps[pa][:, 257:258], ones[:], gcol, ['ones', 'g_g'], [psk[pa]]); yield
                    kb.I('act', [psk[pa]], [K_('cs')], lambda e: e.activation(out=cs[:, 0:2], in_=ps[pa][:, 256:258], func=AF.Copy)); yield
                    kb.I('act', [K_('cs')], [K_('cs')], lambda e: e.activation(out=cs[:, 2:3], in_=cs[:, 0:1], func=AF.Exp)); yield
                    kb.I('dve', [K_('cs'), 'g_bt'], [K_('cs')], lambda e: e.tensor_tensor(out=cs[:, 3:4], in0=cs[:, 2:3], in1=bcol, op=ALU.mult)); yield
                    kb.I('act', [K_('cs')], [K_('cs')], lambda e: e.activation(out=cs[:, 4:5], in_=cs[:, 0:1], func=AF.Exp, scale=-1.0, bias=cs[:, 1:2])); yield
                    kb.I('act', [K_('cs')], [K_('cs')], lambda e: e.activation(out=cs[:, 5:6], in_=cs[:, 1:2], func=AF.Exp)); yield
                    kb.I('dve', [psk[pa], K_('cs')], [K_('Ei')], lambda e: e.tensor_scalar(out=B["Ei"][:], in0=ps[pa][:, 0:128], scalar1=cs[:, 0:1], scalar2=0.0, op0=ALU.subtract, op1=ALU.min)); yield
                    kb.I('act', [K_('Ei')], [K_('Ei')], lambda e: e.activation(out=B["Ei"][:], in_=B["Ei"][:], func=AF.Exp)); yield
                    kb.I('act', [psk[pa]], [K_('eg')], lambda e: e.activation(out=B["eg"][:], in_=ps[pa][:, 0:128], func=AF.Exp)); yield
                    kb.I('dve', [K_('Ei'), 'msk'], [K_('EmI')], lambda e: e.tensor_tensor(out=B["EmI"][:], in0=B["Ei"][:], in1=msk[:, 2 + d, :], op=ALU.mult)); yield
                    kb.I('dve', [K_('Ei'), psk[pa]], [K_('tt')], lambda e: e.tensor_tensor(out=B["tt"][:], in0=B["Ei"][:], in1=ps[pa][:, 128:256], op=ALU.mult)); yield
                    pq = nb()
                    mm(ps[pq][:, 0:128], KT[:, tok], KT[:, tok], ['g_T1'], [psk[pq]]); yield
                    mm(ps[pq][:, 128:256], KT[:, tok], QT[:, tok], ['g_T1', 'g_T0'], [psk[pq]]); yield
                    kb.I('dve', [psk[pq], K_('tt')], [K_('Ya')], lambda e: e.scalar_tensor_tensor(out=B["Ya"][:], in0=ps[pq][:, 0:128], scalar=-1.0, in1=B["tt"][:], op0=ALU.mult, op1=ALU.mult)); yield
                    kb.I('dve', [psk[pq], K_('EmI')], [K_('qkm')], lambda e: e.tensor_tensor(out=B["qkm"][:], in0=ps[pq][:, 128:256], in1=B["EmI"][:], op=ALU.mult)); yield
                    kb.I('dve', [K_('eg'), 'g_T0'], [K_('qd')], lambda e: e.tensor_tensor(out=B["qd"][:], in0=QT[:, tok], in1=B["eg"][:], op=ALU.mult)); yield
                    pz = nb()
                    tr(ps[pz][:, 0:128], B["Ya"][:], [K_('Ya')], [psk[pz]]); yield
                    kb.I('act', [psk[pz]], [K_('YTa')], lambda e: e.activation(out=B["YTa"][:], in_=ps[pz][:, 0:128], func=AF.Copy)); yield
                    kb.I('dve', [K_('Ya'), 'ident'], [K_('Ra')], lambda e: e.tensor_tensor(out=B["Ra"][:], in0=B["Ya"][:], in1=ident[:], op=ALU.add)); yield
                    cur = 'a'
                    for lev in range(6):
                        nx = 'b' if cur == 'a' else 'a'
                        yc, ytc, rc = B["Y" + cur], B["YT" + cur], B["R" + cur]
                        yn, ytn, rn = B["Y" + nx], B["YT" + nx], B["R" + nx]
                        pl = nb()
                        mm(ps[pl][:, 128:256], yc[:], ytc[:], [K_('Y' + cur), K_('YT' + cur)], [psk[pl]]); yield
                        kb.I('act', [psk[pl]], [K_('YT' + nx)], lambda e: e.activation(out=ytn[:], in_=ps[pl][:, 128:256], func=AF.Copy)); yield
                        if lev < 5:
                            mm(ps[pl][:, 0:128], ytc[:], yc[:], [K_('Y' + cur), K_('YT' + cur)], [psk[pl]]); yield
                            kb.I('act', [psk[pl]], [K_('Y' + nx)], lambda e: e.activation(out=yn[:], in_=ps[pl][:, 0:128], func=AF.Copy)); yield
                        mm(ps[pl][:, 256:384], ytn[:], rc[:], [K_('YT' + nx), K_('R' + cur)], [psk[pl]]); yield
                        kb.I('dve', [psk[pl], K_('R' + cur)], [K_('R' + nx)], lambda e: e.tensor_tensor(out=rn[:], in0=rc[:], in1=ps[pl][:, 256:384], op=ALU.add)); yield
                        cur = nx
                    Rf, Rk_ = B["R" + cur], K_('R' + cur)
                    kb.I('dve', ['g_Vtm', 'g_bt'], [K_('rv')], lambda e: e.tensor_scalar(out=B["rv"][:], in0=Vtm[:, n, :], scalar1=bcol, scalar2=None, op0=ALU.mult)); yield
                    kb.I('dve', ['g_Ktm', K_('cs')], [K_('rk')], lambda e: e.tensor_scalar(out=B["rk"][:], in0=Ktm[:, n, :], scalar1=cs[:, 3:4], scalar2=None, op0=ALU.mult)); yield
                    kb.I('dve', ['g_Ktm', K_('cs')], [K_('Kd')], lambda e: e.tensor_scalar(out=B["Kd"][:], in0=Ktm[:, n, :], scalar1=cs[:, 4:5], scalar2=None, op0=ALU.mult)); yield
                    pu = nb()
                    mm(ps[pu][:, 0:128], Rf[:], B["rv"][:], [Rk_, K_('rv')], [psk[pu]]); yield
                    mm(ps[pu][:, 128:256], B["rk"][:], Rf[:], [Rk_, K_('rk')], [psk[pu]]); yield
                    kb.I('act', [psk[pu]], [K_('U')], lambda e: e.activation(out=B["U"][:], in_=ps[pu][:, 0:128], func=AF.Copy)); yield
                    kb.I('act', [psk[pu]], [K_('WT')], lambda e: e.activation(out=B["WT"][:], in_=ps[pu][:, 128:256], func=AF.Copy)); yield
                    while done[d] != k:
                        yield
                    Sc, Sn = S[d][k % 2], S[d][(k + 1) % 2]
                    skc, skn = f'c_S{d}{k % 2}', f'c_S{d}{(k + 1) % 2}'
                    p1 = nb()
                    mm(ps[p1][:, 0:128], B["WT"][:], Sc[:], [K_('WT'), skc], [psk[p1]]); yield
                    kb.I('dve', [psk[p1], K_('U')], [K_('vn')], lambda e: e.tensor_tensor(out=B["vn"][:], in0=B["U"][:], in1=ps[p1][:, 0:128], op=ALU.subtract)); yield
                    mm(ps[p1][:, 128:256], B["qd"][:], Sc[:], [K_('qd'), skc], [psk[p1]], start=True, stop=False); yield
                    mm(ps[p1][:, 128:256], B["qkm"][:], B["vn"][:], [K_('qkm'), K_('vn')], [psk[p1]], start=False, stop=True); yield
                    kb.I('dve', [psk[p1], ('g_O', n)], [('g_O', n)], lambda e: e.tensor_tensor(out=O[:, n, :], in0=O[:, n, :], in1=ps[p1][:, 128:256], op=ALU.add)); yield
                    mm(ps[p1][:, 256:384], B["Kd"][:], B["vn"][:], [K_('Kd'), K_('vn')], [psk[p1]]); yield
                    kb.I('dve', [psk[p1], skc, K_('cs')], [skn], lambda e: e.scalar_tensor_tensor(out=Sn[:], in0=Sc[:], scalar=cs[:, 5:6], in1=ps[p1][:, 256:384], op0=ALU.mult, op1=ALU.add))
                    done[d] = k + 1
                    yield

            active = [gen(0, 0), gen(1, 0), gen(0, 1), gen(1, 1)]
            while active:
                for g_ in list(active):
                    try:
                        next(g_)
                    except StopIteration:
                        active.remove(g_)
            kb.barrier()

    def w_out_phase(b):
        with ExitStack() as st:
            wo = sb("o_w", [128, KC, D], stack=st)
            kb.DMA('sp', [], ['o_w'], lambda e: e.dma_start(out=wo[:], in_=ev_w_out[:, :, :]))
            gt = {}
            for kind, row in (('c', 2), ('l', b)):
                g = sb("o_g" + kind, [128, D], stack=st)
                kb.DMA('sp', [('modsD', 0)], ['o_g' + kind], lambda e: e.dma_start(out=g[:], in_=modsD[0, row:row + 1, 2 * D:3 * D].to_broadcast([128, D])))
                gt[kind] = g
            mt = [sb(f"o_m{i}", [128, KC, 128], stack=st) for i in range(2)]
            xt = [sb(f"o_x{i}", [128, D], stack=st) for i in range(2)]
            for t in range(NT):
                i = t % 2
                kind = 'c' if t < 2 else 'l'
                kb.DMA('sp', [('mixT', b)], [f'o_m{i}'], lambda e: e.dma_start(out=mt[i][:], in_=mixT[b, :, :, t * 128:(t + 1) * 128].rearrange("k p t -> p k t")))
                kb.DMA('sp', ['xin'], [f'o_x{i}'], lambda e: e.dma_start(out=xt[i][:], in_=xin[b, t * 128:(t + 1) * 128, :]))
                for hlf in range(2):
                    pb = psr.next()
                    for k in range(KC):
                        mm(ps[pb][:, :], mt[i][:, k, :], wo[:, k, hlf * 512:(hlf + 1) * 512], [f'o_m{i}', 'o_w'], [psk[pb]], start=(k == 0), stop=(k == KC - 1))
                    if 'yl0' in dbg_out and b == 0:
                        yb = sb(f"o_y{t}_{hlf}", [128, 512], stack=st)
                        kb.I('act', [psk[pb]], [f'o_y{t}_{hlf}'], lambda e: e.activation(out=yb[:], in_=ps[pb][:, :], func=AF.Copy))
                        kb.DMA('sp', [f'o_y{t}_{hlf}'], ['dbg_yl0'], lambda e: e.dma_start(out=dbg_out['yl0'][t * 128:(t + 1) * 128, hlf * 512:(hlf + 1) * 512], in_=yb[:]))
                    kb.I('dve', [psk[pb], 'o_g' + kind], [psk[pb]], lambda e: e.tensor_tensor(out=ps[pb][:, :], in0=ps[pb][:, :], in1=gt[kind][:, hlf * 512:(hlf + 1) * 512], op=ALU.mult))
                    kb.I('dve', [psk[pb], f'o_x{i}'], [f'o_x{i}'], lambda e: e.tensor_tensor(out=xt[i][:, hlf * 512:(hlf + 1) * 512], in0=xt[i][:, hlf * 512:(hlf + 1) * 512], in1=ps[pb][:, :], op=ALU.add))
                kb.DMA('sp', [f'o_x{i}'], [('xs', b)], lambda e: e.dma_start(out=xs[b, t * 128:(t + 1) * 128, :], in_=xt[i][:]))
            kb.barrier()

    peer_wq = din("peer_wq", [2, 128, KC, 2048])
    peer_kT = din("peer_kT", [2, 128, 16, 128])
    peer_u = [din(f"peer_u{i}", [16384, D]) for i in range(2)]
    peer_v = [din(f"peer_v{i}", [16384, D]) for i in range(2)]
    pconst = din("pconst", [1, 32])
    xs1 = dscr("xs1", [NB, LL, D])
    NEG = -1.0e30

    def peer_phase(l, b, tiles, src_fn, dst_fn, final=False):
        with ExitStack() as st:
            wqb = [sb(f"p_wq{i}", [128, KC, 512], stack=st) for i in range(2)]
            kT = sb("p_kT", [128, 16, 128], stack=st)
            kb.DMA('sp', [], ['p_kT'], lambda e: e.dma_start(out=kT[:], in_=peer_kT[l]))
            pc = sb("p_pc", [128, 32], stack=st)
            kb.DMA('sp', [], ['p_pc'], lambda e: e.dma_start(out=pc[:], in_=pconst[0:1, :].to_broadcast([128, 32])))
            nw = sb("p_nw", [128, D], stack=st)
            kb.DMA('sp', [], ['p_nw'], lambda e: e.dma_start(out=nw[:], in_=norms[2 + l].to_broadcast([128, D])))
            A = sb("p_A", [128, D], stack=st); S = sb("p_S", [128, D], stack=st)
            Gk = {}
            for kind_ in sorted(set('c' if t_ < 2 else 'l' for t_ in tiles)):
                Gk[kind_] = sb("p_G" + kind_, [128, D], stack=st)
                row_ = 2 if kind_ == 'c' else b
                kb.DMA('sp', [('modsD', l)], ['p_G' + kind_], lambda e: e.dma_start(out=Gk[kind_][:], in_=modsD[l, row_:row_ + 1, 5 * D:6 * D].to_broadcast([128, D])))
            fnw = None
            if final:
                fnw = sb("p_fnw", [128, D], stack=st)
                kb.DMA('sp', [], ['p_fnw'], lambda e: e.dma_start(out=fnw[:], in_=norms[4].to_broadcast([128, D])))
            xt = [sb(f"p_x{i}", [128, D], stack=st) for i in range(2)]
            h2b = [sb(f"p_h2{i}", [128, D], stack=st) for i in range(2)]
            ssf = sb("p_ssf", [128, 1], stack=st)
            sq = sb("p_sq", [128, D], stack=st)
            ss = sb("p_ss", [128, 1], stack=st)
            h2T = sb("p_h2T", [128, KC, 128], stack=st)
            qT = sb("p_qT", [128, 16, 128], stack=st)
            s1 = sb("p_s1", [128, 16, 128], stack=st)
            s2 = sb("p_s2", [128, 16, 128], stack=st)
            m8 = sb("p_m8", [128, 16, 16], stack=st)
            i8 = sb("p_i8", [128, 16, 16], U32, stack=st)
            i8f = sb("p_i8f", [128, 16, 16], stack=st)
            cand = sb("p_cand", [128, 8, 256], stack=st)
            cand2 = s2[:, :, :].rearrange("p (h a) k -> p h (a k)", a=2)
            sc = sb("p_sc", [128, 8, 16], stack=st)
            ci = sb("p_ci", [128, 8, 16], U32, stack=st)
            cf = sb("p_cf", [128, 8, 16], stack=st)
            fi = sb("p_fi", [128, 8, 16], stack=st)
            fj = sb("p_fj", [128, 8, 16], stack=st)
            oh = sb("p_oh", [128, 8, 16, 16], stack=st)
            g1 = sb("p_g1", [128, 8, 16], stack=st)
            g2_ = sb("p_g2", [128, 8, 16], stack=st)
            eidb = [sb(f"p_eid{i}", [128, 128], I32, stack=st) for i in range(2)]
            gateb = [sb(f"p_gate{i}", [128, 8, 16], stack=st) for i in range(2)]
            gsum = sb("p_gsum", [128, 8], stack=st)
            act = sb("p_act", [128, 128], stack=st)
            t1 = sb("p_t1", [128, 128], stack=st)
            wgt = sb("p_wgt", [128, 128], stack=st)
            NG = 13
            gb = [sb(f"p_gb{i}", [128, D], stack=st) for i in range(NG)]
            gring = Ring(list(range(NG)))
            acc = sb("p_acc", [128, D], stack=st)
            junk = sq
            NDG = 4
            dg = [sb(f"p_dg{i}", [128, 128], stack=st) for i in range(NDG)]
            dring = Ring(list(range(NDG)))
            psr = Ring(list(range(6)))
            cur_kind = [None]

            def front(ti, t):
                i = ti % 2
                kind = 'c' if t < 2 else 'l'
                row = 2 if kind == 'c' else b
                if kind != cur_kind[0]:
                    cur_kind[0] = kind
                    kb.DMA('sp', [('modsD', l)], ['p_A'], lambda e: e.dma_start(out=A[:], in_=modsD[l, row:row + 1, 4 * D:5 * D].to_broadcast([128, D])))
                    yield
                    kb.DMA('sp', [('modsD', l)], ['p_S'], lambda e: e.dma_start(out=S[:], in_=modsD[l, row:row + 1, 3 * D:4 * D].to_broadcast([128, D])))
                    yield
                    kb.I('dve', ['p_A', 'p_nw'], ['p_A'], lambda e: e.scalar_tensor_tensor(out=A[:], in0=A[:], scalar=1.0, in1=nw[:], op0=ALU.add, op1=ALU.mult))
                    yield
                sap, skey = src_fn(t)
                kb.DMA('sp', [skey], [f'p_x{i}'], lambda e: e.dma_start(out=xt[i][:], in_=sap))
                yield
                kb.I('act', [f'p_x{i}'], ['p_sq', 'p_ss'], lambda e: e.activation(out=sq[:], in_=xt[i][:], func=AF.Square, accum_out=ss[:]))
                yield
                kb.I('act', ['p_ss', 'epsc'], ['p_ss'], lambda e: e.activation(out=ss[:], in_=ss[:], func=AF.Sqrt, scale=1.0 / D, bias=epsc[:]))
                yield
                kb.I('dve', ['p_ss'], ['p_ss'], lambda e: e.reciprocal(out=ss[:], in_=ss[:]))
                yield
                kb.I('dve', [f'p_x{i}', 'p_ss', 'p_A'], [f'p_h2{i}'], lambda e: e.scalar_tensor_tensor(out=h2b[i][:], in0=xt[i][:], scalar=ss[:], in1=A[:], op0=ALU.mult, op1=ALU.mult))
                yield
                kb.I('dve', [f'p_h2{i}', 'p_S'], [f'p_h2{i}'], lambda e: e.tensor_tensor(out=h2b[i][:], in0=h2b[i][:], in1=S[:], op=ALU.add))
                yield
                for half in range(2):
                    pb = psr.next()
                    for kk in range(4):
                        k = half * 4 + kk
                        tr(ps[pb][:, kk * 128:(kk + 1) * 128], h2b[i][:, k * 128:(k + 1) * 128], [f'p_h2{i}'], [psk[pb]])
                        yield
                    kb.I('act', [psk[pb]], ['p_h2T'], lambda e: e.activation(out=h2T[:, half * 4:half * 4 + 4, :], in_=ps[pb][:, :].rearrange("p (k t) -> p k t", k=4), func=AF.Copy))
                    yield
                for q0 in range(0, 16, 4):
                    pb = psr.next()
                    wi_ = (q0 // 4) % 2
                    wq = wqb[wi_]
                    kb.DMA('sp', [], [f'p_wq{wi_}'], lambda e: e.dma_start(out=wq[:], in_=peer_wq[l, :, :, q0 * 128:(q0 + 4) * 128]))
                    yield
                    for qq in range(4):
                        for k in range(KC):
                            mm(ps[pb][:, qq * 128:(qq + 1) * 128], wq[:, k, qq * 128:(qq + 1) * 128], h2T[:, k, :], [f'p_wq{wi_}', 'p_h2T'], [psk[pb]], start=(k == 0), stop=(k == KC - 1))
                            yield
                    kb.I('act', [psk[pb]], ['p_qT'], lambda e: e.activation(out=qT[:, q0:q0 + 4, :], in_=ps[pb][:, :].rearrange("p (k t) -> p k t", k=4), func=AF.Copy))
                    yield
                for q0 in range(0, 16, 4):
                    pb = psr.next()
                    for qq in range(4):
                        hp = q0 + qq
                        mm(ps[pb][:, qq * 128:(qq + 1) * 128], qT[:, hp, :], kT[:, hp, :], ['p_qT', 'p_kT'], [psk[pb]])
                        yield
                    kb.I('act', [psk[pb]], ['p_s1'], lambda e: e.activation(out=s1[:, q0:q0 + 4, :], in_=ps[pb][:, :].rearrange("p (k t) -> p k t", k=4), func=AF.Copy))
                    yield
                for hp in range(16):
                    kb.I('dve', ['p_s1'], ['p_m8'], lambda e: e.max(out=m8[:, hp, 0:8], in_=s1[:, hp, :]))
                    yield
                    kb.I('dve', ['p_s1', 'p_m8'], ['p_i8'], lambda e: e.max_index(out=i8[:, hp, 0:8], in_max=m8[:, hp, 0:8], in_values=s1[:, hp, :]))
                    yield
                    kb.I('dve', ['p_s1', 'p_m8'], ['p_s2'], lambda e: e.match_replace(out=s2[:, hp, :], in_to_replace=m8[:, hp, 0:8], in_values=s1[:, hp, :], imm_value=NEG))
                    yield
                    kb.I('dve', ['p_s2'], ['p_m8'], lambda e: e.max(out=m8[:, hp, 8:16], in_=s2[:, hp, :]))
                    yield
                    kb.I('dve', ['p_s2', 'p_m8'], ['p_i8'], lambda e: e.max_index(out=i8[:, hp, 8:16], in_max=m8[:, hp, 8:16], in_values=s2[:, hp, :]))
                    yield
                kb.I('dve', ['p_i8'], ['p_i8f'], lambda e: e.tensor_copy(out=i8f[:], in_=i8[:]))
                yield
                m8v = m8[:, :, :].rearrange("p (h two) k -> p h two k", two=2)
                i8v = i8f[:, :, :].rearrange("p (h two) k -> p h two k", two=2)
                candv = cand[:, :, :].rearrange("p h (i j) -> p h i j", j=16)
                kb.I('dve', ['p_m8'], ['p_cand'], lambda e: e.tensor_tensor(out=candv, in0=m8v[:, :, 0, :].unsqueeze(3).to_broadcast([128, 8, 16, 16]), in1=m8v[:, :, 1, :].unsqueeze(2).to_broadcast([128, 8, 16, 16]), op=ALU.add))
                yield
                for h in range(8):
                    kb.I('dve', ['p_cand'], ['p_sc'], lambda e: e.max(out=sc[:, h, 0:8], in_=cand[:, h, :]))
                    yield
                    kb.I('dve', ['p_cand', 'p_sc'], ['p_ci'], lambda e: e.max_index(out=ci[:, h, 0:8], in_max=sc[:, h, 0:8], in_values=cand[:, h, :]))
                    yield
                    kb.I('dve', ['p_cand', 'p_sc'], ['p_s2'], lambda e: e.match_replace(out=cand2[:, h, :], in_to_replace=sc[:, h, 0:8], in_values=cand[:, h, :], imm_value=NEG))
                    yield
                    kb.I('dve', ['p_s2'], ['p_sc'], lambda e: e.max(out=sc[:, h, 8:16], in_=cand2[:, h, :]))
                    yield
                    kb.I('dve', ['p_s2', 'p_sc'], ['p_ci'], lambda e: e.max_index(out=ci[:, h, 8:16], in_max=sc[:, h, 8:16], in_values=cand2[:, h, :]))
                    yield
                kb.I('dve', ['p_ci'], ['p_cf'], lambda e: e.tensor_copy(out=cf[:], in_=ci[:]))
                yield
                thr_b = pc[:, 0:16].unsqueeze(1).unsqueeze(1).to_broadcast([128, 8, 16, 16])
                iot_b = pc[:, 16:32].unsqueeze(1).unsqueeze(1).to_broadcast([128, 8, 16, 16])
                kb.I('dve', ['p_cf', 'p_pc'], ['p_oh'], lambda e: e.tensor_tensor(out=oh[:], in0=cf[:].unsqueeze(3).to_broadcast([128, 8, 16, 16]), in1=thr_b, op=ALU.is_ge))
                yield
                kb.I('dve', ['p_oh'], ['p_fi'], lambda e: e.tensor_reduce(out=fi[:], in_=oh[:], axis=AX.X, op=ALU.add))
                yield
                kb.I('dve', ['p_fi', 'p_cf'], ['p_fj'], lambda e: e.scalar_tensor_tensor(out=fj[:], in0=fi[:], scalar=-16.0, in1=cf[:], op0=ALU.mult, op1=ALU.add))
                yield
                for (fx, half_, gdst, gk) in ((fi, 0, g1, 'p_g1'), (fj, 1, g2_, 'p_g2')):
                    fk = 'p_fi' if half_ == 0 else 'p_fj'
                    kb.I('dve', [fk, 'p_pc'], ['p_oh'], lambda e: e.tensor_tensor(out=oh[:], in0=fx[:].unsqueeze(3).to_broadcast([128, 8, 16, 16]), in1=iot_b, op=ALU.is_equal))
                    yield
                    kb.I('dve', ['p_oh', 'p_i8f'], ['p_oh'], lambda e: e.tensor_tensor(out=oh[:], in0=oh[:], in1=i8v[:, :, half_, :].unsqueeze(2).to_broadcast([128, 8, 16, 16]), op=ALU.mult))
                    yield
                    kb.I('dve', ['p_oh'], [gk], lambda e: e.tensor_reduce(out=gdst[:], in_=oh[:], axis=AX.X, op=ALU.add))
                    yield
                kb.I('dve', ['p_g1', 'p_g2'], ['p_g1'], lambda e: e.scalar_tensor_tensor(out=g1[:], in0=g1[:], scalar=128.0, in1=g2_[:], op0=ALU.mult, op1=ALU.add))
                yield
                kb.I('dve', ['p_g1'], [f'p_eid{i}'], lambda e: e.tensor_copy(out=eidb[i][:, :].rearrange("p (h k) -> p h k", k=16), in_=g1[:]))
                yield
                kb.I('dve', ['p_sc'], [f'p_gate{i}'], lambda e: e.tensor_tensor(out=gateb[i][:], in0=sc[:], in1=sc[:, :, 0:1].to_broadcast([128, 8, 16]), op=ALU.subtract))
                yield
                kb.I('act', [f'p_gate{i}'], [f'p_gate{i}'], lambda e: e.activation(out=gateb[i][:], in_=gateb[i][:], func=AF.Exp))
                yield
                kb.I('dve', [f'p_gate{i}'], ['p_gsum'], lambda e: e.tensor_reduce(out=gsum[:], in_=gateb[i][:], axis=AX.X, op=ALU.add))
                yield
                kb.I('dve', ['p_gsum'], ['p_gsum'], lambda e: e.reciprocal(out=gsum[:], in_=gsum[:]))
                yield
                kb.I('dve', [f'p_gate{i}', 'p_gsum'], [f'p_gate{i}'], lambda e: e.tensor_tensor(out=gateb[i][:], in0=gateb[i][:], in1=gsum[:].unsqueeze(2).to_broadcast([128, 8, 16]), op=ALU.mult))
                yield

            def pump(gen, n):
                if gen is None:
                    return
                k = 0
                while n is None or k < n:
                    try:
                        next(gen)
                    except StopIteration:
                        return
                    k += 1

            def back(ti, t, nxt):
                i = ti % 2
                kind = 'c' if t < 2 else 'l'
                for slot in range(128):
                    gi = gring.next()
                    kb.DMA('pool', [f'p_eid{i}'], [f'p_gb{gi}'], lambda e: e.indirect_dma_start(out=gb[gi][:, :], out_offset=None, in_=peer_u[l][:, :], in_offset=bass.IndirectOffsetOnAxis(ap=eidb[i][:, slot:slot + 1], axis=0)))
                    kb.I('dve', [f'p_gb{gi}', f'p_h2{i}'], [('p_act', slot)], lambda e: e.scalar_tensor_tensor(out=junk[:], in0=gb[gi][:], scalar=1.0, in1=h2b[i][:], op0=ALU.mult, op1=ALU.mult, accum_out=act[:, slot:slot + 1]))
                    pump(nxt, 2)
                kb.I('dve', [('p_act', s_) for s_ in range(128)], ['p_t1', 'p_act'], lambda e: e.tensor_tensor(out=t1[:], in0=act[:], in1=act[:], op=ALU.mult))
                kb.I('dve', ['p_t1'], ['p_t1'], lambda e: e.tensor_scalar(out=t1[:], in0=t1[:], scalar1=0.044715, scalar2=1.0, op0=ALU.mult, op1=ALU.add))
                kb.I('dve', ['p_t1', 'p_act'], ['p_t1'], lambda e: e.tensor_tensor(out=t1[:], in0=t1[:], in1=act[:], op=ALU.mult))
                kb.I('act', ['p_t1'], ['p_t1'], lambda e: e.activation(out=t1[:], in_=t1[:], func=AF.Sigmoid, scale=1.5957691216057308))
                kb.I('dve', ['p_t1', 'p_act'], ['p_t1'], lambda e: e.tensor_tensor(out=t1[:], in0=t1[:], in1=act[:], op=ALU.mult))
                kb.I('dve', ['p_t1', f'p_gate{i}'], ['p_wgt'], lambda e: e.tensor_tensor(out=wgt[:], in0=t1[:], in1=gateb[i][:, :, :].rearrange("p h k -> p (h k)"), op=ALU.mult))
                for slot in range(128):
                    gi = gring.next()
                    kb.DMA('pool', [f'p_eid{i}'], [f'p_gb{gi}'], lambda e: e.indirect_dma_start(out=gb[gi][:, :], out_offset=None, in_=peer_v[l][:, :], in_offset=bass.IndirectOffsetOnAxis(ap=eidb[i][:, slot:slot + 1], axis=0)))
                    di = dring.next()
                    kb.I('dve', ['ident', 'p_wgt'], [f'p_dg{di}'], lambda e: e.tensor_scalar(out=dg[di][:], in0=ident[:], scalar1=wgt[:, slot:slot + 1], scalar2=None, op0=ALU.mult))
                    for hf in range(2):
                        mm(ps[6 + hf][:, :], dg[di][:], gb[gi][:, hf * 512:(hf + 1) * 512], [f'p_dg{di}', f'p_gb{gi}'], [psk[6 + hf]], start=(slot == 0), stop=(slot == 127))
                    pump(nxt, 2)
                for hf in range(2):
                    kb.I('dve', [psk[6 + hf], 'p_G' + kind], ['p_acc'], lambda e: e.tensor_tensor(out=acc[:, hf * 512:(hf + 1) * 512], in0=ps[6 + hf][:, :], in1=Gk[kind][:, hf * 512:(hf + 1) * 512], op=ALU.mult))
                kb.I('dve', ['p_acc', f'p_x{i}'], [f'p_x{i}'], lambda e: e.tensor_tensor(out=xt[i][:], in0=xt[i][:], in1=acc[:], op=ALU.add))
                if final:
                    kb.I('act', [f'p_x{i}'], ['p_ssf'], lambda e: e.activation(out=junk[:], in_=xt[i][:], func=AF.Square, accum_out=ssf[:]))
                    kb.I('act', ['p_ssf', 'epsc'], ['p_ssf'], lambda e: e.activation(out=ssf[:], in_=ssf[:], func=AF.Sqrt, scale=1.0 / D, bias=epsc[:]))
                    kb.I('dve', ['p_ssf'], ['p_ssf'], lambda e: e.reciprocal(out=ssf[:], in_=ssf[:]))
                    kb.I('dve', [f'p_x{i}', 'p_ssf', 'p_fnw'], [f'p_x{i}'], lambda e: e.scalar_tensor_tensor(out=xt[i][:], in0=xt[i][:], scalar=ssf[:], in1=fnw[:], op0=ALU.mult, op1=ALU.mult))
                dst_fn(t, xt[i], f'p_x{i}')
                pump(nxt, None)

            gens = [front(ti, t) for ti, t in enumerate(tiles)]
            pump(gens[0], None)
            for ti, t in enumerate(tiles):
                back(ti, t, gens[ti + 1] if ti + 1 < len(tiles) else None)
            kb.barrier()

    od_w_in = din("od_w_in", [128, KC, 2064])
    od_w_out = din("od_w_out", [128, KC, D])
    c_gc = din("c_gc", [1, 16])
    c_norm = din("c_norm", [1, 512])
    d_w = din("d_w", [4, 128, 128])
    d_scale = din("d_scale", [512, 1])
    poolm = din("poolm", [4, 128, 128])
    mixT1 = dscr("mixT1", [NB, KC, 128, LT])

    def xs_cm_src(b, t):
        if t < 2:
            return [(slice(0, 128), xs[b, t * 128:(t + 1) * 128, :])]
        v = xs[b, LC:LT, :].rearrange("(r w) d -> r w d", w=GRID_W)
        return [(slice(wi * 32, (wi + 1) * 32), v[:, (t - 2) * 4 + wi, :]) for wi in range(4)]

    def layer1_mixer(b):
        with ExitStack() as ph:
            hT = sb("hT1", [128, KC, LT], stack=ph)
            with ExitStack() as st:
                nw = sb("n1_nw", [128, D], stack=st)
                kb.DMA('sp', [], ['n1_nw'], lambda e: e.dma_start(out=nw[:], in_=norms[1].to_broadcast([128, D])))
                md = {}
                for kind, row in (('c', 2), ('l', b)):
                    A = sb("n1_A" + kind, [128, D], stack=st)
                    S = sb("n1_S" + kind, [128, D], stack=st)
                    kb.DMA('sp', [('modsD', 1)], ['n1_A' + kind], lambda e: e.dma_start(out=A[:], in_=modsD[1, row:row + 1, D:2 * D].to_broadcast([128, D])))
                    kb.DMA('sp', [('modsD', 1)], ['n1_S' + kind], lambda e: e.dma_start(out=S[:], in_=modsD[1, row:row + 1, 0:D].to_broadcast([128, D])))
                    kb.I('dve', ['n1_A' + kind, 'n1_nw'], ['n1_A' + kind], lambda e: e.scalar_tensor_tensor(out=A[:], in0=A[:], scalar=1.0, in1=nw[:], op0=ALU.add, op1=ALU.mult))
                    md[kind] = (A, S)
                xt = [sb(f"n1_x{i}", [128, D], stack=st) for i in range(2)]
                ht = [sb(f"n1_h{i}", [128, D], stack=st) for i in range(2)]
                sq = sb("n1_sq", [128, D], stack=st)
                ss = [sb(f"n1_ss{i}", [128, 1], stack=st) for i in range(2)]
                for t in range(NT):
                    i = t % 2
                    kind = 'c' if t < 2 else 'l'
                    A, S = md[kind]
                    for (psl, sap) in xs_cm_src(b, t):
                        kb.DMA('sp', [('xs', b)], [f'n1_x{i}'], lambda e: e.dma_start(out=xt[i][psl, :], in_=sap))
                    kb.I('act', [f'n1_x{i}'], ['n1_sq', f'n1_ss{i}'], lambda e: e.activation(out=sq[:], in_=xt[i][:], func=AF.Square, accum_out=ss[i][:]))
                    kb.I('act', [f'n1_ss{i}', 'epsc'], [f'n1_ss{i}'], lambda e: e.activation(out=ss[i][:], in_=ss[i][:], func=AF.Sqrt, scale=1.0 / D, bias=epsc[:]))
                    kb.I('dve', [f'n1_ss{i}'], [f'n1_ss{i}'], lambda e: e.reciprocal(out=ss[i][:], in_=ss[i][:]))
                    kb.I('dve', [f'n1_x{i}', f'n1_ss{i}', 'n1_A' + kind], [f'n1_h{i}'], lambda e: e.scalar_tensor_tensor(out=ht[i][:], in0=xt[i][:], scalar=ss[i][:], in1=A[:], op0=ALU.mult, op1=ALU.mult))
                    kb.I('dve', [f'n1_h{i}', 'n1_S' + kind], [f'n1_h{i}'], lambda e: e.tensor_tensor(out=ht[i][:], in0=ht[i][:], in1=S[:], op=ALU.add))
                    for half in range(2):
                        pb = psr.next()
                        for kk in range(4):
                            k = half * 4 + kk
                            tr(ps[pb][:, kk * 128:(kk + 1) * 128], ht[i][:, k * 128:(k + 1) * 128], [f'n1_h{i}'], [psk[pb]])
                        kb.I('act', [psk[pb]], [('hT', t)], lambda e: e.activation(out=hT[:, half * 4:half * 4 + 4, t * 128:(t + 1) * 128], in_=ps[pb][:, :].rearrange("p (k t) -> p k t", k=4), func=AF.Copy))
                kb.barrier()
            with ExitStack() as st:
                wd = sb("q_wd", [128, KC, 512], stack=st)
                kb.DMA('sp', [], ['q_wd'], lambda e: e.dma_start(out=wd[:], in_=od_w_in[:, :, 1552:2064]))
                pm = sb("q_pm", [128, 4, 128], stack=st)
                dw = sb("q_dw", [128, 4, 128], stack=st)
                dsc = sb("q_dsc", [128, 4], stack=st)
                for g in range(4):
                    kb.DMA('sp', [], ['q_pm'], lambda e: e.dma_start(out=pm[:, g, :], in_=poolm[g]))
                    kb.DMA('sp', [], ['q_dw'], lambda e: e.dma_start(out=dw[:, g, :], in_=d_w[g]))
                    kb.DMA('sp', [], ['q_dsc'], lambda e: e.dma_start(out=dsc[:, g:g + 1], in_=d_scale[g * 128:(g + 1) * 128, :]))
                dtm = sb("q_dtm", [128, 512], stack=st)
                pT = sb("q_pT", [128, 128], stack=st)
                yT = sb("q_yT", [128, 4, LT], stack=st)
                for t in range(2, NT):
                    pb = psr.next()
                    for k in range(KC):
                        mm(ps[pb][:, :], hT[:, k, t * 128:(t + 1) * 128], wd[:, k, :], ['q_wd', ('hT', t)], [psk[pb]], start=(k == 0), stop=(k == KC - 1))
                    kb.I('act', [psk[pb]], ['q_dtm'], lambda e: e.activation(out=dtm[:], in_=ps[pb][:, :], func=AF.Copy))
                    for g in range(4):
                        p2 = psr.next()
                        mm(ps[p2][:, 0:128], dtm[:, g * 128:(g + 1) * 128], pm[:, g, :], ['q_dtm', 'q_pm'], [psk[p2]])
                        kb.I('act', [psk[p2]], ['q_pT'], lambda e: e.activation(out=pT[:], in_=ps[p2][:, 0:128], func=AF.Copy))
                        mm(ps[p2][:, 128:256], dw[:, g, :], pT[:], ['q_dw', 'q_pT'], [psk[p2]])
                        kb.I('dve', [psk[p2], 'q_dsc'], ['q_yT'], lambda e: e.tensor_scalar(out=yT[:, g, t * 128:(t + 1) * 128], in0=ps[p2][:, 128:256], scalar1=dsc[:, g:g + 1], scalar2=None, op0=ALU.mult))
                for g in range(4):
                    kb.DMA('sp', ['q_yT'], [('mixT1', b)], lambda e: e.dma_start(out=mixT1[b, 4 + g, :, LC:LT], in_=yT[:, g, LC:LT]))
                kb.barrier()
            mlstm(b, hT)
            kb.barrier()

    def mlstm(b, hT):
        with ExitStack() as st:
            wh = sb("m_wh", [128, KC, 64 + 64 + 128 + 128], stack=st)
            wg16 = sb("m_wg16", [128, KC, 16], stack=st)
            gcst = sb("m_gcst", [128, 16], stack=st)
            cnw = sb("m_cnw", [128, 512], stack=st)
            QT = sb("m_QT", [64, LT], stack=st)
            KT = sb("m_KT", [64, LT], stack=st)
            Ktm = sb("m_Ktm", [128, NT, 64], stack=st)
            V1 = sb("m_V1", [128, NT, 132], stack=st)
            og = sb("m_og", [128, NT, 128], stack=st)
            O = sb("m_O", [128, NT, 128], stack=st)
            MOKEYS = [('m_O', n_) for n_ in range(NT)]
            tmp = sb("m_tmp", [128, LT], stack=st)
            graw = sb("m_graw", [128, NT, 16], stack=st)
            LI = sb("m_li", [128, NT, 8], stack=st)
            LF = sb("m_lf", [128, NT, 8], stack=st)
            kb.DMA('sp', [], ['m_gcst'], lambda e: e.dma_start(out=gcst[:], in_=c_gc[0:1, :].to_broadcast([128, 16])))
            kb.DMA('sp', [], ['m_cnw'], lambda e: e.dma_start(out=cnw[:], in_=c_norm[0:1, :].to_broadcast([128, 512])))
            kb.DMA('sp', [], ['m_wg16'], lambda e: e.dma_start(out=wg16[:], in_=od_w_in[:, :, 1024:1040]))
            pb = psr.next()
            for n in range(NT):
                for k in range(KC):
                    mm(ps[pb][:, n * 16:(n + 1) * 16], hT[:, k, n * 128:(n + 1) * 128], wg16[:, k, :], ['m_wg16', ('hT', n)], [psk[pb]], start=(k == 0), stop=(k == KC - 1))
            kb.I('act', [psk[pb]], ['m_graw'], lambda e: e.activation(out=graw[:], in_=ps[pb][:, 0:NT * 16].rearrange("p (n c) -> p n c", c=16), func=AF.Copy))
            kb.I('dve', ['m_graw', 'm_gcst'], ['m_li'], lambda e: e.tensor_tensor(out=LI[:], in0=graw[:, :, 0:8], in1=gcst[:, 0:8].unsqueeze(1).to_broadcast([128, NT, 8]), op=ALU.add))
            kb.I('dve', ['m_graw', 'm_gcst'], ['m_lf'], lambda e: e.tensor_tensor(out=LF[:], in0=graw[:, :, 8:16], in1=gcst[:, 8:16].unsqueeze(1).to_broadcast([128, NT, 8]), op=ALU.add))
            kb.I('act', ['m_lf'], ['m_lf'], lambda e: e.activation(out=LF[:], in_=LF[:], func=AF.Exp, scale=-1.0))
            kb.I('act', ['m_lf', 'onec'], ['m_lf'], lambda e: e.activation(out=LF[:], in_=LF[:], func=AF.Ln, bias=onec[:]))
            kb.I('dve', ['m_lf'], ['m_lf'], lambda e: e.tensor_scalar(out=LF[:], in0=LF[:], scalar1=-1.0, scalar2=None, op0=ALU.mult))
            kb.I('dve', [], ['m_V1'], lambda e: e.memset(V1[:], 1.0))
            for hd in range(4):
                for (o0, c0, w_) in ((0, hd * 64, 64), (64, 256 + hd * 64, 64), (128, 512 + hd * 128, 128), (256, 1040 + hd * 128, 128)):
                    kb.DMA('sp', [], ['m_wh'], lambda e: e.dma_start(out=wh[:, :, o0:o0 + w_], in_=od_w_in[:, :, c0:c0 + w_]))
                for (dst, dk_, o0, scl) in ((QT, 'm_QT', 0, 1.0), (KT, 'm_KT', 64, 0.125)):
                    for (t0, n) in TOKBLKS:
                        pb = psr.next()
                        for k in range(KC):
                            mm(ps[pb][0:64, 0:n], wh[:, k, o0:o0 + 64], hT[:, k, t0:t0 + n], ['m_wh'] + [('hT', tt) for tt in range(t0 // 128, (t0 + n) // 128)], [psk[pb]], start=(k == 0), stop=(k == KC - 1))
                        kb.I('act', [psk[pb]], [dk_], lambda e: e.activation(out=dst[:, t0:t0 + n], in_=ps[pb][0:64, 0:n], func=AF.Copy, scale=scl))
                for n0 in range(0, NT, 8):
                    pb = psr.next()
                    nn = min(8, NT - n0)
                    for i in range(nn):
                        kb.I('pe', ['m_KT', 'ident'], [psk[pb]], lambda e: e.transpose(out=ps[pb][:, i * 64:(i + 1) * 64], in_=KT[:, (n0 + i) * 128:(n0 + i + 1) * 128], identity=ident[0:64, 0:64]))
                    kb.I('act', [psk[pb]], ['m_Ktm'], lambda e: e.activation(out=Ktm[:, n0:n0 + nn, :], in_=ps[pb][:, 0:nn * 64].rearrange("p (n c) -> p n c", c=64), func=AF.Copy))
                for (o0, dst, dk_, fn_, wdt) in ((128, V1, 'm_V1', AF.Copy, 128), (256, og, 'm_og', AF.Sigmoid, 128)):
                    for n0 in range(0, NT, 4):
                        pb = psr.next()
                        nn = min(4, NT - n0)
                        for i in range(nn):
                            n = n0 + i
                            for k in range(KC):
                                mm(ps[pb][:, i * 128:(i + 1) * 128], hT[:, k, n * 128:(n + 1) * 128], wh[:, k, o0:o0 + 128], ['m_wh', ('hT', n)], [psk[pb]], start=(k == 0), stop=(k == KC - 1))
                        kb.I('act', [psk[pb]], [dk_], lambda e: e.activation(out=dst[:, n0:n0 + nn, 0:128], in_=ps[pb][:, 0:nn * 128].rearrange("p (n c) -> p n c", c=128), func=fn_))
                mlstm_chains(b, hd, QT, KT, Ktm, V1, LI, LF, O)
                ssq = sb(f"m_ssq{hd}", [128, NT], stack=st)
                O2 = tmp[:, :].rearrange("p (n c) -> p n c", c=128)
                kb.I('dve', MOKEYS, ['m_tmp'], lambda e: e.tensor_tensor(out=O2, in0=O[:], in1=O[:], op=ALU.mult))
                kb.I('dve', ['m_tmp'], ['m_ssq'], lambda e: e.tensor_reduce(out=ssq[:], in_=O2, axis=AX.X, op=ALU.add))
                kb.I('act', ['m_ssq', 'epsc'], ['m_ssq'], lambda e: e.activation(out=ssq[:], in_=ssq[:], func=AF.Sqrt, scale=1.0 / 128, bias=epsc[:]))
                kb.I('dve', ['m_ssq'], ['m_ssq'], lambda e: e.reciprocal(out=ssq[:], in_=ssq[:]))
                kb.I('dve', MOKEYS + ['m_ssq'], MOKEYS, lambda e: e.tensor_tensor(out=O[:], in0=O[:], in1=ssq[:].unsqueeze(2).to_broadcast([128, NT, 128]), op=ALU.mult))
                kb.I('dve', MOKEYS + ['m_cnw'], MOKEYS, lambda e: e.tensor_tensor(out=O[:], in0=O[:], in1=cnw[:, hd * 128:(hd + 1) * 128].unsqueeze(1).to_broadcast([128, NT, 128]), op=ALU.mult))
                kb.I('dve', MOKEYS + ['m_og'], MOKEYS, lambda e: e.tensor_tensor(out=O[:], in0=O[:], in1=og[:], op=ALU.mult))
                for n0 in range(0, NT, 4):
                    pb = psr.next()
                    nn = min(4, NT - n0)
                    for i in range(nn):
                        tr(ps[pb][:, i * 128:(i + 1) * 128], O[:, n0 + i, :], MOKEYS, [psk[pb]])
                    kb.I('act', [psk[pb]], ['m_tmp'], lambda e: e.activation(out=tmp[:, n0 * 128:(n0 + nn) * 128], in_=ps[pb][:, 0:nn * 128], func=AF.Copy))
                kb.DMA('sp', ['m_tmp'], [('mixT1', b)], lambda e: e.dma_start(out=mixT1[b, hd, :, LC:LT], in_=tmp[:, LC:LT]))
            kb.barrier()

    def mlstm_chains(b, hd, QT, KT, Ktm, V1, LI, LF, O):
        MOK = [('m_O', n_) for n_ in range(NT)]
        with ExitStack() as st:
            sets = {}
            for d_ in range(2):
                for p_ in range(2):
                    B = {nm: sb(f"k_{nm}{d_}{p_}", [128, 128], stack=st) for nm in ("Tg", "Ei", "ST")}
                    B["eg"] = sb(f"k_eg{d_}{p_}", [64, 128], stack=st)
                    B["qd"] = sb(f"k_qd{d_}{p_}", [64, 128], stack=st)
                    B["Kd"] = sb(f"k_Kd{d_}{p_}", [128, 64], stack=st)
                    B["cs"] = sb(f"k_cs{d_}{p_}", [128, 8], stack=st)
                    sets[(d_, p_)] = B
            C = {d_: [sb(f"k_C{d_}{i}", [64, 132], stack=st) for i in range(2)] for d_ in range(2)}
            kb.I('dve', [], MOK, lambda e: e.memset(O[:], 0.0))
            for d_ in range(2):
                kb.I('dve', [], [f'k_C{d_}0'], lambda e: e.memset(C[d_][0][:], 0.0))
            done = {0: 0, 1: 0}

            def gen(d, p):
                col = d * 4 + hd
                order = ([0, 1] + list(range(2, NT))) if d == 0 else ([1, 0] + list(range(NT - 1, 1, -1)))
                B = sets[(d, p)]
                tag = f"{d}{p}"
                K_ = lambda nm: f"k_{nm}{tag}"
                cs = B["cs"]
                banks = [2 * (2 * d + p), 2 * (2 * d + p) + 1]
                bi = [0]

                def nb():
                    x = banks[bi[0] % 2]
                    bi[0] += 1
                    return x
                for k in range(p, NT, 2):
                    n = order[k]
                    fcol = LF[:, n, col:col + 1]
                    icol = LI[:, n, col:col + 1]
                    tok = slice(n * 128, (n + 1) * 128)
                    kb.I('dve', ['m_lf', 'msk'], [K_('Tg')], lambda e: e.tensor_scalar(out=B["Tg"][:], in0=msk[:, d, :], scalar1=fcol, scalar2=None, op0=ALU.mult)); yield
                    pa = nb()
                    mm(ps[pa][:, 0:128], ones[:], B["Tg"][:], ['ones', K_('Tg')], [psk[pa]]); yield
                    mm(ps[pa][:, 256:257], msk[:, d, :], fcol, ['msk', 'm_lf'], [psk[pa]]); yield
                    mm(ps[pa][:, 257:258], ones[:], fcol, ['ones', 'm_lf'], [psk[pa]]); yield
                    kb.I('act', [psk[pa]], [K_('cs')], lambda e: e.activation(out=cs[:, 0:2], in_=ps[pa][:, 256:258], func=AF.Copy)); yield
                    kb.I('dve', [K_('cs')], [K_('cs')], lambda e: e.tensor_tensor(out=cs[:, 2:3], in0=cs[:, 1:2], in1=cs[:, 0:1], op=ALU.subtract)); yield
                    kb.I('dve', [K_('cs'), 'm_li'], [K_('cs')], lambda e: e.tensor_tensor(out=cs[:, 2:3], in0=cs[:, 2:3], in1=icol, op=ALU.add)); yield
                    kb.I('act', [K_('cs')], [K_('cs')], lambda e: e.activation(out=cs[:, 3:5], in_=cs[:, 1:3], func=AF.Exp)); yield
                    kb.I('dve', ['m_Ktm', K_('cs')], [K_('Kd')], lambda e: e.tensor_scalar(out=B["Kd"][:], in0=Ktm[:, n, :], scalar1=cs[:, 4:5], scalar2=None, op0=ALU.mult)); yield
                    pq = None
                    if n >= 2:
                        kb.I('dve', [psk[pa], K_('cs')], [K_('Ei')], lambda e: e.tensor_scalar(out=B["Ei"][:], in0=ps[pa][:, 0:128], scalar1=cs[:, 0:1], scalar2=0.0, op0=ALU.subtract, op1=ALU.min)); yield
                        kb.I('act', [K_('Ei'), 'm_li'], [K_('Ei')], lambda e: e.activation(out=B["Ei"][:], in_=B["Ei"][:], func=AF.Exp, bias=icol)); yield
                        kb.I('dve', [K_('Ei'), 'msk'], [K_('Ei')], lambda e: e.tensor_tensor(out=B["Ei"][:], in0=B["Ei"][:], in1=msk[:, 2 + d, :], op=ALU.mult)); yield
                        kb.I('act', [psk[pa]], [K_('eg')], lambda e: e.activation(out=B["eg"][:], in_=ps[pa][0:64, 0:128], func=AF.Exp)); yield
                        kb.I('dve', [K_('eg'), 'm_QT'], [K_('qd')], lambda e: e.tensor_tensor(out=B["qd"][:], in0=QT[:, tok], in1=B["eg"][:], op=ALU.mult)); yield
                        pq = nb()
                        mm(ps[pq][:, 0:128], KT[:, tok], QT[:, tok], ['m_KT', 'm_QT'], [psk[pq]]); yield
                        kb.I('dve', [psk[pq], K_('Ei')], [K_('ST')], lambda e: e.tensor_tensor(out=B["ST"][:], in0=ps[pq][:, 0:128], in1=B["Ei"][:], op=ALU.mult)); yield
                    while done[d] != k:
                        yield
                    Cc, Cn = C[d][k % 2], C[d][(k + 1) % 2]
                    ckc, ckn = f'k_C{d}{k % 2}', f'k_C{d}{(k + 1) % 2}'
                    if n >= 2:
                        mm(ps[pq][:, 128:257], B["qd"][:], Cc[:, 0:129], [K_('qd'), ckc], [psk[pq]], start=True, stop=False); yield
                        mm(ps[pq][:, 128:257], B["ST"][:], V1[:, n, 0:129], [K_('ST'), 'm_V1'], [psk[pq]], start=False, stop=True); yield
                        kb.I('act', [psk[pq]], [K_('cs')], lambda e: e.activation(out=cs[:, 5:6], in_=ps[pq][:, 256:257], func=AF.Abs)); yield
                        kb.I('dve', [K_('cs')], [K_('cs')], lambda e: e.tensor_scalar(out=cs[:, 5:6], in0=cs[:, 5:6], scalar1=1.0, scalar2=None, op0=ALU.max)); yield
                        kb.I('dve', [K_('cs')], [K_('cs')], lambda e: e.reciprocal(out=cs[:, 5:6], in_=cs[:, 5:6])); yield
                        kb.I('dve', [psk[pq], K_('cs'), ('m_O', n)], [('m_O', n)], lambda e: e.scalar_tensor_tensor(out=O[:, n, :], in0=ps[pq][:, 128:256], scalar=cs[:, 5:6], in1=O[:, n, :], op0=ALU.mult, op1=ALU.add)); yield
                    p3 = nb()
                    mm(ps[p3][0:64, 0:129], B["Kd"][:], V1[:, n, 0:129], [K_('Kd'), 'm_V1'], [psk[p3]]); yield
                    kb.I('dve', [psk[p3], ckc, K_('cs')], [ckn], lambda e: e.scalar_tensor_tensor(out=Cn[:, 0:129], in0=Cc[:, 0:129], scalar=cs[0:64, 3:4], in1=ps[p3][0:64, 0:129], op0=ALU.mult, op1=ALU.add))
                    done[d] = k + 1
                    yield

            active = [gen(0, 0), gen(1, 0), gen(0, 1), gen(1, 1)]
            while active:
                for g_ in list(active):
                    try:
                        next(g_)
                    except StopIteration:
                        active.remove(g_)
            kb.barrier()

    def w_out1_phase(b):
        with ExitStack() as st:
            wo = sb("o1_w", [128, KC, D], stack=st)
            kb.DMA('sp', [], ['o1_w'], lambda e: e.dma_start(out=wo[:], in_=od_w_out[:, :, :]))
            g = sb("o1_g", [128, D], stack=st)
            kb.DMA('sp', [('modsD', 1)], ['o1_g'], lambda e: e.dma_start(out=g[:], in_=modsD[1, b:b + 1, 2 * D:3 * D].to_broadcast([128, D])))
            mt = [sb(f"o1_m{i}", [128, KC, 128], stack=st) for i in range(2)]
            xt = [sb(f"o1_x{i}", [128, D], stack=st) for i in range(2)]
            for t in range(2, NT):
                i = t % 2
                kb.DMA('sp', [('mixT1', b)], [f'o1_m{i}'], lambda e: e.dma_start(out=mt[i][:], in_=mixT1[b, :, :, t * 128:(t + 1) * 128].rearrange("k p t -> p k t")))
                for (psl, sap) in xs_cm_src(b, t):
                    kb.DMA('sp', [('xs', b)], [f'o1_x{i}'], lambda e: e.dma_start(out=xt[i][psl, :], in_=sap))
                for hlf in range(2):
                    pb = psr.next()
                    for k in range(KC):
                        mm(ps[pb][:, :], mt[i][:, k, :], wo[:, k, hlf * 512:(hlf + 1) * 512], [f'o1_m{i}', 'o1_w'], [psk[pb]], start=(k == 0), stop=(k == KC - 1))
                    kb.I('dve', [psk[pb], 'o1_g'], [psk[pb]], lambda e: e.tensor_tensor(out=ps[pb][:, :], in0=ps[pb][:, :], in1=g[:, hlf * 512:(hlf + 1) * 512], op=ALU.mult))
                    kb.I('dve', [psk[pb], f'o1_x{i}'], [f'o1_x{i}'], lambda e: e.tensor_tensor(out=xt[i][:, hlf * 512:(hlf + 1) * 512], in0=xt[i][:, hlf * 512:(hlf + 1) * 512], in1=ps[pb][:, :], op=ALU.add))
                kb.DMA('sp', [f'o1_x{i}'], [('xs1', b)], lambda e: e.dma_start(out=xs1[b, (t - 2) * 128:(t - 1) * 128, :], in_=xt[i][:]))
            kb.barrier()

    def layer1(b, tiles=None):
        layer1_mixer(b)
        w_out1_phase(b)
        ov = out_d[b].rearrange("(r w) d -> r w d", w=GRID_W)

        def dst1(t, tile, key):
            for wi in range(4):
                kb.DMA('sp', [key], [('out', b)], lambda e: e.dma_start(out=ov[:, (t - 2) * 4 + wi, :], in_=tile[wi * 32:(wi + 1) * 32, :]))
        peer_phase(1, b, tiles or list(range(2, NT)), lambda t: (xs1[b, (t - 2) * 128:(t - 1) * 128, :], ('xs1', b)), dst1, final=True)

    nb_run = 1 if (stop_after or '').endswith('_b0') else NB
    for b in range(0 if l1only else nb_run):
        layer0_mixer(b)
        if stop_after in ('hT0', 'lru'):
            break
        w_out_phase(b)
        if stop_after == 'mix0_b0':
            continue

        def dst0(t, tile, key, b=b):
            kb.DMA('sp', [key], [('xs', b)], lambda e: e.dma_start(out=xs[b, t * 128:(t + 1) * 128, :], in_=tile[:]))
        peer_tiles = list(range(NT)) if stop_after != 'peer0_b0' else [0, 2]
        peer_phase(0, b, peer_tiles, lambda t, b=b: (xs[b, t * 128:(t + 1) * 128, :], ('xs', b)), dst0)
    if stop_after is None or stop_after.startswith('l1'):
        for b in range(nb_run):
            if stop_after == 'l1mix_b0':
                layer1_mixer(b)
                w_out1_phase(b)
            elif l1only:
                layer1(b, tiles=[2, 17])
            else:
                layer1(b)
    if 'xs' in dbg_out:
        kb.barrier()
        kb.DMA('sp', [('xs', 0), ('xs', 1)], ['dbg_xs'], lambda e: e.dma_start(out=dbg_out['xs'], in_=xs))
    if 'xs1' in dbg_out:
        kb.barrier()
        kb.DMA('sp', [('xs1', 0), ('xs1', 1)], ['dbg_xs1'], lambda e: e.dma_start(out=dbg_out['xs1'], in_=xs1))

    kb.finish()
    es.close()
    return nc


def make_in_maps(inputs):
    x = np.asarray(inputs['x'], np.float32)
    ctx = np.asarray(inputs['ctx'], np.float32)
    c = np.asarray(inputs['c'], np.float32)
    c_ctx = np.asarray(inputs['c_ctx'], np.float32)
    ada_w = np.ascontiguousarray(np.asarray(inputs['ada_w'], np.float32).reshape(2, KC, 128, 6 * D))
    ada_b = np.ascontiguousarray(np.asarray(inputs['ada_b'], np.float32).reshape(2, 1, 6 * D))
    norms = np.stack([inputs['norm_mix'][0], inputs['norm_mix'][1], inputs['norm_ffn'][0], inputs['norm_ffn'][1],
                      inputs['final_norm']]).astype(np.float32).reshape(5, 1, D)
    ident = np.eye(128, dtype=np.float32)
    ev_w_in = np.ascontiguousarray(np.asarray(inputs['ev_w_in'][0], np.float32).reshape(KC, 128, 3088).transpose(1, 0, 2))
    ev_w_out = np.ascontiguousarray(np.asarray(inputs['ev_w_out'][0], np.float32).reshape(KC, 128, D).transpose(1, 0, 2))
    a_convT = np.ascontiguousarray(np.asarray(inputs['a_conv'][0], np.float32).T)
    a_gc = np.concatenate([np.asarray(inputs['a_alog'][0]).reshape(8), np.asarray(inputs['a_dtb'][0]).reshape(8)]).astype(np.float32).reshape(1, 16)
    a_norm = np.asarray(inputs['a_norm'][0], np.float32).reshape(1, 128)
    lru_pc = np.zeros((512, 11), np.float32)
    lru_pc[:, 0:4] = np.asarray(inputs['b_conv_w'][0]).T
    lru_pc[:, 4] = np.asarray(inputs['b_conv_b'][0])
    for d in range(2):
        lru_pc[:, 5 + 3 * d] = np.asarray(inputs['b_ba'][0, d]).reshape(512)
        lru_pc[:, 6 + 3 * d] = np.asarray(inputs['b_bx'][0, d]).reshape(512)
        lru_pc[:, 7 + 3 * d] = np.asarray(inputs['b_lam'][0, d]).reshape(512)
    lru_w = np.zeros((2, 2, 4, 128, 128), np.float32)
    for ax, nm in enumerate(('b_wa', 'b_wx')):
        w = np.asarray(inputs[nm][0], np.float32)
        for d in range(2):
            for ct in range(4):
                lru_w[ax, d, ct, 0:64, 0:64] = w[d, 2 * ct]
                lru_w[ax, d, ct, 64:128, 64:128] = w[d, 2 * ct + 1]
    ii = np.arange(128)
    kk_, aa_ = ii[:, None], ii[None, :]
    masks = np.stack([(kk_ <= aa_), (kk_ >= aa_), (aa_ >= kk_), (aa_ <= kk_), (kk_ > aa_), (kk_ < aa_)]).astype(np.float32)
    peer_wq = np.ascontiguousarray(np.asarray(inputs['peer_wq'], np.float32).reshape(2, KC, 128, 2048).transpose(0, 2, 1, 3))
    peer_kT = np.ascontiguousarray(np.asarray(inputs['peer_keys'], np.float32).reshape(2, 16, 128, 128).transpose(0, 3, 1, 2))
    peer_u = np.asarray(inputs['peer_u'], np.float32)
    peer_v = np.asarray(inputs['peer_v'], np.float32)
    peer_u0, peer_u1, peer_v0, peer_v1 = peer_u[0], peer_u[1], peer_v[0], peer_v[1]
    pconst = np.concatenate([16.0 * (np.arange(16) + 1), np.arange(16)]).astype(np.float32).reshape(1, 32)
    od_w_in = np.ascontiguousarray(np.asarray(inputs['od_w_in'][0], np.float32).reshape(KC, 128, 2064).transpose(1, 0, 2))
    od_w_out = np.ascontiguousarray(np.asarray(inputs['od_w_out'][0], np.float32).reshape(KC, 128, D).transpose(1, 0, 2))
    c_gc = np.concatenate([np.asarray(inputs['c_ibias'][0]).reshape(8), np.asarray(inputs['c_fbias'][0]).reshape(8)]).astype(np.float32).reshape(1, 16)
    c_norm = np.asarray(inputs['c_norm'][0], np.float32).reshape(1, 512)
    d_w = np.asarray(inputs['d_w'][0], np.float32)
    d_scale = np.asarray(inputs['d_scale'][0], np.float32).reshape(512, 1)
    poolm = np.zeros((4, 128, 128), np.float32)
    seg = ROWS
    for gi, w in enumerate((2, 4, 8, 16)):
        P = np.zeros((seg, seg), np.float32)
        for t_ in range(seg):
            lo = max(t_ - w // 2, 0)
            hi = min(t_ - w // 2 + w, seg)
            P[t_, lo:hi] = 1.0 / (hi - lo)
        P = P - np.eye(seg, dtype=np.float32)
        for sgi in range(128 // seg):
            poolm[gi, sgi * seg:(sgi + 1) * seg, sgi * seg:(sgi + 1) * seg] = P.T
    shared = dict(od_w_in=od_w_in, od_w_out=od_w_out, c_gc=c_gc, c_norm=c_norm, d_w=d_w, d_scale=d_scale, poolm=poolm, peer_wq=peer_wq, peer_kT=peer_kT, peer_u0=peer_u0, peer_u1=peer_u1, peer_v0=peer_v0, peer_v1=peer_v1, pconst=pconst, ada_w=ada_w, ada_b=ada_b, norms=norms, ident=ident, ev_w_in=ev_w_in, ev_w_out=ev_w_out, a_convT=a_convT,
                  a_gc=a_gc, a_norm=a_norm, lru_pc=lru_pc, lru_w=lru_w, masks=masks)
    maps = []
    for ci in range(NCORES):
        b0 = ci * NB
        xin = np.concatenate([ctx[b0:b0 + NB], x[b0:b0 + NB]], axis=1)
        c3 = np.stack([c[b0], c[b0 + 1], c_ctx], axis=1)
        c3T = np.ascontiguousarray(c3.reshape(KC, 128, 3).transpose(1, 0, 2))
        maps.append(dict(xin=np.ascontiguousarray(xin), c3T=c3T, **shared))
    return maps


def kernel(**inputs):
    nc = build_program()
    maps = make_in_maps(inputs)
    res = run_bass_kernel_spmd(nc, maps, core_ids=list(range(NCORES)))
    outs = [r["out"] for r in res.results]
    return np.concatenate(outs, axis=0).astype(np.float32)
```
